# Optimizing a Trainium2 kernel written in Bass

```python
import math
import jax
import jax.numpy as jnp
from jax import lax
import numpy as np


D_MODEL = 2048
BATCH = 4
SEQ = 8192
DEPTH = 1

N_META = 16
CHUNK = 128
LEAD = (-N_META) % CHUNK
NORM_EPS = 1e-6

RET_HEADS = 8
RET_QK_HEAD = D_MODEL // RET_HEADS
RET_V_HEAD = 2 * RET_QK_HEAD
RET_QK = RET_HEADS * RET_QK_HEAD
RET_V = RET_HEADS * RET_V_HEAD
ROPE_BASE = 10000.0

SSM_INNER = 2 * D_MODEL
SSM_HEAD_DIM = 64
SSM_HEADS = SSM_INNER // SSM_HEAD_DIM
SSM_GROUPS = 8
SSM_STATE = 128
CONV_WIDTH = 4
CONV_DIM = SSM_INNER + 2 * SSM_GROUPS * SSM_STATE

PEER_HEADS = 8
PEER_NKEYS = 128
PEER_EXPERTS = PEER_NKEYS * PEER_NKEYS
PEER_TOPK = 16
PEER_QUERY = 256
PEER_HALF = PEER_QUERY // 2
PEER_BLOCK = 128

IN_SIZES = (RET_QK, RET_QK, RET_V, RET_V, SSM_INNER, CONV_DIM, SSM_HEADS, D_MODEL, D_MODEL)
N_PROJ = 2 * RET_QK + 2 * RET_V + SSM_INNER + CONV_DIM + SSM_HEADS + 2 * D_MODEL

kernel_name = "hybrid_retention_ssd_peer_meta"


def _rms(x):
    xf = x.astype(jnp.float32)
    return xf * lax.rsqrt(jnp.mean(xf * xf, axis=-1, keepdims=True) + NORM_EPS)


def rmsnorm(x, g):
    return (_rms(x) * g.astype(jnp.float32)).astype(x.dtype)


def pad_front(t):
    return jnp.pad(t, ((0, 0), (LEAD, 0)) + ((0, 0),) * (t.ndim - 2))


def to_chunks(t):
    b, lp = t.shape[:2]
    return jnp.swapaxes(t.reshape((b, lp // CHUNK, CHUNK) + t.shape[2:]), 0, 1)


def from_chunks(t):
    t = jnp.swapaxes(t, 0, 1)
    return t.reshape((t.shape[0], t.shape[1] * t.shape[2]) + t.shape[3:])


def rotary(t, pos):
    half = t.shape[-1] // 2
    inv = ROPE_BASE ** (-jnp.arange(half, dtype=jnp.float32) / half)
    ang = pos[:, None] * inv[None, :]
    cos = jnp.cos(ang)[None, :, None, :]
    sin = jnp.sin(ang)[None, :, None, :]
    tf = t.astype(jnp.float32)
    t1 = tf[..., 0::2]
    t2 = tf[..., 1::2]
    return jnp.stack([t1 * cos - t2 * sin, t1 * sin + t2 * cos], axis=-1).reshape(t.shape)


def retention_chunked(q, k, v, log_gamma):
    q, k, v = (a.astype(jnp.float32) for a in (q, k, v))
    b, lp, nh, dk = q.shape
    dv = v.shape[-1]
    idx = jnp.arange(CHUNK, dtype=jnp.float32)
    rel = idx[:, None] - idx[None, :]
    dmask = jnp.where(rel >= 0, jnp.exp(log_gamma[:, None, None] * jnp.maximum(rel, 0.0)), 0.0)
    xi = jnp.exp(log_gamma[None, :] * (idx[:, None] + 1.0))
    zeta = jnp.exp(log_gamma[None, :] * (CHUNK - 1.0 - idx[:, None]))
    chunk_decay = jnp.exp(log_gamma * CHUNK)

    def step(state, inp):
        qc, kc, vc = inp
        s = jnp.einsum('bihd,bjhd->bhij', qc, kc) * dmask[None]
        inner = jnp.einsum('bhij,bjhe->bihe', s, vc)
        cross = jnp.einsum('bihd,bhde->bihe', qc, state) * xi[None, :, :, None]
        state = state * chunk_decay[None, :, None, None] + jnp.einsum(
            'bjhd,bjhe->bhde', kc * zeta[None, :, :, None], vc)
        return state, inner + cross

    state0 = jnp.zeros((b, nh, dk, dv), jnp.float32)
    _, ys = lax.scan(step, state0, (to_chunks(q), to_chunks(k), to_chunks(v)))
    return from_chunks(ys)


def ssd_chunked(xdt, a, bm, cm):
    b, lp, nh, p = xdt.shape
    g = bm.shape[2]
    n = bm.shape[3]
    hg = nh // g
    xdt = xdt.astype(jnp.float32).reshape(b, lp, g, hg, p)
    a = a.astype(jnp.float32).reshape(b, lp, g, hg)
    causal = jnp.tril(jnp.ones((CHUNK, CHUNK), dtype=bool))

    def step(state, inp):
        xc, ac, bc, cc = inp
        acs = jnp.cumsum(ac, axis=1)
        diff = acs[:, :, None] - acs[:, None, :]
        lmat = jnp.exp(jnp.where(causal[None, :, :, None, None], diff, -jnp.inf))
        cb = jnp.einsum('bign,bjgn->bijg', cc, bc)
        y_diag = jnp.einsum('bijg,bijgh,bjghp->bighp', cb, lmat, xc)
        y_off = jnp.einsum('bign,bghpn->bighp', cc, state) * jnp.exp(acs)[..., None]
        dec = jnp.exp(acs[:, -1:] - acs)
        state = state * jnp.exp(acs[:, -1])[..., None, None] + jnp.einsum(
            'bjgn,bjgh,bjghp->bghpn', bc, dec, xc)
        return state, y_diag + y_off

    state0 = jnp.zeros((b, g, hg, p, n), jnp.float32)
    _, ys = lax.scan(step, state0, (to_chunks(xdt), to_chunks(a),
                                    to_chunks(bm.astype(jnp.float32)), to_chunks(cm.astype(jnp.float32))))
    return from_chunks(ys).reshape(b, lp, nh, p)


def causal_dwconv(x, w, bias):
    y = lax.conv_general_dilated(
        x, w[:, None, :].astype(x.dtype), window_strides=(1,),
        padding=((CONV_WIDTH - 1, 0),), dimension_numbers=('NWC', 'WIO', 'NWC'),
        feature_group_count=x.shape[-1])
    return y + bias.astype(x.dtype)


def peer_ffn(xn, w_q, sub_keys, exp_u, exp_v):
    b, seq_len, d = xn.shape
    n_tok = b * seq_len
    n_blk = -(-n_tok // PEER_BLOCK)
    xt = jnp.pad(xn.reshape(n_tok, d), ((0, n_blk * PEER_BLOCK - n_tok), (0, 0)))
    xt = xt.reshape(n_blk, PEER_BLOCK, d)

    def block(xb):
        q = (xb @ w_q).reshape(PEER_BLOCK, PEER_HEADS, 2, PEER_HALF)
        s = jnp.einsum('thcd,hckd->thck', q, sub_keys).astype(jnp.float32)
        s1, i1 = lax.top_k(s[:, :, 0], PEER_TOPK)
        s2, i2 = lax.top_k(s[:, :, 1], PEER_TOPK)
        cand = (s1[..., :, None] + s2[..., None, :]).reshape(PEER_BLOCK, PEER_HEADS, PEER_TOPK * PEER_TOPK)
        cand_id = (i1[..., :, None] * PEER_NKEYS + i2[..., None, :]).reshape(
            PEER_BLOCK, PEER_HEADS, PEER_TOPK * PEER_TOPK)
        top_s, top_pos = lax.top_k(cand, PEER_TOPK)
        eid = jnp.take_along_axis(cand_id, top_pos, axis=-1)
        gate = jax.nn.softmax(top_s, axis=-1)
        act = jax.nn.gelu(jnp.einsum('td,thkd->thk', xb, exp_u[eid]).astype(jnp.float32), approximate=False)
        return jnp.einsum('thk,thkd->td', (gate * act).astype(xb.dtype), exp_v[eid])

    y = lax.map(block, xt)
    return y.reshape(n_blk * PEER_BLOCK, d)[:n_tok].reshape(b, seq_len, d)


def setup_inputs(seed: int = 0) -> dict:
    key = jax.random.key(seed)
    ks = jax.random.split(key, 20)
    f32 = jnp.float32

    def nrm(k, shape, scale):
        return scale * jax.random.normal(k, shape, f32)

    x = nrm(ks[0], (BATCH, SEQ, D_MODEL), 1.0)
    meta_tokens = nrm(ks[1], (N_META, D_MODEL), 1.0)
    norm_mix_g = 1.0 + nrm(ks[2], (DEPTH, D_MODEL), 0.01)
    w_in = nrm(ks[3], (DEPTH, D_MODEL, N_PROJ), D_MODEL ** -0.5)
    conv_w = nrm(ks[4], (DEPTH, CONV_WIDTH, CONV_DIM), CONV_WIDTH ** -0.5)
    conv_b = nrm(ks[5], (DEPTH, CONV_DIM), 0.01)
    dt0 = jnp.exp(jax.random.uniform(ks[6], (DEPTH, SSM_HEADS), f32, math.log(1e-3), math.log(1e-1)))
    dt_bias = dt0 + jnp.log(-jnp.expm1(-dt0))
    a_log = jnp.log(jax.random.uniform(ks[7], (DEPTH, SSM_HEADS), f32, 1.0, 16.0))
    d_skip = 1.0 + nrm(ks[8], (DEPTH, SSM_HEADS), 0.01)
    ssm_norm_g = 1.0 + nrm(ks[9], (DEPTH, SSM_INNER), 0.01)
    w_ret_o = nrm(ks[10], (DEPTH, RET_V, D_MODEL), RET_V ** -0.5)
    w_ssm_o = nrm(ks[11], (DEPTH, SSM_INNER, D_MODEL), SSM_INNER ** -0.5)
    w_out = nrm(ks[12], (DEPTH, D_MODEL, D_MODEL), D_MODEL ** -0.5)
    norm_ffn_g = 1.0 + nrm(ks[13], (DEPTH, D_MODEL), 0.01)
    peer_w_q = nrm(ks[14], (DEPTH, D_MODEL, PEER_HEADS * PEER_QUERY), D_MODEL ** -0.5)
    peer_sub_keys = nrm(ks[15], (DEPTH, PEER_HEADS, 2, PEER_NKEYS, PEER_HALF), PEER_HALF ** -0.5)
    peer_u = nrm(ks[16], (DEPTH, PEER_EXPERTS, D_MODEL), D_MODEL ** -0.5)
    peer_v = nrm(ks[17], (DEPTH, PEER_EXPERTS, D_MODEL), PEER_HEADS ** -0.5)
    norm_final_g = 1.0 + nrm(ks[18], (D_MODEL,), 0.01)
    return {"x": x, "meta_tokens": meta_tokens, "norm_mix_g": norm_mix_g, "w_in": w_in,
            "conv_w": conv_w, "conv_b": conv_b, "dt_bias": dt_bias, "a_log": a_log,
            "d_skip": d_skip, "ssm_norm_g": ssm_norm_g, "w_ret_o": w_ret_o, "w_ssm_o": w_ssm_o,
            "w_out": w_out, "norm_ffn_g": norm_ffn_g, "peer_w_q": peer_w_q,
            "peer_sub_keys": peer_sub_keys, "peer_u": peer_u, "peer_v": peer_v,
            "norm_final_g": norm_final_g}


def reference(x, meta_tokens, norm_mix_g, w_in, conv_w, conv_b, dt_bias, a_log, d_skip,
              ssm_norm_g, w_ret_o, w_ssm_o, w_out, norm_ffn_g, peer_w_q, peer_sub_keys,
              peer_u, peer_v, norm_final_g):
    f32 = jnp.float32
    b = x.shape[0]
    h = jnp.concatenate(
        [jnp.broadcast_to(meta_tokens.astype(x.dtype)[None], (b, N_META, D_MODEL)), x], axis=1)
    seq_len = h.shape[1]
    pos = jnp.arange(seq_len, dtype=f32)
    log_gamma = jnp.log(1.0 - 2.0 ** (-5.0 - jnp.arange(RET_HEADS, dtype=f32)))
    split_points = np.cumsum(np.array(IN_SIZES))[:-1].tolist()

    for l in range(DEPTH):
        n = rmsnorm(h, norm_mix_g[l])
        proj = n @ w_in[l]
        q, k, v, g_ret, z, xbc, dt_raw, gate_ret, gate_ssm = jnp.split(proj, split_points, axis=-1)

        q = rotary(q.reshape(b, seq_len, RET_HEADS, RET_QK_HEAD), pos)
        k = rotary(k.reshape(b, seq_len, RET_HEADS, RET_QK_HEAD), pos) * (RET_QK_HEAD ** -0.5)
        v = v.reshape(b, seq_len, RET_HEADS, RET_V_HEAD)
        o = retention_chunked(pad_front(q), pad_front(k), pad_front(v), log_gamma)[:, LEAD:]
        o = _rms(o).reshape(b, seq_len, RET_V)
        y_ret = (jax.nn.silu(g_ret.astype(f32)) * o) @ w_ret_o[l]

        xbc = jax.nn.silu(causal_dwconv(xbc, conv_w[l], conv_b[l]))
        xs, bm, cm = jnp.split(xbc, [SSM_INNER, SSM_INNER + SSM_GROUPS * SSM_STATE], axis=-1)
        xs = xs.reshape(b, seq_len, SSM_HEADS, SSM_HEAD_DIM).astype(f32)
        bm = bm.reshape(b, seq_len, SSM_GROUPS, SSM_STATE)
        cm = cm.reshape(b, seq_len, SSM_GROUPS, SSM_STATE)
        dt = jax.nn.softplus(dt_raw.astype(f32) + dt_bias[l].astype(f32))
        a_cont = -jnp.exp(a_log[l].astype(f32))
        y = ssd_chunked(pad_front(xs * dt[..., None]), pad_front(dt * a_cont),
                        pad_front(bm), pad_front(cm))[:, LEAD:]
        y = y + d_skip[l].astype(f32)[:, None] * xs
        y = y.reshape(b, seq_len, SSM_INNER) * jax.nn.silu(z.astype(f32))
        y = _rms(y.reshape(b, seq_len, SSM_GROUPS, SSM_INNER // SSM_GROUPS)).reshape(
            b, seq_len, SSM_INNER) * ssm_norm_g[l].astype(f32)
        y_ssm = y @ w_ssm_o[l]

        merged = jax.nn.sigmoid(gate_ret.astype(f32)) * y_ret + jax.nn.sigmoid(gate_ssm.astype(f32)) * y_ssm
        h = h + (merged @ w_out[l]).astype(h.dtype)

        h = h + peer_ffn(rmsnorm(h, norm_ffn_g[l]), peer_w_q[l], peer_sub_keys[l],
                         peer_u[l], peer_v[l]).astype(h.dtype)

    return rmsnorm(h, norm_final_g)[:, N_META:]
```

```python
import numpy as np
from contextlib import ExitStack
import concourse.bass as bass
import concourse.mybir as mybir
from concourse.bass_utils import run_bass_kernel_spmd

F32 = mybir.dt.float32
BF16 = mybir.dt.bfloat16
I32 = mybir.dt.int32
U32 = mybir.dt.uint32
AF = mybir.ActivationFunctionType
ALU = mybir.AluOpType
AX = mybir.AxisListType

D = 2048
N_META = 16
LEAD = 112
EPS = 1e-6
RH, RDK, RDV = 8, 256, 512
SSM_INNER, SH, SP_, SG, SN = 4096, 64, 64, 8, 128
CONV_DIM = 6144
NPROJ = 26688
C_Q, C_K, C_V, C_G, C_Z, C_XBC, C_DT, C_GR, C_GS = 0, 2048, 4096, 8192, 12288, 16384, 22528, 22592, 24640
PTM_W = 16384
PH, PNK, PTOPK, PHALF = 8, 128, 16, 128
NEXP = 16384
HALO = 8


class Buf:
    __slots__ = ("writers", "readers", "name")

    def __init__(self, name=""):
        self.writers = {}
        self.readers = {}
        self.name = name


class KB:
    NQ = 20

    def __init__(self, nc):
        self.nc = nc
        self.engs = {"pe": nc.tensor, "act": nc.scalar, "dve": nc.vector, "pool": nc.gpsimd, "sp": nc.sync}
        self.epoch = 0
        self.csem = {e: nc.alloc_semaphore("c_" + e) for e in ("pe", "act", "dve", "pool")}
        self.ccnt = {e: 0 for e in self.csem}
        self.dsem = {q: [nc.alloc_semaphore("d_%s%d" % (q, i)) for i in range(self.NQ)] for q in ("sp", "pool")}
        self.dcnt = {q: 0 for q in self.dsem}
        self.waited = {e: {} for e in self.engs}
        self.n_ins = 0
        self.limit = None
        self.n_ops = 0

    def _wait(self, e, toks):
        w = self.waited[e]
        need = {}
        for key, (sem, val) in toks:
            if key[0] == "c":
                if key[2] < self.epoch:
                    continue
                if e == "pe" and key[1] == "pe":
                    continue
            if w.get(key, 0) >= val:
                continue
            if key not in need or need[key][1] < val:
                need[key] = (sem, val)
        for key, (sem, val) in need.items():
            self.engs[e].wait_ge(sem, val)
            w[key] = val
            self.n_ins += 1

    @staticmethod
    def _merge(dst, key, sv):
        if key not in dst or dst[key][1] < sv[1]:
            dst[key] = sv

    def _deps(self, reads, writes, accumulate=False):
        toks = []
        for b in reads:
            toks += list(b.writers.items())
        for b in writes:
            if not accumulate:
                toks += list(b.writers.items())
            toks += list(b.readers.items())
        return toks

    def _commit(self, key, sv, reads, writes, accumulate=False):
        for b in reads:
            self._merge(b.readers, key, sv)
        for b in writes:
            if accumulate:
                self._merge(b.writers, key, sv)
            else:
                b.writers = {key: sv}
                b.readers = {}

    def op(self, e, fn, reads=(), writes=()):
        self.n_ops += 1
        if self.limit is not None and self.n_ops > self.limit:
            return
        self._wait(e, self._deps(reads, writes))
        ins = fn(self.engs[e])
        self.ccnt[e] += 1
        ins.then_inc(self.csem[e], 1)
        self.n_ins += 1
        self._commit(("c", e, self.epoch), (self.csem[e], self.ccnt[e]), reads, writes)

    def dma(self, q, fn, reads=(), writes=(), accumulate=False):
        self.n_ops += 1
        if self.limit is not None and self.n_ops > self.limit:
            return
        i = self.dcnt[q]
        slot, gen = i % self.NQ, i // self.NQ
        key = ("d", q, slot)
        sem = self.dsem[q][slot]
        toks = self._deps(reads, writes, accumulate)
        if gen > 0:
            toks.append((key, (sem, 16 * gen)))
        self._wait(q, toks)
        ins = fn(self.engs[q])
        ins.then_inc(sem, 16)
        self.dcnt[q] += 1
        self.n_ins += 1
        self._commit(key, (sem, 16 * (gen + 1)), reads, writes, accumulate)

    def all_tokens(self):
        toks = [(("c", e, self.epoch), (self.csem[e], self.ccnt[e])) for e in self.csem if self.ccnt[e] > 0]
        for q in self.dsem:
            n = self.dcnt[q]
            for slot in range(min(n, self.NQ)):
                gens = (n - 1 - slot) // self.NQ + 1
                toks.append((("d", q, slot), (self.dsem[q][slot], 16 * gens)))
        return toks

    def barrier(self):
        toks = self.all_tokens()
        for e in self.engs:
            self._wait(e, toks)
        if self.limit is not None and self.n_ops > self.limit:
            return
        self.epoch += 1
        self.csem = {e: self.nc.alloc_semaphore("c_%s_%d" % (e, self.epoch)) for e in self.csem}
        self.ccnt = {e: 0 for e in self.csem}

    def maybe_epoch(self, thresh=30000):
        if max(self.ccnt.values()) > thresh:
            self.barrier()


def bc_mid(ap2d, n):
    p, f = ap2d.shape
    return ap2d.unsqueeze(1).to_broadcast([p, n, f])


def bc_last(ap2d, n):
    p, f = ap2d.shape
    return ap2d.unsqueeze(2).to_broadcast([p, f, n])


class Cfg:
    def __init__(self, pre=33, main=32, debug=False, phases=("proj", "ret", "ssd", "out", "peer"), tiny=()):
        self.tiny = set(tiny)
        self.pre = pre
        self.main = main
        self.debug = debug
        self.phases = phases
        self.nch = pre + main
        self.nt = self.nch * 128
        self.nt_main = main * 128


def build_program(cfg):
    nc = bass.Bass("TRN2", target_bir_lowering=False)
    kb = KB(nc)
    kb.limit = getattr(cfg, "limit", None)
    NT, NTM = cfg.nt, cfg.nt_main
    ROW0 = cfg.pre * 128
    dbg_kind = "ExternalOutput" if cfg.debug else "Internal"

    def ext_in(name, shape, dt=F32):
        if name in cfg.tiny:
            shape = [1, 8]
        return nc.dram_tensor(name, list(shape), dt, kind="ExternalInput").ap()

    hin = ext_in("hin", [NT, D])
    rowmask = ext_in("rowmask", [NT, 1])
    ropec = ext_in("ropec", [NT, 128])
    ropes = ext_in("ropes", [NT, 128])
    cst_f = ext_in("cst_f", [128, 4096])
    norm_mix_g = ext_in("norm_mix_g", [1, D])
    w_in = ext_in("w_in", [D, NPROJ])
    conv_w = ext_in("conv_w", [4, CONV_DIM])
    conv_b = ext_in("conv_b", [1, CONV_DIM])
    dt_bias = ext_in("dt_bias", [1, SH])
    a_log = ext_in("a_log", [1, SH])
    d_skip = ext_in("d_skip", [1, SH])
    ssm_norm_g = ext_in("ssm_norm_g", [1, SSM_INNER])
    w_ret_o = ext_in("w_ret_o", [RH * RDV, D])
    w_ssm_o = ext_in("w_ssm_o", [SSM_INNER, D])
    w_out = ext_in("w_out", [D, D])
    norm_ffn_g = ext_in("norm_ffn_g", [1, D])
    peer_w_q = ext_in("peer_w_q", [D, D])
    peer_sub_keys = ext_in("peer_sub_keys", [16 * 128, 128])
    peer_u = ext_in("peer_u", [NEXP, D])
    peer_v = ext_in("peer_v", [NEXP, D])
    norm_final_g = ext_in("norm_final_g", [1, D])
    out = nc.dram_tensor("out", [NTM, D], F32, kind="ExternalOutput").ap()

    PTMS = [nc.dram_tensor("PTM%d" % i, [NT, 4096], BF16, kind=dbg_kind).ap() for i in range(4)]

    class _PTM:
        def __getitem__(self, key):
            rows, cols = key
            i = cols.start // 4096
            assert (cols.stop - 1) // 4096 == i
            return PTMS[i][rows, cols.start - i * 4096:cols.stop - i * 4096]
    PTM = _PTM()
    XBCT = nc.dram_tensor("XBCT", [CONV_DIM, HALO + NT], BF16, kind=dbg_kind).ap()
    DTS = nc.dram_tensor("DTS", [NT, SH], F32, kind=dbg_kind).ap()
    GT = nc.dram_tensor("GT", [2 * D, NTM], BF16, kind=dbg_kind).ap()
    OGT = nc.dram_tensor("OGT", [RH * RDV, NTM], BF16, kind=dbg_kind).ap()
    YST = nc.dram_tensor("YST", [SSM_INNER, NTM], BF16, kind=dbg_kind).ap()
    H1 = nc.dram_tensor("H1", [NTM, D], F32, kind=dbg_kind).ap()
    XN2 = nc.dram_tensor("XN2", [NTM, D], BF16, kind=dbg_kind).ap()
    SC = nc.dram_tensor("SC", [NTM, 16 * 128], F32, kind=dbg_kind).ap()
    b_XN2, b_SC = Buf("XN2"), Buf("SC")
    NEX = 128 if "peer_u" in cfg.tiny else NEXP
    UVB = nc.dram_tensor("UVB", [NEX, 2 * D], BF16, kind="Internal").ap()
    b_UVB = Buf("UVB")
    WRB = nc.dram_tensor("WRB", [RH * RDV, D], BF16, kind="Internal").ap()
    WSB = nc.dram_tensor("WSB", [SSM_INNER, D], BF16, kind="Internal").ap()
    WOB = nc.dram_tensor("WOB", [D, D], BF16, kind="Internal").ap()
    WQB = nc.dram_tensor("WQB", [D, D], BF16, kind="Internal").ap()
    b_WB = Buf("WB")
    b_PTM, b_XBCT, b_DTS, b_GT, b_OGT, b_YST, b_H1, b_out = (Buf(n) for n in
                                                              ("PTM", "XBCT", "DTS", "GT", "OGT", "YST", "H1", "out"))

    es_glob = ExitStack()

    uniq = [0]

    def sb(es, name, shape, dt):
        uniq[0] += 1
        return es.enter_context(nc.sbuf_tensor("%s_%d" % (name, uniq[0]), list(shape), dt)).ap()

    def ps(es, name, shape, dt):
        uniq[0] += 1
        return es.enter_context(nc.psum_tensor("%s_%d" % (name, uniq[0]), list(shape), dt)).ap()

    ident_bf = sb(es_glob, "ident_bf", [128, 128], BF16)
    ident_f = sb(es_glob, "ident_f", [128, 128], F32)
    b_ident = Buf("ident")
    kb.op("pool", lambda e: e.memset(ident_f, 0.0), writes=[b_ident])
    kb.op("pool", lambda e: e.affine_select(out=ident_f, in_=ident_f, pattern=[[-1, 128]], compare_op=ALU.not_equal,
                                            fill=1.0, base=0, channel_multiplier=1), reads=[b_ident], writes=[b_ident])
    kb.op("dve", lambda e: e.tensor_copy(out=ident_bf, in_=ident_f), reads=[b_ident], writes=[b_ident])

    def phase_proj():
        with ExitStack() as es:
            TBMAX = 17 * 128
            g_bc = sb(es, "g_bc", [128, D], F32)
            b_g = Buf("g_bc")
            kb.dma("sp", lambda e: e.dma_start(out=g_bc, in_=norm_mix_g.partition_broadcast(128)), writes=[b_g])
            nT = sb(es, "nT", [128, 16, TBMAX], BF16)
            b_nT = Buf("nT")
            h_t = [sb(es, "h_t%d" % i, [128, D], F32) for i in range(2)]
            b_h = [Buf("h_t") for _ in range(2)]
            n_bf = [sb(es, "n_bf%d" % i, [128, D], BF16) for i in range(2)]
            b_n = [Buf("n_bf") for _ in range(2)]
            junk = sb(es, "junk", [128, D], BF16)
            b_junk = Buf("junk")
            stat = sb(es, "stat", [128, 4], F32)
            b_stat = Buf("stat")
            wst = [sb(es, "wst%d" % i, [128, 16, 512], F32) for i in range(2)]
            b_wst = [Buf("wst") for _ in range(2)]
            wbf = [sb(es, "wbf%d" % i, [128, 16, 512], BF16) for i in range(2)]
            b_wbf = [Buf("wbf") for _ in range(2)]
            ob = [sb(es, "ob%d" % i, [128, 512], BF16) for i in range(4)]
            b_ob = [Buf("ob") for _ in range(4)]
            obf = [sb(es, "obf%d" % i, [128, 64], F32) for i in range(2)]
            b_obf = [Buf("obf") for _ in range(2)]
            zt = sb(es, "zt", [128, HALO], BF16)
            b_zt = Buf("zt")
            ptr = ps(es, "ptr", [128, 16, 128], BF16)
            b_ptr = Buf("ptr")
            pmm = [ps(es, "pmm%d" % i, [128, 512], F32) for i in range(4)]
            b_pmm = [Buf("pmm") for _ in range(4)]

            kb.op("pool", lambda e: e.memset(zt, 0.0), writes=[b_zt])
            for r in range(CONV_DIM // 128):
                kb.dma("sp", lambda e, r=r: e.dma_start(out=XBCT[r * 128:(r + 1) * 128, 0:HALO], in_=zt),
                       reads=[b_zt], writes=[b_XBCT], accumulate=True)

            w_in_v = w_in.rearrange("(k p) n -> p k n", p=128)
            cnt = {"w": 0, "ob": 0, "pm": 0, "ev": 0, "obf": 0}

            def evac(dst, src, reads, writes):
                eng = "act" if cnt["ev"] % 2 == 0 else "dve"
                cnt["ev"] += 1
                if eng == "act":
                    kb.op("act", lambda e: e.activation(out=dst, in_=src, func=AF.Copy), reads=reads, writes=writes)
                else:
                    kb.op("dve", lambda e: e.tensor_copy(out=dst, in_=src), reads=reads, writes=writes)

            def do_block(ch0, nchk, colgroups):
                ntok = nchk * 128
                r0 = ch0 * 128
                for t in range(nchk):
                    i = t % 2
                    rows = slice(r0 + t * 128, r0 + (t + 1) * 128)
                    kb.dma("sp", lambda e, i=i, rows=rows: e.dma_start(out=h_t[i], in_=hin[rows, :]), writes=[b_h[i]])
                    kb.op("act", lambda e, i=i: e.activation(out=junk, in_=h_t[i], func=AF.Square, accum_out=stat[:, 0:1]),
                          reads=[b_h[i]], writes=[b_junk, b_stat])
                    kb.op("dve", lambda e: e.tensor_scalar(out=stat[:, 1:2], in0=stat[:, 0:1], scalar1=1.0 / D, scalar2=EPS,
                                                           op0=ALU.mult, op1=ALU.add), reads=[b_stat], writes=[b_stat])
                    kb.op("act", lambda e: e.activation(out=stat[:, 3:4], in_=stat[:, 1:2], func=AF.Sqrt),
                          reads=[b_stat], writes=[b_stat])
                    kb.op("dve", lambda e: e.reciprocal(out=stat[:, 2:3], in_=stat[:, 3:4]), reads=[b_stat], writes=[b_stat])
                    kb.op("dve", lambda e, i=i: e.scalar_tensor_tensor(out=n_bf[i], in0=h_t[i], scalar=stat[:, 2:3], in1=g_bc,
                                                                       op0=ALU.mult, op1=ALU.mult),
                          reads=[b_h[i], b_stat, b_g], writes=[b_n[i]])
                    for k in range(16):
                        kb.op("pe", lambda e, i=i, k=k: e.transpose(out=ptr[:, k, :], in_=n_bf[i][:, k * 128:(k + 1) * 128],
                                                                    identity=ident_bf),
                              reads=[b_n[i], b_ident], writes=[b_ptr])
                    evac(nT[:, :, t * 128:(t + 1) * 128], ptr, [b_ptr], [b_nT])
                for (kind, c0, ncol, dst_row0) in colgroups:
                    kb.maybe_epoch()
                    wi = cnt["w"] % 2
                    cnt["w"] += 1
                    kb.dma("sp", lambda e, wi=wi, c0=c0, ncol=ncol: e.dma_start(out=wst[wi][:, :, 0:ncol],
                                                                                 in_=w_in_v[:, :, c0:c0 + ncol]),
                           writes=[b_wst[wi]])
                    kb.op("pool", lambda e, wi=wi, ncol=ncol: e.tensor_copy(out=wbf[wi][:, :, 0:ncol], in_=wst[wi][:, :, 0:ncol]),
                          reads=[b_wst[wi]], writes=[b_wbf[wi]])
                    if kind == "tm":
                        for t in range(nchk):
                            pi = cnt["pm"] % 4
                            cnt["pm"] += 1
                            for k in range(16):
                                kb.op("pe", lambda e, pi=pi, k=k, t=t, wi=wi, ncol=ncol: e.matmul(
                                    pmm[pi][:, 0:ncol], lhsT=nT[:, k, t * 128:(t + 1) * 128], rhs=wbf[wi][:, k, 0:ncol],
                                    start=(k == 0), stop=(k == 15)), reads=[b_nT, b_wbf[wi]], writes=[b_pmm[pi]])
                            oi = cnt["ob"] % 4
                            cnt["ob"] += 1
                            evac(ob[oi][:, 0:ncol], pmm[pi][:, 0:ncol], [b_pmm[pi]], [b_ob[oi]])
                            rows = slice(r0 + t * 128, r0 + (t + 1) * 128)
                            kb.dma("sp", lambda e, oi=oi, rows=rows, c0=c0, ncol=ncol: e.dma_start(
                                out=PTM[rows, c0:c0 + ncol], in_=ob[oi][:, 0:ncol]),
                                reads=[b_ob[oi]], writes=[b_PTM], accumulate=True)
                    elif kind == "dt":
                        for t in range(nchk):
                            pi = cnt["pm"] % 4
                            cnt["pm"] += 1
                            for k in range(16):
                                kb.op("pe", lambda e, pi=pi, k=k, t=t, wi=wi: e.matmul(
                                    pmm[pi][:, 0:64], lhsT=nT[:, k, t * 128:(t + 1) * 128], rhs=wbf[wi][:, k, 0:64],
                                    start=(k == 0), stop=(k == 15)), reads=[b_nT, b_wbf[wi]], writes=[b_pmm[pi]])
                            oi = cnt["obf"] % 2
                            cnt["obf"] += 1
                            evac(obf[oi], pmm[pi][:, 0:64], [b_pmm[pi]], [b_obf[oi]])
                            rows = slice(r0 + t * 128, r0 + (t + 1) * 128)
                            kb.dma("sp", lambda e, oi=oi, rows=rows: e.dma_start(out=DTS[rows, :], in_=obf[oi]),
                                   reads=[b_obf[oi]], writes=[b_DTS], accumulate=True)
                    else:
                        for m in range(ncol // 128):
                            for tg in range(0, ntok, 512):
                                n = min(512, ntok - tg)
                                pi = cnt["pm"] % 4
                                cnt["pm"] += 1
                                for k in range(16):
                                    kb.op("pe", lambda e, pi=pi, k=k, m=m, tg=tg, n=n, wi=wi: e.matmul(
                                        pmm[pi][:, 0:n], lhsT=wbf[wi][:, k, m * 128:(m + 1) * 128], rhs=nT[:, k, tg:tg + n],
                                        start=(k == 0), stop=(k == 15)), reads=[b_nT, b_wbf[wi]], writes=[b_pmm[pi]])
                                oi = cnt["ob"] % 4
                                cnt["ob"] += 1
                                evac(ob[oi][:, 0:n], pmm[pi][:, 0:n], [b_pmm[pi]], [b_ob[oi]])
                                rr = dst_row0 + m * 128
                                if kind == "xbc":
                                    kb.dma("sp", lambda e, oi=oi, rr=rr, tg=tg, n=n: e.dma_start(
                                        out=XBCT[rr:rr + 128, HALO + r0 + tg:HALO + r0 + tg + n], in_=ob[oi][:, 0:n]),
                                        reads=[b_ob[oi]], writes=[b_XBCT], accumulate=True)
                                else:
                                    cc = r0 - ROW0 + tg
                                    kb.dma("sp", lambda e, oi=oi, rr=rr, cc=cc, n=n: e.dma_start(
                                        out=GT[rr:rr + 128, cc:cc + n], in_=ob[oi][:, 0:n]),
                                        reads=[b_ob[oi]], writes=[b_GT], accumulate=True)

            def groups(c_lo, c_hi, kind, dst_row0=0):
                return [(kind, c, min(512, c_hi - c), dst_row0 + (c - c_lo)) for c in range(c_lo, c_hi, 512)]

            pre_groups = (groups(C_K, C_G, "tm") + groups(C_XBC, C_DT, "xbc")
                          + [("dt", C_DT, 64, 0)])
            main_groups = (groups(0, PTM_W, "tm") + groups(C_XBC, C_DT, "xbc") + [("dt", C_DT, 64, 0)]
                           + groups(C_GR, NPROJ, "gt"))

            def blocks(c0, n):
                res, c = [], c0
                while n > 0:
                    m = min(n, 17 if n == 17 else 16)
                    res.append((c, m))
                    c += m
                    n -= m
                return res

            for (c, m) in blocks(0, cfg.pre):
                do_block(c, m, pre_groups)
            for (c, m) in blocks(cfg.pre, cfg.main):
                do_block(c, m, main_groups)
        kb.barrier()

    CO_UT, CO_SL, CO_ONE, CO_DQ, CO_DK, CO_DKZ, CO_G128 = 0, 128, 256, 384, 392, 400, 408

    def load_consts(es):
        cst = sb(es, "cst", [128, 512], F32)
        b_cst = Buf("cst")
        kb.dma("sp", lambda e: e.dma_start(out=cst, in_=cst_f[:, 0:512]), writes=[b_cst])
        return cst, b_cst

    def transpose_store(src_bf, b_src, nblk, ptr, b_ptr, oT, b_oT, dst_view, b_dst, col0):
        for r in range(0, nblk, 16):
            for k in range(16):
                kb.op("pe", lambda e, k=k, r=r: e.transpose(out=ptr[:, k, :], in_=src_bf[:, (r + k) * 128:(r + k + 1) * 128],
                                                            identity=ident_bf), reads=[b_src, b_ident], writes=[b_ptr])
            kb.op("act", lambda e, r=r: e.activation(out=oT[:, r:r + 16, :], in_=ptr, func=AF.Copy),
                  reads=[b_ptr], writes=[b_oT])
        kb.dma("sp", lambda e: e.dma_start(out=dst_view[:, :, col0:col0 + 128], in_=oT[:, 0:nblk, :]),
               reads=[b_oT], writes=[b_dst], accumulate=True)

    precast_state = {"done": False}

    def precast_units(es, ptile, b_ptile):
        gcol = sb(es, "gcol", [128, 32], F32); b_gcol = Buf()
        tmpg = sb(es, "tmpg", [32, 128], F32); b_tg = Buf()
        kb.dma("sp", lambda e: e.dma_start(out=tmpg, in_=ssm_norm_g.rearrange("o (kt p) -> (o kt) p", p=128)), writes=[b_tg])
        kb.op("pe", lambda e: e.transpose(out=ptile[:, 0:32], in_=tmpg, identity=ident_f[0:32, 0:32]),
              reads=[b_tg, b_ident], writes=[b_ptile])
        kb.op("dve", lambda e: e.tensor_copy(out=gcol, in_=ptile[:, 0:32]), reads=[b_ptile], writes=[b_gcol])
        stg = [sb(es, "stgw%d" % i, [128, D], F32) for i in range(3)]; b_stg = [Buf() for _ in range(3)]
        stb = [sb(es, "stbw%d" % i, [128, D], BF16) for i in range(3)]; b_stb = [Buf() for _ in range(3)]
        jobs = []
        for (src, dst, nblk, scale_g, b_dst) in ((w_ret_o, WRB, 32, False, b_WB), (w_ssm_o, WSB, 32, True, b_WB),
                                                 (w_out, WOB, 16, False, b_WB), (peer_w_q, WQB, 16, False, b_WB)):
            for r in range(nblk):
                jobs.append((src[r * 128:(r + 1) * 128, :], dst[r * 128:(r + 1) * 128, :], r if scale_g else None, b_dst))
        for r in range(NEX // 128):
            jobs.append((peer_u[r * 128:(r + 1) * 128, :], UVB[r * 128:(r + 1) * 128, 0:D], None, b_UVB))
            jobs.append((peer_v[r * 128:(r + 1) * 128, :], UVB[r * 128:(r + 1) * 128, D:2 * D], None, b_UVB))
        for n, (src_ap, dst_ap, gr, b_dst) in enumerate(jobs):
            i = n % 3
            kb.dma("sp", lambda e, i=i, src_ap=src_ap: e.dma_start(out=stg[i], in_=src_ap), writes=[b_stg[i]])
            if gr is not None:
                kb.op("pool", lambda e, i=i, gr=gr: e.tensor_scalar(out=stb[i], in0=stg[i], scalar1=gcol[:, gr:gr + 1], scalar2=None,
                                                                    op0=ALU.mult), reads=[b_stg[i], b_gcol], writes=[b_stb[i]])
            else:
                kb.op("pool", lambda e, i=i: e.tensor_copy(out=stb[i], in_=stg[i]), reads=[b_stg[i]], writes=[b_stb[i]])
            kb.dma("sp", lambda e, i=i, dst_ap=dst_ap: e.dma_start(out=dst_ap, in_=stb[i]), reads=[b_stb[i]], writes=[b_dst],
                   accumulate=True)
            yield n
        precast_state["done"] = True

    N_PRECAST = 96 + 2 * (NEX // 128)

    def phase_ret():
        with ExitStack() as es:
            cst, b_cst = load_consts(es)
            UT = cst[:, CO_UT:CO_UT + 128]
            st_f = sb(es, "st_f", [128, RH, 2, 512], F32)
            st_b = sb(es, "st_b", [128, RH, 2, 512], BF16)
            b_stf = [Buf("stf") for _ in range(RH)]
            b_stb = [Buf("stb") for _ in range(RH)]
            for h in range(RH):
                kb.op("pool", lambda e, h=h: e.memset(st_f[:, h], 0.0), writes=[b_stf[h]])
                kb.op("pool", lambda e, h=h: e.memset(st_b[:, h], 0.0), writes=[b_stb[h]])
            k_in = sb(es, "k_in", [128, 2048], BF16); b_kin = Buf()
            q_in = sb(es, "q_in", [128, 2048], BF16); b_qin = Buf()
            v_in = sb(es, "v_in", [128, 4096], BF16); b_vin = Buf()
            g_in = sb(es, "g_in", [128, 4096], BF16); b_gin = Buf()
            cos_t = sb(es, "cos_t", [128, 128], F32); b_cos = Buf()
            sin_t = sb(es, "sin_t", [128, 128], F32); b_sin = Buf()
            tmp1 = sb(es, "tmp1", [128, 8, 128], F32); b_t1 = Buf()
            tmp2 = sb(es, "tmp2", [128, 8, 128], F32); b_t2 = Buf()
            kr = sb(es, "kr", [128, 8, 2, 128], F32); b_kr = Buf()
            kt_ = sb(es, "kt_", [128, 8, 256], BF16); b_kt = Buf()
            kz_ = sb(es, "kz_", [128, 8, 256], BF16); b_kz = Buf()
            qt_ = sb(es, "qt_", [128, 8, 256], BF16); b_qt = Buf()
            qT = sb(es, "qT", [128, 16, 128], BF16); b_qT = Buf()
            kT = sb(es, "kT", [128, 16, 128], BF16); b_kT = Buf()
            sTm = [sb(es, "sTm%d" % i, [128, 128], BF16) for i in range(2)]; b_sTm = [Buf(), Buf()]
            o_sb = sb(es, "o_sb", [128, RH, 512], F32); b_osb = [Buf() for _ in range(RH)]
            sg = sb(es, "sg", [128, RH, 512], BF16); b_sg = Buf()
            og = sb(es, "og", [128, RH * 512], BF16); b_og = Buf()
            oT = sb(es, "oT", [128, 32, 128], BF16); b_oT = Buf()
            junk = sb(es, "junkr", [128, 512], BF16); b_junk = Buf()
            ssq = sb(es, "ssq", [128, 32], F32); b_ssq = Buf()
            ptr = ps(es, "ptr_r", [128, 16, 128], BF16); b_ptr = Buf()
            ps_s = ps(es, "ps_s", [128, 512], F32); b_pss = Buf()
            ps_o = [ps(es, "ps_o%d" % i, [128, 512], F32) for i in range(2)]; b_pso = [Buf(), Buf()]
            ps_u = [ps(es, "ps_u%d" % i, [128, 512], F32) for i in range(2)]; b_psu = [Buf(), Buf()]
            OGT_v = OGT.rearrange("(kt p) n -> p kt n", p=128)
            pc_gen = precast_units(es, ps_s, b_pss)
            pc_per_chunk = -(-N_PRECAST // cfg.nch)

            def rope(src, b_src, dst_list):
                v4 = src.rearrange("p (h f two) -> p h f two", h=8, two=2)
                t1, t2 = v4[:, :, :, 0], v4[:, :, :, 1]
                cb_, sb_ = bc_mid(cos_t, 8), bc_mid(sin_t, 8)
                kb.op("dve", lambda e: e.tensor_tensor(out=tmp1, in0=t1, in1=cb_, op=ALU.mult), reads=[b_src, b_cos], writes=[b_t1])
                kb.op("dve", lambda e: e.tensor_tensor(out=tmp2, in0=t2, in1=sb_, op=ALU.mult), reads=[b_src, b_sin], writes=[b_t2])
                kb.op("dve", lambda e: e.tensor_tensor(out=kr[:, :, 0, :], in0=tmp1, in1=tmp2, op=ALU.subtract),
                      reads=[b_t1, b_t2], writes=[b_kr])
                kb.op("dve", lambda e: e.tensor_tensor(out=tmp1, in0=t1, in1=sb_, op=ALU.mult), reads=[b_src, b_sin], writes=[b_t1])
                kb.op("dve", lambda e: e.tensor_tensor(out=tmp2, in0=t2, in1=cb_, op=ALU.mult), reads=[b_src, b_cos], writes=[b_t2])
                kb.op("dve", lambda e: e.tensor_tensor(out=kr[:, :, 1, :], in0=tmp1, in1=tmp2, op=ALU.add),
                      reads=[b_t1, b_t2, b_kr], writes=[b_kr])
                krv = kr.rearrange("p h two f -> p h (two f)")
                for (dst, b_dst, co) in dst_list:
                    kb.op("dve", lambda e, dst=dst, co=co: e.tensor_tensor(out=dst, in0=krv, in1=bc_last(cst[:, co:co + 8], 256),
                                                                           op=ALU.mult), reads=[b_kr, b_cst], writes=[b_dst])

            for ch in range(cfg.nch):
                kb.maybe_epoch()
                for _ in range(pc_per_chunk):
                    next(pc_gen, None)
                main = ch >= cfg.pre
                r0 = ch * 128
                rows = slice(r0, r0 + 128)
                kb.dma("sp", lambda e, rows=rows: e.dma_start(out=k_in, in_=PTM[rows, C_K:C_K + 2048]), reads=[b_PTM], writes=[b_kin])
                kb.dma("sp", lambda e, rows=rows: e.dma_start(out=v_in, in_=PTM[rows, C_V:C_V + 4096]), reads=[b_PTM], writes=[b_vin])
                kb.dma("sp", lambda e, rows=rows: e.dma_start(out=cos_t, in_=ropec[rows, :]), writes=[b_cos])
                kb.dma("sp", lambda e, rows=rows: e.dma_start(out=sin_t, in_=ropes[rows, :]), writes=[b_sin])
                if main:
                    kb.dma("sp", lambda e, rows=rows: e.dma_start(out=q_in, in_=PTM[rows, C_Q:C_Q + 2048]), reads=[b_PTM], writes=[b_qin])
                    kb.dma("sp", lambda e, rows=rows: e.dma_start(out=g_in, in_=PTM[rows, C_G:C_G + 4096]), reads=[b_PTM], writes=[b_gin])
                    rope(k_in, b_kin, [(kt_, b_kt, CO_DK), (kz_, b_kz, CO_DKZ)])
                    rope(q_in, b_qin, [(qt_, b_qt, CO_DQ)])
                    kb.op("act", lambda e: e.activation(out=sg.rearrange("p h f -> p (h f)"), in_=g_in, func=AF.Silu),
                          reads=[b_gin], writes=[b_sg])
                    for (src, b_src, dstT, b_dstT) in ((qt_, b_qt, qT, b_qT), (kt_, b_kt, kT, b_kT)):
                        s2 = src.rearrange("p h f -> p (h f)")
                        for k in range(16):
                            kb.op("pe", lambda e, k=k, s2=s2: e.transpose(out=ptr[:, k, :], in_=s2[:, k * 128:(k + 1) * 128],
                                                                          identity=ident_bf), reads=[b_src, b_ident], writes=[b_ptr])
                        kb.op("act", lambda e, dstT=dstT: e.activation(out=dstT, in_=ptr, func=AF.Copy), reads=[b_ptr], writes=[b_dstT])
                else:
                    rope(k_in, b_kin, [(kz_, b_kz, CO_DKZ)])
                for h in range(RH):
                    vh = v_in[:, h * 512:(h + 1) * 512]
                    if main:
                        pi = h % 2
                        for c in range(2):
                            kb.op("pe", lambda e, h=h, c=c: e.matmul(ps_s[:, 0:128], lhsT=kT[:, 2 * h + c, :], rhs=qT[:, 2 * h + c, :],
                                                                     start=(c == 0), stop=(c == 1)), reads=[b_kT, b_qT], writes=[b_pss])
                        kb.op("dve", lambda e, pi=pi: e.tensor_tensor(out=sTm[pi], in0=ps_s[:, 0:128], in1=UT, op=ALU.mult),
                              reads=[b_pss, b_cst], writes=[b_sTm[pi]])
                        kb.op("pe", lambda e, pi=pi, vh=vh: e.matmul(ps_o[pi], lhsT=sTm[pi], rhs=vh, start=True, stop=False),
                              reads=[b_sTm[pi], b_vin], writes=[b_pso[pi]])
                        for c in range(2):
                            kb.op("pe", lambda e, pi=pi, h=h, c=c: e.matmul(ps_o[pi], lhsT=qT[:, 2 * h + c, :], rhs=st_b[:, h, c, :],
                                                                            start=False, stop=(c == 1)),
                                  reads=[b_qT, b_stb[h]], writes=[b_pso[pi]])
                        kb.op("dve", lambda e, pi=pi, h=h: e.tensor_copy(out=o_sb[:, h, :], in_=ps_o[pi]), reads=[b_pso[pi]], writes=[b_osb[h]])
                        kb.op("act", lambda e, h=h: e.activation(out=junk, in_=o_sb[:, h, :], func=AF.Square,
                                                                 accum_out=ssq[:, h:h + 1]), reads=[b_osb[h]], writes=[b_junk, b_ssq])
                    for c in range(2):
                        kb.op("pe", lambda e, h=h, c=c, vh=vh: e.matmul(ps_u[c], lhsT=kz_[:, h, c * 128:(c + 1) * 128], rhs=vh,
                                                                        start=True, stop=True), reads=[b_kz, b_vin], writes=[b_psu[c]])
                        kb.op("dve", lambda e, h=h, c=c: e.scalar_tensor_tensor(out=st_f[:, h, c, :], in0=st_f[:, h, c, :],
                                                                                 scalar=cst[:, CO_G128 + h:CO_G128 + h + 1],
                                                                                 in1=ps_u[c], op0=ALU.mult, op1=ALU.add),
                              reads=[b_psu[c], b_cst, b_stf[h]], writes=[b_stf[h]])
                    kb.op("act", lambda e, h=h: e.activation(out=st_b[:, h], in_=st_f[:, h], func=AF.Copy), reads=[b_stf[h]], writes=[b_stb[h]])
                if main:
                    kb.op("dve", lambda e: e.tensor_scalar(out=ssq[:, 8:16], in0=ssq[:, 0:8], scalar1=1.0 / RDV, scalar2=EPS,
                                                           op0=ALU.mult, op1=ALU.add), reads=[b_ssq], writes=[b_ssq])
                    kb.op("act", lambda e: e.activation(out=ssq[:, 16:24], in_=ssq[:, 8:16], func=AF.Sqrt), reads=[b_ssq], writes=[b_ssq])
                    kb.op("dve", lambda e: e.reciprocal(out=ssq[:, 24:32], in_=ssq[:, 16:24]), reads=[b_ssq], writes=[b_ssq])
                    kb.op("dve", lambda e: e.tensor_tensor(out=o_sb, in0=o_sb, in1=bc_last(ssq[:, 24:32], 512), op=ALU.mult),
                          reads=b_osb + [b_ssq], writes=b_osb)
                    kb.op("dve", lambda e: e.tensor_tensor(out=og.rearrange("p (h f) -> p h f", h=RH), in0=o_sb, in1=sg, op=ALU.mult),
                          reads=b_osb + [b_sg], writes=[b_og])
                    transpose_store(og, b_og, 32, ptr, b_ptr, oT, b_oT, OGT_v, b_OGT, r0 - ROW0)
            for _ in pc_gen:
                pass
        kb.barrier()

    if "proj" in cfg.phases:
        phase_proj()
    def phase_ssd():
        with ExitStack() as es:
            cst, b_cst = load_consts(es)
            UT = cst[:, CO_UT:CO_UT + 128]
            SLm = cst[:, CO_SL:CO_SL + 128]
            ONES = cst[:, CO_ONE:CO_ONE + 128]
            diag = sb(es, "diag", [128, 48, 4, 128], BF16); b_diag = Buf()
            cwT = sb(es, "cwT", [128, 48, 8], F32); b_cwT = Buf()
            cb_bc = sb(es, "cb_bc", [128, 5120], BF16); b_cb = Buf()
            par = sb(es, "par", [128, 4, 64], F32); b_par = Buf()
            sst_f = sb(es, "sst_f", [128, SH, SP_], F32)
            sst_b = sb(es, "sst_b", [128, SH * SP_], BF16)
            b_sf = [Buf() for _ in range(SG)]; b_sbb = [Buf() for _ in range(SG)]
            ptr = ps(es, "ptr_s", [128, 16, 128], BF16); b_ptr = Buf()
            pcA = ps(es, "pcA", [128, 512], F32); b_pcA = Buf()
            pcB = ps(es, "pcB", [128, 512], F32); b_pcB = Buf()
            psm = ps(es, "psm", [128, 512], F32); b_psm = Buf()
            pseg = ps(es, "pseg", [128, 1024], F32); b_pseg = Buf()
            pst = ps(es, "pst", [128, 512], F32); b_pst = Buf()
            YST_v = YST.rearrange("(kt p) n -> p kt n", p=128)
            XB_v = XBCT.rearrange("(t p) c -> p t c", p=128)

            with ExitStack() as es2:
                cw5 = sb(es2, "cw5", [8, CONV_DIM], F32); b_cw5 = Buf()
                cbs = sb(es2, "cbs", [128, 5120], F32); b_cbs = Buf()
                kb.dma("sp", lambda e: e.dma_start(out=cw5[0:4, :], in_=conv_w), writes=[b_cw5])
                kb.dma("sp", lambda e: e.dma_start(out=cw5[4:5, :], in_=conv_b), writes=[b_cw5], accumulate=True)
                kb.dma("sp", lambda e: e.dma_start(out=cbs, in_=conv_b[0:1, 0:5120].partition_broadcast(128)), writes=[b_cbs])
                kb.op("dve", lambda e: e.tensor_copy(out=cb_bc, in_=cbs), reads=[b_cbs], writes=[b_cb])
                for t in range(48):
                    kb.op("pe", lambda e, t=t: e.transpose(out=psm[:, t * 8:t * 8 + 5], in_=cw5[0:5, t * 128:(t + 1) * 128],
                                                           identity=ident_f[0:5, 0:5]), reads=[b_cw5, b_ident], writes=[b_psm])
                kb.op("dve", lambda e: e.tensor_copy(out=cwT[:, :, 0:5], in_=psm[:, 0:384].rearrange("p (t e) -> p t e", e=8)[:, :, 0:5]),
                      reads=[b_psm], writes=[b_cwT])
                for t in range(48):
                    for w in range(4):
                        kb.op("dve", lambda e, t=t, w=w: e.tensor_scalar(out=diag[:, t, w, :], in0=ident_f, scalar1=cwT[:, t, w:w + 1],
                                                                         scalar2=None, op0=ALU.mult),
                              reads=[b_ident, b_cwT], writes=[b_diag])
                kb.dma("sp", lambda e: e.dma_start(out=par[:, 0, :], in_=dt_bias.partition_broadcast(128)), writes=[b_par])
                kb.dma("sp", lambda e: e.dma_start(out=par[:, 3, :], in_=a_log.partition_broadcast(128)), writes=[b_par], accumulate=True)
                kb.dma("sp", lambda e: e.dma_start(out=par[:, 2, :], in_=d_skip.partition_broadcast(128)), writes=[b_par], accumulate=True)
                kb.op("act", lambda e: e.activation(out=par[:, 1, :], in_=par[:, 3, :], func=AF.Exp), reads=[b_par], writes=[b_par])
                kb.op("dve", lambda e: e.tensor_scalar(out=par[:, 1, :], in0=par[:, 1, :], scalar1=-1.0, scalar2=None, op0=ALU.mult),
                      reads=[b_par], writes=[b_par])
                for g in range(SG):
                    kb.op("pool", lambda e, g=g: e.memset(sst_f[:, g * 8:(g + 1) * 8, :], 0.0), writes=[b_sf[g]])
                    kb.op("pool", lambda e, g=g: e.memset(sst_b[:, g * 512:(g + 1) * 512], 0.0), writes=[b_sbb[g]])
                kb.barrier()
            xw = sb(es, "xw", [128, 48, 131], BF16); b_xw = Buf()
            z_in = sb(es, "z_in", [128, 4096], BF16); b_zin = Buf()
            xs = sb(es, "xs", [128, SH, SP_], F32); b_xs = [Buf() for _ in range(SG)]
            xdt = sb(es, "xdt", [128, SH, SP_], BF16); b_xdt = Buf()
            decx = sb(es, "decx", [128, SH * SP_], BF16); b_decx = [Buf() for _ in range(SG)]
            Bt = sb(es, "Bt", [128, 1024], BF16); b_Bt = Buf()
            BCT = sb(es, "BCT", [128, 16, 128], BF16); b_BCT = Buf()
            segL = [sb(es, "segL%d" % i, [128, 8, 128], F32) for i in range(2)]; b_segL = [Buf(), Buf()]
            Lg = [sb(es, "Lg%d" % i, [128, 8, 128], F32) for i in range(2)]; b_Lg = [Buf(), Buf()]
            MT = [sb(es, "MT%d" % i, [128, 8, 128], BF16) for i in range(2)]; b_MT = [Buf(), Buf()]
            cbm = sb(es, "cbm", [128, 128], F32); b_cbm = Buf()
            tmpc = [sb(es, "tmpc%d" % i, [128, 512], F32) for i in range(2)]; b_tmpc = [Buf(), Buf()]
            sz = sb(es, "sz", [128, 4096], BF16); b_sz = Buf()
            ys = sb(es, "ys", [128, 4096], BF16); b_ys = Buf()
            oT = sb(es, "oT_s", [128, 32, 128], BF16); b_oT = Buf()
            sm = sb(es, "sm", [128, 8, 64], F32); b_sm = Buf()
            sm2 = sb(es, "sm2", [128, 128], F32); b_sm2 = Buf()
            dtr = sb(es, "dtr", [128, 64], F32); b_dtr = Buf()
            msk = sb(es, "msk", [128, 1], F32); b_msk = Buf()
            junk = sb(es, "junks", [128, 512], BF16); b_junk = Buf()
            ssq = sb(es, "ssq2", [128, 32], F32); b_ssq = Buf()
            xs2 = xs.rearrange("p h f -> p (h f)")
            xdt2 = xdt.rearrange("p h f -> p (h f)")
            sst_f2 = sst_f.rearrange("p h f -> p (h f)")

            for ch in range(cfg.nch):
                kb.maybe_epoch()
                main = ch >= cfg.pre
                r0 = ch * 128
                rows = slice(r0, r0 + 128)
                c0 = HALO + r0 - 3
                T = 48 if main else 40
                kb.dma("sp", lambda e, T=T, c0=c0: e.dma_start(out=xw[:, 0:T, :], in_=XB_v[:, 0:T, c0:c0 + 131]),
                       reads=[b_XBCT], writes=[b_xw])
                kb.dma("sp", lambda e, rows=rows: e.dma_start(out=dtr, in_=DTS[rows, :]), reads=[b_DTS], writes=[b_dtr])
                kb.dma("sp", lambda e, rows=rows: e.dma_start(out=msk, in_=rowmask[rows, :]), writes=[b_msk])
                if main:
                    kb.dma("sp", lambda e, rows=rows: e.dma_start(out=z_in, in_=PTM[rows, C_Z:C_Z + 4096]), reads=[b_PTM], writes=[b_zin])
                S = lambda i: sm[:, i, :]
                kb.op("dve", lambda e: e.tensor_tensor(out=S(0), in0=dtr, in1=par[:, 0, :], op=ALU.add), reads=[b_dtr, b_par], writes=[b_sm])
                kb.op("act", lambda e: e.activation(out=S(1), in_=S(0), func=AF.Abs), reads=[b_sm], writes=[b_sm])
                kb.op("act", lambda e: e.activation(out=S(2), in_=S(1), func=AF.Exp, scale=-1.0), reads=[b_sm], writes=[b_sm])
                kb.op("act", lambda e: e.activation(out=S(3), in_=S(2), func=AF.Ln, bias=ONES[:, 0:1]), reads=[b_sm, b_cst], writes=[b_sm])
                kb.op("dve", lambda e: e.scalar_tensor_tensor(out=S(4), in0=S(0), scalar=0.0, in1=S(3), op0=ALU.max, op1=ALU.add),
                      reads=[b_sm], writes=[b_sm])
                kb.op("dve", lambda e: e.tensor_scalar(out=S(5), in0=S(4), scalar1=msk[:, 0:1], scalar2=None, op0=ALU.mult),
                      reads=[b_sm, b_msk], writes=[b_sm])
                kb.op("dve", lambda e: e.tensor_tensor(out=S(6), in0=S(4), in1=par[:, 1, :], op=ALU.mult), reads=[b_sm, b_par], writes=[b_sm])
                a_ap = S(6)
                for bnk in range(10):
                    pc, b_pc = (pcA, b_pcA) if bnk % 2 == 0 else (pcB, b_pcB)
                    for tt in range(4):
                        t = bnk * 4 + tt
                        for w in range(4):
                            kb.op("pe", lambda e, pc=pc, t=t, tt=tt, w=w: e.matmul(pc[:, tt * 128:(tt + 1) * 128], lhsT=xw[:, t, w:w + 128],
                                                                                   rhs=diag[:, t, w, :], start=(w == 0), stop=(w == 3)),
                                  reads=[b_xw, b_diag], writes=[b_pc])
                    ti = bnk % 2
                    kb.op("dve", lambda e, pc=pc, ti=ti, bnk=bnk: e.tensor_tensor(out=tmpc[ti], in0=pc, in1=cb_bc[:, bnk * 512:(bnk + 1) * 512],
                                                                                  op=ALU.add), reads=[b_pc, b_cb], writes=[b_tmpc[ti]])
                    if bnk < 8:
                        kb.op("act", lambda e, ti=ti, bnk=bnk: e.activation(out=xs2[:, bnk * 512:(bnk + 1) * 512], in_=tmpc[ti], func=AF.Silu),
                              reads=[b_tmpc[ti]], writes=[b_xs[bnk]])
                    else:
                        kb.op("act", lambda e, ti=ti, bnk=bnk: e.activation(out=Bt[:, (bnk - 8) * 512:(bnk - 7) * 512], in_=tmpc[ti], func=AF.Silu),
                              reads=[b_tmpc[ti]], writes=[b_Bt])
                for bnk in range(4 if main else 2):
                    pc, b_pc = (pcA, b_pcA) if bnk % 2 == 0 else (pcB, b_pcB)
                    for tt in range(4):
                        t = 32 + bnk * 4 + tt
                        for w in range(4):
                            kb.op("pe", lambda e, pc=pc, t=t, tt=tt, w=w: e.matmul(pc[:, tt * 128:(tt + 1) * 128], lhsT=diag[:, t, w, :],
                                                                                   rhs=xw[:, t, w:w + 128], start=(w == 0), stop=(w == 3)),
                                  reads=[b_xw, b_diag], writes=[b_pc])
                    for tt in range(4):
                        t = 32 + bnk * 4 + tt
                        kb.op("act", lambda e, pc=pc, t=t, tt=tt: e.activation(out=BCT[:, t - 32, :], in_=pc[:, tt * 128:(tt + 1) * 128],
                                                                               func=AF.Silu, bias=cwT[:, t, 4:5]),
                              reads=[b_pc, b_cwT], writes=[b_BCT])
                kb.op("dve", lambda e: e.tensor_tensor(out=xdt, in0=xs, in1=bc_last(S(5), 64), op=ALU.mult), reads=b_xs + [b_sm], writes=[b_xdt])
                if main:
                    kb.op("dve", lambda e: e.tensor_tensor(out=xs, in0=xs, in1=bc_last(par[:, 2, :], 64), op=ALU.mult),
                          reads=b_xs + [b_par], writes=b_xs)
                    kb.op("act", lambda e: e.activation(out=sz, in_=z_in, func=AF.Silu), reads=[b_zin], writes=[b_sz])
                kb.op("pe", lambda e: e.matmul(psm[:, 0:64], lhsT=UT, rhs=a_ap, start=True, stop=True), reads=[b_cst, b_sm], writes=[b_psm])
                kb.op("pe", lambda e: e.matmul(psm[:, 64:128], lhsT=ONES, rhs=a_ap, start=True, stop=True), reads=[b_cst, b_sm], writes=[b_psm])
                kb.op("act", lambda e: e.activation(out=sm2, in_=psm[:, 0:128], func=AF.Exp), reads=[b_psm], writes=[b_sm2])
                eacs, cdec = sm2[:, 0:64], sm2[:, 64:128]
                for g in range(SG):
                    i2 = g % 2
                    hs = slice(g * 8, (g + 1) * 8)
                    cs = slice(g * 512, (g + 1) * 512)
                    kb.op("pool", lambda e, i2=i2, hs=hs: e.tensor_tensor(out=segL[i2], in0=bc_mid(SLm, 8), in1=bc_last(a_ap[:, hs], 128),
                                                                          op=ALU.mult), reads=[b_cst, b_sm], writes=[b_segL[i2]])
                    for hh in range(8):
                        kb.op("pe", lambda e, i2=i2, hh=hh: e.matmul(pseg[:, hh * 128:(hh + 1) * 128], lhsT=segL[i2][:, hh, :], rhs=UT,
                                                                     start=True, stop=True), reads=[b_segL[i2], b_cst], writes=[b_pseg])
                    Lg2 = Lg[i2].rearrange("p h i -> p (h i)")
                    for hf in range(2):
                        kb.op("act", lambda e, Lg2=Lg2, hf=hf: e.activation(out=Lg2[:, hf * 512:(hf + 1) * 512],
                                                                            in_=pseg[:, hf * 512:(hf + 1) * 512], func=AF.Exp),
                              reads=[b_pseg], writes=[b_Lg[i2]])
                    dec_g = Lg[i2][:, :, 127]
                    kb.op("dve", lambda e, i2=i2, hs=hs, dec_g=dec_g: e.tensor_tensor(out=decx.rearrange("p (h f) -> p h f", f=64)[:, hs, :],
                                                                                      in0=xdt[:, hs, :],
                                                                                      in1=dec_g.unsqueeze(2).to_broadcast([128, 8, 64]),
                                                                                      op=ALU.mult),
                          reads=[b_xdt, b_Lg[i2]], writes=[b_decx[g]])
                    if main:
                        kb.op("pe", lambda e, g=g: e.matmul(psm[:, 128:256], lhsT=BCT[:, g, :], rhs=BCT[:, 8 + g, :], start=True, stop=True),
                              reads=[b_BCT], writes=[b_psm])
                        kb.op("dve", lambda e: e.tensor_tensor(out=cbm, in0=psm[:, 128:256], in1=UT, op=ALU.mult),
                              reads=[b_psm, b_cst], writes=[b_cbm])
                        kb.op("pool", lambda e, i2=i2: e.tensor_tensor(out=MT[i2], in0=Lg[i2], in1=bc_mid(cbm, 8), op=ALU.mult),
                              reads=[b_Lg[i2], b_cbm], writes=[b_MT[i2]])
                        for hh in range(8):
                            kb.op("pe", lambda e, i2=i2, hh=hh, g=g: e.matmul(pcA[:, hh * 64:(hh + 1) * 64], lhsT=MT[i2][:, hh, :],
                                                                              rhs=xdt[:, g * 8 + hh, :], start=True, stop=True),
                                  reads=[b_MT[i2], b_xdt], writes=[b_pcA])
                        kb.op("pe", lambda e, g=g, cs=cs: e.matmul(pcB, lhsT=BCT[:, 8 + g, :], rhs=sst_b[:, cs], start=True, stop=True),
                              reads=[b_BCT, b_sbb[g]], writes=[b_pcB])
                        kb.op("dve", lambda e, hs=hs: e.tensor_tensor(out=tmpc[0].rearrange("p (h f) -> p h f", f=64),
                                                                      in0=pcB.rearrange("p (h f) -> p h f", f=64),
                                                                      in1=bc_last(eacs[:, hs], 64), op=ALU.mult),
                              reads=[b_pcB, b_sm2], writes=[b_tmpc[0]])
                        kb.op("dve", lambda e: e.tensor_tensor(out=tmpc[1], in0=tmpc[0], in1=pcA, op=ALU.add),
                              reads=[b_tmpc[0], b_pcA], writes=[b_tmpc[1]])
                        kb.op("dve", lambda e, cs=cs: e.tensor_tensor(out=xs2[:, cs], in0=xs2[:, cs], in1=tmpc[1], op=ALU.add),
                              reads=[b_xs[g], b_tmpc[1]], writes=[b_xs[g]])
                    kb.op("pe", lambda e, g=g, cs=cs: e.matmul(pst, lhsT=Bt[:, g * 128:(g + 1) * 128], rhs=decx[:, cs], start=True, stop=True),
                          reads=[b_Bt, b_decx[g]], writes=[b_pst])
                    kb.op("dve", lambda e, hs=hs: e.tensor_tensor(out=sst_f[:, hs, :], in0=sst_f[:, hs, :], in1=bc_last(cdec[:, hs], 64),
                                                                  op=ALU.mult), reads=[b_sf[g], b_sm2], writes=[b_sf[g]])
                    kb.op("dve", lambda e, cs=cs: e.tensor_tensor(out=sst_f2[:, cs], in0=sst_f2[:, cs], in1=pst, op=ALU.add),
                          reads=[b_sf[g], b_pst], writes=[b_sf[g]])
                    kb.op("act", lambda e, cs=cs: e.activation(out=sst_b[:, cs], in_=sst_f2[:, cs], func=AF.Copy),
                          reads=[b_sf[g]], writes=[b_sbb[g]])
                if main:
                    kb.op("dve", lambda e: e.tensor_tensor(out=xs2, in0=xs2, in1=sz, op=ALU.mult), reads=b_xs + [b_sz], writes=b_xs)
                    for g in range(SG):
                        kb.op("act", lambda e, g=g: e.activation(out=junk, in_=xs2[:, g * 512:(g + 1) * 512], func=AF.Square,
                                                                 accum_out=ssq[:, g:g + 1]), reads=[b_xs[g]], writes=[b_junk, b_ssq])
                    kb.op("dve", lambda e: e.tensor_scalar(out=ssq[:, 8:16], in0=ssq[:, 0:8], scalar1=1.0 / 512, scalar2=EPS,
                                                           op0=ALU.mult, op1=ALU.add), reads=[b_ssq], writes=[b_ssq])
                    kb.op("act", lambda e: e.activation(out=ssq[:, 16:24], in_=ssq[:, 8:16], func=AF.Sqrt), reads=[b_ssq], writes=[b_ssq])
                    kb.op("dve", lambda e: e.reciprocal(out=ssq[:, 24:32], in_=ssq[:, 16:24]), reads=[b_ssq], writes=[b_ssq])
                    kb.op("dve", lambda e: e.tensor_tensor(out=ys.rearrange("p (g f) -> p g f", f=512),
                                                           in0=xs2.rearrange("p (g f) -> p g f", f=512),
                                                           in1=bc_last(ssq[:, 24:32], 512), op=ALU.mult),
                          reads=b_xs + [b_ssq], writes=[b_ys])
                    transpose_store(ys, b_ys, 32, ptr, b_ptr, oT, b_oT, YST_v, b_YST, r0 - ROW0)
        kb.barrier()

    if "ret" in cfg.phases:
        phase_ret()
    def phase_out():
        with ExitStack() as es:
            TBC = 3
            TBM = TBC * 128
            g_bc = sb(es, "gf_bc", [128, D], F32); b_g = Buf()
            kb.dma("sp", lambda e: e.dma_start(out=g_bc, in_=norm_ffn_g.partition_broadcast(128)), writes=[b_g])
            skT = sb(es, "skT", [128, 16, 128], F32); b_skT = Buf()
            ptr = ps(es, "ptr_o", [128, 16, 128], BF16); b_ptr = Buf()
            pA = ps(es, "pA", [128, 512], F32); b_pA = Buf()
            pB = ps(es, "pB", [128, 512], F32); b_pB = Buf()
            pC = [ps(es, "pC%d" % i, [128, 512], F32) for i in range(2)]; b_pC = [Buf(), Buf()]
            pD = ps(es, "pD", [128, 512], F32); b_pD = Buf()

            with ExitStack() as es2:
                skr = sb(es2, "skr", [128, 16, 128], F32); b_skr = Buf()
                kb.dma("sp", lambda e: e.dma_start(out=skr, in_=peer_sub_keys.rearrange("(j k) d -> k j d", k=128)), writes=[b_skr])
                for j in range(16):
                    kb.op("pe", lambda e, j=j: e.transpose(out=pC[j % 2][:, 0:128], in_=skr[:, j, :], identity=ident_f),
                          reads=[b_skr, b_ident], writes=[b_pC[j % 2]])
                    kb.op("dve", lambda e, j=j: e.tensor_copy(out=skT[:, j, :], in_=pC[j % 2][:, 0:128]), reads=[b_pC[j % 2]], writes=[b_skT])
                if not precast_state["done"]:
                    for _ in precast_units(es2, pA, b_pA):
                        pass
                kb.barrier()

            ogT = sb(es, "ogT", [128, 32, TBM], BF16); b_ogT = Buf()
            ysT = sb(es, "ysT", [128, 32, TBM], BF16); b_ysT = Buf()
            mT = sb(es, "mT", [128, 16, TBM], BF16); b_mT = Buf()
            xT = sb(es, "xT", [128, 16, TBM], BF16); b_xT = Buf()
            hb = sb(es, "hb", [128, TBC, D], F32); b_hb = [Buf() for _ in range(TBC)]
            NWB = 4
            wbf = [sb(es, "wbf_o%d" % i, [128, 4096], BF16) for i in range(NWB)]; b_wbf = [Buf() for _ in range(NWB)]
            grt = [sb(es, "grt%d" % i, [128, 2, TBM], BF16) for i in range(2)]; b_grt = [Buf(), Buf()]
            sgt = [sb(es, "sgt%d" % i, [128, 2, TBM], F32) for i in range(2)]; b_sgt = [Buf(), Buf()]
            t1 = sb(es, "t1o", [128, TBM], F32); b_t1 = Buf()
            t2 = sb(es, "t2o", [128, TBM], F32); b_t2 = Buf()
            n_bf = sb(es, "n_bf_o", [128, D], BF16); b_n = Buf()
            junk = sb(es, "junk_o", [128, D], BF16); b_junk = Buf()
            stat = sb(es, "stat_o", [128, 4], F32); b_stat = Buf()
            qpT = sb(es, "qpT", [128, TBM], F32); b_qpT = Buf()
            sct = sb(es, "sct", [128, TBC, 128], F32); b_sct = Buf()
            OGT_v = OGT.rearrange("(kt p) n -> p kt n", p=128)
            YST_v = YST.rearrange("(kt p) n -> p kt n", p=128)
            wr_v = WRB.rearrange("(kt p) c -> p kt c", p=128)
            ws_v = WSB.rearrange("(kt p) c -> p kt c", p=128)
            wo_v = WOB.rearrange("(kt p) c -> p kt c", p=128)
            wq_v = WQB.rearrange("(kt p) c -> p kt c", p=128)
            SC_v = SC.rearrange("(t p) c -> p t c", p=128)
            cnt = {"w": 0, "g": 0, "pc": 0}

            def load_w(view, kts, c0, ncol, scale_g=False):
                wi = cnt["w"] % NWB
                cnt["w"] += 1
                dsb = wbf[wi][:, 0:kts * ncol].rearrange("p (k c) -> p k c", c=ncol)
                kb.dma("sp", lambda e: e.dma_start(out=dsb, in_=view[:, :, c0:c0 + ncol]), reads=[b_WB], writes=[b_wbf[wi]])
                return dsb, b_wbf[wi]

            ch = 0
            while ch < cfg.main:
                nsub = min(TBC, cfg.main - ch)
                ntok = nsub * 128
                c0 = ch * 128
                kb.dma("sp", lambda e, c0=c0, ntok=ntok: e.dma_start(out=ogT[:, :, 0:ntok], in_=OGT_v[:, :, c0:c0 + ntok]),
                       reads=[b_OGT], writes=[b_ogT])
                kb.dma("sp", lambda e, c0=c0, ntok=ntok: e.dma_start(out=ysT[:, :, 0:ntok], in_=YST_v[:, :, c0:c0 + ntok]),
                       reads=[b_YST], writes=[b_ysT])
                for t in range(nsub):
                    rows = slice(ROW0 + c0 + t * 128, ROW0 + c0 + (t + 1) * 128)
                    kb.dma("sp", lambda e, t=t, rows=rows: e.dma_start(out=hb[:, t, :], in_=hin[rows, :]), writes=[b_hb[t]])
                for m in range(16):
                    kb.maybe_epoch()
                    wr, b_wr = load_w(wr_v, 32, m * 128, 128)
                    for kt in range(32):
                        kb.op("pe", lambda e, kt=kt, wr=wr, ntok=ntok: e.matmul(pA[:, 0:ntok], lhsT=wr[:, kt, :], rhs=ogT[:, kt, 0:ntok],
                                                                                start=(kt == 0), stop=(kt == 31)),
                              reads=[b_wr, b_ogT], writes=[b_pA])
                    ws, b_ws = load_w(ws_v, 32, m * 128, 128, scale_g=True)
                    for kt in range(32):
                        kb.op("pe", lambda e, kt=kt, ws=ws, ntok=ntok: e.matmul(pB[:, 0:ntok], lhsT=ws[:, kt, :], rhs=ysT[:, kt, 0:ntok],
                                                                                start=(kt == 0), stop=(kt == 31)),
                              reads=[b_ws, b_ysT], writes=[b_pB])
                    gi = cnt["g"] % 2
                    cnt["g"] += 1
                    kb.dma("sp", lambda e, gi=gi, m=m, c0=c0, ntok=ntok: e.dma_start(out=grt[gi][:, 0, 0:ntok],
                                                                                     in_=GT[m * 128:(m + 1) * 128, c0:c0 + ntok]),
                           reads=[b_GT], writes=[b_grt[gi]])
                    kb.dma("sp", lambda e, gi=gi, m=m, c0=c0, ntok=ntok: e.dma_start(out=grt[gi][:, 1, 0:ntok],
                                                                                     in_=GT[D + m * 128:D + (m + 1) * 128, c0:c0 + ntok]),
                           reads=[b_GT], writes=[b_grt[gi]], accumulate=True)
                    kb.op("act", lambda e, gi=gi, ntok=ntok: e.activation(out=sgt[gi][:, :, 0:ntok], in_=grt[gi][:, :, 0:ntok], func=AF.Sigmoid),
                          reads=[b_grt[gi]], writes=[b_sgt[gi]])
                    kb.op("dve", lambda e, gi=gi, ntok=ntok: e.tensor_tensor(out=t1[:, 0:ntok], in0=pA[:, 0:ntok], in1=sgt[gi][:, 0, 0:ntok],
                                                                             op=ALU.mult), reads=[b_pA, b_sgt[gi]], writes=[b_t1])
                    kb.op("dve", lambda e, gi=gi, ntok=ntok: e.tensor_tensor(out=t2[:, 0:ntok], in0=pB[:, 0:ntok], in1=sgt[gi][:, 1, 0:ntok],
                                                                             op=ALU.mult), reads=[b_pB, b_sgt[gi]], writes=[b_t2])
                    kb.op("dve", lambda e, m=m, ntok=ntok: e.tensor_tensor(out=mT[:, m, 0:ntok], in0=t1[:, 0:ntok], in1=t2[:, 0:ntok],
                                                                           op=ALU.add), reads=[b_t1, b_t2], writes=[b_mT])
                for nb in range(8):
                    wo, b_wo = load_w(wo_v, 16, nb * 256, 256)
                    for t in range(nsub):
                        pi = cnt["pc"] % 2
                        cnt["pc"] += 1
                        for kt in range(16):
                            kb.op("pe", lambda e, pi=pi, kt=kt, t=t, wo=wo: e.matmul(pC[pi][:, 0:256], lhsT=mT[:, kt, t * 128:(t + 1) * 128],
                                                                                     rhs=wo[:, kt, :], start=(kt == 0), stop=(kt == 15)),
                                  reads=[b_mT, b_wo], writes=[b_pC[pi]])
                        kb.op("dve", lambda e, pi=pi, t=t, nb=nb: e.tensor_tensor(out=hb[:, t, nb * 256:(nb + 1) * 256],
                                                                                  in0=hb[:, t, nb * 256:(nb + 1) * 256], in1=pC[pi][:, 0:256],
                                                                                  op=ALU.add), reads=[b_hb[t], b_pC[pi]], writes=[b_hb[t]])
                for t in range(nsub):
                    rows = slice(c0 + t * 128, c0 + (t + 1) * 128)
                    kb.dma("sp", lambda e, t=t, rows=rows: e.dma_start(out=H1[rows, :], in_=hb[:, t, :]), reads=[b_hb[t]], writes=[b_H1],
                           accumulate=True)
                    kb.op("act", lambda e, t=t: e.activation(out=junk, in_=hb[:, t, :], func=AF.Square, accum_out=stat[:, 0:1]),
                          reads=[b_hb[t]], writes=[b_junk, b_stat])
                    kb.op("dve", lambda e: e.tensor_scalar(out=stat[:, 1:2], in0=stat[:, 0:1], scalar1=1.0 / D, scalar2=EPS,
                                                           op0=ALU.mult, op1=ALU.add), reads=[b_stat], writes=[b_stat])
                    kb.op("act", lambda e: e.activation(out=stat[:, 3:4], in_=stat[:, 1:2], func=AF.Sqrt), reads=[b_stat], writes=[b_stat])
                    kb.op("dve", lambda e: e.reciprocal(out=stat[:, 2:3], in_=stat[:, 3:4]), reads=[b_stat], writes=[b_stat])
                    kb.op("dve", lambda e, t=t: e.scalar_tensor_tensor(out=n_bf, in0=hb[:, t, :], scalar=stat[:, 2:3], in1=g_bc,
                                                                       op0=ALU.mult, op1=ALU.mult), reads=[b_hb[t], b_stat, b_g], writes=[b_n])
                    kb.dma("sp", lambda e, rows=rows: e.dma_start(out=XN2[rows, :], in_=n_bf), reads=[b_n], writes=[b_XN2], accumulate=True)
                    for k in range(16):
                        kb.op("pe", lambda e, k=k: e.transpose(out=ptr[:, k, :], in_=n_bf[:, k * 128:(k + 1) * 128], identity=ident_bf),
                              reads=[b_n, b_ident], writes=[b_ptr])
                    kb.op("act", lambda e, t=t: e.activation(out=xT[:, :, t * 128:(t + 1) * 128], in_=ptr, func=AF.Copy),
                          reads=[b_ptr], writes=[b_xT])
                for j in range(16):
                    wq, b_wq = load_w(wq_v, 16, j * 128, 128)
                    pi = cnt["pc"] % 2
                    cnt["pc"] += 1
                    for kt in range(16):
                        kb.op("pe", lambda e, pi=pi, kt=kt, wq=wq, ntok=ntok: e.matmul(pC[pi][:, 0:ntok], lhsT=wq[:, kt, :], rhs=xT[:, kt, 0:ntok],
                                                                                       start=(kt == 0), stop=(kt == 15)),
                              reads=[b_wq, b_xT], writes=[b_pC[pi]])
                    kb.op("act", lambda e, pi=pi, ntok=ntok: e.activation(out=qpT[:, 0:ntok], in_=pC[pi][:, 0:ntok], func=AF.Copy),
                          reads=[b_pC[pi]], writes=[b_qpT])
                    for t in range(nsub):
                        kb.op("pe", lambda e, t=t, j=j: e.matmul(pD[:, t * 128:(t + 1) * 128], lhsT=qpT[:, t * 128:(t + 1) * 128], rhs=skT[:, j, :],
                                                                 start=True, stop=True), reads=[b_qpT, b_skT], writes=[b_pD])
                    kb.op("dve", lambda e, nsub=nsub, ntok=ntok: e.tensor_copy(out=sct[:, 0:nsub, :],
                                                                               in_=pD[:, 0:ntok].rearrange("p (t k) -> p t k", k=128)),
                          reads=[b_pD], writes=[b_sct])
                    kb.dma("sp", lambda e, j=j, ch=ch, nsub=nsub: e.dma_start(out=SC_v[:, ch:ch + nsub, j * 128:(j + 1) * 128], in_=sct[:, 0:nsub, :]),
                           reads=[b_sct], writes=[b_SC], accumulate=True)
                ch += nsub
        kb.barrier()

    if "ssd" in cfg.phases:
        phase_ssd()
    def phase_peer():
        with ExitStack() as es:
            NEG = -1.0e30
            NB = 6
            gf_bc = sb(es, "gfin_bc", [128, D], F32); b_gf = Buf()
            kb.dma("sp", lambda e: e.dma_start(out=gf_bc, in_=norm_final_g.partition_broadcast(128)), writes=[b_gf])
            iota = sb(es, "iota", [128, 256], F32); b_iota = Buf()
            iota_i = sb(es, "iota_i", [128, 256], I32)
            kb.op("pool", lambda e: e.iota(iota_i, pattern=[[1, 256]], base=0, channel_multiplier=0), writes=[b_iota])
            kb.op("dve", lambda e: e.tensor_copy(out=iota, in_=iota_i), reads=[b_iota], writes=[b_iota])
            if not precast_state["done"]:
                with ExitStack() as es2:
                    ptmp = ps(es2, "ptmp", [128, 512], F32); b_ptmp = Buf()
                    for _ in precast_units(es2, ptmp, b_ptmp):
                        pass
                    kb.barrier()
            sc = sb(es, "sc", [128, 16, 128], F32); b_sc = Buf()
            work = sb(es, "work", [128, 256], F32); b_work = Buf()
            v16 = sb(es, "v16", [128, 16, 16], F32); b_v16 = Buf()
            i16 = sb(es, "i16", [128, 16, 16], U32); b_i16 = Buf()
            i16f = sb(es, "i16f", [128, 16, 16], F32); b_i16f = Buf()
            i16s = sb(es, "i16s", [128, 16, 16], F32); b_i16s = Buf()
            cand = sb(es, "cand", [128, 8, 256], F32); b_cand = Buf()
            cid = sb(es, "cid", [128, 8, 256], F32); b_cid = Buf()
            top = sb(es, "top", [128, 8, 16], F32); b_top = Buf()
            pos = sb(es, "pos", [128, 8, 16], U32); b_pos = Buf()
            posf = sb(es, "posf", [128, 8, 16], F32); b_posf = Buf()
            eq = sb(es, "eq", [128, 16, 256], F32); b_eq = Buf()
            eidf = sb(es, "eidf", [128, 128], F32); b_eidf = Buf()
            eid = sb(es, "eid", [128, 128], I32); b_eid = Buf()
            gsm = sb(es, "gsm", [128, 4, 128], F32); b_gsm = Buf()
            gz = sb(es, "gz", [128, 16], F32); b_gz = Buf()
            act_t = sb(es, "act_t", [128, 128], F32); b_act = Buf()
            wgt = sb(es, "wgt", [128, 128], F32); b_wgt = Buf()
            GS, NGB = 4, 3
            dgs = [sb(es, "dgs%d" % i, [128, GS, 128], BF16) for i in range(NGB)]; b_dgs = [Buf() for _ in range(NGB)]
            uv = [sb(es, "uv%d" % i, [128, GS, 2 * D], BF16) for i in range(NGB)]
            b_uv = [[Buf() for _ in range(GS)] for _ in range(NGB)]
            b_actg = [Buf() for _ in range(128 // GS)]
            xn = sb(es, "xn", [128, D], BF16); b_xn = Buf()
            h1t = sb(es, "h1t", [128, D], F32); b_h1 = Buf()
            junk = sb(es, "junk_p", [128, D], BF16); b_junk = Buf()
            prod = [sb(es, "prod%d" % i, [128, D], BF16) for i in range(2)]; b_prod = [Buf(), Buf()]
            stat = sb(es, "stat_p", [128, 4], F32); b_stat = Buf()
            ot = sb(es, "ot", [128, D], F32); b_ot = Buf()
            pacc = [ps(es, "pacc%d" % i, [128, 512], F32) for i in range(4)]; b_pacc = [Buf() for _ in range(4)]
            cnt = {"u": 0, "v": 0}

            for ch in range(cfg.main):
                kb.maybe_epoch()
                rows = slice(ch * 128, (ch + 1) * 128)
                kb.dma("sp", lambda e, rows=rows: e.dma_start(out=sc.rearrange("p j k -> p (j k)"), in_=SC[rows, :]), reads=[b_SC], writes=[b_sc])
                kb.dma("sp", lambda e, rows=rows: e.dma_start(out=xn, in_=XN2[rows, :]), reads=[b_XN2], writes=[b_xn])
                kb.dma("sp", lambda e, rows=rows: e.dma_start(out=h1t, in_=H1[rows, :]), reads=[b_H1], writes=[b_h1])
                for j in range(16):
                    kb.op("dve", lambda e, j=j: e.max(out=v16[:, j, 0:8], in_=sc[:, j, :]), reads=[b_sc], writes=[b_v16])
                    kb.op("dve", lambda e, j=j: e.match_replace(out=work[:, 0:128], in_to_replace=v16[:, j, 0:8], in_values=sc[:, j, :],
                                                                imm_value=NEG), reads=[b_sc, b_v16], writes=[b_work])
                    kb.op("dve", lambda e, j=j: e.max(out=v16[:, j, 8:16], in_=work[:, 0:128]), reads=[b_work, b_v16], writes=[b_v16])
                    kb.op("dve", lambda e, j=j: e.max_index(out=i16[:, j, 0:8], in_max=v16[:, j, 0:8], in_values=sc[:, j, :]),
                          reads=[b_sc, b_v16], writes=[b_i16])
                    kb.op("dve", lambda e, j=j: e.max_index(out=i16[:, j, 8:16], in_max=v16[:, j, 8:16], in_values=sc[:, j, :]),
                          reads=[b_sc, b_v16, b_i16], writes=[b_i16])
                kb.op("dve", lambda e: e.tensor_copy(out=i16f, in_=i16), reads=[b_i16], writes=[b_i16f])
                v4 = v16.rearrange("p (h c) k -> p h c k", c=2)
                f4 = i16f.rearrange("p (h c) k -> p h c k", c=2)
                s4 = i16s.rearrange("p (h c) k -> p h c k", c=2)
                c4 = cand.rearrange("p h (a b) -> p h a b", b=16)
                d4 = cid.rearrange("p h (a b) -> p h a b", b=16)
                kb.op("dve", lambda e: e.tensor_tensor(out=c4, in0=v4[:, :, 0, :].unsqueeze(3).to_broadcast([128, 8, 16, 16]),
                                                       in1=v4[:, :, 1, :].unsqueeze(2).to_broadcast([128, 8, 16, 16]), op=ALU.add),
                      reads=[b_v16], writes=[b_cand])
                kb.op("dve", lambda e: e.tensor_scalar(out=i16s, in0=i16f, scalar1=float(PNK), scalar2=None, op0=ALU.mult),
                      reads=[b_i16f], writes=[b_i16s])
                kb.op("dve", lambda e: e.tensor_tensor(out=d4, in0=s4[:, :, 0, :].unsqueeze(3).to_broadcast([128, 8, 16, 16]),
                                                       in1=f4[:, :, 1, :].unsqueeze(2).to_broadcast([128, 8, 16, 16]), op=ALU.add),
                      reads=[b_i16f, b_i16s], writes=[b_cid])
                for h in range(PH):
                    kb.op("dve", lambda e, h=h: e.max(out=top[:, h, 0:8], in_=cand[:, h, :]), reads=[b_cand], writes=[b_top])
                    kb.op("dve", lambda e, h=h: e.match_replace(out=work, in_to_replace=top[:, h, 0:8], in_values=cand[:, h, :],
                                                                imm_value=NEG), reads=[b_cand, b_top], writes=[b_work])
                    kb.op("dve", lambda e, h=h: e.max(out=top[:, h, 8:16], in_=work), reads=[b_work, b_top], writes=[b_top])
                    kb.op("dve", lambda e, h=h: e.max_index(out=pos[:, h, 0:8], in_max=top[:, h, 0:8], in_values=cand[:, h, :]),
                          reads=[b_cand, b_top], writes=[b_pos])
                    kb.op("dve", lambda e, h=h: e.max_index(out=pos[:, h, 8:16], in_max=top[:, h, 8:16], in_values=cand[:, h, :]),
                          reads=[b_cand, b_top, b_pos], writes=[b_pos])
                kb.op("dve", lambda e: e.tensor_copy(out=posf, in_=pos), reads=[b_pos], writes=[b_posf])
                for h in range(PH):
                    kb.op("dve", lambda e, h=h: e.tensor_tensor(out=eq, in0=iota.unsqueeze(1).to_broadcast([128, 16, 256]),
                                                                in1=posf[:, h, :].unsqueeze(2).to_broadcast([128, 16, 256]),
                                                                op=ALU.is_equal), reads=[b_iota, b_posf], writes=[b_eq])
                    kb.op("dve", lambda e, h=h: e.tensor_tensor(out=eq, in0=eq, in1=cid[:, h, :].unsqueeze(1).to_broadcast([128, 16, 256]),
                                                                op=ALU.mult), reads=[b_eq, b_cid], writes=[b_eq])
                    kb.op("dve", lambda e, h=h: e.tensor_reduce(out=eidf[:, h * 16:(h + 1) * 16], in_=eq, axis=AX.X, op=ALU.add),
                          reads=[b_eq], writes=[b_eidf])
                kb.op("dve", lambda e: e.tensor_copy(out=eid, in_=eidf), reads=[b_eidf], writes=[b_eid])
                G = lambda i: gsm[:, i, :]
                t3 = top
                kb.op("dve", lambda e: e.tensor_tensor(out=G(0).rearrange("p (h k) -> p h k", k=16), in0=t3,
                                                       in1=t3[:, :, 0:1].to_broadcast([128, 8, 16]), op=ALU.subtract),
                      reads=[b_top], writes=[b_gsm])
                kb.op("act", lambda e: e.activation(out=G(1), in_=G(0), func=AF.Exp), reads=[b_gsm], writes=[b_gsm])
                kb.op("dve", lambda e: e.tensor_reduce(out=gz[:, 0:8], in_=G(1).rearrange("p (h k) -> p h k", k=16), axis=AX.X, op=ALU.add),
                      reads=[b_gsm], writes=[b_gz])
                kb.op("dve", lambda e: e.reciprocal(out=gz[:, 8:16], in_=gz[:, 0:8]), reads=[b_gz], writes=[b_gz])
                kb.op("dve", lambda e: e.tensor_tensor(out=G(2).rearrange("p (h k) -> p h k", k=16),
                                                       in0=G(1).rearrange("p (h k) -> p h k", k=16),
                                                       in1=bc_last(gz[:, 8:16], 16), op=ALU.mult), reads=[b_gsm, b_gz], writes=[b_gsm])
                NG = 128 // GS

                def finish_group(gq):
                    bi = gq % NGB
                    s0 = gq * GS
                    kb.op("act", lambda e: e.activation(out=gsm[:, 3, s0:s0 + GS], in_=act_t[:, s0:s0 + GS], func=AF.Gelu),
                          reads=[b_actg[gq]], writes=[b_actg[gq]])
                    kb.op("dve", lambda e: e.tensor_tensor(out=wgt[:, s0:s0 + GS], in0=gsm[:, 3, s0:s0 + GS], in1=gsm[:, 2, s0:s0 + GS],
                                                           op=ALU.mult), reads=[b_actg[gq], b_gsm], writes=[b_actg[gq]])
                    kb.op("dve", lambda e: e.tensor_tensor(out=dgs[bi], in0=ident_f.unsqueeze(1).to_broadcast([128, GS, 128]),
                                                           in1=bc_last(wgt[:, s0:s0 + GS], 128), op=ALU.mult),
                          reads=[b_ident, b_actg[gq]], writes=[b_dgs[bi]])
                    for i in range(GS):
                        s_ = s0 + i
                        for nb in range(4):
                            kb.op("pe", lambda e, i=i, s_=s_, nb=nb: e.matmul(pacc[nb], lhsT=dgs[bi][:, i, :],
                                                                              rhs=uv[bi][:, i, D + nb * 512:D + (nb + 1) * 512],
                                                                              start=(s_ == 0), stop=(s_ == 127)),
                                  reads=[b_dgs[bi], b_uv[bi][i]], writes=[b_pacc[nb]])

                for gq in range(NG):
                    bi = gq % NGB
                    s0 = gq * GS
                    for i in range(GS):
                        s_ = s0 + i
                        kb.dma("pool", lambda e, bi=bi, i=i, s_=s_: e.indirect_dma_start(
                            out=uv[bi][:, i, :], out_offset=None, in_=UVB,
                            in_offset=bass.IndirectOffsetOnAxis(ap=eid[:, s_:s_ + 1], axis=0)),
                            reads=[b_eid, b_UVB], writes=[b_uv[bi][i]])
                    for i in range(GS):
                        s_ = s0 + i
                        pj = s_ % 2
                        kb.op("dve", lambda e, bi=bi, i=i, pj=pj: e.tensor_tensor(out=prod[pj], in0=uv[bi][:, i, 0:D], in1=xn, op=ALU.mult),
                              reads=[b_uv[bi][i], b_xn], writes=[b_prod[pj]])
                        kb.op("act", lambda e, pj=pj, s_=s_: e.activation(out=junk, in_=prod[pj], func=AF.Copy, accum_out=act_t[:, s_:s_ + 1]),
                              reads=[b_prod[pj]], writes=[b_junk, b_actg[gq]])
                    if gq >= 1:
                        finish_group(gq - 1)
                finish_group(NG - 1)
                for nb in range(4):
                    kb.op("dve", lambda e, nb=nb: e.tensor_tensor(out=h1t[:, nb * 512:(nb + 1) * 512], in0=h1t[:, nb * 512:(nb + 1) * 512],
                                                                  in1=pacc[nb], op=ALU.add), reads=[b_h1, b_pacc[nb]], writes=[b_h1])
                kb.op("act", lambda e: e.activation(out=junk, in_=h1t, func=AF.Square, accum_out=stat[:, 0:1]),
                      reads=[b_h1], writes=[b_junk, b_stat])
                kb.op("dve", lambda e: e.tensor_scalar(out=stat[:, 1:2], in0=stat[:, 0:1], scalar1=1.0 / D, scalar2=EPS,
                                                       op0=ALU.mult, op1=ALU.add), reads=[b_stat], writes=[b_stat])
                kb.op("act", lambda e: e.activation(out=stat[:, 3:4], in_=stat[:, 1:2], func=AF.Sqrt), reads=[b_stat], writes=[b_stat])
                kb.op("dve", lambda e: e.reciprocal(out=stat[:, 2:3], in_=stat[:, 3:4]), reads=[b_stat], writes=[b_stat])
                kb.op("dve", lambda e: e.scalar_tensor_tensor(out=ot, in0=h1t, scalar=stat[:, 2:3], in1=gf_bc, op0=ALU.mult, op1=ALU.mult),
                      reads=[b_h1, b_stat, b_gf], writes=[b_ot])
                kb.dma("sp", lambda e, rows=rows: e.dma_start(out=out[rows, :], in_=ot), reads=[b_ot], writes=[b_out], accumulate=True)
        kb.barrier()

    if "out" in cfg.phases:
        phase_out()
    if "peer" in cfg.phases:
        phase_peer()

    kb.barrier()
    es_glob.close()
    return nc, kb


def make_consts():
    c = np.zeros((128, 4096), np.float64)
    i = np.arange(128)
    c[:, 0:128] = (i[None, :] >= i[:, None])
    c[:, 128:256] = (i[:, None] > i[None, :])
    c[:, 256:384] = 1.0
    gam = 1.0 - 2.0 ** (-5.0 - np.arange(8))
    lg = np.log(gam)
    c[:, 384:392] = np.exp(lg[None, :] * (i[:, None] + 1.0))
    c[:, 392:400] = np.exp(-lg[None, :] * (i[:, None] + 1.0)) / 16.0
    c[:, 400:408] = np.exp(lg[None, :] * (127.0 - i[:, None])) / 16.0
    c[:, 408:416] = np.exp(lg[None, :] * 128.0)
    return c.astype(np.float32)


def core_streams(x, meta_tokens, cfg):
    B, S, _ = x.shape
    maps = []
    inv = (10000.0 ** (-np.arange(128, dtype=np.float32) / np.float32(128))).astype(np.float32)
    for core in range(8):
        b, s = core // 2, core % 2
        full = np.zeros((LEAD + N_META + S, D), np.float32)
        full[LEAD:LEAD + N_META] = meta_tokens
        full[LEAD + N_META:] = x[b]
        valid = np.zeros((full.shape[0], 1), np.float32)
        valid[LEAD:] = 1.0
        pos = np.maximum(np.arange(full.shape[0]) - LEAD, 0).astype(np.float32)
        hin = np.zeros((cfg.nt, D), np.float32)
        msk = np.zeros((cfg.nt, 1), np.float32)
        p = np.zeros((cfg.nt,), np.float32)
        if s == 0:
            n = (cfg.main + 1) * 128
            r = (cfg.pre - 1) * 128
            hin[r:] = full[0:n]
            msk[r:] = valid[0:n]
            p[r:] = pos[0:n]
        else:
            hin[:] = full[0:cfg.nt]
            msk[:] = valid[0:cfg.nt]
            p[:] = pos[0:cfg.nt]
        ang = p[:, None] * inv[None, :]
        maps.append({"hin": hin, "rowmask": msk, "ropec": np.cos(ang).astype(np.float32),
                     "ropes": np.sin(ang).astype(np.float32)})
    return maps


_PROG = {}


def kernel(x, meta_tokens, norm_mix_g, w_in, conv_w, conv_b, dt_bias, a_log, d_skip, ssm_norm_g, w_ret_o,
           w_ssm_o, w_out, norm_ffn_g, peer_w_q, peer_sub_keys, peer_u, peer_v, norm_final_g):
    cfg = Cfg()
    x = np.asarray(x, np.float32)
    maps = core_streams(x, np.asarray(meta_tokens, np.float32), cfg)
    shared = {
        "cst_f": make_consts(),
        "norm_mix_g": np.asarray(norm_mix_g, np.float32).reshape(1, D),
        "w_in": np.asarray(w_in, np.float32).reshape(D, NPROJ),
        "conv_w": np.asarray(conv_w, np.float32).reshape(4, CONV_DIM),
        "conv_b": np.asarray(conv_b, np.float32).reshape(1, CONV_DIM),
        "dt_bias": np.asarray(dt_bias, np.float32).reshape(1, SH),
        "a_log": np.asarray(a_log, np.float32).reshape(1, SH),
        "d_skip": np.asarray(d_skip, np.float32).reshape(1, SH),
        "ssm_norm_g": np.asarray(ssm_norm_g, np.float32).reshape(1, SSM_INNER),
        "w_ret_o": np.asarray(w_ret_o, np.float32).reshape(RH * RDV, D),
        "w_ssm_o": np.asarray(w_ssm_o, np.float32).reshape(SSM_INNER, D),
        "w_out": np.asarray(w_out, np.float32).reshape(D, D),
        "norm_ffn_g": np.asarray(norm_ffn_g, np.float32).reshape(1, D),
        "peer_w_q": np.asarray(peer_w_q, np.float32).reshape(D, D),
        "peer_sub_keys": np.asarray(peer_sub_keys, np.float32).reshape(16 * 128, 128),
        "peer_u": np.asarray(peer_u, np.float32).reshape(NEXP, D),
        "peer_v": np.asarray(peer_v, np.float32).reshape(NEXP, D),
        "norm_final_g": np.asarray(norm_final_g, np.float32).reshape(1, D),
    }
    for m in maps:
        m.update(shared)
    nc, _ = build_program(cfg)
    res = run_bass_kernel_spmd(nc, maps, core_ids=list(range(8)))
    B, S, _ = x.shape
    outp = np.zeros((B, S, D), np.float32)
    half = cfg.main * 128
    for core in range(8):
        b, s = core // 2, core % 2
        outp[b, s * half:(s + 1) * half] = res.results[core]["out"]
    return outp
```

```python
import numpy as np
from contextlib import ExitStack
import concourse.bass as bass
import concourse.mybir as mybir
from concourse.bass_utils import run_bass_kernel_spmd

F32 = mybir.dt.float32
BF16 = mybir.dt.bfloat16
I32 = mybir.dt.int32
U32 = mybir.dt.uint32
AF = mybir.ActivationFunctionType
ALU = mybir.AluOpType
AX = mybir.AxisListType

D = 2048
N_META = 16
LEAD = 112
EPS = 1e-6
RH, RDK, RDV = 8, 256, 512
SSM_INNER, SH, SP_, SG, SN = 4096, 64, 64, 8, 128
CONV_DIM = 6144
NPROJ = 26688
C_Q, C_K, C_V, C_G, C_Z, C_XBC, C_DT, C_GR, C_GS = 0, 2048, 4096, 8192, 12288, 16384, 22528, 22592, 24640
PTM_W = 16384
PH, PNK, PTOPK, PHALF = 8, 128, 16, 128
NEXP = 16384
HALO = 8


class Buf:
    __slots__ = ("writers", "readers", "name")

    def __init__(self, name=""):
        self.writers = {}
        self.readers = {}
        self.name = name


class KB:
    NQ = 20

    def __init__(self, nc):
        self.nc = nc
        self.engs = {"pe": nc.tensor, "act": nc.scalar, "dve": nc.vector, "pool": nc.gpsimd, "sp": nc.sync}
        self.epoch = 0
        self.csem = {e: nc.alloc_semaphore("c_" + e) for e in ("pe", "act", "dve", "pool")}
        self.ccnt = {e: 0 for e in self.csem}
        self.dsem = {q: [nc.alloc_semaphore("d_%s%d" % (q, i)) for i in range(self.NQ)] for q in ("sp", "pool")}
        self.dcnt = {q: 0 for q in self.dsem}
        self.waited = {e: {} for e in self.engs}
        self.n_ins = 0
        self.limit = None
        self.n_ops = 0

    def _wait(self, e, toks):
        w = self.waited[e]
        need = {}
        for key, (sem, val) in toks:
            if key[0] == "c":
                if key[2] < self.epoch:
                    continue
                if e == "pe" and key[1] == "pe":
                    continue
            if w.get(key, 0) >= val:
                continue
            if key not in need or need[key][1] < val:
                need[key] = (sem, val)
        for key, (sem, val) in need.items():
            self.engs[e].wait_ge(sem, val)
            w[key] = val
            self.n_ins += 1

    @staticmethod
    def _merge(dst, key, sv):
        if key not in dst or dst[key][1] < sv[1]:
            dst[key] = sv

    def _deps(self, reads, writes, accumulate=False):
        toks = []
        for b in reads:
            toks += list(b.writers.items())
        for b in writes:
            if not accumulate:
                toks += list(b.writers.items())
            toks += list(b.readers.items())
        return toks

    def _commit(self, key, sv, reads, writes, accumulate=False):
        for b in reads:
            self._merge(b.readers, key, sv)
        for b in writes:
            if accumulate:
                self._merge(b.writers, key, sv)
            else:
                b.writers = {key: sv}
                b.readers = {}

    def op(self, e, fn, reads=(), writes=()):
        self.n_ops += 1
        if self.limit is not None and self.n_ops > self.limit:
            return
        self._wait(e, self._deps(reads, writes))
        ins = fn(self.engs[e])
        self.ccnt[e] += 1
        ins.then_inc(self.csem[e], 1)
        self.n_ins += 1
        self._commit(("c", e, self.epoch), (self.csem[e], self.ccnt[e]), reads, writes)

    def dma(self, q, fn, reads=(), writes=(), accumulate=False):
        self.n_ops += 1
        if self.limit is not None and self.n_ops > self.limit:
            return
        i = self.dcnt[q]
        slot, gen = i % self.NQ, i // self.NQ
        key = ("d", q, slot)
        sem = self.dsem[q][slot]
        toks = self._deps(reads, writes, accumulate)
        if gen > 0:
            toks.append((key, (sem, 16 * gen)))
        self._wait(q, toks)
        ins = fn(self.engs[q])
        ins.then_inc(sem, 16)
        self.dcnt[q] += 1
        self.n_ins += 1
        self._commit(key, (sem, 16 * (gen + 1)), reads, writes, accumulate)

    def all_tokens(self):
        toks = [(("c", e, self.epoch), (self.csem[e], self.ccnt[e])) for e in self.csem if self.ccnt[e] > 0]
        for q in self.dsem:
            n = self.dcnt[q]
            for slot in range(min(n, self.NQ)):
                gens = (n - 1 - slot) // self.NQ + 1
                toks.append((("d", q, slot), (self.dsem[q][slot], 16 * gens)))
        return toks

    def barrier(self):
        toks = self.all_tokens()
        for e in self.engs:
            self._wait(e, toks)
        if self.limit is not None and self.n_ops > self.limit:
            return
        self.epoch += 1
        self.csem = {e: self.nc.alloc_semaphore("c_%s_%d" % (e, self.epoch)) for e in self.csem}
        self.ccnt = {e: 0 for e in self.csem}

    def maybe_epoch(self, thresh=30000):
        if max(self.ccnt.values()) > thresh:
            self.barrier()


def bc_mid(ap2d, n):
    p, f = ap2d.shape
    return ap2d.unsqueeze(1).to_broadcast([p, n, f])


def bc_last(ap2d, n):
    p, f = ap2d.shape
    return ap2d.unsqueeze(2).to_broadcast([p, f, n])


class Cfg:
    def __init__(self, pre=33, main=32, debug=False, phases=("proj", "ret", "ssd", "out", "peer"), tiny=()):
        self.tiny = set(tiny)
        self.pre = pre
        self.main = main
        self.debug = debug
        self.phases = phases
        self.nch = pre + main
        self.nt = self.nch * 128
        self.nt_main = main * 128


def build_program(cfg):
    nc = bass.Bass("TRN2", target_bir_lowering=False)
    kb = KB(nc)
    kb.limit = getattr(cfg, "limit", None)
    NT, NTM = cfg.nt, cfg.nt_main
    ROW0 = cfg.pre * 128
    dbg_kind = "ExternalOutput" if cfg.debug else "Internal"

    def ext_in(name, shape, dt=F32):
        if name in cfg.tiny:
            shape = [1, 8]
        return nc.dram_tensor(name, list(shape), dt, kind="ExternalInput").ap()

    hin = ext_in("hin", [NT, D])
    rowmask = ext_in("rowmask", [NT, 1])
    ropec = ext_in("ropec", [NT, 128])
    ropes = ext_in("ropes", [NT, 128])
    cst_f = ext_in("cst_f", [128, 4096])
    norm_mix_g = ext_in("norm_mix_g", [1, D])
    w_in = ext_in("w_in", [D, NPROJ])
    conv_w = ext_in("conv_w", [4, CONV_DIM])
    conv_b = ext_in("conv_b", [1, CONV_DIM])
    dt_bias = ext_in("dt_bias", [1, SH])
    a_log = ext_in("a_log", [1, SH])
    d_skip = ext_in("d_skip", [1, SH])
    ssm_norm_g = ext_in("ssm_norm_g", [1, SSM_INNER])
    w_ret_o = ext_in("w_ret_o", [RH * RDV, D])
    w_ssm_o = ext_in("w_ssm_o", [SSM_INNER, D])
    w_out = ext_in("w_out", [D, D])
    norm_ffn_g = ext_in("norm_ffn_g", [1, D])
    peer_w_q = ext_in("peer_w_q", [D, D])
    peer_sub_keys = ext_in("peer_sub_keys", [16 * 128, 128])
    peer_u = ext_in("peer_u", [NEXP, D])
    peer_v = ext_in("peer_v", [NEXP, D])
    norm_final_g = ext_in("norm_final_g", [1, D])
    out = nc.dram_tensor("out", [NTM, D], F32, kind="ExternalOutput").ap()

    PTMS = [nc.dram_tensor("PTM%d" % i, [NT, 4096], BF16, kind=dbg_kind).ap() for i in range(4)]

    class _PTM:
        def __getitem__(self, key):
            rows, cols = key
            i = cols.start // 4096
            assert (cols.stop - 1) // 4096 == i
            return PTMS[i][rows, cols.start - i * 4096:cols.stop - i * 4096]
    PTM = _PTM()
    XBCT = nc.dram_tensor("XBCT", [CONV_DIM, HALO + NT], BF16, kind=dbg_kind).ap()
    DTS = nc.dram_tensor("DTS", [NT, SH], F32, kind=dbg_kind).ap()
    GT = nc.dram_tensor("GT", [2 * D, NTM], BF16, kind=dbg_kind).ap()
    OGT = nc.dram_tensor("OGT", [RH * RDV, NTM], BF16, kind=dbg_kind).ap()
    YST = nc.dram_tensor("YST", [SSM_INNER, NTM], BF16, kind=dbg_kind).ap()
    H1 = nc.dram_tensor("H1", [NTM, D], F32, kind=dbg_kind).ap()
    XN2 = nc.dram_tensor("XN2", [NTM, D], BF16, kind=dbg_kind).ap()
    SC = nc.dram_tensor("SC", [NTM, 16 * 128], F32, kind=dbg_kind).ap()
    b_XN2, b_SC = Buf("XN2"), Buf("SC")
    NEX = 128 if "peer_u" in cfg.tiny else NEXP
    UVB = nc.dram_tensor("UVB", [NEX, 2 * D], BF16, kind="Internal").ap()
    b_UVB = Buf("UVB")
    WRB = nc.dram_tensor("WRB", [RH * RDV, D], BF16, kind="Internal").ap()
    WSB = nc.dram_tensor("WSB", [SSM_INNER, D], BF16, kind="Internal").ap()
    WOB = nc.dram_tensor("WOB", [D, D], BF16, kind="Internal").ap()
    WQB = nc.dram_tensor("WQB", [D, D], BF16, kind="Internal").ap()
    b_WB = Buf("WB")
    b_PTM, b_XBCT, b_DTS, b_GT, b_OGT, b_YST, b_H1, b_out = (Buf(n) for n in
                                                              ("PTM", "XBCT", "DTS", "GT", "OGT", "YST", "H1", "out"))

    es_glob = ExitStack()

    uniq = [0]

    def sb(es, name, shape, dt):
        uniq[0] += 1
        return es.enter_context(nc.sbuf_tensor("%s_%d" % (name, uniq[0]), list(shape), dt)).ap()

    def ps(es, name, shape, dt):
        uniq[0] += 1
        return es.enter_context(nc.psum_tensor("%s_%d" % (name, uniq[0]), list(shape), dt)).ap()

    ident_bf = sb(es_glob, "ident_bf", [128, 128], BF16)
    ident_f = sb(es_glob, "ident_f", [128, 128], F32)
    b_ident = Buf("ident")
    kb.op("pool", lambda e: e.memset(ident_f, 0.0), writes=[b_ident])
    kb.op("pool", lambda e: e.affine_select(out=ident_f, in_=ident_f, pattern=[[-1, 128]], compare_op=ALU.not_equal,
                                            fill=1.0, base=0, channel_multiplier=1), reads=[b_ident], writes=[b_ident])
    kb.op("dve", lambda e: e.tensor_copy(out=ident_bf, in_=ident_f), reads=[b_ident], writes=[b_ident])

    def phase_proj():
        with ExitStack() as es:
            TBMAX = 17 * 128
            g_bc = sb(es, "g_bc", [128, D], F32)
            b_g = Buf("g_bc")
            kb.dma("sp", lambda e: e.dma_start(out=g_bc, in_=norm_mix_g.partition_broadcast(128)), writes=[b_g])
            nT = sb(es, "nT", [128, 16, TBMAX], BF16)
            b_nT = Buf("nT")
            h_t = [sb(es, "h_t%d" % i, [128, D], F32) for i in range(2)]
            b_h = [Buf("h_t") for _ in range(2)]
            n_bf = [sb(es, "n_bf%d" % i, [128, D], BF16) for i in range(2)]
            b_n = [Buf("n_bf") for _ in range(2)]
            junk = sb(es, "junk", [128, D], BF16)
            b_junk = Buf("junk")
            stat = sb(es, "stat", [128, 4], F32)
            b_stat = Buf("stat")
            wst = [sb(es, "wst%d" % i, [128, 16, 512], F32) for i in range(2)]
            b_wst = [Buf("wst") for _ in range(2)]
            wbf = [sb(es, "wbf%d" % i, [128, 16, 512], BF16) for i in range(2)]
            b_wbf = [Buf("wbf") for _ in range(2)]
            ob = [sb(es, "ob%d" % i, [128, 512], BF16) for i in range(4)]
            b_ob = [Buf("ob") for _ in range(4)]
            obf = [sb(es, "obf%d" % i, [128, 64], F32) for i in range(2)]
            b_obf = [Buf("obf") for _ in range(2)]
            zt = sb(es, "zt", [128, HALO], BF16)
            b_zt = Buf("zt")
            ptr = ps(es, "ptr", [128, 16, 128], BF16)
            b_ptr = Buf("ptr")
            pmm = [ps(es, "pmm%d" % i, [128, 512], F32) for i in range(4)]
            b_pmm = [Buf("pmm") for _ in range(4)]

            kb.op("pool", lambda e: e.memset(zt, 0.0), writes=[b_zt])
            for r in range(CONV_DIM // 128):
                kb.dma("sp", lambda e, r=r: e.dma_start(out=XBCT[r * 128:(r + 1) * 128, 0:HALO], in_=zt),
                       reads=[b_zt], writes=[b_XBCT], accumulate=True)

            w_in_v = w_in.rearrange("(k p) n -> p k n", p=128)
            cnt = {"w": 0, "ob": 0, "pm": 0, "ev": 0, "obf": 0}

            def evac(dst, src, reads, writes):
                eng = "act" if cnt["ev"] % 2 == 0 else "dve"
                cnt["ev"] += 1
                if eng == "act":
                    kb.op("act", lambda e: e.activation(out=dst, in_=src, func=AF.Copy), reads=reads, writes=writes)
                else:
                    kb.op("dve", lambda e: e.tensor_copy(out=dst, in_=src), reads=reads, writes=writes)

            def do_block(ch0, nchk, colgroups):
                ntok = nchk * 128
                r0 = ch0 * 128
                for t in range(nchk):
                    i = t % 2
                    rows = slice(r0 + t * 128, r0 + (t + 1) * 128)
                    kb.dma("sp", lambda e, i=i, rows=rows: e.dma_start(out=h_t[i], in_=hin[rows, :]), writes=[b_h[i]])
                    kb.op("act", lambda e, i=i: e.activation(out=junk, in_=h_t[i], func=AF.Square, accum_out=stat[:, 0:1]),
                          reads=[b_h[i]], writes=[b_junk, b_stat])
                    kb.op("dve", lambda e: e.tensor_scalar(out=stat[:, 1:2], in0=stat[:, 0:1], scalar1=1.0 / D, scalar2=EPS,
                                                           op0=ALU.mult, op1=ALU.add), reads=[b_stat], writes=[b_stat])
                    kb.op("act", lambda e: e.activation(out=stat[:, 3:4], in_=stat[:, 1:2], func=AF.Sqrt),
                          reads=[b_stat], writes=[b_stat])
                    kb.op("dve", lambda e: e.reciprocal(out=stat[:, 2:3], in_=stat[:, 3:4]), reads=[b_stat], writes=[b_stat])
                    kb.op("dve", lambda e, i=i: e.scalar_tensor_tensor(out=n_bf[i], in0=h_t[i], scalar=stat[:, 2:3], in1=g_bc,
                                                                       op0=ALU.mult, op1=ALU.mult),
                          reads=[b_h[i], b_stat, b_g], writes=[b_n[i]])
                    for k in range(16):
                        kb.op("pe", lambda e, i=i, k=k: e.transpose(out=ptr[:, k, :], in_=n_bf[i][:, k * 128:(k + 1) * 128],
                                                                    identity=ident_bf),
                              reads=[b_n[i], b_ident], writes=[b_ptr])
                    evac(nT[:, :, t * 128:(t + 1) * 128], ptr, [b_ptr], [b_nT])
                for (kind, c0, ncol, dst_row0) in colgroups:
                    kb.maybe_epoch()
                    wi = cnt["w"] % 2
                    cnt["w"] += 1
                    kb.dma("sp", lambda e, wi=wi, c0=c0, ncol=ncol: e.dma_start(out=wst[wi][:, :, 0:ncol],
                                                                                 in_=w_in_v[:, :, c0:c0 + ncol]),
                           writes=[b_wst[wi]])
                    kb.op("pool", lambda e, wi=wi, ncol=ncol: e.tensor_copy(out=wbf[wi][:, :, 0:ncol], in_=wst[wi][:, :, 0:ncol]),
                          reads=[b_wst[wi]], writes=[b_wbf[wi]])
                    if kind == "tm":
                        for t in range(nchk):
                            pi = cnt["pm"] % 4
                            cnt["pm"] += 1
                            for k in range(16):
                                kb.op("pe", lambda e, pi=pi, k=k, t=t, wi=wi, ncol=ncol: e.matmul(
                                    pmm[pi][:, 0:ncol], lhsT=nT[:, k, t * 128:(t + 1) * 128], rhs=wbf[wi][:, k, 0:ncol],
                                    start=(k == 0), stop=(k == 15)), reads=[b_nT, b_wbf[wi]], writes=[b_pmm[pi]])
                            oi = cnt["ob"] % 4
                            cnt["ob"] += 1
                            evac(ob[oi][:, 0:ncol], pmm[pi][:, 0:ncol], [b_pmm[pi]], [b_ob[oi]])
                            rows = slice(r0 + t * 128, r0 + (t + 1) * 128)
                            kb.dma("sp", lambda e, oi=oi, rows=rows, c0=c0, ncol=ncol: e.dma_start(
                                out=PTM[rows, c0:c0 + ncol], in_=ob[oi][:, 0:ncol]),
                                reads=[b_ob[oi]], writes=[b_PTM], accumulate=True)
                    elif kind == "dt":
                        for t in range(nchk):
                            pi = cnt["pm"] % 4
                            cnt["pm"] += 1
                            for k in range(16):
                                kb.op("pe", lambda e, pi=pi, k=k, t=t, wi=wi: e.matmul(
                                    pmm[pi][:, 0:64], lhsT=nT[:, k, t * 128:(t + 1) * 128], rhs=wbf[wi][:, k, 0:64],
                                    start=(k == 0), stop=(k == 15)), reads=[b_nT, b_wbf[wi]], writes=[b_pmm[pi]])
                            oi = cnt["obf"] % 2
                            cnt["obf"] += 1
                            evac(obf[oi], pmm[pi][:, 0:64], [b_pmm[pi]], [b_obf[oi]])
                            rows = slice(r0 + t * 128, r0 + (t + 1) * 128)
                            kb.dma("sp", lambda e, oi=oi, rows=rows: e.dma_start(out=DTS[rows, :], in_=obf[oi]),
                                   reads=[b_obf[oi]], writes=[b_DTS], accumulate=True)
                    else:
                        for m in range(ncol // 128):
                            for tg in range(0, ntok, 512):
                                n = min(512, ntok - tg)
                                pi = cnt["pm"] % 4
                                cnt["pm"] += 1
                                for k in range(16):
                                    kb.op("pe", lambda e, pi=pi, k=k, m=m, tg=tg, n=n, wi=wi: e.matmul(
                                        pmm[pi][:, 0:n], lhsT=wbf[wi][:, k, m * 128:(m + 1) * 128], rhs=nT[:, k, tg:tg + n],
                                        start=(k == 0), stop=(k == 15)), reads=[b_nT, b_wbf[wi]], writes=[b_pmm[pi]])
                                oi = cnt["ob"] % 4
                                cnt["ob"] += 1
                                evac(ob[oi][:, 0:n], pmm[pi][:, 0:n], [b_pmm[pi]], [b_ob[oi]])
                                rr = dst_row0 + m * 128
                                if kind == "xbc":
                                    kb.dma("sp", lambda e, oi=oi, rr=rr, tg=tg, n=n: e.dma_start(
                                        out=XBCT[rr:rr + 128, HALO + r0 + tg:HALO + r0 + tg + n], in_=ob[oi][:, 0:n]),
                                        reads=[b_ob[oi]], writes=[b_XBCT], accumulate=True)
                                else:
                                    cc = r0 - ROW0 + tg
                                    kb.dma("sp", lambda e, oi=oi, rr=rr, cc=cc, n=n: e.dma_start(
                                        out=GT[rr:rr + 128, cc:cc + n], in_=ob[oi][:, 0:n]),
                                        reads=[b_ob[oi]], writes=[b_GT], accumulate=True)

            def groups(c_lo, c_hi, kind, dst_row0=0):
                return [(kind, c, min(512, c_hi - c), dst_row0 + (c - c_lo)) for c in range(c_lo, c_hi, 512)]

            pre_groups = (groups(C_K, C_G, "tm") + groups(C_XBC, C_DT, "xbc")
                          + [("dt", C_DT, 64, 0)])
            main_groups = (groups(0, PTM_W, "tm") + groups(C_XBC, C_DT, "xbc") + [("dt", C_DT, 64, 0)]
                           + groups(C_GR, NPROJ, "gt"))

            def blocks(c0, n):
                res, c = [], c0
                while n > 0:
                    m = min(n, 17 if n == 17 else 16)
                    res.append((c, m))
                    c += m
                    n -= m
                return res

            for (c, m) in blocks(0, cfg.pre):
                do_block(c, m, pre_groups)
            for (c, m) in blocks(cfg.pre, cfg.main):
                do_block(c, m, main_groups)
        kb.barrier()

    CO_UT, CO_SL, CO_ONE, CO_DQ, CO_DK, CO_DKZ, CO_G128 = 0, 128, 256, 384, 392, 400, 408

    def load_consts(es):
        cst = sb(es, "cst", [128, 512], F32)
        b_cst = Buf("cst")
        kb.dma("sp", lambda e: e.dma_start(out=cst, in_=cst_f[:, 0:512]), writes=[b_cst])
        return cst, b_cst

    def transpose_store(src_bf, b_src, nblk, ptr, b_ptr, oT, b_oT, dst_view, b_dst, col0):
        for r in range(0, nblk, 16):
            for k in range(16):
                kb.op("pe", lambda e, k=k, r=r: e.transpose(out=ptr[:, k, :], in_=src_bf[:, (r + k) * 128:(r + k + 1) * 128],
                                                            identity=ident_bf), reads=[b_src, b_ident], writes=[b_ptr])
            kb.op("act", lambda e, r=r: e.activation(out=oT[:, r:r + 16, :], in_=ptr, func=AF.Copy),
                  reads=[b_ptr], writes=[b_oT])
        kb.dma("sp", lambda e: e.dma_start(out=dst_view[:, :, col0:col0 + 128], in_=oT[:, 0:nblk, :]),
               reads=[b_oT], writes=[b_dst], accumulate=True)

    precast_state = {"done": False}

    def precast_units(es, ptile, b_ptile):
        gcol = sb(es, "gcol", [128, 32], F32); b_gcol = Buf()
        tmpg = sb(es, "tmpg", [32, 128], F32); b_tg = Buf()
        kb.dma("sp", lambda e: e.dma_start(out=tmpg, in_=ssm_norm_g.rearrange("o (kt p) -> (o kt) p", p=128)), writes=[b_tg])
        kb.op("pe", lambda e: e.transpose(out=ptile[:, 0:32], in_=tmpg, identity=ident_f[0:32, 0:32]),
              reads=[b_tg, b_ident], writes=[b_ptile])
        kb.op("dve", lambda e: e.tensor_copy(out=gcol, in_=ptile[:, 0:32]), reads=[b_ptile], writes=[b_gcol])
        stg = [sb(es, "stgw%d" % i, [128, D], F32) for i in range(3)]; b_stg = [Buf() for _ in range(3)]
        stb = [sb(es, "stbw%d" % i, [128, D], BF16) for i in range(3)]; b_stb = [Buf() for _ in range(3)]
        jobs = []
        for (src, dst, nblk, scale_g, b_dst) in ((w_ret_o, WRB, 32, False, b_WB), (w_ssm_o, WSB, 32, True, b_WB),
                                                 (w_out, WOB, 16, False, b_WB), (peer_w_q, WQB, 16, False, b_WB)):
            for r in range(nblk):
                jobs.append((src[r * 128:(r + 1) * 128, :], dst[r * 128:(r + 1) * 128, :], r if scale_g else None, b_dst))
        for r in range(NEX // 128):
            jobs.append((peer_u[r * 128:(r + 1) * 128, :], UVB[r * 128:(r + 1) * 128, 0:D], None, b_UVB))
            jobs.append((peer_v[r * 128:(r + 1) * 128, :], UVB[r * 128:(r + 1) * 128, D:2 * D], None, b_UVB))
        def ld(n):
            src_ap = jobs[n][0]
            i = n % 3
            kb.dma("sp", lambda e: e.dma_start(out=stg[i], in_=src_ap), writes=[b_stg[i]])

        def cast(n):
            i = n % 3
            gr = jobs[n][2]
            if gr is not None:
                kb.op("pool", lambda e: e.tensor_scalar(out=stb[i], in0=stg[i], scalar1=gcol[:, gr:gr + 1], scalar2=None,
                                                        op0=ALU.mult), reads=[b_stg[i], b_gcol], writes=[b_stb[i]])
            else:
                kb.op("pool", lambda e: e.tensor_copy(out=stb[i], in_=stg[i]), reads=[b_stg[i]], writes=[b_stb[i]])

        def st(n):
            i = n % 3
            dst_ap, b_dst = jobs[n][1], jobs[n][3]
            kb.dma("sp", lambda e: e.dma_start(out=dst_ap, in_=stb[i]), reads=[b_stb[i]], writes=[b_dst], accumulate=True)

        NJ = len(jobs)
        for n in range(NJ + 2):
            if n < NJ:
                ld(n)
            if 0 <= n - 1 < NJ:
                cast(n - 1)
            if 0 <= n - 2 < NJ:
                st(n - 2)
            yield n
        precast_state["done"] = True

    N_PRECAST = 96 + 2 * (NEX // 128) + 2

    def phase_ret():
        with ExitStack() as es:
            cst, b_cst = load_consts(es)
            UT = cst[:, CO_UT:CO_UT + 128]
            st_f = sb(es, "st_f", [128, RH, 2, 512], F32)
            st_b = sb(es, "st_b", [128, RH, 2, 512], BF16)
            b_stf = [Buf("stf") for _ in range(RH)]
            b_stb = [Buf("stb") for _ in range(RH)]
            for h in range(RH):
                kb.op("pool", lambda e, h=h: e.memset(st_f[:, h], 0.0), writes=[b_stf[h]])
                kb.op("pool", lambda e, h=h: e.memset(st_b[:, h], 0.0), writes=[b_stb[h]])
            k_in = sb(es, "k_in", [128, 2048], BF16); b_kin = Buf()
            q_in = sb(es, "q_in", [128, 2048], BF16); b_qin = Buf()
            v_in = sb(es, "v_in", [128, 4096], BF16); b_vin = Buf()
            g_in = sb(es, "g_in", [128, 4096], BF16); b_gin = Buf()
            cos_t = sb(es, "cos_t", [128, 128], F32); b_cos = Buf()
            sin_t = sb(es, "sin_t", [128, 128], F32); b_sin = Buf()
            tmp1 = sb(es, "tmp1", [128, 8, 128], F32); b_t1 = Buf()
            tmp2 = sb(es, "tmp2", [128, 8, 128], F32); b_t2 = Buf()
            kr = sb(es, "kr", [128, 8, 2, 128], F32); b_kr = Buf()
            kt_ = sb(es, "kt_", [128, 8, 256], BF16); b_kt = Buf()
            kz_ = sb(es, "kz_", [128, 8, 256], BF16); b_kz = Buf()
            qt_ = sb(es, "qt_", [128, 8, 256], BF16); b_qt = Buf()
            qT = sb(es, "qT", [128, 16, 128], BF16); b_qT = Buf()
            kT = sb(es, "kT", [128, 16, 128], BF16); b_kT = Buf()
            sTm = [sb(es, "sTm%d" % i, [128, 128], BF16) for i in range(2)]; b_sTm = [Buf(), Buf()]
            o_sb = sb(es, "o_sb", [128, RH, 512], F32); b_osb = [Buf() for _ in range(RH)]
            sg = sb(es, "sg", [128, RH, 512], BF16); b_sg = Buf()
            og = sb(es, "og", [128, RH * 512], BF16); b_og = Buf()
            oT = sb(es, "oT", [128, 32, 128], BF16); b_oT = Buf()
            junk = sb(es, "junkr", [128, 512], BF16); b_junk = Buf()
            ssq = sb(es, "ssq", [128, 32], F32); b_ssq = Buf()
            ptr = ps(es, "ptr_r", [128, 16, 128], BF16); b_ptr = Buf()
            ps_s = ps(es, "ps_s", [128, 512], F32); b_pss = Buf()
            ps_o = [ps(es, "ps_o%d" % i, [128, 512], F32) for i in range(2)]; b_pso = [Buf(), Buf()]
            ps_u = [ps(es, "ps_u%d" % i, [128, 512], F32) for i in range(2)]; b_psu = [Buf(), Buf()]
            OGT_v = OGT.rearrange("(kt p) n -> p kt n", p=128)
            pc_gen = precast_units(es, ps_s, b_pss)
            pc_per_chunk = -(-N_PRECAST // cfg.nch)

            def rope(src, b_src, dst_list):
                v4 = src.rearrange("p (h f two) -> p h f two", h=8, two=2)
                t1, t2 = v4[:, :, :, 0], v4[:, :, :, 1]
                cb_, sb_ = bc_mid(cos_t, 8), bc_mid(sin_t, 8)
                kb.op("dve", lambda e: e.tensor_tensor(out=tmp1, in0=t1, in1=cb_, op=ALU.mult), reads=[b_src, b_cos], writes=[b_t1])
                kb.op("dve", lambda e: e.tensor_tensor(out=tmp2, in0=t2, in1=sb_, op=ALU.mult), reads=[b_src, b_sin], writes=[b_t2])
                kb.op("dve", lambda e: e.tensor_tensor(out=kr[:, :, 0, :], in0=tmp1, in1=tmp2, op=ALU.subtract),
                      reads=[b_t1, b_t2], writes=[b_kr])
                kb.op("dve", lambda e: e.tensor_tensor(out=tmp1, in0=t1, in1=sb_, op=ALU.mult), reads=[b_src, b_sin], writes=[b_t1])
                kb.op("dve", lambda e: e.tensor_tensor(out=tmp2, in0=t2, in1=cb_, op=ALU.mult), reads=[b_src, b_cos], writes=[b_t2])
                kb.op("dve", lambda e: e.tensor_tensor(out=kr[:, :, 1, :], in0=tmp1, in1=tmp2, op=ALU.add),
                      reads=[b_t1, b_t2, b_kr], writes=[b_kr])
                krv = kr.rearrange("p h two f -> p h (two f)")
                for (dst, b_dst, co) in dst_list:
                    kb.op("dve", lambda e, dst=dst, co=co: e.tensor_tensor(out=dst, in0=krv, in1=bc_last(cst[:, co:co + 8], 256),
                                                                           op=ALU.mult), reads=[b_kr, b_cst], writes=[b_dst])

            for ch in range(cfg.nch):
                kb.maybe_epoch()
                for _ in range(pc_per_chunk):
                    next(pc_gen, None)
                main = ch >= cfg.pre
                r0 = ch * 128
                rows = slice(r0, r0 + 128)
                kb.dma("sp", lambda e, rows=rows: e.dma_start(out=k_in, in_=PTM[rows, C_K:C_K + 2048]), reads=[b_PTM], writes=[b_kin])
                kb.dma("sp", lambda e, rows=rows: e.dma_start(out=v_in, in_=PTM[rows, C_V:C_V + 4096]), reads=[b_PTM], writes=[b_vin])
                kb.dma("sp", lambda e, rows=rows: e.dma_start(out=cos_t, in_=ropec[rows, :]), writes=[b_cos])
                kb.dma("sp", lambda e, rows=rows: e.dma_start(out=sin_t, in_=ropes[rows, :]), writes=[b_sin])
                if main:
                    kb.dma("sp", lambda e, rows=rows: e.dma_start(out=q_in, in_=PTM[rows, C_Q:C_Q + 2048]), reads=[b_PTM], writes=[b_qin])
                    kb.dma("sp", lambda e, rows=rows: e.dma_start(out=g_in, in_=PTM[rows, C_G:C_G + 4096]), reads=[b_PTM], writes=[b_gin])
                    rope(k_in, b_kin, [(kt_, b_kt, CO_DK), (kz_, b_kz, CO_DKZ)])
                    rope(q_in, b_qin, [(qt_, b_qt, CO_DQ)])
                    kb.op("act", lambda e: e.activation(out=sg.rearrange("p h f -> p (h f)"), in_=g_in, func=AF.Silu),
                          reads=[b_gin], writes=[b_sg])
                    for (src, b_src, dstT, b_dstT) in ((qt_, b_qt, qT, b_qT), (kt_, b_kt, kT, b_kT)):
                        s2 = src.rearrange("p h f -> p (h f)")
                        for k in range(16):
                            kb.op("pe", lambda e, k=k, s2=s2: e.transpose(out=ptr[:, k, :], in_=s2[:, k * 128:(k + 1) * 128],
                                                                          identity=ident_bf), reads=[b_src, b_ident], writes=[b_ptr])
                        kb.op("act", lambda e, dstT=dstT: e.activation(out=dstT, in_=ptr, func=AF.Copy), reads=[b_ptr], writes=[b_dstT])
                else:
                    rope(k_in, b_kin, [(kz_, b_kz, CO_DKZ)])
                for h in range(RH):
                    vh = v_in[:, h * 512:(h + 1) * 512]
                    if main:
                        pi = h % 2
                        for c in range(2):
                            kb.op("pe", lambda e, h=h, c=c: e.matmul(ps_s[:, 0:128], lhsT=kT[:, 2 * h + c, :], rhs=qT[:, 2 * h + c, :],
                                                                     start=(c == 0), stop=(c == 1)), reads=[b_kT, b_qT], writes=[b_pss])
                        kb.op("dve", lambda e, pi=pi: e.tensor_tensor(out=sTm[pi], in0=ps_s[:, 0:128], in1=UT, op=ALU.mult),
                              reads=[b_pss, b_cst], writes=[b_sTm[pi]])
                        kb.op("pe", lambda e, pi=pi, vh=vh: e.matmul(ps_o[pi], lhsT=sTm[pi], rhs=vh, start=True, stop=False),
                              reads=[b_sTm[pi], b_vin], writes=[b_pso[pi]])
                        for c in range(2):
                            kb.op("pe", lambda e, pi=pi, h=h, c=c: e.matmul(ps_o[pi], lhsT=qT[:, 2 * h + c, :], rhs=st_b[:, h, c, :],
                                                                            start=False, stop=(c == 1)),
                                  reads=[b_qT, b_stb[h]], writes=[b_pso[pi]])
                        kb.op("dve", lambda e, pi=pi, h=h: e.tensor_copy(out=o_sb[:, h, :], in_=ps_o[pi]), reads=[b_pso[pi]], writes=[b_osb[h]])
                        kb.op("act", lambda e, h=h: e.activation(out=junk, in_=o_sb[:, h, :], func=AF.Square,
                                                                 accum_out=ssq[:, h:h + 1]), reads=[b_osb[h]], writes=[b_junk, b_ssq])
                    for c in range(2):
                        kb.op("pe", lambda e, h=h, c=c, vh=vh: e.matmul(ps_u[c], lhsT=kz_[:, h, c * 128:(c + 1) * 128], rhs=vh,
                                                                        start=True, stop=True), reads=[b_kz, b_vin], writes=[b_psu[c]])
                        kb.op("dve", lambda e, h=h, c=c: e.scalar_tensor_tensor(out=st_f[:, h, c, :], in0=st_f[:, h, c, :],
                                                                                 scalar=cst[:, CO_G128 + h:CO_G128 + h + 1],
                                                                                 in1=ps_u[c], op0=ALU.mult, op1=ALU.add),
                              reads=[b_psu[c], b_cst, b_stf[h]], writes=[b_stf[h]])
                    kb.op("act", lambda e, h=h: e.activation(out=st_b[:, h], in_=st_f[:, h], func=AF.Copy), reads=[b_stf[h]], writes=[b_stb[h]])
                if main:
                    kb.op("dve", lambda e: e.tensor_scalar(out=ssq[:, 8:16], in0=ssq[:, 0:8], scalar1=1.0 / RDV, scalar2=EPS,
                                                           op0=ALU.mult, op1=ALU.add), reads=[b_ssq], writes=[b_ssq])
                    kb.op("act", lambda e: e.activation(out=ssq[:, 16:24], in_=ssq[:, 8:16], func=AF.Sqrt), reads=[b_ssq], writes=[b_ssq])
                    kb.op("dve", lambda e: e.reciprocal(out=ssq[:, 24:32], in_=ssq[:, 16:24]), reads=[b_ssq], writes=[b_ssq])
                    kb.op("dve", lambda e: e.tensor_tensor(out=o_sb, in0=o_sb, in1=bc_last(ssq[:, 24:32], 512), op=ALU.mult),
                          reads=b_osb + [b_ssq], writes=b_osb)
                    kb.op("dve", lambda e: e.tensor_tensor(out=og.rearrange("p (h f) -> p h f", h=RH), in0=o_sb, in1=sg, op=ALU.mult),
                          reads=b_osb + [b_sg], writes=[b_og])
                    transpose_store(og, b_og, 32, ptr, b_ptr, oT, b_oT, OGT_v, b_OGT, r0 - ROW0)
            for _ in pc_gen:
                pass
        kb.barrier()

    if "proj" in cfg.phases:
        phase_proj()
    def phase_ssd():
        with ExitStack() as es:
            cst, b_cst = load_consts(es)
            UT = cst[:, CO_UT:CO_UT + 128]
            SLm = cst[:, CO_SL:CO_SL + 128]
            ONES = cst[:, CO_ONE:CO_ONE + 128]
            diag = sb(es, "diag", [128, 48, 4, 128], BF16); b_diag = Buf()
            cwT = sb(es, "cwT", [128, 48, 8], F32); b_cwT = Buf()
            cb_bc = sb(es, "cb_bc", [128, 5120], BF16); b_cb = Buf()
            par = sb(es, "par", [128, 4, 64], F32); b_par = Buf()
            sst_f = sb(es, "sst_f", [128, SH, SP_], F32)
            sst_b = sb(es, "sst_b", [128, SH * SP_], BF16)
            b_sf = [Buf() for _ in range(SG)]; b_sbb = [Buf() for _ in range(SG)]
            ptr = ps(es, "ptr_s", [128, 16, 128], BF16); b_ptr = Buf()
            pcA = ps(es, "pcA", [128, 512], F32); b_pcA = Buf()
            pcB = ps(es, "pcB", [128, 512], F32); b_pcB = Buf()
            psm = ps(es, "psm", [128, 512], F32); b_psm = Buf()
            pseg = ps(es, "pseg", [128, 1024], F32); b_pseg = Buf()
            pst = ps(es, "pst", [128, 512], F32); b_pst = Buf()
            YST_v = YST.rearrange("(kt p) n -> p kt n", p=128)
            XB_v = XBCT.rearrange("(t p) c -> p t c", p=128)

            with ExitStack() as es2:
                cw5 = sb(es2, "cw5", [8, CONV_DIM], F32); b_cw5 = Buf()
                cbs = sb(es2, "cbs", [128, 5120], F32); b_cbs = Buf()
                kb.dma("sp", lambda e: e.dma_start(out=cw5[0:4, :], in_=conv_w), writes=[b_cw5])
                kb.dma("sp", lambda e: e.dma_start(out=cw5[4:5, :], in_=conv_b), writes=[b_cw5], accumulate=True)
                kb.dma("sp", lambda e: e.dma_start(out=cbs, in_=conv_b[0:1, 0:5120].partition_broadcast(128)), writes=[b_cbs])
                kb.op("dve", lambda e: e.tensor_copy(out=cb_bc, in_=cbs), reads=[b_cbs], writes=[b_cb])
                for t in range(48):
                    kb.op("pe", lambda e, t=t: e.transpose(out=psm[:, t * 8:t * 8 + 5], in_=cw5[0:5, t * 128:(t + 1) * 128],
                                                           identity=ident_f[0:5, 0:5]), reads=[b_cw5, b_ident], writes=[b_psm])
                kb.op("dve", lambda e: e.tensor_copy(out=cwT[:, :, 0:5], in_=psm[:, 0:384].rearrange("p (t e) -> p t e", e=8)[:, :, 0:5]),
                      reads=[b_psm], writes=[b_cwT])
                for t in range(48):
                    for w in range(4):
                        kb.op("dve", lambda e, t=t, w=w: e.tensor_scalar(out=diag[:, t, w, :], in0=ident_f, scalar1=cwT[:, t, w:w + 1],
                                                                         scalar2=None, op0=ALU.mult),
                              reads=[b_ident, b_cwT], writes=[b_diag])
                kb.dma("sp", lambda e: e.dma_start(out=par[:, 0, :], in_=dt_bias.partition_broadcast(128)), writes=[b_par])
                kb.dma("sp", lambda e: e.dma_start(out=par[:, 3, :], in_=a_log.partition_broadcast(128)), writes=[b_par], accumulate=True)
                kb.dma("sp", lambda e: e.dma_start(out=par[:, 2, :], in_=d_skip.partition_broadcast(128)), writes=[b_par], accumulate=True)
                kb.op("act", lambda e: e.activation(out=par[:, 1, :], in_=par[:, 3, :], func=AF.Exp), reads=[b_par], writes=[b_par])
                kb.op("dve", lambda e: e.tensor_scalar(out=par[:, 1, :], in0=par[:, 1, :], scalar1=-1.0, scalar2=None, op0=ALU.mult),
                      reads=[b_par], writes=[b_par])
                for g in range(SG):
                    kb.op("pool", lambda e, g=g: e.memset(sst_f[:, g * 8:(g + 1) * 8, :], 0.0), writes=[b_sf[g]])
                    kb.op("pool", lambda e, g=g: e.memset(sst_b[:, g * 512:(g + 1) * 512], 0.0), writes=[b_sbb[g]])
                kb.barrier()
            xw = sb(es, "xw", [128, 48, 131], BF16); b_xw = Buf()
            z_in = sb(es, "z_in", [128, 4096], BF16); b_zin = Buf()
            xs = sb(es, "xs", [128, SH, SP_], F32); b_xs = [Buf() for _ in range(SG)]
            xdt = sb(es, "xdt", [128, SH, SP_], BF16); b_xdt = Buf()
            decx = sb(es, "decx", [128, SH * SP_], BF16); b_decx = [Buf() for _ in range(SG)]
            Bt = sb(es, "Bt", [128, 1024], BF16); b_Bt = Buf()
            BCT = sb(es, "BCT", [128, 16, 128], BF16); b_BCT = Buf()
            segL = [sb(es, "segL%d" % i, [128, 8, 128], F32) for i in range(2)]; b_segL = [Buf(), Buf()]
            Lg = [sb(es, "Lg%d" % i, [128, 8, 128], F32) for i in range(2)]; b_Lg = [Buf(), Buf()]
            MT = [sb(es, "MT%d" % i, [128, 8, 128], BF16) for i in range(2)]; b_MT = [Buf(), Buf()]
            cbm = sb(es, "cbm", [128, 128], F32); b_cbm = Buf()
            tmpc = [sb(es, "tmpc%d" % i, [128, 512], F32) for i in range(2)]; b_tmpc = [Buf(), Buf()]
            sz = sb(es, "sz", [128, 4096], BF16); b_sz = Buf()
            ys = sb(es, "ys", [128, 4096], BF16); b_ys = Buf()
            oT = sb(es, "oT_s", [128, 32, 128], BF16); b_oT = Buf()
            sm = sb(es, "sm", [128, 8, 64], F32); b_sm = Buf()
            sm2 = sb(es, "sm2", [128, 128], F32); b_sm2 = Buf()
            dtr = sb(es, "dtr", [128, 64], F32); b_dtr = Buf()
            msk = sb(es, "msk", [128, 1], F32); b_msk = Buf()
            junk = sb(es, "junks", [128, 512], BF16); b_junk = Buf()
            ssq = sb(es, "ssq2", [128, 32], F32); b_ssq = Buf()
            xs2 = xs.rearrange("p h f -> p (h f)")
            xdt2 = xdt.rearrange("p h f -> p (h f)")
            sst_f2 = sst_f.rearrange("p h f -> p (h f)")

            for ch in range(cfg.nch):
                kb.maybe_epoch()
                main = ch >= cfg.pre
                r0 = ch * 128
                rows = slice(r0, r0 + 128)
                c0 = HALO + r0 - 3
                T = 48 if main else 40
                kb.dma("sp", lambda e, T=T, c0=c0: e.dma_start(out=xw[:, 0:T, :], in_=XB_v[:, 0:T, c0:c0 + 131]),
                       reads=[b_XBCT], writes=[b_xw])
                kb.dma("sp", lambda e, rows=rows: e.dma_start(out=dtr, in_=DTS[rows, :]), reads=[b_DTS], writes=[b_dtr])
                kb.dma("sp", lambda e, rows=rows: e.dma_start(out=msk, in_=rowmask[rows, :]), writes=[b_msk])
                if main:
                    kb.dma("sp", lambda e, rows=rows: e.dma_start(out=z_in, in_=PTM[rows, C_Z:C_Z + 4096]), reads=[b_PTM], writes=[b_zin])
                S = lambda i: sm[:, i, :]
                kb.op("dve", lambda e: e.tensor_tensor(out=S(0), in0=dtr, in1=par[:, 0, :], op=ALU.add), reads=[b_dtr, b_par], writes=[b_sm])
                kb.op("act", lambda e: e.activation(out=S(1), in_=S(0), func=AF.Abs), reads=[b_sm], writes=[b_sm])
                kb.op("act", lambda e: e.activation(out=S(2), in_=S(1), func=AF.Exp, scale=-1.0), reads=[b_sm], writes=[b_sm])
                kb.op("act", lambda e: e.activation(out=S(3), in_=S(2), func=AF.Ln, bias=ONES[:, 0:1]), reads=[b_sm, b_cst], writes=[b_sm])
                kb.op("dve", lambda e: e.scalar_tensor_tensor(out=S(4), in0=S(0), scalar=0.0, in1=S(3), op0=ALU.max, op1=ALU.add),
                      reads=[b_sm], writes=[b_sm])
                kb.op("dve", lambda e: e.tensor_scalar(out=S(5), in0=S(4), scalar1=msk[:, 0:1], scalar2=None, op0=ALU.mult),
                      reads=[b_sm, b_msk], writes=[b_sm])
                kb.op("dve", lambda e: e.tensor_tensor(out=S(6), in0=S(4), in1=par[:, 1, :], op=ALU.mult), reads=[b_sm, b_par], writes=[b_sm])
                a_ap = S(6)
                for bnk in range(10):
                    pc, b_pc = (pcA, b_pcA) if bnk % 2 == 0 else (pcB, b_pcB)
                    for tt in range(4):
                        t = bnk * 4 + tt
                        for w in range(4):
                            kb.op("pe", lambda e, pc=pc, t=t, tt=tt, w=w: e.matmul(pc[:, tt * 128:(tt + 1) * 128], lhsT=xw[:, t, w:w + 128],
                                                                                   rhs=diag[:, t, w, :], start=(w == 0), stop=(w == 3)),
                                  reads=[b_xw, b_diag], writes=[b_pc])
                    ti = bnk % 2
                    kb.op("dve", lambda e, pc=pc, ti=ti, bnk=bnk: e.tensor_tensor(out=tmpc[ti], in0=pc, in1=cb_bc[:, bnk * 512:(bnk + 1) * 512],
                                                                                  op=ALU.add), reads=[b_pc, b_cb], writes=[b_tmpc[ti]])
                    if bnk < 8:
                        kb.op("act", lambda e, ti=ti, bnk=bnk: e.activation(out=xs2[:, bnk * 512:(bnk + 1) * 512], in_=tmpc[ti], func=AF.Silu),
                              reads=[b_tmpc[ti]], writes=[b_xs[bnk]])
                    else:
                        kb.op("act", lambda e, ti=ti, bnk=bnk: e.activation(out=Bt[:, (bnk - 8) * 512:(bnk - 7) * 512], in_=tmpc[ti], func=AF.Silu),
                              reads=[b_tmpc[ti]], writes=[b_Bt])
                for bnk in range(4 if main else 2):
                    pc, b_pc = (pcA, b_pcA) if bnk % 2 == 0 else (pcB, b_pcB)
                    for tt in range(4):
                        t = 32 + bnk * 4 + tt
                        for w in range(4):
                            kb.op("pe", lambda e, pc=pc, t=t, tt=tt, w=w: e.matmul(pc[:, tt * 128:(tt + 1) * 128], lhsT=diag[:, t, w, :],
                                                                                   rhs=xw[:, t, w:w + 128], start=(w == 0), stop=(w == 3)),
                                  reads=[b_xw, b_diag], writes=[b_pc])
                    for tt in range(4):
                        t = 32 + bnk * 4 + tt
                        kb.op("act", lambda e, pc=pc, t=t, tt=tt: e.activation(out=BCT[:, t - 32, :], in_=pc[:, tt * 128:(tt + 1) * 128],
                                                                               func=AF.Silu, bias=cwT[:, t, 4:5]),
                              reads=[b_pc, b_cwT], writes=[b_BCT])
                kb.op("dve", lambda e: e.tensor_tensor(out=xdt, in0=xs, in1=bc_last(S(5), 64), op=ALU.mult), reads=b_xs + [b_sm], writes=[b_xdt])
                if main:
                    kb.op("dve", lambda e: e.tensor_tensor(out=xs, in0=xs, in1=bc_last(par[:, 2, :], 64), op=ALU.mult),
                          reads=b_xs + [b_par], writes=b_xs)
                    kb.op("act", lambda e: e.activation(out=sz, in_=z_in, func=AF.Silu), reads=[b_zin], writes=[b_sz])
                kb.op("pe", lambda e: e.matmul(psm[:, 0:64], lhsT=UT, rhs=a_ap, start=True, stop=True), reads=[b_cst, b_sm], writes=[b_psm])
                kb.op("pe", lambda e: e.matmul(psm[:, 64:128], lhsT=ONES, rhs=a_ap, start=True, stop=True), reads=[b_cst, b_sm], writes=[b_psm])
                kb.op("act", lambda e: e.activation(out=sm2, in_=psm[:, 0:128], func=AF.Exp), reads=[b_psm], writes=[b_sm2])
                eacs, cdec = sm2[:, 0:64], sm2[:, 64:128]
                for g in range(SG):
                    i2 = g % 2
                    hs = slice(g * 8, (g + 1) * 8)
                    cs = slice(g * 512, (g + 1) * 512)
                    kb.op("pool", lambda e, i2=i2, hs=hs: e.tensor_tensor(out=segL[i2], in0=bc_mid(SLm, 8), in1=bc_last(a_ap[:, hs], 128),
                                                                          op=ALU.mult), reads=[b_cst, b_sm], writes=[b_segL[i2]])
                    for hh in range(8):
                        kb.op("pe", lambda e, i2=i2, hh=hh: e.matmul(pseg[:, hh * 128:(hh + 1) * 128], lhsT=segL[i2][:, hh, :], rhs=UT,
                                                                     start=True, stop=True), reads=[b_segL[i2], b_cst], writes=[b_pseg])
                    Lg2 = Lg[i2].rearrange("p h i -> p (h i)")
                    for hf in range(2):
                        kb.op("act", lambda e, Lg2=Lg2, hf=hf: e.activation(out=Lg2[:, hf * 512:(hf + 1) * 512],
                                                                            in_=pseg[:, hf * 512:(hf + 1) * 512], func=AF.Exp),
                              reads=[b_pseg], writes=[b_Lg[i2]])
                    dec_g = Lg[i2][:, :, 127]
                    kb.op("dve", lambda e, i2=i2, hs=hs, dec_g=dec_g: e.tensor_tensor(out=decx.rearrange("p (h f) -> p h f", f=64)[:, hs, :],
                                                                                      in0=xdt[:, hs, :],
                                                                                      in1=dec_g.unsqueeze(2).to_broadcast([128, 8, 64]),
                                                                                      op=ALU.mult),
                          reads=[b_xdt, b_Lg[i2]], writes=[b_decx[g]])
                    if main:
                        kb.op("pe", lambda e, g=g: e.matmul(psm[:, 128:256], lhsT=BCT[:, g, :], rhs=BCT[:, 8 + g, :], start=True, stop=True),
                              reads=[b_BCT], writes=[b_psm])
                        kb.op("dve", lambda e: e.tensor_tensor(out=cbm, in0=psm[:, 128:256], in1=UT, op=ALU.mult),
                              reads=[b_psm, b_cst], writes=[b_cbm])
                        kb.op("pool", lambda e, i2=i2: e.tensor_tensor(out=MT[i2], in0=Lg[i2], in1=bc_mid(cbm, 8), op=ALU.mult),
                              reads=[b_Lg[i2], b_cbm], writes=[b_MT[i2]])
                        for hh in range(8):
                            kb.op("pe", lambda e, i2=i2, hh=hh, g=g: e.matmul(pcA[:, hh * 64:(hh + 1) * 64], lhsT=MT[i2][:, hh, :],
                                                                              rhs=xdt[:, g * 8 + hh, :], start=True, stop=True),
                                  reads=[b_MT[i2], b_xdt], writes=[b_pcA])
                        kb.op("pe", lambda e, g=g, cs=cs: e.matmul(pcB, lhsT=BCT[:, 8 + g, :], rhs=sst_b[:, cs], start=True, stop=True),
                              reads=[b_BCT, b_sbb[g]], writes=[b_pcB])
                        kb.op("dve", lambda e, hs=hs: e.tensor_tensor(out=tmpc[0].rearrange("p (h f) -> p h f", f=64),
                                                                      in0=pcB.rearrange("p (h f) -> p h f", f=64),
                                                                      in1=bc_last(eacs[:, hs], 64), op=ALU.mult),
                              reads=[b_pcB, b_sm2], writes=[b_tmpc[0]])
                        kb.op("dve", lambda e: e.tensor_tensor(out=tmpc[1], in0=tmpc[0], in1=pcA, op=ALU.add),
                              reads=[b_tmpc[0], b_pcA], writes=[b_tmpc[1]])
                        kb.op("dve", lambda e, cs=cs: e.tensor_tensor(out=xs2[:, cs], in0=xs2[:, cs], in1=tmpc[1], op=ALU.add),
                              reads=[b_xs[g], b_tmpc[1]], writes=[b_xs[g]])
                    kb.op("pe", lambda e, g=g, cs=cs: e.matmul(pst, lhsT=Bt[:, g * 128:(g + 1) * 128], rhs=decx[:, cs], start=True, stop=True),
                          reads=[b_Bt, b_decx[g]], writes=[b_pst])
                    kb.op("dve", lambda e, hs=hs: e.tensor_tensor(out=sst_f[:, hs, :], in0=sst_f[:, hs, :], in1=bc_last(cdec[:, hs], 64),
                                                                  op=ALU.mult), reads=[b_sf[g], b_sm2], writes=[b_sf[g]])
                    kb.op("dve", lambda e, cs=cs: e.tensor_tensor(out=sst_f2[:, cs], in0=sst_f2[:, cs], in1=pst, op=ALU.add),
                          reads=[b_sf[g], b_pst], writes=[b_sf[g]])
                    kb.op("act", lambda e, cs=cs: e.activation(out=sst_b[:, cs], in_=sst_f2[:, cs], func=AF.Copy),
                          reads=[b_sf[g]], writes=[b_sbb[g]])
                if main:
                    kb.op("dve", lambda e: e.tensor_tensor(out=xs2, in0=xs2, in1=sz, op=ALU.mult), reads=b_xs + [b_sz], writes=b_xs)
                    for g in range(SG):
                        kb.op("act", lambda e, g=g: e.activation(out=junk, in_=xs2[:, g * 512:(g + 1) * 512], func=AF.Square,
                                                                 accum_out=ssq[:, g:g + 1]), reads=[b_xs[g]], writes=[b_junk, b_ssq])
                    kb.op("dve", lambda e: e.tensor_scalar(out=ssq[:, 8:16], in0=ssq[:, 0:8], scalar1=1.0 / 512, scalar2=EPS,
                                                           op0=ALU.mult, op1=ALU.add), reads=[b_ssq], writes=[b_ssq])
                    kb.op("act", lambda e: e.activation(out=ssq[:, 16:24], in_=ssq[:, 8:16], func=AF.Sqrt), reads=[b_ssq], writes=[b_ssq])
                    kb.op("dve", lambda e: e.reciprocal(out=ssq[:, 24:32], in_=ssq[:, 16:24]), reads=[b_ssq], writes=[b_ssq])
                    kb.op("dve", lambda e: e.tensor_tensor(out=ys.rearrange("p (g f) -> p g f", f=512),
                                                           in0=xs2.rearrange("p (g f) -> p g f", f=512),
                                                           in1=bc_last(ssq[:, 24:32], 512), op=ALU.mult),
                          reads=b_xs + [b_ssq], writes=[b_ys])
                    transpose_store(ys, b_ys, 32, ptr, b_ptr, oT, b_oT, YST_v, b_YST, r0 - ROW0)
        kb.barrier()

    if "ret" in cfg.phases:
        phase_ret()
    def phase_out():
        with ExitStack() as es:
            TBC = 3
            TBM = TBC * 128
            g_bc = sb(es, "gf_bc", [128, D], F32); b_g = Buf()
            kb.dma("sp", lambda e: e.dma_start(out=g_bc, in_=norm_ffn_g.partition_broadcast(128)), writes=[b_g])
            skT = sb(es, "skT", [128, 16, 128], F32); b_skT = Buf()
            ptr = ps(es, "ptr_o", [128, 16, 128], BF16); b_ptr = Buf()
            pA = ps(es, "pA", [128, 512], F32); b_pA = Buf()
            pB = ps(es, "pB", [128, 512], F32); b_pB = Buf()
            pC = [ps(es, "pC%d" % i, [128, 512], F32) for i in range(2)]; b_pC = [Buf(), Buf()]
            pD = ps(es, "pD", [128, 512], F32); b_pD = Buf()

            with ExitStack() as es2:
                skr = sb(es2, "skr", [128, 16, 128], F32); b_skr = Buf()
                kb.dma("sp", lambda e: e.dma_start(out=skr, in_=peer_sub_keys.rearrange("(j k) d -> k j d", k=128)), writes=[b_skr])
                for j in range(16):
                    kb.op("pe", lambda e, j=j: e.transpose(out=pC[j % 2][:, 0:128], in_=skr[:, j, :], identity=ident_f),
                          reads=[b_skr, b_ident], writes=[b_pC[j % 2]])
                    kb.op("dve", lambda e, j=j: e.tensor_copy(out=skT[:, j, :], in_=pC[j % 2][:, 0:128]), reads=[b_pC[j % 2]], writes=[b_skT])
                if not precast_state["done"]:
                    for _ in precast_units(es2, pA, b_pA):
                        pass
                kb.barrier()

            ogT = sb(es, "ogT", [128, 32, TBM], BF16); b_ogT = Buf()
            ysT = sb(es, "ysT", [128, 32, TBM], BF16); b_ysT = Buf()
            mT = sb(es, "mT", [128, 16, TBM], BF16); b_mT = Buf()
            xT = sb(es, "xT", [128, 16, TBM], BF16); b_xT = Buf()
            hb = sb(es, "hb", [128, TBC, D], F32); b_hb = [Buf() for _ in range(TBC)]
            NWB = 4
            wbf = [sb(es, "wbf_o%d" % i, [128, 4096], BF16) for i in range(NWB)]; b_wbf = [Buf() for _ in range(NWB)]
            grt = [sb(es, "grt%d" % i, [128, 2, TBM], BF16) for i in range(2)]; b_grt = [Buf(), Buf()]
            sgt = [sb(es, "sgt%d" % i, [128, 2, TBM], F32) for i in range(2)]; b_sgt = [Buf(), Buf()]
            t1 = sb(es, "t1o", [128, TBM], F32); b_t1 = Buf()
            t2 = sb(es, "t2o", [128, TBM], F32); b_t2 = Buf()
            n_bf = sb(es, "n_bf_o", [128, D], BF16); b_n = Buf()
            junk = sb(es, "junk_o", [128, D], BF16); b_junk = Buf()
            stat = sb(es, "stat_o", [128, 4], F32); b_stat = Buf()
            qpT = sb(es, "qpT", [128, TBM], F32); b_qpT = Buf()
            sct = sb(es, "sct", [128, TBC, 128], F32); b_sct = Buf()
            OGT_v = OGT.rearrange("(kt p) n -> p kt n", p=128)
            YST_v = YST.rearrange("(kt p) n -> p kt n", p=128)
            wr_v = WRB.rearrange("(kt p) c -> p kt c", p=128)
            ws_v = WSB.rearrange("(kt p) c -> p kt c", p=128)
            wo_v = WOB.rearrange("(kt p) c -> p kt c", p=128)
            wq_v = WQB.rearrange("(kt p) c -> p kt c", p=128)
            SC_v = SC.rearrange("(t p) c -> p t c", p=128)
            cnt = {"w": 0, "g": 0, "pc": 0}

            def load_w(view, kts, c0, ncol, scale_g=False):
                wi = cnt["w"] % NWB
                cnt["w"] += 1
                dsb = wbf[wi][:, 0:kts * ncol].rearrange("p (k c) -> p k c", c=ncol)
                kb.dma("sp", lambda e: e.dma_start(out=dsb, in_=view[:, :, c0:c0 + ncol]), reads=[b_WB], writes=[b_wbf[wi]])
                return dsb, b_wbf[wi]

            ch = 0
            while ch < cfg.main:
                nsub = min(TBC, cfg.main - ch)
                ntok = nsub * 128
                c0 = ch * 128
                kb.dma("sp", lambda e, c0=c0, ntok=ntok: e.dma_start(out=ogT[:, :, 0:ntok], in_=OGT_v[:, :, c0:c0 + ntok]),
                       reads=[b_OGT], writes=[b_ogT])
                kb.dma("sp", lambda e, c0=c0, ntok=ntok: e.dma_start(out=ysT[:, :, 0:ntok], in_=YST_v[:, :, c0:c0 + ntok]),
                       reads=[b_YST], writes=[b_ysT])
                for t in range(nsub):
                    rows = slice(ROW0 + c0 + t * 128, ROW0 + c0 + (t + 1) * 128)
                    kb.dma("sp", lambda e, t=t, rows=rows: e.dma_start(out=hb[:, t, :], in_=hin[rows, :]), writes=[b_hb[t]])
                for m in range(16):
                    kb.maybe_epoch()
                    wr, b_wr = load_w(wr_v, 32, m * 128, 128)
                    for kt in range(32):
                        kb.op("pe", lambda e, kt=kt, wr=wr, ntok=ntok: e.matmul(pA[:, 0:ntok], lhsT=wr[:, kt, :], rhs=ogT[:, kt, 0:ntok],
                                                                                start=(kt == 0), stop=(kt == 31)),
                              reads=[b_wr, b_ogT], writes=[b_pA])
                    ws, b_ws = load_w(ws_v, 32, m * 128, 128, scale_g=True)
                    for kt in range(32):
                        kb.op("pe", lambda e, kt=kt, ws=ws, ntok=ntok: e.matmul(pB[:, 0:ntok], lhsT=ws[:, kt, :], rhs=ysT[:, kt, 0:ntok],
                                                                                start=(kt == 0), stop=(kt == 31)),
                              reads=[b_ws, b_ysT], writes=[b_pB])
                    gi = cnt["g"] % 2
                    cnt["g"] += 1
                    kb.dma("sp", lambda e, gi=gi, m=m, c0=c0, ntok=ntok: e.dma_start(out=grt[gi][:, 0, 0:ntok],
                                                                                     in_=GT[m * 128:(m + 1) * 128, c0:c0 + ntok]),
                           reads=[b_GT], writes=[b_grt[gi]])
                    kb.dma("sp", lambda e, gi=gi, m=m, c0=c0, ntok=ntok: e.dma_start(out=grt[gi][:, 1, 0:ntok],
                                                                                     in_=GT[D + m * 128:D + (m + 1) * 128, c0:c0 + ntok]),
                           reads=[b_GT], writes=[b_grt[gi]], accumulate=True)
                    kb.op("act", lambda e, gi=gi, ntok=ntok: e.activation(out=sgt[gi][:, :, 0:ntok], in_=grt[gi][:, :, 0:ntok], func=AF.Sigmoid),
                          reads=[b_grt[gi]], writes=[b_sgt[gi]])
                    kb.op("dve", lambda e, gi=gi, ntok=ntok: e.tensor_tensor(out=t1[:, 0:ntok], in0=pA[:, 0:ntok], in1=sgt[gi][:, 0, 0:ntok],
                                                                             op=ALU.mult), reads=[b_pA, b_sgt[gi]], writes=[b_t1])
                    kb.op("dve", lambda e, gi=gi, ntok=ntok: e.tensor_tensor(out=t2[:, 0:ntok], in0=pB[:, 0:ntok], in1=sgt[gi][:, 1, 0:ntok],
                                                                             op=ALU.mult), reads=[b_pB, b_sgt[gi]], writes=[b_t2])
                    kb.op("dve", lambda e, m=m, ntok=ntok: e.tensor_tensor(out=mT[:, m, 0:ntok], in0=t1[:, 0:ntok], in1=t2[:, 0:ntok],
                                                                           op=ALU.add), reads=[b_t1, b_t2], writes=[b_mT])
                for nb in range(8):
                    wo, b_wo = load_w(wo_v, 16, nb * 256, 256)
                    for t in range(nsub):
                        pi = cnt["pc"] % 2
                        cnt["pc"] += 1
                        for kt in range(16):
                            kb.op("pe", lambda e, pi=pi, kt=kt, t=t, wo=wo: e.matmul(pC[pi][:, 0:256], lhsT=mT[:, kt, t * 128:(t + 1) * 128],
                                                                                     rhs=wo[:, kt, :], start=(kt == 0), stop=(kt == 15)),
                                  reads=[b_mT, b_wo], writes=[b_pC[pi]])
                        kb.op("dve", lambda e, pi=pi, t=t, nb=nb: e.tensor_tensor(out=hb[:, t, nb * 256:(nb + 1) * 256],
                                                                                  in0=hb[:, t, nb * 256:(nb + 1) * 256], in1=pC[pi][:, 0:256],
                                                                                  op=ALU.add), reads=[b_hb[t], b_pC[pi]], writes=[b_hb[t]])
                for t in range(nsub):
                    rows = slice(c0 + t * 128, c0 + (t + 1) * 128)
                    kb.dma("sp", lambda e, t=t, rows=rows: e.dma_start(out=H1[rows, :], in_=hb[:, t, :]), reads=[b_hb[t]], writes=[b_H1],
                           accumulate=True)
                    kb.op("act", lambda e, t=t: e.activation(out=junk, in_=hb[:, t, :], func=AF.Square, accum_out=stat[:, 0:1]),
                          reads=[b_hb[t]], writes=[b_junk, b_stat])
                    kb.op("dve", lambda e: e.tensor_scalar(out=stat[:, 1:2], in0=stat[:, 0:1], scalar1=1.0 / D, scalar2=EPS,
                                                           op0=ALU.mult, op1=ALU.add), reads=[b_stat], writes=[b_stat])
                    kb.op("act", lambda e: e.activation(out=stat[:, 3:4], in_=stat[:, 1:2], func=AF.Sqrt), reads=[b_stat], writes=[b_stat])
                    kb.op("dve", lambda e: e.reciprocal(out=stat[:, 2:3], in_=stat[:, 3:4]), reads=[b_stat], writes=[b_stat])
                    kb.op("dve", lambda e, t=t: e.scalar_tensor_tensor(out=n_bf, in0=hb[:, t, :], scalar=stat[:, 2:3], in1=g_bc,
                                                                       op0=ALU.mult, op1=ALU.mult), reads=[b_hb[t], b_stat, b_g], writes=[b_n])
                    kb.dma("sp", lambda e, rows=rows: e.dma_start(out=XN2[rows, :], in_=n_bf), reads=[b_n], writes=[b_XN2], accumulate=True)
                    for k in range(16):
                        kb.op("pe", lambda e, k=k: e.transpose(out=ptr[:, k, :], in_=n_bf[:, k * 128:(k + 1) * 128], identity=ident_bf),
                              reads=[b_n, b_ident], writes=[b_ptr])
                    kb.op("act", lambda e, t=t: e.activation(out=xT[:, :, t * 128:(t + 1) * 128], in_=ptr, func=AF.Copy),
                          reads=[b_ptr], writes=[b_xT])
                for j in range(16):
                    wq, b_wq = load_w(wq_v, 16, j * 128, 128)
                    pi = cnt["pc"] % 2
                    cnt["pc"] += 1
                    for kt in range(16):
                        kb.op("pe", lambda e, pi=pi, kt=kt, wq=wq, ntok=ntok: e.matmul(pC[pi][:, 0:ntok], lhsT=wq[:, kt, :], rhs=xT[:, kt, 0:ntok],
                                                                                       start=(kt == 0), stop=(kt == 15)),
                              reads=[b_wq, b_xT], writes=[b_pC[pi]])
                    kb.op("act", lambda e, pi=pi, ntok=ntok: e.activation(out=qpT[:, 0:ntok], in_=pC[pi][:, 0:ntok], func=AF.Copy),
                          reads=[b_pC[pi]], writes=[b_qpT])
                    for t in range(nsub):
                        kb.op("pe", lambda e, t=t, j=j: e.matmul(pD[:, t * 128:(t + 1) * 128], lhsT=qpT[:, t * 128:(t + 1) * 128], rhs=skT[:, j, :],
                                                                 start=True, stop=True), reads=[b_qpT, b_skT], writes=[b_pD])
                    kb.op("dve", lambda e, nsub=nsub, ntok=ntok: e.tensor_copy(out=sct[:, 0:nsub, :],
                                                                               in_=pD[:, 0:ntok].rearrange("p (t k) -> p t k", k=128)),
                          reads=[b_pD], writes=[b_sct])
                    kb.dma("sp", lambda e, j=j, ch=ch, nsub=nsub: e.dma_start(out=SC_v[:, ch:ch + nsub, j * 128:(j + 1) * 128], in_=sct[:, 0:nsub, :]),
                           reads=[b_sct], writes=[b_SC], accumulate=True)
                ch += nsub
        kb.barrier()

    if "ssd" in cfg.phases:
        phase_ssd()
    def phase_peer():
        with ExitStack() as es:
            NEG = -1.0e30
            NB = 6
            gf_bc = sb(es, "gfin_bc", [128, D], F32); b_gf = Buf()
            kb.dma("sp", lambda e: e.dma_start(out=gf_bc, in_=norm_final_g.partition_broadcast(128)), writes=[b_gf])
            iota = sb(es, "iota", [128, 256], F32); b_iota = Buf()
            iota_i = sb(es, "iota_i", [128, 256], I32)
            kb.op("pool", lambda e: e.iota(iota_i, pattern=[[1, 256]], base=0, channel_multiplier=0), writes=[b_iota])
            kb.op("dve", lambda e: e.tensor_copy(out=iota, in_=iota_i), reads=[b_iota], writes=[b_iota])
            if not precast_state["done"]:
                with ExitStack() as es2:
                    ptmp = ps(es2, "ptmp", [128, 512], F32); b_ptmp = Buf()
                    for _ in precast_units(es2, ptmp, b_ptmp):
                        pass
                    kb.barrier()
            sc = sb(es, "sc", [128, 16, 128], F32); b_sc = Buf()
            work = sb(es, "work", [128, 256], F32); b_work = Buf()
            v16 = sb(es, "v16", [128, 16, 16], F32); b_v16 = Buf()
            i16 = sb(es, "i16", [128, 16, 16], U32); b_i16 = Buf()
            i16f = sb(es, "i16f", [128, 16, 16], F32); b_i16f = Buf()
            i16s = sb(es, "i16s", [128, 16, 16], F32); b_i16s = Buf()
            cand = sb(es, "cand", [128, 8, 256], F32); b_cand = Buf()
            cid = sb(es, "cid", [128, 8, 256], F32); b_cid = Buf()
            top = sb(es, "top", [128, 8, 16], F32); b_top = Buf()
            pos = sb(es, "pos", [128, 8, 16], U32); b_pos = Buf()
            posf = sb(es, "posf", [128, 8, 16], F32); b_posf = Buf()
            eq = sb(es, "eq", [128, 16, 256], F32); b_eq = Buf()
            eidf = sb(es, "eidf", [128, 128], F32); b_eidf = Buf()
            eid = sb(es, "eid", [128, 128], I32); b_eid = Buf()
            gsm = sb(es, "gsm", [128, 4, 128], F32); b_gsm = Buf()
            gz = sb(es, "gz", [128, 16], F32); b_gz = Buf()
            act_t = sb(es, "act_t", [128, 128], F32); b_act = Buf()
            wgt = sb(es, "wgt", [128, 128], F32); b_wgt = Buf()
            GS, NGB = 4, 3
            dgs = [sb(es, "dgs%d" % i, [128, GS, 128], BF16) for i in range(NGB)]; b_dgs = [Buf() for _ in range(NGB)]
            uv = [sb(es, "uv%d" % i, [128, GS, 2 * D], BF16) for i in range(NGB)]
            b_uv = [[Buf() for _ in range(GS)] for _ in range(NGB)]
            b_actg = [Buf() for _ in range(128 // GS)]
            xn = sb(es, "xn", [128, D], BF16); b_xn = Buf()
            h1t = sb(es, "h1t", [128, D], F32); b_h1 = Buf()
            junk = sb(es, "junk_p", [128, D], BF16); b_junk = Buf()
            prod = [sb(es, "prod%d" % i, [128, D], BF16) for i in range(2)]; b_prod = [Buf(), Buf()]
            stat = sb(es, "stat_p", [128, 4], F32); b_stat = Buf()
            ot = sb(es, "ot", [128, D], F32); b_ot = Buf()
            pacc = [ps(es, "pacc%d" % i, [128, 512], F32) for i in range(4)]; b_pacc = [Buf() for _ in range(4)]
            cnt = {"u": 0, "v": 0}

            for ch in range(cfg.main):
                kb.maybe_epoch()
                rows = slice(ch * 128, (ch + 1) * 128)
                kb.dma("sp", lambda e, rows=rows: e.dma_start(out=sc.rearrange("p j k -> p (j k)"), in_=SC[rows, :]), reads=[b_SC], writes=[b_sc])
                kb.dma("sp", lambda e, rows=rows: e.dma_start(out=xn, in_=XN2[rows, :]), reads=[b_XN2], writes=[b_xn])
                kb.dma("sp", lambda e, rows=rows: e.dma_start(out=h1t, in_=H1[rows, :]), reads=[b_H1], writes=[b_h1])
                for j in range(16):
                    kb.op("dve", lambda e, j=j: e.max(out=v16[:, j, 0:8], in_=sc[:, j, :]), reads=[b_sc], writes=[b_v16])
                    kb.op("dve", lambda e, j=j: e.match_replace(out=work[:, 0:128], in_to_replace=v16[:, j, 0:8], in_values=sc[:, j, :],
                                                                imm_value=NEG), reads=[b_sc, b_v16], writes=[b_work])
                    kb.op("dve", lambda e, j=j: e.max(out=v16[:, j, 8:16], in_=work[:, 0:128]), reads=[b_work, b_v16], writes=[b_v16])
                    kb.op("dve", lambda e, j=j: e.max_index(out=i16[:, j, 0:8], in_max=v16[:, j, 0:8], in_values=sc[:, j, :]),
                          reads=[b_sc, b_v16], writes=[b_i16])
                    kb.op("dve", lambda e, j=j: e.max_index(out=i16[:, j, 8:16], in_max=v16[:, j, 8:16], in_values=sc[:, j, :]),
                          reads=[b_sc, b_v16, b_i16], writes=[b_i16])
                kb.op("dve", lambda e: e.tensor_copy(out=i16f, in_=i16), reads=[b_i16], writes=[b_i16f])
                v4 = v16.rearrange("p (h c) k -> p h c k", c=2)
                f4 = i16f.rearrange("p (h c) k -> p h c k", c=2)
                s4 = i16s.rearrange("p (h c) k -> p h c k", c=2)
                c4 = cand.rearrange("p h (a b) -> p h a b", b=16)
                d4 = cid.rearrange("p h (a b) -> p h a b", b=16)
                kb.op("dve", lambda e: e.tensor_tensor(out=c4, in0=v4[:, :, 0, :].unsqueeze(3).to_broadcast([128, 8, 16, 16]),
                                                       in1=v4[:, :, 1, :].unsqueeze(2).to_broadcast([128, 8, 16, 16]), op=ALU.add),
                      reads=[b_v16], writes=[b_cand])
                kb.op("dve", lambda e: e.tensor_scalar(out=i16s, in0=i16f, scalar1=float(PNK), scalar2=None, op0=ALU.mult),
                      reads=[b_i16f], writes=[b_i16s])
                kb.op("dve", lambda e: e.tensor_tensor(out=d4, in0=s4[:, :, 0, :].unsqueeze(3).to_broadcast([128, 8, 16, 16]),
                                                       in1=f4[:, :, 1, :].unsqueeze(2).to_broadcast([128, 8, 16, 16]), op=ALU.add),
                      reads=[b_i16f, b_i16s], writes=[b_cid])
                for h in range(PH):
                    kb.op("dve", lambda e, h=h: e.max(out=top[:, h, 0:8], in_=cand[:, h, :]), reads=[b_cand], writes=[b_top])
                    kb.op("dve", lambda e, h=h: e.match_replace(out=work, in_to_replace=top[:, h, 0:8], in_values=cand[:, h, :],
                                                                imm_value=NEG), reads=[b_cand, b_top], writes=[b_work])
                    kb.op("dve", lambda e, h=h: e.max(out=top[:, h, 8:16], in_=work), reads=[b_work, b_top], writes=[b_top])
                    kb.op("dve", lambda e, h=h: e.max_index(out=pos[:, h, 0:8], in_max=top[:, h, 0:8], in_values=cand[:, h, :]),
                          reads=[b_cand, b_top], writes=[b_pos])
                    kb.op("dve", lambda e, h=h: e.max_index(out=pos[:, h, 8:16], in_max=top[:, h, 8:16], in_values=cand[:, h, :]),
                          reads=[b_cand, b_top, b_pos], writes=[b_pos])
                kb.op("dve", lambda e: e.tensor_copy(out=posf, in_=pos), reads=[b_pos], writes=[b_posf])
                for h in range(PH):
                    kb.op("dve", lambda e, h=h: e.tensor_tensor(out=eq, in0=iota.unsqueeze(1).to_broadcast([128, 16, 256]),
                                                                in1=posf[:, h, :].unsqueeze(2).to_broadcast([128, 16, 256]),
                                                                op=ALU.is_equal), reads=[b_iota, b_posf], writes=[b_eq])
                    kb.op("dve", lambda e, h=h: e.tensor_tensor(out=eq, in0=eq, in1=cid[:, h, :].unsqueeze(1).to_broadcast([128, 16, 256]),
                                                                op=ALU.mult), reads=[b_eq, b_cid], writes=[b_eq])
                    kb.op("dve", lambda e, h=h: e.tensor_reduce(out=eidf[:, h * 16:(h + 1) * 16], in_=eq, axis=AX.X, op=ALU.add),
                          reads=[b_eq], writes=[b_eidf])
                kb.op("dve", lambda e: e.tensor_copy(out=eid, in_=eidf), reads=[b_eidf], writes=[b_eid])
                G = lambda i: gsm[:, i, :]
                t3 = top
                kb.op("dve", lambda e: e.tensor_tensor(out=G(0).rearrange("p (h k) -> p h k", k=16), in0=t3,
                                                       in1=t3[:, :, 0:1].to_broadcast([128, 8, 16]), op=ALU.subtract),
                      reads=[b_top], writes=[b_gsm])
                kb.op("act", lambda e: e.activation(out=G(1), in_=G(0), func=AF.Exp), reads=[b_gsm], writes=[b_gsm])
                kb.op("dve", lambda e: e.tensor_reduce(out=gz[:, 0:8], in_=G(1).rearrange("p (h k) -> p h k", k=16), axis=AX.X, op=ALU.add),
                      reads=[b_gsm], writes=[b_gz])
                kb.op("dve", lambda e: e.reciprocal(out=gz[:, 8:16], in_=gz[:, 0:8]), reads=[b_gz], writes=[b_gz])
                kb.op("dve", lambda e: e.tensor_tensor(out=G(2).rearrange("p (h k) -> p h k", k=16),
                                                       in0=G(1).rearrange("p (h k) -> p h k", k=16),
                                                       in1=bc_last(gz[:, 8:16], 16), op=ALU.mult), reads=[b_gsm, b_gz], writes=[b_gsm])
                NG = 128 // GS

                def finish_group(gq):
                    bi = gq % NGB
                    s0 = gq * GS
                    kb.op("act", lambda e: e.activation(out=gsm[:, 3, s0:s0 + GS], in_=act_t[:, s0:s0 + GS], func=AF.Gelu),
                          reads=[b_actg[gq]], writes=[b_actg[gq]])
                    kb.op("dve", lambda e: e.tensor_tensor(out=wgt[:, s0:s0 + GS], in0=gsm[:, 3, s0:s0 + GS], in1=gsm[:, 2, s0:s0 + GS],
                                                           op=ALU.mult), reads=[b_actg[gq], b_gsm], writes=[b_actg[gq]])
                    kb.op("dve", lambda e: e.tensor_tensor(out=dgs[bi], in0=ident_f.unsqueeze(1).to_broadcast([128, GS, 128]),
                                                           in1=bc_last(wgt[:, s0:s0 + GS], 128), op=ALU.mult),
                          reads=[b_ident, b_actg[gq]], writes=[b_dgs[bi]])
                    for i in range(GS):
                        s_ = s0 + i
                        for nb in range(4):
                            kb.op("pe", lambda e, i=i, s_=s_, nb=nb: e.matmul(pacc[nb], lhsT=dgs[bi][:, i, :],
                                                                              rhs=uv[bi][:, i, D + nb * 512:D + (nb + 1) * 512],
                                                                              start=(s_ == 0), stop=(s_ == 127)),
                                  reads=[b_dgs[bi], b_uv[bi][i]], writes=[b_pacc[nb]])

                for gq in range(NG):
                    bi = gq % NGB
                    s0 = gq * GS
                    for i in range(GS):
                        s_ = s0 + i
                        kb.dma("pool", lambda e, bi=bi, i=i, s_=s_: e.indirect_dma_start(
                            out=uv[bi][:, i, :], out_offset=None, in_=UVB,
                            in_offset=bass.IndirectOffsetOnAxis(ap=eid[:, s_:s_ + 1], axis=0)),
                            reads=[b_eid, b_UVB], writes=[b_uv[bi][i]])
                    for i in range(GS):
                        s_ = s0 + i
                        pj = s_ % 2
                        kb.op("dve", lambda e, bi=bi, i=i, pj=pj: e.tensor_tensor(out=prod[pj], in0=uv[bi][:, i, 0:D], in1=xn, op=ALU.mult),
                              reads=[b_uv[bi][i], b_xn], writes=[b_prod[pj]])
                        kb.op("act", lambda e, pj=pj, s_=s_: e.activation(out=junk, in_=prod[pj], func=AF.Copy, accum_out=act_t[:, s_:s_ + 1]),
                              reads=[b_prod[pj]], writes=[b_junk, b_actg[gq]])
                    if gq >= 1:
                        finish_group(gq - 1)
                finish_group(NG - 1)
                for nb in range(4):
                    kb.op("dve", lambda e, nb=nb: e.tensor_tensor(out=h1t[:, nb * 512:(nb + 1) * 512], in0=h1t[:, nb * 512:(nb + 1) * 512],
                                                                  in1=pacc[nb], op=ALU.add), reads=[b_h1, b_pacc[nb]], writes=[b_h1])
                kb.op("act", lambda e: e.activation(out=junk, in_=h1t, func=AF.Square, accum_out=stat[:, 0:1]),
                      reads=[b_h1], writes=[b_junk, b_stat])
                kb.op("dve", lambda e: e.tensor_scalar(out=stat[:, 1:2], in0=stat[:, 0:1], scalar1=1.0 / D, scalar2=EPS,
                                                       op0=ALU.mult, op1=ALU.add), reads=[b_stat], writes=[b_stat])
                kb.op("act", lambda e: e.activation(out=stat[:, 3:4], in_=stat[:, 1:2], func=AF.Sqrt), reads=[b_stat], writes=[b_stat])
                kb.op("dve", lambda e: e.reciprocal(out=stat[:, 2:3], in_=stat[:, 3:4]), reads=[b_stat], writes=[b_stat])
                kb.op("dve", lambda e: e.scalar_tensor_tensor(out=ot, in0=h1t, scalar=stat[:, 2:3], in1=gf_bc, op0=ALU.mult, op1=ALU.mult),
                      reads=[b_h1, b_stat, b_gf], writes=[b_ot])
                kb.dma("sp", lambda e, rows=rows: e.dma_start(out=out[rows, :], in_=ot), reads=[b_ot], writes=[b_out], accumulate=True)
        kb.barrier()

    if "out" in cfg.phases:
        phase_out()
    if "peer" in cfg.phases:
        phase_peer()

    kb.barrier()
    es_glob.close()
    return nc, kb


def make_consts():
    c = np.zeros((128, 4096), np.float64)
    i = np.arange(128)
    c[:, 0:128] = (i[None, :] >= i[:, None])
    c[:, 128:256] = (i[:, None] > i[None, :])
    c[:, 256:384] = 1.0
    gam = 1.0 - 2.0 ** (-5.0 - np.arange(8))
    lg = np.log(gam)
    c[:, 384:392] = np.exp(lg[None, :] * (i[:, None] + 1.0))
    c[:, 392:400] = np.exp(-lg[None, :] * (i[:, None] + 1.0)) / 16.0
    c[:, 400:408] = np.exp(lg[None, :] * (127.0 - i[:, None])) / 16.0
    c[:, 408:416] = np.exp(lg[None, :] * 128.0)
    return c.astype(np.float32)


def core_streams(x, meta_tokens, cfg):
    B, S, _ = x.shape
    maps = []
    inv = (10000.0 ** (-np.arange(128, dtype=np.float32) / np.float32(128))).astype(np.float32)
    for core in range(8):
        b, s = core // 2, core % 2
        full = np.zeros((LEAD + N_META + S, D), np.float32)
        full[LEAD:LEAD + N_META] = meta_tokens
        full[LEAD + N_META:] = x[b]
        valid = np.zeros((full.shape[0], 1), np.float32)
        valid[LEAD:] = 1.0
        pos = np.maximum(np.arange(full.shape[0]) - LEAD, 0).astype(np.float32)
        hin = np.zeros((cfg.nt, D), np.float32)
        msk = np.zeros((cfg.nt, 1), np.float32)
        p = np.zeros((cfg.nt,), np.float32)
        if s == 0:
            n = (cfg.main + 1) * 128
            r = (cfg.pre - 1) * 128
            hin[r:] = full[0:n]
            msk[r:] = valid[0:n]
            p[r:] = pos[0:n]
        else:
            hin[:] = full[0:cfg.nt]
            msk[:] = valid[0:cfg.nt]
            p[:] = pos[0:cfg.nt]
        ang = p[:, None] * inv[None, :]
        maps.append({"hin": hin, "rowmask": msk, "ropec": np.cos(ang).astype(np.float32),
                     "ropes": np.sin(ang).astype(np.float32)})
    return maps


_PROG = {}


def kernel(x, meta_tokens, norm_mix_g, w_in, conv_w, conv_b, dt_bias, a_log, d_skip, ssm_norm_g, w_ret_o,
           w_ssm_o, w_out, norm_ffn_g, peer_w_q, peer_sub_keys, peer_u, peer_v, norm_final_g):
    cfg = Cfg()
    x = np.asarray(x, np.float32)
    maps = core_streams(x, np.asarray(meta_tokens, np.float32), cfg)
    shared = {
        "cst_f": make_consts(),
        "norm_mix_g": np.asarray(norm_mix_g, np.float32).reshape(1, D),
        "w_in": np.asarray(w_in, np.float32).reshape(D, NPROJ),
        "conv_w": np.asarray(conv_w, np.float32).reshape(4, CONV_DIM),
        "conv_b": np.asarray(conv_b, np.float32).reshape(1, CONV_DIM),
        "dt_bias": np.asarray(dt_bias, np.float32).reshape(1, SH),
        "a_log": np.asarray(a_log, np.float32).reshape(1, SH),
        "d_skip": np.asarray(d_skip, np.float32).reshape(1, SH),
        "ssm_norm_g": np.asarray(ssm_norm_g, np.float32).reshape(1, SSM_INNER),
        "w_ret_o": np.asarray(w_ret_o, np.float32).reshape(RH * RDV, D),
        "w_ssm_o": np.asarray(w_ssm_o, np.float32).reshape(SSM_INNER, D),
        "w_out": np.asarray(w_out, np.float32).reshape(D, D),
        "norm_ffn_g": np.asarray(norm_ffn_g, np.float32).reshape(1, D),
        "peer_w_q": np.asarray(peer_w_q, np.float32).reshape(D, D),
        "peer_sub_keys": np.asarray(peer_sub_keys, np.float32).reshape(16 * 128, 128),
        "peer_u": np.asarray(peer_u, np.float32).reshape(NEXP, D),
        "peer_v": np.asarray(peer_v, np.float32).reshape(NEXP, D),
        "norm_final_g": np.asarray(norm_final_g, np.float32).reshape(1, D),
    }
    for m in maps:
        m.update(shared)
    nc, _ = build_program(cfg)
    res = run_bass_kernel_spmd(nc, maps, core_ids=list(range(8)))
    B, S, _ = x.shape
    outp = np.zeros((B, S, D), np.float32)
    half = cfg.main * 128
    for core in range(8):
        b, s = core // 2, core % 2
        outp[b, s * half:(s + 1) * half] = res.results[core]["out"]
    return outp
```

```python
import numpy as np
from contextlib import ExitStack
import concourse.bass as bass
import concourse.mybir as mybir
from concourse.bass_utils import run_bass_kernel_spmd

F32 = mybir.dt.float32
BF16 = mybir.dt.bfloat16
I32 = mybir.dt.int32
U32 = mybir.dt.uint32
AF = mybir.ActivationFunctionType
ALU = mybir.AluOpType
AX = mybir.AxisListType

D = 2048
N_META = 16
LEAD = 112
EPS = 1e-6
RH, RDK, RDV = 8, 256, 512
SSM_INNER, SH, SP_, SG, SN = 4096, 64, 64, 8, 128
CONV_DIM = 6144
NPROJ = 26688
C_Q, C_K, C_V, C_G, C_Z, C_XBC, C_DT, C_GR, C_GS = 0, 2048, 4096, 8192, 12288, 16384, 22528, 22592, 24640
PTM_W = 16384
PH, PNK, PTOPK, PHALF = 8, 128, 16, 128
NEXP = 16384
HALO = 8


class Buf:
    __slots__ = ("writers", "readers", "name")

    def __init__(self, name=""):
        self.writers = {}
        self.readers = {}
        self.name = name


class KB:
    NQ = 20

    def __init__(self, nc):
        self.nc = nc
        self.engs = {"pe": nc.tensor, "act": nc.scalar, "dve": nc.vector, "pool": nc.gpsimd, "sp": nc.sync}
        self.epoch = 0
        self.csem = {e: nc.alloc_semaphore("c_" + e) for e in ("pe", "act", "dve", "pool")}
        self.ccnt = {e: 0 for e in self.csem}
        self.dsem = {q: [nc.alloc_semaphore("d_%s%d" % (q, i)) for i in range(self.NQ)] for q in ("sp", "pool")}
        self.dcnt = {q: 0 for q in self.dsem}
        self.waited = {e: {} for e in self.engs}
        self.n_ins = 0
        self.limit = None
        self.n_ops = 0
        self.recording = False
        self.deferred_q = []

    def _wait(self, e, toks):
        w = self.waited[e]
        need = {}
        for key, (sem, val) in toks:
            if key[0] == "c":
                if key[2] < self.epoch:
                    continue
                if e == "pe" and key[1] == "pe":
                    continue
            if w.get(key, 0) >= val:
                continue
            if key not in need or need[key][1] < val:
                need[key] = (sem, val)
        for key, (sem, val) in need.items():
            self.engs[e].wait_ge(sem, val)
            w[key] = val
            self.n_ins += 1

    @staticmethod
    def _merge(dst, key, sv):
        if key not in dst or dst[key][1] < sv[1]:
            dst[key] = sv

    def _deps(self, reads, writes, accumulate=False):
        toks = []
        for b in reads:
            toks += list(b.writers.items())
        for b in writes:
            if not accumulate:
                toks += list(b.writers.items())
            toks += list(b.readers.items())
        return toks

    def _commit(self, key, sv, reads, writes, accumulate=False):
        for b in reads:
            self._merge(b.readers, key, sv)
        for b in writes:
            if accumulate:
                self._merge(b.writers, key, sv)
            else:
                b.writers = {key: sv}
                b.readers = {}

    def flush(self, n=None):
        q = self.deferred_q
        k = len(q) if n is None else min(n, len(q))
        rec, self.recording = self.recording, False
        for _ in range(k):
            kind, args = q.pop(0)
            (self.op if kind == "op" else self.dma)(*args)
        self.recording = rec

    def op(self, e, fn, reads=(), writes=()):
        if self.recording:
            self.deferred_q.append(("op", (e, fn, list(reads), list(writes))))
            return
        self.n_ops += 1
        if self.limit is not None and self.n_ops > self.limit:
            return
        self._wait(e, self._deps(reads, writes))
        ins = fn(self.engs[e])
        self.ccnt[e] += 1
        ins.then_inc(self.csem[e], 1)
        self.n_ins += 1
        self._commit(("c", e, self.epoch), (self.csem[e], self.ccnt[e]), reads, writes)

    def dma(self, q, fn, reads=(), writes=(), accumulate=False):
        if self.recording:
            self.deferred_q.append(("dma", (q, fn, list(reads), list(writes), accumulate)))
            return
        self.n_ops += 1
        if self.limit is not None and self.n_ops > self.limit:
            return
        i = self.dcnt[q]
        slot, gen = i % self.NQ, i // self.NQ
        key = ("d", q, slot)
        sem = self.dsem[q][slot]
        toks = self._deps(reads, writes, accumulate)
        if gen > 0:
            toks.append((key, (sem, 16 * gen)))
        self._wait(q, toks)
        ins = fn(self.engs[q])
        ins.then_inc(sem, 16)
        self.dcnt[q] += 1
        self.n_ins += 1
        self._commit(key, (sem, 16 * (gen + 1)), reads, writes, accumulate)

    def all_tokens(self):
        toks = [(("c", e, self.epoch), (self.csem[e], self.ccnt[e])) for e in self.csem if self.ccnt[e] > 0]
        for q in self.dsem:
            n = self.dcnt[q]
            for slot in range(min(n, self.NQ)):
                gens = (n - 1 - slot) // self.NQ + 1
                toks.append((("d", q, slot), (self.dsem[q][slot], 16 * gens)))
        return toks

    def barrier(self):
        toks = self.all_tokens()
        for e in self.engs:
            self._wait(e, toks)
        if self.limit is not None and self.n_ops > self.limit:
            return
        self.epoch += 1
        self.csem = {e: self.nc.alloc_semaphore("c_%s_%d" % (e, self.epoch)) for e in self.csem}
        self.ccnt = {e: 0 for e in self.csem}

    def maybe_epoch(self, thresh=30000):
        if max(self.ccnt.values()) > thresh:
            self.barrier()


def bc_mid(ap2d, n):
    p, f = ap2d.shape
    return ap2d.unsqueeze(1).to_broadcast([p, n, f])


def bc_last(ap2d, n):
    p, f = ap2d.shape
    return ap2d.unsqueeze(2).to_broadcast([p, f, n])


class Cfg:
    def __init__(self, pre=33, main=32, debug=False, phases=("proj", "ret", "ssd", "out", "peer"), tiny=()):
        self.tiny = set(tiny)
        self.pre = pre
        self.main = main
        self.debug = debug
        self.phases = phases
        self.nch = pre + main
        self.nt = self.nch * 128
        self.nt_main = main * 128


def build_program(cfg):
    nc = bass.Bass("TRN2", target_bir_lowering=False)
    kb = KB(nc)
    kb.limit = getattr(cfg, "limit", None)
    NT, NTM = cfg.nt, cfg.nt_main
    ROW0 = cfg.pre * 128
    dbg_kind = "ExternalOutput" if cfg.debug else "Internal"

    def ext_in(name, shape, dt=F32):
        if name in cfg.tiny:
            shape = [1, 8]
        return nc.dram_tensor(name, list(shape), dt, kind="ExternalInput").ap()

    hin = ext_in("hin", [NT, D])
    rowmask = ext_in("rowmask", [NT, 1])
    ropec = ext_in("ropec", [NT, 128])
    ropes = ext_in("ropes", [NT, 128])
    cst_f = ext_in("cst_f", [128, 4096])
    norm_mix_g = ext_in("norm_mix_g", [1, D])
    w_in = ext_in("w_in", [D, NPROJ])
    conv_w = ext_in("conv_w", [4, CONV_DIM])
    conv_b = ext_in("conv_b", [1, CONV_DIM])
    dt_bias = ext_in("dt_bias", [1, SH])
    a_log = ext_in("a_log", [1, SH])
    d_skip = ext_in("d_skip", [1, SH])
    ssm_norm_g = ext_in("ssm_norm_g", [1, SSM_INNER])
    w_ret_o = ext_in("w_ret_o", [RH * RDV, D])
    w_ssm_o = ext_in("w_ssm_o", [SSM_INNER, D])
    w_out = ext_in("w_out", [D, D])
    norm_ffn_g = ext_in("norm_ffn_g", [1, D])
    peer_w_q = ext_in("peer_w_q", [D, D])
    peer_sub_keys = ext_in("peer_sub_keys", [16 * 128, 128])
    peer_u = ext_in("peer_u", [NEXP, D])
    peer_v = ext_in("peer_v", [NEXP, D])
    norm_final_g = ext_in("norm_final_g", [1, D])
    out = nc.dram_tensor("out", [NTM, D], F32, kind="ExternalOutput").ap()

    PTMS = [nc.dram_tensor("PTM%d" % i, [NT, 4096], BF16, kind=dbg_kind).ap() for i in range(4)]

    class _PTM:
        def __getitem__(self, key):
            rows, cols = key
            i = cols.start // 4096
            assert (cols.stop - 1) // 4096 == i
            return PTMS[i][rows, cols.start - i * 4096:cols.stop - i * 4096]
    PTM = _PTM()
    XBCT = nc.dram_tensor("XBCT", [CONV_DIM, HALO + NT], BF16, kind=dbg_kind).ap()
    DTS = nc.dram_tensor("DTS", [NT, SH], F32, kind=dbg_kind).ap()
    GT = nc.dram_tensor("GT", [2 * D, NTM], BF16, kind=dbg_kind).ap()
    OGT = nc.dram_tensor("OGT", [RH * RDV, NTM], BF16, kind=dbg_kind).ap()
    YST = nc.dram_tensor("YST", [SSM_INNER, NTM], BF16, kind=dbg_kind).ap()
    H1 = nc.dram_tensor("H1", [NTM, D], F32, kind=dbg_kind).ap()
    XN2 = nc.dram_tensor("XN2", [NTM, D], BF16, kind=dbg_kind).ap()
    SC = nc.dram_tensor("SC", [NTM, 16 * 128], F32, kind=dbg_kind).ap()
    b_XN2, b_SC = Buf("XN2"), Buf("SC")
    NEX = 128 if "peer_u" in cfg.tiny else NEXP
    UVB = nc.dram_tensor("UVB", [NEX, 2 * D], BF16, kind="Internal").ap()
    b_UVB = Buf("UVB")
    WRB = nc.dram_tensor("WRB", [RH * RDV, D], BF16, kind="Internal").ap()
    WSB = nc.dram_tensor("WSB", [SSM_INNER, D], BF16, kind="Internal").ap()
    WOB = nc.dram_tensor("WOB", [D, D], BF16, kind="Internal").ap()
    WQB = nc.dram_tensor("WQB", [D, D], BF16, kind="Internal").ap()
    b_WB = Buf("WB")
    b_PTM, b_XBCT, b_DTS, b_GT, b_OGT, b_YST, b_H1, b_out = (Buf(n) for n in
                                                              ("PTM", "XBCT", "DTS", "GT", "OGT", "YST", "H1", "out"))

    es_glob = ExitStack()

    uniq = [0]

    def sb(es, name, shape, dt):
        uniq[0] += 1
        return es.enter_context(nc.sbuf_tensor("%s_%d" % (name, uniq[0]), list(shape), dt)).ap()

    def ps(es, name, shape, dt):
        uniq[0] += 1
        return es.enter_context(nc.psum_tensor("%s_%d" % (name, uniq[0]), list(shape), dt)).ap()

    ident_bf = sb(es_glob, "ident_bf", [128, 128], BF16)
    ident_f = sb(es_glob, "ident_f", [128, 128], F32)
    b_ident = Buf("ident")
    kb.op("pool", lambda e: e.memset(ident_f, 0.0), writes=[b_ident])
    kb.op("pool", lambda e: e.affine_select(out=ident_f, in_=ident_f, pattern=[[-1, 128]], compare_op=ALU.not_equal,
                                            fill=1.0, base=0, channel_multiplier=1), reads=[b_ident], writes=[b_ident])
    kb.op("dve", lambda e: e.tensor_copy(out=ident_bf, in_=ident_f), reads=[b_ident], writes=[b_ident])

    def phase_proj():
        with ExitStack() as es:
            TBMAX = 17 * 128
            g_bc = sb(es, "g_bc", [128, D], F32)
            b_g = Buf("g_bc")
            kb.dma("sp", lambda e: e.dma_start(out=g_bc, in_=norm_mix_g.partition_broadcast(128)), writes=[b_g])
            nT = sb(es, "nT", [128, 16, TBMAX], BF16)
            b_nT = Buf("nT")
            h_t = [sb(es, "h_t%d" % i, [128, D], F32) for i in range(2)]
            b_h = [Buf("h_t") for _ in range(2)]
            n_bf = [sb(es, "n_bf%d" % i, [128, D], BF16) for i in range(2)]
            b_n = [Buf("n_bf") for _ in range(2)]
            junk = sb(es, "junk", [128, D], BF16)
            b_junk = Buf("junk")
            stat = sb(es, "stat", [128, 4], F32)
            b_stat = Buf("stat")
            wst = [sb(es, "wst%d" % i, [128, 16, 512], F32) for i in range(2)]
            b_wst = [Buf("wst") for _ in range(2)]
            wbf = [sb(es, "wbf%d" % i, [128, 16, 512], BF16) for i in range(2)]
            b_wbf = [Buf("wbf") for _ in range(2)]
            ob = [sb(es, "ob%d" % i, [128, 512], BF16) for i in range(4)]
            b_ob = [Buf("ob") for _ in range(4)]
            obf = [sb(es, "obf%d" % i, [128, 64], F32) for i in range(2)]
            b_obf = [Buf("obf") for _ in range(2)]
            zt = sb(es, "zt", [128, HALO], BF16)
            b_zt = Buf("zt")
            ptr = ps(es, "ptr", [128, 16, 128], BF16)
            b_ptr = Buf("ptr")
            pmm = [ps(es, "pmm%d" % i, [128, 512], F32) for i in range(4)]
            b_pmm = [Buf("pmm") for _ in range(4)]

            kb.op("pool", lambda e: e.memset(zt, 0.0), writes=[b_zt])
            for r in range(CONV_DIM // 128):
                kb.dma("sp", lambda e, r=r: e.dma_start(out=XBCT[r * 128:(r + 1) * 128, 0:HALO], in_=zt),
                       reads=[b_zt], writes=[b_XBCT], accumulate=True)

            w_in_v = w_in.rearrange("(k p) n -> p k n", p=128)
            cnt = {"w": 0, "ob": 0, "pm": 0, "ev": 0, "obf": 0}

            def evac(dst, src, reads, writes):
                eng = "act" if cnt["ev"] % 2 == 0 else "dve"
                cnt["ev"] += 1
                if eng == "act":
                    kb.op("act", lambda e: e.activation(out=dst, in_=src, func=AF.Copy), reads=reads, writes=writes)
                else:
                    kb.op("dve", lambda e: e.tensor_copy(out=dst, in_=src), reads=reads, writes=writes)

            def do_block(ch0, nchk, colgroups):
                ntok = nchk * 128
                r0 = ch0 * 128
                for t in range(nchk):
                    i = t % 2
                    rows = slice(r0 + t * 128, r0 + (t + 1) * 128)
                    kb.dma("sp", lambda e, i=i, rows=rows: e.dma_start(out=h_t[i], in_=hin[rows, :]), writes=[b_h[i]])
                    kb.op("act", lambda e, i=i: e.activation(out=junk, in_=h_t[i], func=AF.Square, accum_out=stat[:, 0:1]),
                          reads=[b_h[i]], writes=[b_junk, b_stat])
                    kb.op("dve", lambda e: e.tensor_scalar(out=stat[:, 1:2], in0=stat[:, 0:1], scalar1=1.0 / D, scalar2=EPS,
                                                           op0=ALU.mult, op1=ALU.add), reads=[b_stat], writes=[b_stat])
                    kb.op("act", lambda e: e.activation(out=stat[:, 3:4], in_=stat[:, 1:2], func=AF.Sqrt),
                          reads=[b_stat], writes=[b_stat])
                    kb.op("dve", lambda e: e.reciprocal(out=stat[:, 2:3], in_=stat[:, 3:4]), reads=[b_stat], writes=[b_stat])
                    kb.op("dve", lambda e, i=i: e.scalar_tensor_tensor(out=n_bf[i], in0=h_t[i], scalar=stat[:, 2:3], in1=g_bc,
                                                                       op0=ALU.mult, op1=ALU.mult),
                          reads=[b_h[i], b_stat, b_g], writes=[b_n[i]])
                    for k in range(16):
                        kb.op("pe", lambda e, i=i, k=k: e.transpose(out=ptr[:, k, :], in_=n_bf[i][:, k * 128:(k + 1) * 128],
                                                                    identity=ident_bf),
                              reads=[b_n[i], b_ident], writes=[b_ptr])
                    evac(nT[:, :, t * 128:(t + 1) * 128], ptr, [b_ptr], [b_nT])
                for (kind, c0, ncol, dst_row0) in colgroups:
                    kb.maybe_epoch()
                    wi = cnt["w"] % 2
                    cnt["w"] += 1
                    kb.dma("sp", lambda e, wi=wi, c0=c0, ncol=ncol: e.dma_start(out=wst[wi][:, :, 0:ncol],
                                                                                 in_=w_in_v[:, :, c0:c0 + ncol]),
                           writes=[b_wst[wi]])
                    kb.op("pool", lambda e, wi=wi, ncol=ncol: e.tensor_copy(out=wbf[wi][:, :, 0:ncol], in_=wst[wi][:, :, 0:ncol]),
                          reads=[b_wst[wi]], writes=[b_wbf[wi]])
                    if kind == "tm":
                        for t in range(nchk):
                            pi = cnt["pm"] % 4
                            cnt["pm"] += 1
                            for k in range(16):
                                kb.op("pe", lambda e, pi=pi, k=k, t=t, wi=wi, ncol=ncol: e.matmul(
                                    pmm[pi][:, 0:ncol], lhsT=nT[:, k, t * 128:(t + 1) * 128], rhs=wbf[wi][:, k, 0:ncol],
                                    start=(k == 0), stop=(k == 15)), reads=[b_nT, b_wbf[wi]], writes=[b_pmm[pi]])
                            oi = cnt["ob"] % 4
                            cnt["ob"] += 1
                            evac(ob[oi][:, 0:ncol], pmm[pi][:, 0:ncol], [b_pmm[pi]], [b_ob[oi]])
                            rows = slice(r0 + t * 128, r0 + (t + 1) * 128)
                            kb.dma("sp", lambda e, oi=oi, rows=rows, c0=c0, ncol=ncol: e.dma_start(
                                out=PTM[rows, c0:c0 + ncol], in_=ob[oi][:, 0:ncol]),
                                reads=[b_ob[oi]], writes=[b_PTM], accumulate=True)
                    elif kind == "dt":
                        for t in range(nchk):
                            pi = cnt["pm"] % 4
                            cnt["pm"] += 1
                            for k in range(16):
                                kb.op("pe", lambda e, pi=pi, k=k, t=t, wi=wi: e.matmul(
                                    pmm[pi][:, 0:64], lhsT=nT[:, k, t * 128:(t + 1) * 128], rhs=wbf[wi][:, k, 0:64],
                                    start=(k == 0), stop=(k == 15)), reads=[b_nT, b_wbf[wi]], writes=[b_pmm[pi]])
                            oi = cnt["obf"] % 2
                            cnt["obf"] += 1
                            evac(obf[oi], pmm[pi][:, 0:64], [b_pmm[pi]], [b_obf[oi]])
                            rows = slice(r0 + t * 128, r0 + (t + 1) * 128)
                            kb.dma("sp", lambda e, oi=oi, rows=rows: e.dma_start(out=DTS[rows, :], in_=obf[oi]),
                                   reads=[b_obf[oi]], writes=[b_DTS], accumulate=True)
                    else:
                        for m in range(ncol // 128):
                            for tg in range(0, ntok, 512):
                                n = min(512, ntok - tg)
                                pi = cnt["pm"] % 4
                                cnt["pm"] += 1
                                for k in range(16):
                                    kb.op("pe", lambda e, pi=pi, k=k, m=m, tg=tg, n=n, wi=wi: e.matmul(
                                        pmm[pi][:, 0:n], lhsT=wbf[wi][:, k, m * 128:(m + 1) * 128], rhs=nT[:, k, tg:tg + n],
                                        start=(k == 0), stop=(k == 15)), reads=[b_nT, b_wbf[wi]], writes=[b_pmm[pi]])
                                oi = cnt["ob"] % 4
                                cnt["ob"] += 1
                                evac(ob[oi][:, 0:n], pmm[pi][:, 0:n], [b_pmm[pi]], [b_ob[oi]])
                                rr = dst_row0 + m * 128
                                if kind == "xbc":
                                    kb.dma("sp", lambda e, oi=oi, rr=rr, tg=tg, n=n: e.dma_start(
                                        out=XBCT[rr:rr + 128, HALO + r0 + tg:HALO + r0 + tg + n], in_=ob[oi][:, 0:n]),
                                        reads=[b_ob[oi]], writes=[b_XBCT], accumulate=True)
                                else:
                                    cc = r0 - ROW0 + tg
                                    kb.dma("sp", lambda e, oi=oi, rr=rr, cc=cc, n=n: e.dma_start(
                                        out=GT[rr:rr + 128, cc:cc + n], in_=ob[oi][:, 0:n]),
                                        reads=[b_ob[oi]], writes=[b_GT], accumulate=True)

            def groups(c_lo, c_hi, kind, dst_row0=0):
                return [(kind, c, min(512, c_hi - c), dst_row0 + (c - c_lo)) for c in range(c_lo, c_hi, 512)]

            pre_groups = (groups(C_K, C_G, "tm") + groups(C_XBC, C_DT, "xbc")
                          + [("dt", C_DT, 64, 0)])
            main_groups = (groups(0, PTM_W, "tm") + groups(C_XBC, C_DT, "xbc") + [("dt", C_DT, 64, 0)]
                           + groups(C_GR, NPROJ, "gt"))

            def blocks(c0, n):
                res, c = [], c0
                while n > 0:
                    m = min(n, 17 if n == 17 else 16)
                    res.append((c, m))
                    c += m
                    n -= m
                return res

            for (c, m) in blocks(0, cfg.pre):
                do_block(c, m, pre_groups)
            for (c, m) in blocks(cfg.pre, cfg.main):
                do_block(c, m, main_groups)
        kb.barrier()

    CO_UT, CO_SL, CO_ONE, CO_DQ, CO_DK, CO_DKZ, CO_G128 = 0, 128, 256, 384, 392, 400, 408

    def load_consts(es):
        cst = sb(es, "cst", [128, 512], F32)
        b_cst = Buf("cst")
        kb.dma("sp", lambda e: e.dma_start(out=cst, in_=cst_f[:, 0:512]), writes=[b_cst])
        return cst, b_cst

    def transpose_store(src_bf, b_src, nblk, ptr, b_ptr, oT, b_oT, dst_view, b_dst, col0):
        for r in range(0, nblk, 16):
            for k in range(16):
                kb.op("pe", lambda e, k=k, r=r: e.transpose(out=ptr[:, k, :], in_=src_bf[:, (r + k) * 128:(r + k + 1) * 128],
                                                            identity=ident_bf), reads=[b_src, b_ident], writes=[b_ptr])
            kb.op("act", lambda e, r=r: e.activation(out=oT[:, r:r + 16, :], in_=ptr, func=AF.Copy),
                  reads=[b_ptr], writes=[b_oT])
        kb.dma("sp", lambda e: e.dma_start(out=dst_view[:, :, col0:col0 + 128], in_=oT[:, 0:nblk, :]),
               reads=[b_oT], writes=[b_dst], accumulate=True)

    precast_state = {"done": False}

    def precast_units(es, ptile, b_ptile):
        gcol = sb(es, "gcol", [128, 32], F32); b_gcol = Buf()
        tmpg = sb(es, "tmpg", [32, 128], F32); b_tg = Buf()
        kb.dma("sp", lambda e: e.dma_start(out=tmpg, in_=ssm_norm_g.rearrange("o (kt p) -> (o kt) p", p=128)), writes=[b_tg])
        kb.op("pe", lambda e: e.transpose(out=ptile[:, 0:32], in_=tmpg, identity=ident_f[0:32, 0:32]),
              reads=[b_tg, b_ident], writes=[b_ptile])
        kb.op("dve", lambda e: e.tensor_copy(out=gcol, in_=ptile[:, 0:32]), reads=[b_ptile], writes=[b_gcol])
        stg = [sb(es, "stgw%d" % i, [128, D], F32) for i in range(3)]; b_stg = [Buf() for _ in range(3)]
        stb = [sb(es, "stbw%d" % i, [128, D], BF16) for i in range(3)]; b_stb = [Buf() for _ in range(3)]
        jobs = []
        for (src, dst, nblk, scale_g, b_dst) in ((w_ret_o, WRB, 32, False, b_WB), (w_ssm_o, WSB, 32, True, b_WB),
                                                 (w_out, WOB, 16, False, b_WB), (peer_w_q, WQB, 16, False, b_WB)):
            for r in range(nblk):
                jobs.append((src[r * 128:(r + 1) * 128, :], dst[r * 128:(r + 1) * 128, :], r if scale_g else None, b_dst))
        for r in range(NEX // 128):
            jobs.append((peer_u[r * 128:(r + 1) * 128, :], UVB[r * 128:(r + 1) * 128, 0:D], None, b_UVB))
            jobs.append((peer_v[r * 128:(r + 1) * 128, :], UVB[r * 128:(r + 1) * 128, D:2 * D], None, b_UVB))
        def ld(n):
            src_ap = jobs[n][0]
            i = n % 3
            kb.dma("sp", lambda e: e.dma_start(out=stg[i], in_=src_ap), writes=[b_stg[i]])

        def cast(n):
            i = n % 3
            gr = jobs[n][2]
            if gr is not None:
                kb.op("pool", lambda e: e.tensor_scalar(out=stb[i], in0=stg[i], scalar1=gcol[:, gr:gr + 1], scalar2=None,
                                                        op0=ALU.mult), reads=[b_stg[i], b_gcol], writes=[b_stb[i]])
            else:
                kb.op("pool", lambda e: e.tensor_copy(out=stb[i], in_=stg[i]), reads=[b_stg[i]], writes=[b_stb[i]])

        def st(n):
            i = n % 3
            dst_ap, b_dst = jobs[n][1], jobs[n][3]
            kb.dma("sp", lambda e: e.dma_start(out=dst_ap, in_=stb[i]), reads=[b_stb[i]], writes=[b_dst], accumulate=True)

        NJ = len(jobs)
        for n in range(NJ + 2):
            if n < NJ:
                ld(n)
            if 0 <= n - 1 < NJ:
                cast(n - 1)
            if 0 <= n - 2 < NJ:
                st(n - 2)
            yield n
        precast_state["done"] = True

    N_PRECAST = 96 + 2 * (NEX // 128) + 2

    def phase_ret():
        with ExitStack() as es:
            cst, b_cst = load_consts(es)
            UT = cst[:, CO_UT:CO_UT + 128]
            st_f = sb(es, "st_f", [128, RH, 2, 512], F32)
            st_b = sb(es, "st_b", [128, RH, 2, 512], BF16)
            b_stf = [Buf("stf") for _ in range(RH)]
            b_stb = [Buf("stb") for _ in range(RH)]
            for h in range(RH):
                kb.op("pool", lambda e, h=h: e.memset(st_f[:, h], 0.0), writes=[b_stf[h]])
                kb.op("pool", lambda e, h=h: e.memset(st_b[:, h], 0.0), writes=[b_stb[h]])
            k_in = sb(es, "k_in", [128, 2048], BF16); b_kin = Buf()
            q_in = sb(es, "q_in", [128, 2048], BF16); b_qin = Buf()
            v_in = sb(es, "v_in", [128, 4096], BF16); b_vin = Buf()
            g_in = sb(es, "g_in", [128, 4096], BF16); b_gin = Buf()
            cos_t = sb(es, "cos_t", [128, 128], F32); b_cos = Buf()
            sin_t = sb(es, "sin_t", [128, 128], F32); b_sin = Buf()
            tmp1 = sb(es, "tmp1", [128, 8, 128], F32); b_t1 = Buf()
            tmp2 = sb(es, "tmp2", [128, 8, 128], F32); b_t2 = Buf()
            kr = sb(es, "kr", [128, 8, 2, 128], F32); b_kr = Buf()
            kt_ = sb(es, "kt_", [128, 8, 256], BF16); b_kt = Buf()
            kz_ = sb(es, "kz_", [128, 8, 256], BF16); b_kz = Buf()
            qt_ = sb(es, "qt_", [128, 8, 256], BF16); b_qt = Buf()
            qT = sb(es, "qT", [128, 16, 128], BF16); b_qT = Buf()
            kT = sb(es, "kT", [128, 16, 128], BF16); b_kT = Buf()
            sTm = [sb(es, "sTm%d" % i, [128, 128], BF16) for i in range(2)]; b_sTm = [Buf(), Buf()]
            o_sb = sb(es, "o_sb", [128, RH, 512], F32); b_osb = [Buf() for _ in range(RH)]
            sg = sb(es, "sg", [128, RH, 512], BF16); b_sg = Buf()
            og = sb(es, "og", [128, RH * 512], BF16); b_og = Buf()
            oT = sb(es, "oT", [128, 32, 128], BF16); b_oT = Buf()
            junk = sb(es, "junkr", [128, 512], BF16); b_junk = Buf()
            ssq = sb(es, "ssq", [128, 32], F32); b_ssq = Buf()
            ptr = ps(es, "ptr_r", [128, 16, 128], BF16); b_ptr = Buf()
            ps_s = ps(es, "ps_s", [128, 512], F32); b_pss = Buf()
            ps_o = [ps(es, "ps_o%d" % i, [128, 512], F32) for i in range(2)]; b_pso = [Buf(), Buf()]
            ps_u = [ps(es, "ps_u%d" % i, [128, 512], F32) for i in range(2)]; b_psu = [Buf(), Buf()]
            OGT_v = OGT.rearrange("(kt p) n -> p kt n", p=128)
            pc_gen = precast_units(es, ps_s, b_pss)
            pc_per_chunk = -(-N_PRECAST // cfg.nch)

            def rope(src, b_src, dst_list):
                v4 = src.rearrange("p (h f two) -> p h f two", h=8, two=2)
                t1, t2 = v4[:, :, :, 0], v4[:, :, :, 1]
                cb_, sb_ = bc_mid(cos_t, 8), bc_mid(sin_t, 8)
                kb.op("dve", lambda e: e.tensor_tensor(out=tmp1, in0=t1, in1=cb_, op=ALU.mult), reads=[b_src, b_cos], writes=[b_t1])
                kb.op("dve", lambda e: e.tensor_tensor(out=tmp2, in0=t2, in1=sb_, op=ALU.mult), reads=[b_src, b_sin], writes=[b_t2])
                kb.op("dve", lambda e: e.tensor_tensor(out=kr[:, :, 0, :], in0=tmp1, in1=tmp2, op=ALU.subtract),
                      reads=[b_t1, b_t2], writes=[b_kr])
                kb.op("dve", lambda e: e.tensor_tensor(out=tmp1, in0=t1, in1=sb_, op=ALU.mult), reads=[b_src, b_sin], writes=[b_t1])
                kb.op("dve", lambda e: e.tensor_tensor(out=tmp2, in0=t2, in1=cb_, op=ALU.mult), reads=[b_src, b_cos], writes=[b_t2])
                kb.op("dve", lambda e: e.tensor_tensor(out=kr[:, :, 1, :], in0=tmp1, in1=tmp2, op=ALU.add),
                      reads=[b_t1, b_t2, b_kr], writes=[b_kr])
                krv = kr.rearrange("p h two f -> p h (two f)")
                for (dst, b_dst, co) in dst_list:
                    kb.op("dve", lambda e, dst=dst, co=co: e.tensor_tensor(out=dst, in0=krv, in1=bc_last(cst[:, co:co + 8], 256),
                                                                           op=ALU.mult), reads=[b_kr, b_cst], writes=[b_dst])

            for ch in range(cfg.nch):
                kb.maybe_epoch()
                for _ in range(pc_per_chunk):
                    next(pc_gen, None)
                main = ch >= cfg.pre
                r0 = ch * 128
                rows = slice(r0, r0 + 128)
                kb.dma("sp", lambda e, rows=rows: e.dma_start(out=k_in, in_=PTM[rows, C_K:C_K + 2048]), reads=[b_PTM], writes=[b_kin])
                kb.dma("sp", lambda e, rows=rows: e.dma_start(out=v_in, in_=PTM[rows, C_V:C_V + 4096]), reads=[b_PTM], writes=[b_vin])
                kb.dma("sp", lambda e, rows=rows: e.dma_start(out=cos_t, in_=ropec[rows, :]), writes=[b_cos])
                kb.dma("sp", lambda e, rows=rows: e.dma_start(out=sin_t, in_=ropes[rows, :]), writes=[b_sin])
                if main:
                    kb.dma("sp", lambda e, rows=rows: e.dma_start(out=q_in, in_=PTM[rows, C_Q:C_Q + 2048]), reads=[b_PTM], writes=[b_qin])
                    kb.dma("sp", lambda e, rows=rows: e.dma_start(out=g_in, in_=PTM[rows, C_G:C_G + 4096]), reads=[b_PTM], writes=[b_gin])
                    rope(k_in, b_kin, [(kt_, b_kt, CO_DK), (kz_, b_kz, CO_DKZ)])
                    rope(q_in, b_qin, [(qt_, b_qt, CO_DQ)])
                    kb.op("act", lambda e: e.activation(out=sg.rearrange("p h f -> p (h f)"), in_=g_in, func=AF.Silu),
                          reads=[b_gin], writes=[b_sg])
                    for (src, b_src, dstT, b_dstT) in ((qt_, b_qt, qT, b_qT), (kt_, b_kt, kT, b_kT)):
                        s2 = src.rearrange("p h f -> p (h f)")
                        for k in range(16):
                            kb.op("pe", lambda e, k=k, s2=s2: e.transpose(out=ptr[:, k, :], in_=s2[:, k * 128:(k + 1) * 128],
                                                                          identity=ident_bf), reads=[b_src, b_ident], writes=[b_ptr])
                        kb.op("act", lambda e, dstT=dstT: e.activation(out=dstT, in_=ptr, func=AF.Copy), reads=[b_ptr], writes=[b_dstT])
                else:
                    rope(k_in, b_kin, [(kz_, b_kz, CO_DKZ)])
                for h in range(RH):
                    vh = v_in[:, h * 512:(h + 1) * 512]
                    if main:
                        pi = h % 2
                        for c in range(2):
                            kb.op("pe", lambda e, h=h, c=c: e.matmul(ps_s[:, 0:128], lhsT=kT[:, 2 * h + c, :], rhs=qT[:, 2 * h + c, :],
                                                                     start=(c == 0), stop=(c == 1)), reads=[b_kT, b_qT], writes=[b_pss])
                        kb.op("dve", lambda e, pi=pi: e.tensor_tensor(out=sTm[pi], in0=ps_s[:, 0:128], in1=UT, op=ALU.mult),
                              reads=[b_pss, b_cst], writes=[b_sTm[pi]])
                        kb.op("pe", lambda e, pi=pi, vh=vh: e.matmul(ps_o[pi], lhsT=sTm[pi], rhs=vh, start=True, stop=False),
                              reads=[b_sTm[pi], b_vin], writes=[b_pso[pi]])
                        for c in range(2):
                            kb.op("pe", lambda e, pi=pi, h=h, c=c: e.matmul(ps_o[pi], lhsT=qT[:, 2 * h + c, :], rhs=st_b[:, h, c, :],
                                                                            start=False, stop=(c == 1)),
                                  reads=[b_qT, b_stb[h]], writes=[b_pso[pi]])
                        kb.op("dve", lambda e, pi=pi, h=h: e.tensor_copy(out=o_sb[:, h, :], in_=ps_o[pi]), reads=[b_pso[pi]], writes=[b_osb[h]])
                        kb.op("act", lambda e, h=h: e.activation(out=junk, in_=o_sb[:, h, :], func=AF.Square,
                                                                 accum_out=ssq[:, h:h + 1]), reads=[b_osb[h]], writes=[b_junk, b_ssq])
                    for c in range(2):
                        kb.op("pe", lambda e, h=h, c=c, vh=vh: e.matmul(ps_u[c], lhsT=kz_[:, h, c * 128:(c + 1) * 128], rhs=vh,
                                                                        start=True, stop=True), reads=[b_kz, b_vin], writes=[b_psu[c]])
                        kb.op("dve", lambda e, h=h, c=c: e.scalar_tensor_tensor(out=st_f[:, h, c, :], in0=st_f[:, h, c, :],
                                                                                 scalar=cst[:, CO_G128 + h:CO_G128 + h + 1],
                                                                                 in1=ps_u[c], op0=ALU.mult, op1=ALU.add),
                              reads=[b_psu[c], b_cst, b_stf[h]], writes=[b_stf[h]])
                    kb.op("act", lambda e, h=h: e.activation(out=st_b[:, h], in_=st_f[:, h], func=AF.Copy), reads=[b_stf[h]], writes=[b_stb[h]])
                if main:
                    kb.op("dve", lambda e: e.tensor_scalar(out=ssq[:, 8:16], in0=ssq[:, 0:8], scalar1=1.0 / RDV, scalar2=EPS,
                                                           op0=ALU.mult, op1=ALU.add), reads=[b_ssq], writes=[b_ssq])
                    kb.op("act", lambda e: e.activation(out=ssq[:, 16:24], in_=ssq[:, 8:16], func=AF.Sqrt), reads=[b_ssq], writes=[b_ssq])
                    kb.op("dve", lambda e: e.reciprocal(out=ssq[:, 24:32], in_=ssq[:, 16:24]), reads=[b_ssq], writes=[b_ssq])
                    kb.op("dve", lambda e: e.tensor_tensor(out=o_sb, in0=o_sb, in1=bc_last(ssq[:, 24:32], 512), op=ALU.mult),
                          reads=b_osb + [b_ssq], writes=b_osb)
                    kb.op("dve", lambda e: e.tensor_tensor(out=og.rearrange("p (h f) -> p h f", h=RH), in0=o_sb, in1=sg, op=ALU.mult),
                          reads=b_osb + [b_sg], writes=[b_og])
                    transpose_store(og, b_og, 32, ptr, b_ptr, oT, b_oT, OGT_v, b_OGT, r0 - ROW0)
            for _ in pc_gen:
                pass
        kb.barrier()

    if "proj" in cfg.phases:
        phase_proj()
    def phase_ssd():
        with ExitStack() as es:
            cst, b_cst = load_consts(es)
            UT = cst[:, CO_UT:CO_UT + 128]
            SLm = cst[:, CO_SL:CO_SL + 128]
            ONES = cst[:, CO_ONE:CO_ONE + 128]
            diag = sb(es, "diag", [128, 48, 4, 128], BF16); b_diag = Buf()
            cwT = sb(es, "cwT", [128, 48, 8], F32); b_cwT = Buf()
            cb_bc = sb(es, "cb_bc", [128, 5120], BF16); b_cb = Buf()
            par = sb(es, "par", [128, 4, 64], F32); b_par = Buf()
            sst_f = sb(es, "sst_f", [128, SH, SP_], F32)
            sst_b = sb(es, "sst_b", [128, SH * SP_], BF16)
            b_sf = [Buf() for _ in range(SG)]; b_sbb = [Buf() for _ in range(SG)]
            ptr = ps(es, "ptr_s", [128, 16, 128], BF16); b_ptr = Buf()
            pcA = ps(es, "pcA", [128, 512], F32); b_pcA = Buf()
            pcB = ps(es, "pcB", [128, 512], F32); b_pcB = Buf()
            psm = ps(es, "psm", [128, 512], F32); b_psm = Buf()
            pseg = ps(es, "pseg", [128, 1024], F32); b_pseg = Buf()
            pst = ps(es, "pst", [128, 512], F32); b_pst = Buf()
            YST_v = YST.rearrange("(kt p) n -> p kt n", p=128)
            XB_v = XBCT.rearrange("(t p) c -> p t c", p=128)

            with ExitStack() as es2:
                cw5 = sb(es2, "cw5", [8, CONV_DIM], F32); b_cw5 = Buf()
                cbs = sb(es2, "cbs", [128, 5120], F32); b_cbs = Buf()
                kb.dma("sp", lambda e: e.dma_start(out=cw5[0:4, :], in_=conv_w), writes=[b_cw5])
                kb.dma("sp", lambda e: e.dma_start(out=cw5[4:5, :], in_=conv_b), writes=[b_cw5], accumulate=True)
                kb.dma("sp", lambda e: e.dma_start(out=cbs, in_=conv_b[0:1, 0:5120].partition_broadcast(128)), writes=[b_cbs])
                kb.op("dve", lambda e: e.tensor_copy(out=cb_bc, in_=cbs), reads=[b_cbs], writes=[b_cb])
                for t in range(48):
                    kb.op("pe", lambda e, t=t: e.transpose(out=psm[:, t * 8:t * 8 + 5], in_=cw5[0:5, t * 128:(t + 1) * 128],
                                                           identity=ident_f[0:5, 0:5]), reads=[b_cw5, b_ident], writes=[b_psm])
                kb.op("dve", lambda e: e.tensor_copy(out=cwT[:, :, 0:5], in_=psm[:, 0:384].rearrange("p (t e) -> p t e", e=8)[:, :, 0:5]),
                      reads=[b_psm], writes=[b_cwT])
                for t in range(48):
                    for w in range(4):
                        kb.op("dve", lambda e, t=t, w=w: e.tensor_scalar(out=diag[:, t, w, :], in0=ident_f, scalar1=cwT[:, t, w:w + 1],
                                                                         scalar2=None, op0=ALU.mult),
                              reads=[b_ident, b_cwT], writes=[b_diag])
                kb.dma("sp", lambda e: e.dma_start(out=par[:, 0, :], in_=dt_bias.partition_broadcast(128)), writes=[b_par])
                kb.dma("sp", lambda e: e.dma_start(out=par[:, 3, :], in_=a_log.partition_broadcast(128)), writes=[b_par], accumulate=True)
                kb.dma("sp", lambda e: e.dma_start(out=par[:, 2, :], in_=d_skip.partition_broadcast(128)), writes=[b_par], accumulate=True)
                kb.op("act", lambda e: e.activation(out=par[:, 1, :], in_=par[:, 3, :], func=AF.Exp), reads=[b_par], writes=[b_par])
                kb.op("dve", lambda e: e.tensor_scalar(out=par[:, 1, :], in0=par[:, 1, :], scalar1=-1.0, scalar2=None, op0=ALU.mult),
                      reads=[b_par], writes=[b_par])
                for g in range(SG):
                    kb.op("pool", lambda e, g=g: e.memset(sst_f[:, g * 8:(g + 1) * 8, :], 0.0), writes=[b_sf[g]])
                    kb.op("pool", lambda e, g=g: e.memset(sst_b[:, g * 512:(g + 1) * 512], 0.0), writes=[b_sbb[g]])
                kb.barrier()
            xw = sb(es, "xw", [128, 48, 131], BF16); b_xw = Buf()
            z_in = sb(es, "z_in", [128, 4096], BF16); b_zin = Buf()
            xs = sb(es, "xs", [128, SH, SP_], F32); b_xs = [Buf() for _ in range(SG)]
            xdt = sb(es, "xdt", [128, SH, SP_], BF16); b_xdt = Buf()
            decx = sb(es, "decx", [128, SH * SP_], BF16); b_decx = [Buf() for _ in range(SG)]
            Bt = sb(es, "Bt", [128, 1024], BF16); b_Bt = Buf()
            BCT = sb(es, "BCT", [128, 16, 128], BF16); b_BCT = Buf()
            segL = [sb(es, "segL%d" % i, [128, 8, 128], F32) for i in range(2)]; b_segL = [Buf(), Buf()]
            Lg = [sb(es, "Lg%d" % i, [128, 8, 128], F32) for i in range(2)]; b_Lg = [Buf(), Buf()]
            MT = [sb(es, "MT%d" % i, [128, 8, 128], BF16) for i in range(2)]; b_MT = [Buf(), Buf()]
            cbm = sb(es, "cbm", [128, 128], F32); b_cbm = Buf()
            tmpc = [sb(es, "tmpc%d" % i, [128, 512], F32) for i in range(2)]; b_tmpc = [Buf(), Buf()]
            sz = sb(es, "sz", [128, 4096], BF16); b_sz = Buf()
            ys = sb(es, "ys", [128, 4096], BF16); b_ys = Buf()
            oT = sb(es, "oT_s", [128, 32, 128], BF16); b_oT = Buf()
            sm = sb(es, "sm", [128, 8, 64], F32); b_sm = Buf()
            sm2 = sb(es, "sm2", [128, 128], F32); b_sm2 = Buf()
            dtr = sb(es, "dtr", [128, 64], F32); b_dtr = Buf()
            msk = sb(es, "msk", [128, 1], F32); b_msk = Buf()
            junk = sb(es, "junks", [128, 512], BF16); b_junk = Buf()
            ssq = sb(es, "ssq2", [128, 32], F32); b_ssq = Buf()
            xs2 = xs.rearrange("p h f -> p (h f)")
            xdt2 = xdt.rearrange("p h f -> p (h f)")
            sst_f2 = sst_f.rearrange("p h f -> p (h f)")

            for ch in range(cfg.nch):
                kb.maybe_epoch()
                main = ch >= cfg.pre
                r0 = ch * 128
                rows = slice(r0, r0 + 128)
                c0 = HALO + r0 - 3
                T = 48 if main else 40
                kb.dma("sp", lambda e, T=T, c0=c0: e.dma_start(out=xw[:, 0:T, :], in_=XB_v[:, 0:T, c0:c0 + 131]),
                       reads=[b_XBCT], writes=[b_xw])
                kb.dma("sp", lambda e, rows=rows: e.dma_start(out=dtr, in_=DTS[rows, :]), reads=[b_DTS], writes=[b_dtr])
                kb.dma("sp", lambda e, rows=rows: e.dma_start(out=msk, in_=rowmask[rows, :]), writes=[b_msk])
                if main:
                    kb.dma("sp", lambda e, rows=rows: e.dma_start(out=z_in, in_=PTM[rows, C_Z:C_Z + 4096]), reads=[b_PTM], writes=[b_zin])
                S = lambda i: sm[:, i, :]
                kb.op("dve", lambda e: e.tensor_tensor(out=S(0), in0=dtr, in1=par[:, 0, :], op=ALU.add), reads=[b_dtr, b_par], writes=[b_sm])
                kb.op("act", lambda e: e.activation(out=S(1), in_=S(0), func=AF.Abs), reads=[b_sm], writes=[b_sm])
                kb.op("act", lambda e: e.activation(out=S(2), in_=S(1), func=AF.Exp, scale=-1.0), reads=[b_sm], writes=[b_sm])
                kb.op("act", lambda e: e.activation(out=S(3), in_=S(2), func=AF.Ln, bias=ONES[:, 0:1]), reads=[b_sm, b_cst], writes=[b_sm])
                kb.op("dve", lambda e: e.scalar_tensor_tensor(out=S(4), in0=S(0), scalar=0.0, in1=S(3), op0=ALU.max, op1=ALU.add),
                      reads=[b_sm], writes=[b_sm])
                kb.op("dve", lambda e: e.tensor_scalar(out=S(5), in0=S(4), scalar1=msk[:, 0:1], scalar2=None, op0=ALU.mult),
                      reads=[b_sm, b_msk], writes=[b_sm])
                kb.op("dve", lambda e: e.tensor_tensor(out=S(6), in0=S(4), in1=par[:, 1, :], op=ALU.mult), reads=[b_sm, b_par], writes=[b_sm])
                a_ap = S(6)
                for bnk in range(10):
                    pc, b_pc = (pcA, b_pcA) if bnk % 2 == 0 else (pcB, b_pcB)
                    for tt in range(4):
                        t = bnk * 4 + tt
                        for w in range(4):
                            kb.op("pe", lambda e, pc=pc, t=t, tt=tt, w=w: e.matmul(pc[:, tt * 128:(tt + 1) * 128], lhsT=xw[:, t, w:w + 128],
                                                                                   rhs=diag[:, t, w, :], start=(w == 0), stop=(w == 3)),
                                  reads=[b_xw, b_diag], writes=[b_pc])
                    ti = bnk % 2
                    kb.op("dve", lambda e, pc=pc, ti=ti, bnk=bnk: e.tensor_tensor(out=tmpc[ti], in0=pc, in1=cb_bc[:, bnk * 512:(bnk + 1) * 512],
                                                                                  op=ALU.add), reads=[b_pc, b_cb], writes=[b_tmpc[ti]])
                    if bnk < 8:
                        kb.op("act", lambda e, ti=ti, bnk=bnk: e.activation(out=xs2[:, bnk * 512:(bnk + 1) * 512], in_=tmpc[ti], func=AF.Silu),
                              reads=[b_tmpc[ti]], writes=[b_xs[bnk]])
                    else:
                        kb.op("act", lambda e, ti=ti, bnk=bnk: e.activation(out=Bt[:, (bnk - 8) * 512:(bnk - 7) * 512], in_=tmpc[ti], func=AF.Silu),
                              reads=[b_tmpc[ti]], writes=[b_Bt])
                for bnk in range(4 if main else 2):
                    pc, b_pc = (pcA, b_pcA) if bnk % 2 == 0 else (pcB, b_pcB)
                    for tt in range(4):
                        t = 32 + bnk * 4 + tt
                        for w in range(4):
                            kb.op("pe", lambda e, pc=pc, t=t, tt=tt, w=w: e.matmul(pc[:, tt * 128:(tt + 1) * 128], lhsT=diag[:, t, w, :],
                                                                                   rhs=xw[:, t, w:w + 128], start=(w == 0), stop=(w == 3)),
                                  reads=[b_xw, b_diag], writes=[b_pc])
                    for tt in range(4):
                        t = 32 + bnk * 4 + tt
                        kb.op("act", lambda e, pc=pc, t=t, tt=tt: e.activation(out=BCT[:, t - 32, :], in_=pc[:, tt * 128:(tt + 1) * 128],
                                                                               func=AF.Silu, bias=cwT[:, t, 4:5]),
                              reads=[b_pc, b_cwT], writes=[b_BCT])
                kb.op("dve", lambda e: e.tensor_tensor(out=xdt, in0=xs, in1=bc_last(S(5), 64), op=ALU.mult), reads=b_xs + [b_sm], writes=[b_xdt])
                if main:
                    kb.op("dve", lambda e: e.tensor_tensor(out=xs, in0=xs, in1=bc_last(par[:, 2, :], 64), op=ALU.mult),
                          reads=b_xs + [b_par], writes=b_xs)
                    kb.op("act", lambda e: e.activation(out=sz, in_=z_in, func=AF.Silu), reads=[b_zin], writes=[b_sz])
                kb.op("pe", lambda e: e.matmul(psm[:, 0:64], lhsT=UT, rhs=a_ap, start=True, stop=True), reads=[b_cst, b_sm], writes=[b_psm])
                kb.op("pe", lambda e: e.matmul(psm[:, 64:128], lhsT=ONES, rhs=a_ap, start=True, stop=True), reads=[b_cst, b_sm], writes=[b_psm])
                kb.op("act", lambda e: e.activation(out=sm2, in_=psm[:, 0:128], func=AF.Exp), reads=[b_psm], writes=[b_sm2])
                eacs, cdec = sm2[:, 0:64], sm2[:, 64:128]
                for g in range(SG):
                    i2 = g % 2
                    hs = slice(g * 8, (g + 1) * 8)
                    cs = slice(g * 512, (g + 1) * 512)
                    kb.op("pool", lambda e, i2=i2, hs=hs: e.tensor_tensor(out=segL[i2], in0=bc_mid(SLm, 8), in1=bc_last(a_ap[:, hs], 128),
                                                                          op=ALU.mult), reads=[b_cst, b_sm], writes=[b_segL[i2]])
                    for hh in range(8):
                        kb.op("pe", lambda e, i2=i2, hh=hh: e.matmul(pseg[:, hh * 128:(hh + 1) * 128], lhsT=segL[i2][:, hh, :], rhs=UT,
                                                                     start=True, stop=True), reads=[b_segL[i2], b_cst], writes=[b_pseg])
                    Lg2 = Lg[i2].rearrange("p h i -> p (h i)")
                    for hf in range(2):
                        kb.op("act", lambda e, Lg2=Lg2, hf=hf: e.activation(out=Lg2[:, hf * 512:(hf + 1) * 512],
                                                                            in_=pseg[:, hf * 512:(hf + 1) * 512], func=AF.Exp),
                              reads=[b_pseg], writes=[b_Lg[i2]])
                    dec_g = Lg[i2][:, :, 127]
                    kb.op("dve", lambda e, i2=i2, hs=hs, dec_g=dec_g: e.tensor_tensor(out=decx.rearrange("p (h f) -> p h f", f=64)[:, hs, :],
                                                                                      in0=xdt[:, hs, :],
                                                                                      in1=dec_g.unsqueeze(2).to_broadcast([128, 8, 64]),
                                                                                      op=ALU.mult),
                          reads=[b_xdt, b_Lg[i2]], writes=[b_decx[g]])
                    if main:
                        kb.op("pe", lambda e, g=g: e.matmul(psm[:, 128:256], lhsT=BCT[:, g, :], rhs=BCT[:, 8 + g, :], start=True, stop=True),
                              reads=[b_BCT], writes=[b_psm])
                        kb.op("dve", lambda e: e.tensor_tensor(out=cbm, in0=psm[:, 128:256], in1=UT, op=ALU.mult),
                              reads=[b_psm, b_cst], writes=[b_cbm])
                        kb.op("pool", lambda e, i2=i2: e.tensor_tensor(out=MT[i2], in0=Lg[i2], in1=bc_mid(cbm, 8), op=ALU.mult),
                              reads=[b_Lg[i2], b_cbm], writes=[b_MT[i2]])
                        for hh in range(8):
                            kb.op("pe", lambda e, i2=i2, hh=hh, g=g: e.matmul(pcA[:, hh * 64:(hh + 1) * 64], lhsT=MT[i2][:, hh, :],
                                                                              rhs=xdt[:, g * 8 + hh, :], start=True, stop=True),
                                  reads=[b_MT[i2], b_xdt], writes=[b_pcA])
                        kb.op("pe", lambda e, g=g, cs=cs: e.matmul(pcB, lhsT=BCT[:, 8 + g, :], rhs=sst_b[:, cs], start=True, stop=True),
                              reads=[b_BCT, b_sbb[g]], writes=[b_pcB])
                        kb.op("dve", lambda e, hs=hs: e.tensor_tensor(out=tmpc[0].rearrange("p (h f) -> p h f", f=64),
                                                                      in0=pcB.rearrange("p (h f) -> p h f", f=64),
                                                                      in1=bc_last(eacs[:, hs], 64), op=ALU.mult),
                              reads=[b_pcB, b_sm2], writes=[b_tmpc[0]])
                        kb.op("dve", lambda e: e.tensor_tensor(out=tmpc[1], in0=tmpc[0], in1=pcA, op=ALU.add),
                              reads=[b_tmpc[0], b_pcA], writes=[b_tmpc[1]])
                        kb.op("dve", lambda e, cs=cs: e.tensor_tensor(out=xs2[:, cs], in0=xs2[:, cs], in1=tmpc[1], op=ALU.add),
                              reads=[b_xs[g], b_tmpc[1]], writes=[b_xs[g]])
                    kb.op("pe", lambda e, g=g, cs=cs: e.matmul(pst, lhsT=Bt[:, g * 128:(g + 1) * 128], rhs=decx[:, cs], start=True, stop=True),
                          reads=[b_Bt, b_decx[g]], writes=[b_pst])
                    kb.op("dve", lambda e, hs=hs: e.tensor_tensor(out=sst_f[:, hs, :], in0=sst_f[:, hs, :], in1=bc_last(cdec[:, hs], 64),
                                                                  op=ALU.mult), reads=[b_sf[g], b_sm2], writes=[b_sf[g]])
                    kb.op("dve", lambda e, cs=cs: e.tensor_tensor(out=sst_f2[:, cs], in0=sst_f2[:, cs], in1=pst, op=ALU.add),
                          reads=[b_sf[g], b_pst], writes=[b_sf[g]])
                    kb.op("act", lambda e, cs=cs: e.activation(out=sst_b[:, cs], in_=sst_f2[:, cs], func=AF.Copy),
                          reads=[b_sf[g]], writes=[b_sbb[g]])
                if main:
                    kb.op("dve", lambda e: e.tensor_tensor(out=xs2, in0=xs2, in1=sz, op=ALU.mult), reads=b_xs + [b_sz], writes=b_xs)
                    for g in range(SG):
                        kb.op("act", lambda e, g=g: e.activation(out=junk, in_=xs2[:, g * 512:(g + 1) * 512], func=AF.Square,
                                                                 accum_out=ssq[:, g:g + 1]), reads=[b_xs[g]], writes=[b_junk, b_ssq])
                    kb.op("dve", lambda e: e.tensor_scalar(out=ssq[:, 8:16], in0=ssq[:, 0:8], scalar1=1.0 / 512, scalar2=EPS,
                                                           op0=ALU.mult, op1=ALU.add), reads=[b_ssq], writes=[b_ssq])
                    kb.op("act", lambda e: e.activation(out=ssq[:, 16:24], in_=ssq[:, 8:16], func=AF.Sqrt), reads=[b_ssq], writes=[b_ssq])
                    kb.op("dve", lambda e: e.reciprocal(out=ssq[:, 24:32], in_=ssq[:, 16:24]), reads=[b_ssq], writes=[b_ssq])
                    kb.op("dve", lambda e: e.tensor_tensor(out=ys.rearrange("p (g f) -> p g f", f=512),
                                                           in0=xs2.rearrange("p (g f) -> p g f", f=512),
                                                           in1=bc_last(ssq[:, 24:32], 512), op=ALU.mult),
                          reads=b_xs + [b_ssq], writes=[b_ys])
                    transpose_store(ys, b_ys, 32, ptr, b_ptr, oT, b_oT, YST_v, b_YST, r0 - ROW0)
        kb.barrier()

    if "ret" in cfg.phases:
        phase_ret()
    def phase_out():
        with ExitStack() as es:
            TBC = 3
            TBM = TBC * 128
            g_bc = sb(es, "gf_bc", [128, D], F32); b_g = Buf()
            kb.dma("sp", lambda e: e.dma_start(out=g_bc, in_=norm_ffn_g.partition_broadcast(128)), writes=[b_g])
            skT = sb(es, "skT", [128, 16, 128], F32); b_skT = Buf()
            ptr = ps(es, "ptr_o", [128, 16, 128], BF16); b_ptr = Buf()
            pA = ps(es, "pA", [128, 512], F32); b_pA = Buf()
            pB = ps(es, "pB", [128, 512], F32); b_pB = Buf()
            pC = [ps(es, "pC%d" % i, [128, 512], F32) for i in range(2)]; b_pC = [Buf(), Buf()]
            pD = ps(es, "pD", [128, 512], F32); b_pD = Buf()

            with ExitStack() as es2:
                skr = sb(es2, "skr", [128, 16, 128], F32); b_skr = Buf()
                kb.dma("sp", lambda e: e.dma_start(out=skr, in_=peer_sub_keys.rearrange("(j k) d -> k j d", k=128)), writes=[b_skr])
                for j in range(16):
                    kb.op("pe", lambda e, j=j: e.transpose(out=pC[j % 2][:, 0:128], in_=skr[:, j, :], identity=ident_f),
                          reads=[b_skr, b_ident], writes=[b_pC[j % 2]])
                    kb.op("dve", lambda e, j=j: e.tensor_copy(out=skT[:, j, :], in_=pC[j % 2][:, 0:128]), reads=[b_pC[j % 2]], writes=[b_skT])
                if not precast_state["done"]:
                    for _ in precast_units(es2, pA, b_pA):
                        pass
                kb.barrier()

            ogT = sb(es, "ogT", [128, 32, TBM], BF16); b_ogT = Buf()
            ysT = sb(es, "ysT", [128, 32, TBM], BF16); b_ysT = Buf()
            mT = sb(es, "mT", [128, 16, TBM], BF16); b_mT = Buf()
            xT = sb(es, "xT", [128, 16, TBM], BF16); b_xT = Buf()
            hb = sb(es, "hb", [128, TBC, D], F32); b_hb = [Buf() for _ in range(TBC)]
            NWB = 4
            wbf = [sb(es, "wbf_o%d" % i, [128, 4096], BF16) for i in range(NWB)]; b_wbf = [Buf() for _ in range(NWB)]
            grt = [sb(es, "grt%d" % i, [128, 2, TBM], BF16) for i in range(2)]; b_grt = [Buf(), Buf()]
            sgt = [sb(es, "sgt%d" % i, [128, 2, TBM], F32) for i in range(2)]; b_sgt = [Buf(), Buf()]
            t1 = sb(es, "t1o", [128, TBM], F32); b_t1 = Buf()
            t2 = sb(es, "t2o", [128, TBM], F32); b_t2 = Buf()
            n_bf = sb(es, "n_bf_o", [128, D], BF16); b_n = Buf()
            junk = sb(es, "junk_o", [128, D], BF16); b_junk = Buf()
            stat = sb(es, "stat_o", [128, 4], F32); b_stat = Buf()
            qpT = sb(es, "qpT", [128, TBM], F32); b_qpT = Buf()
            sct = sb(es, "sct", [128, TBC, 128], F32); b_sct = Buf()
            OGT_v = OGT.rearrange("(kt p) n -> p kt n", p=128)
            YST_v = YST.rearrange("(kt p) n -> p kt n", p=128)
            wr_v = WRB.rearrange("(kt p) c -> p kt c", p=128)
            ws_v = WSB.rearrange("(kt p) c -> p kt c", p=128)
            wo_v = WOB.rearrange("(kt p) c -> p kt c", p=128)
            wq_v = WQB.rearrange("(kt p) c -> p kt c", p=128)
            SC_v = SC.rearrange("(t p) c -> p t c", p=128)
            cnt = {"w": 0, "g": 0, "pc": 0}

            def load_w(view, kts, c0, ncol, scale_g=False):
                wi = cnt["w"] % NWB
                cnt["w"] += 1
                dsb = wbf[wi][:, 0:kts * ncol].rearrange("p (k c) -> p k c", c=ncol)
                kb.dma("sp", lambda e: e.dma_start(out=dsb, in_=view[:, :, c0:c0 + ncol]), reads=[b_WB], writes=[b_wbf[wi]])
                return dsb, b_wbf[wi]

            ch = 0
            while ch < cfg.main:
                nsub = min(TBC, cfg.main - ch)
                ntok = nsub * 128
                c0 = ch * 128
                kb.dma("sp", lambda e, c0=c0, ntok=ntok: e.dma_start(out=ogT[:, :, 0:ntok], in_=OGT_v[:, :, c0:c0 + ntok]),
                       reads=[b_OGT], writes=[b_ogT])
                kb.dma("sp", lambda e, c0=c0, ntok=ntok: e.dma_start(out=ysT[:, :, 0:ntok], in_=YST_v[:, :, c0:c0 + ntok]),
                       reads=[b_YST], writes=[b_ysT])
                for t in range(nsub):
                    rows = slice(ROW0 + c0 + t * 128, ROW0 + c0 + (t + 1) * 128)
                    kb.dma("sp", lambda e, t=t, rows=rows: e.dma_start(out=hb[:, t, :], in_=hin[rows, :]), writes=[b_hb[t]])
                for m in range(16):
                    kb.maybe_epoch()
                    wr, b_wr = load_w(wr_v, 32, m * 128, 128)
                    for kt in range(32):
                        kb.op("pe", lambda e, kt=kt, wr=wr, ntok=ntok: e.matmul(pA[:, 0:ntok], lhsT=wr[:, kt, :], rhs=ogT[:, kt, 0:ntok],
                                                                                start=(kt == 0), stop=(kt == 31)),
                              reads=[b_wr, b_ogT], writes=[b_pA])
                    ws, b_ws = load_w(ws_v, 32, m * 128, 128, scale_g=True)
                    for kt in range(32):
                        kb.op("pe", lambda e, kt=kt, ws=ws, ntok=ntok: e.matmul(pB[:, 0:ntok], lhsT=ws[:, kt, :], rhs=ysT[:, kt, 0:ntok],
                                                                                start=(kt == 0), stop=(kt == 31)),
                              reads=[b_ws, b_ysT], writes=[b_pB])
                    gi = cnt["g"] % 2
                    cnt["g"] += 1
                    kb.dma("sp", lambda e, gi=gi, m=m, c0=c0, ntok=ntok: e.dma_start(out=grt[gi][:, 0, 0:ntok],
                                                                                     in_=GT[m * 128:(m + 1) * 128, c0:c0 + ntok]),
                           reads=[b_GT], writes=[b_grt[gi]])
                    kb.dma("sp", lambda e, gi=gi, m=m, c0=c0, ntok=ntok: e.dma_start(out=grt[gi][:, 1, 0:ntok],
                                                                                     in_=GT[D + m * 128:D + (m + 1) * 128, c0:c0 + ntok]),
                           reads=[b_GT], writes=[b_grt[gi]], accumulate=True)
                    kb.op("act", lambda e, gi=gi, ntok=ntok: e.activation(out=sgt[gi][:, :, 0:ntok], in_=grt[gi][:, :, 0:ntok], func=AF.Sigmoid),
                          reads=[b_grt[gi]], writes=[b_sgt[gi]])
                    kb.op("dve", lambda e, gi=gi, ntok=ntok: e.tensor_tensor(out=t1[:, 0:ntok], in0=pA[:, 0:ntok], in1=sgt[gi][:, 0, 0:ntok],
                                                                             op=ALU.mult), reads=[b_pA, b_sgt[gi]], writes=[b_t1])
                    kb.op("dve", lambda e, gi=gi, ntok=ntok: e.tensor_tensor(out=t2[:, 0:ntok], in0=pB[:, 0:ntok], in1=sgt[gi][:, 1, 0:ntok],
                                                                             op=ALU.mult), reads=[b_pB, b_sgt[gi]], writes=[b_t2])
                    kb.op("dve", lambda e, m=m, ntok=ntok: e.tensor_tensor(out=mT[:, m, 0:ntok], in0=t1[:, 0:ntok], in1=t2[:, 0:ntok],
                                                                           op=ALU.add), reads=[b_t1, b_t2], writes=[b_mT])
                for nb in range(8):
                    wo, b_wo = load_w(wo_v, 16, nb * 256, 256)
                    for t in range(nsub):
                        pi = cnt["pc"] % 2
                        cnt["pc"] += 1
                        for kt in range(16):
                            kb.op("pe", lambda e, pi=pi, kt=kt, t=t, wo=wo: e.matmul(pC[pi][:, 0:256], lhsT=mT[:, kt, t * 128:(t + 1) * 128],
                                                                                     rhs=wo[:, kt, :], start=(kt == 0), stop=(kt == 15)),
                                  reads=[b_mT, b_wo], writes=[b_pC[pi]])
                        kb.op("dve", lambda e, pi=pi, t=t, nb=nb: e.tensor_tensor(out=hb[:, t, nb * 256:(nb + 1) * 256],
                                                                                  in0=hb[:, t, nb * 256:(nb + 1) * 256], in1=pC[pi][:, 0:256],
                                                                                  op=ALU.add), reads=[b_hb[t], b_pC[pi]], writes=[b_hb[t]])
                for t in range(nsub):
                    rows = slice(c0 + t * 128, c0 + (t + 1) * 128)
                    kb.dma("sp", lambda e, t=t, rows=rows: e.dma_start(out=H1[rows, :], in_=hb[:, t, :]), reads=[b_hb[t]], writes=[b_H1],
                           accumulate=True)
                    kb.op("act", lambda e, t=t: e.activation(out=junk, in_=hb[:, t, :], func=AF.Square, accum_out=stat[:, 0:1]),
                          reads=[b_hb[t]], writes=[b_junk, b_stat])
                    kb.op("dve", lambda e: e.tensor_scalar(out=stat[:, 1:2], in0=stat[:, 0:1], scalar1=1.0 / D, scalar2=EPS,
                                                           op0=ALU.mult, op1=ALU.add), reads=[b_stat], writes=[b_stat])
                    kb.op("act", lambda e: e.activation(out=stat[:, 3:4], in_=stat[:, 1:2], func=AF.Sqrt), reads=[b_stat], writes=[b_stat])
                    kb.op("dve", lambda e: e.reciprocal(out=stat[:, 2:3], in_=stat[:, 3:4]), reads=[b_stat], writes=[b_stat])
                    kb.op("dve", lambda e, t=t: e.scalar_tensor_tensor(out=n_bf, in0=hb[:, t, :], scalar=stat[:, 2:3], in1=g_bc,
                                                                       op0=ALU.mult, op1=ALU.mult), reads=[b_hb[t], b_stat, b_g], writes=[b_n])
                    kb.dma("sp", lambda e, rows=rows: e.dma_start(out=XN2[rows, :], in_=n_bf), reads=[b_n], writes=[b_XN2], accumulate=True)
                    for k in range(16):
                        kb.op("pe", lambda e, k=k: e.transpose(out=ptr[:, k, :], in_=n_bf[:, k * 128:(k + 1) * 128], identity=ident_bf),
                              reads=[b_n, b_ident], writes=[b_ptr])
                    kb.op("act", lambda e, t=t: e.activation(out=xT[:, :, t * 128:(t + 1) * 128], in_=ptr, func=AF.Copy),
                          reads=[b_ptr], writes=[b_xT])
                for j in range(16):
                    wq, b_wq = load_w(wq_v, 16, j * 128, 128)
                    pi = cnt["pc"] % 2
                    cnt["pc"] += 1
                    for kt in range(16):
                        kb.op("pe", lambda e, pi=pi, kt=kt, wq=wq, ntok=ntok: e.matmul(pC[pi][:, 0:ntok], lhsT=wq[:, kt, :], rhs=xT[:, kt, 0:ntok],
                                                                                       start=(kt == 0), stop=(kt == 15)),
                              reads=[b_wq, b_xT], writes=[b_pC[pi]])
                    kb.op("act", lambda e, pi=pi, ntok=ntok: e.activation(out=qpT[:, 0:ntok], in_=pC[pi][:, 0:ntok], func=AF.Copy),
                          reads=[b_pC[pi]], writes=[b_qpT])
                    for t in range(nsub):
                        kb.op("pe", lambda e, t=t, j=j: e.matmul(pD[:, t * 128:(t + 1) * 128], lhsT=qpT[:, t * 128:(t + 1) * 128], rhs=skT[:, j, :],
                                                                 start=True, stop=True), reads=[b_qpT, b_skT], writes=[b_pD])
                    kb.op("dve", lambda e, nsub=nsub, ntok=ntok: e.tensor_copy(out=sct[:, 0:nsub, :],
                                                                               in_=pD[:, 0:ntok].rearrange("p (t k) -> p t k", k=128)),
                          reads=[b_pD], writes=[b_sct])
                    kb.dma("sp", lambda e, j=j, ch=ch, nsub=nsub: e.dma_start(out=SC_v[:, ch:ch + nsub, j * 128:(j + 1) * 128], in_=sct[:, 0:nsub, :]),
                           reads=[b_sct], writes=[b_SC], accumulate=True)
                ch += nsub
        kb.barrier()

    if "ssd" in cfg.phases:
        phase_ssd()
    def phase_peer():
        with ExitStack() as es:
            NEG = -1.0e30
            NB = 6
            gf_bc = sb(es, "gfin_bc", [128, D], F32); b_gf = Buf()
            kb.dma("sp", lambda e: e.dma_start(out=gf_bc, in_=norm_final_g.partition_broadcast(128)), writes=[b_gf])
            iota = sb(es, "iota", [128, 256], F32); b_iota = Buf()
            iota_i = sb(es, "iota_i", [128, 256], I32)
            kb.op("pool", lambda e: e.iota(iota_i, pattern=[[1, 256]], base=0, channel_multiplier=0), writes=[b_iota])
            kb.op("dve", lambda e: e.tensor_copy(out=iota, in_=iota_i), reads=[b_iota], writes=[b_iota])
            if not precast_state["done"]:
                with ExitStack() as es2:
                    ptmp = ps(es2, "ptmp", [128, 512], F32); b_ptmp = Buf()
                    for _ in precast_units(es2, ptmp, b_ptmp):
                        pass
                    kb.barrier()
            sc = sb(es, "sc", [128, 16, 128], F32); b_sc = Buf()
            work = sb(es, "work", [128, 256], F32); b_work = Buf()
            v16 = sb(es, "v16", [128, 16, 16], F32); b_v16 = Buf()
            i16 = sb(es, "i16", [128, 16, 16], U32); b_i16 = Buf()
            i16f = sb(es, "i16f", [128, 16, 16], F32); b_i16f = Buf()
            i16s = sb(es, "i16s", [128, 16, 16], F32); b_i16s = Buf()
            cand = sb(es, "cand", [128, 8, 256], F32); b_cand = Buf()
            cid = sb(es, "cid", [128, 8, 256], F32); b_cid = Buf()
            top = sb(es, "top", [128, 8, 16], F32); b_top = Buf()
            pos = sb(es, "pos", [128, 8, 16], U32); b_pos = Buf()
            posf = sb(es, "posf", [128, 8, 16], F32); b_posf = Buf()
            eq = sb(es, "eq", [128, 16, 256], F32); b_eq = Buf()
            eidf = sb(es, "eidf", [128, 128], F32); b_eidf = Buf()
            eid2 = [sb(es, "eid%d" % i, [128, 128], I32) for i in range(2)]; b_eid2 = [Buf(), Buf()]
            gsm2 = [sb(es, "gsm%d" % i, [128, 4, 128], F32) for i in range(2)]; b_gsm2 = [Buf(), Buf()]
            gz = sb(es, "gz", [128, 16], F32); b_gz = Buf()
            act_t = sb(es, "act_t", [128, 128], F32); b_act = Buf()
            wgt = sb(es, "wgt", [128, 128], F32); b_wgt = Buf()
            GS, NGB = 4, 3
            dgs = [sb(es, "dgs%d" % i, [128, GS, 128], BF16) for i in range(NGB)]; b_dgs = [Buf() for _ in range(NGB)]
            uv = [sb(es, "uv%d" % i, [128, GS, 2 * D], BF16) for i in range(NGB)]
            b_uv = [[Buf() for _ in range(GS)] for _ in range(NGB)]
            b_actg = [Buf() for _ in range(128 // GS)]
            xn2_ = [sb(es, "xn%d" % i, [128, D], BF16) for i in range(2)]; b_xn2 = [Buf(), Buf()]
            h1t2 = [sb(es, "h1t%d" % i, [128, D], F32) for i in range(2)]; b_h12 = [Buf(), Buf()]
            junk = sb(es, "junk_p", [128, D], BF16); b_junk = Buf()
            prod = [sb(es, "prod%d" % i, [128, D], BF16) for i in range(2)]; b_prod = [Buf(), Buf()]
            stat = sb(es, "stat_p", [128, 4], F32); b_stat = Buf()
            ot = sb(es, "ot", [128, D], F32); b_ot = Buf()
            pacc = [ps(es, "pacc%d" % i, [128, 512], F32) for i in range(4)]; b_pacc = [Buf() for _ in range(4)]
            cnt = {"u": 0, "v": 0}

            def routing(ch):
                rows = slice(ch * 128, (ch + 1) * 128)
                par = ch % 2
                eid, b_eid, gsm, b_gsm = eid2[par], b_eid2[par], gsm2[par], b_gsm2[par]
                xn, b_xn, h1t, b_h1 = xn2_[par], b_xn2[par], h1t2[par], b_h12[par]
                kb.dma("sp", lambda e, rows=rows: e.dma_start(out=sc.rearrange("p j k -> p (j k)"), in_=SC[rows, :]), reads=[b_SC], writes=[b_sc])
                kb.dma("sp", lambda e, rows=rows: e.dma_start(out=xn, in_=XN2[rows, :]), reads=[b_XN2], writes=[b_xn])
                kb.dma("sp", lambda e, rows=rows: e.dma_start(out=h1t, in_=H1[rows, :]), reads=[b_H1], writes=[b_h1])
                for j in range(16):
                    kb.op("dve", lambda e, j=j: e.max(out=v16[:, j, 0:8], in_=sc[:, j, :]), reads=[b_sc], writes=[b_v16])
                    kb.op("dve", lambda e, j=j: e.match_replace(out=work[:, 0:128], in_to_replace=v16[:, j, 0:8], in_values=sc[:, j, :],
                                                                imm_value=NEG), reads=[b_sc, b_v16], writes=[b_work])
                    kb.op("dve", lambda e, j=j: e.max(out=v16[:, j, 8:16], in_=work[:, 0:128]), reads=[b_work, b_v16], writes=[b_v16])
                    kb.op("dve", lambda e, j=j: e.max_index(out=i16[:, j, 0:8], in_max=v16[:, j, 0:8], in_values=sc[:, j, :]),
                          reads=[b_sc, b_v16], writes=[b_i16])
                    kb.op("dve", lambda e, j=j: e.max_index(out=i16[:, j, 8:16], in_max=v16[:, j, 8:16], in_values=sc[:, j, :]),
                          reads=[b_sc, b_v16, b_i16], writes=[b_i16])
                kb.op("dve", lambda e: e.tensor_copy(out=i16f, in_=i16), reads=[b_i16], writes=[b_i16f])
                v4 = v16.rearrange("p (h c) k -> p h c k", c=2)
                f4 = i16f.rearrange("p (h c) k -> p h c k", c=2)
                s4 = i16s.rearrange("p (h c) k -> p h c k", c=2)
                c4 = cand.rearrange("p h (a b) -> p h a b", b=16)
                d4 = cid.rearrange("p h (a b) -> p h a b", b=16)
                kb.op("dve", lambda e: e.tensor_tensor(out=c4, in0=v4[:, :, 0, :].unsqueeze(3).to_broadcast([128, 8, 16, 16]),
                                                       in1=v4[:, :, 1, :].unsqueeze(2).to_broadcast([128, 8, 16, 16]), op=ALU.add),
                      reads=[b_v16], writes=[b_cand])
                kb.op("dve", lambda e: e.tensor_scalar(out=i16s, in0=i16f, scalar1=float(PNK), scalar2=None, op0=ALU.mult),
                      reads=[b_i16f], writes=[b_i16s])
                kb.op("dve", lambda e: e.tensor_tensor(out=d4, in0=s4[:, :, 0, :].unsqueeze(3).to_broadcast([128, 8, 16, 16]),
                                                       in1=f4[:, :, 1, :].unsqueeze(2).to_broadcast([128, 8, 16, 16]), op=ALU.add),
                      reads=[b_i16f, b_i16s], writes=[b_cid])
                for h in range(PH):
                    kb.op("dve", lambda e, h=h: e.max(out=top[:, h, 0:8], in_=cand[:, h, :]), reads=[b_cand], writes=[b_top])
                    kb.op("dve", lambda e, h=h: e.match_replace(out=work, in_to_replace=top[:, h, 0:8], in_values=cand[:, h, :],
                                                                imm_value=NEG), reads=[b_cand, b_top], writes=[b_work])
                    kb.op("dve", lambda e, h=h: e.max(out=top[:, h, 8:16], in_=work), reads=[b_work, b_top], writes=[b_top])
                    kb.op("dve", lambda e, h=h: e.max_index(out=pos[:, h, 0:8], in_max=top[:, h, 0:8], in_values=cand[:, h, :]),
                          reads=[b_cand, b_top], writes=[b_pos])
                    kb.op("dve", lambda e, h=h: e.max_index(out=pos[:, h, 8:16], in_max=top[:, h, 8:16], in_values=cand[:, h, :]),
                          reads=[b_cand, b_top, b_pos], writes=[b_pos])
                kb.op("dve", lambda e: e.tensor_copy(out=posf, in_=pos), reads=[b_pos], writes=[b_posf])
                for h in range(PH):
                    kb.op("dve", lambda e, h=h: e.tensor_tensor(out=eq, in0=iota.unsqueeze(1).to_broadcast([128, 16, 256]),
                                                                in1=posf[:, h, :].unsqueeze(2).to_broadcast([128, 16, 256]),
                                                                op=ALU.is_equal), reads=[b_iota, b_posf], writes=[b_eq])
                    kb.op("dve", lambda e, h=h: e.tensor_tensor(out=eq, in0=eq, in1=cid[:, h, :].unsqueeze(1).to_broadcast([128, 16, 256]),
                                                                op=ALU.mult), reads=[b_eq, b_cid], writes=[b_eq])
                    kb.op("dve", lambda e, h=h: e.tensor_reduce(out=eidf[:, h * 16:(h + 1) * 16], in_=eq, axis=AX.X, op=ALU.add),
                          reads=[b_eq], writes=[b_eidf])
                kb.op("dve", lambda e: e.tensor_copy(out=eid, in_=eidf), reads=[b_eidf], writes=[b_eid])
                G = lambda i: gsm[:, i, :]
                t3 = top
                kb.op("dve", lambda e: e.tensor_tensor(out=G(0).rearrange("p (h k) -> p h k", k=16), in0=t3,
                                                       in1=t3[:, :, 0:1].to_broadcast([128, 8, 16]), op=ALU.subtract),
                      reads=[b_top], writes=[b_gsm])
                kb.op("act", lambda e: e.activation(out=G(1), in_=G(0), func=AF.Exp), reads=[b_gsm], writes=[b_gsm])
                kb.op("dve", lambda e: e.tensor_reduce(out=gz[:, 0:8], in_=G(1).rearrange("p (h k) -> p h k", k=16), axis=AX.X, op=ALU.add),
                      reads=[b_gsm], writes=[b_gz])
                kb.op("dve", lambda e: e.reciprocal(out=gz[:, 8:16], in_=gz[:, 0:8]), reads=[b_gz], writes=[b_gz])
                kb.op("dve", lambda e: e.tensor_tensor(out=G(2).rearrange("p (h k) -> p h k", k=16),
                                                       in0=G(1).rearrange("p (h k) -> p h k", k=16),
                                                       in1=bc_last(gz[:, 8:16], 16), op=ALU.mult), reads=[b_gsm, b_gz], writes=[b_gsm])

            def mix(ch):
                rows = slice(ch * 128, (ch + 1) * 128)
                par = ch % 2
                eid, b_eid, gsm, b_gsm = eid2[par], b_eid2[par], gsm2[par], b_gsm2[par]
                xn, b_xn, h1t, b_h1 = xn2_[par], b_xn2[par], h1t2[par], b_h12[par]
                NG = 128 // GS

                def finish_group(gq):
                    bi = gq % NGB
                    s0 = gq * GS
                    kb.op("act", lambda e: e.activation(out=gsm[:, 3, s0:s0 + GS], in_=act_t[:, s0:s0 + GS], func=AF.Gelu),
                          reads=[b_actg[gq]], writes=[b_actg[gq]])
                    kb.op("dve", lambda e: e.tensor_tensor(out=wgt[:, s0:s0 + GS], in0=gsm[:, 3, s0:s0 + GS], in1=gsm[:, 2, s0:s0 + GS],
                                                           op=ALU.mult), reads=[b_actg[gq], b_gsm], writes=[b_actg[gq]])
                    kb.op("dve", lambda e: e.tensor_tensor(out=dgs[bi], in0=ident_f.unsqueeze(1).to_broadcast([128, GS, 128]),
                                                           in1=bc_last(wgt[:, s0:s0 + GS], 128), op=ALU.mult),
                          reads=[b_ident, b_actg[gq]], writes=[b_dgs[bi]])
                    for i in range(GS):
                        s_ = s0 + i
                        for nb in range(4):
                            kb.op("pe", lambda e, i=i, s_=s_, nb=nb: e.matmul(pacc[nb], lhsT=dgs[bi][:, i, :],
                                                                              rhs=uv[bi][:, i, D + nb * 512:D + (nb + 1) * 512],
                                                                              start=(s_ == 0), stop=(s_ == 127)),
                                  reads=[b_dgs[bi], b_uv[bi][i]], writes=[b_pacc[nb]])

                for gq in range(NG):
                    bi = gq % NGB
                    s0 = gq * GS
                    for i in range(GS):
                        s_ = s0 + i
                        kb.dma("pool", lambda e, bi=bi, i=i, s_=s_: e.indirect_dma_start(
                            out=uv[bi][:, i, :], out_offset=None, in_=UVB,
                            in_offset=bass.IndirectOffsetOnAxis(ap=eid[:, s_:s_ + 1], axis=0)),
                            reads=[b_eid, b_UVB], writes=[b_uv[bi][i]])
                    for i in range(GS):
                        s_ = s0 + i
                        pj = s_ % 2
                        kb.op("dve", lambda e, bi=bi, i=i, pj=pj: e.tensor_tensor(out=prod[pj], in0=uv[bi][:, i, 0:D], in1=xn, op=ALU.mult),
                              reads=[b_uv[bi][i], b_xn], writes=[b_prod[pj]])
                        kb.op("act", lambda e, pj=pj, s_=s_: e.activation(out=junk, in_=prod[pj], func=AF.Copy, accum_out=act_t[:, s_:s_ + 1]),
                              reads=[b_prod[pj]], writes=[b_junk, b_actg[gq]])
                    if gq >= 1:
                        finish_group(gq - 1)
                    kb.flush(6)
                finish_group(NG - 1)
                for nb in range(4):
                    kb.op("dve", lambda e, nb=nb: e.tensor_tensor(out=h1t[:, nb * 512:(nb + 1) * 512], in0=h1t[:, nb * 512:(nb + 1) * 512],
                                                                  in1=pacc[nb], op=ALU.add), reads=[b_h1, b_pacc[nb]], writes=[b_h1])
                kb.op("act", lambda e: e.activation(out=junk, in_=h1t, func=AF.Square, accum_out=stat[:, 0:1]),
                      reads=[b_h1], writes=[b_junk, b_stat])
                kb.op("dve", lambda e: e.tensor_scalar(out=stat[:, 1:2], in0=stat[:, 0:1], scalar1=1.0 / D, scalar2=EPS,
                                                       op0=ALU.mult, op1=ALU.add), reads=[b_stat], writes=[b_stat])
                kb.op("act", lambda e: e.activation(out=stat[:, 3:4], in_=stat[:, 1:2], func=AF.Sqrt), reads=[b_stat], writes=[b_stat])
                kb.op("dve", lambda e: e.reciprocal(out=stat[:, 2:3], in_=stat[:, 3:4]), reads=[b_stat], writes=[b_stat])
                kb.op("dve", lambda e: e.scalar_tensor_tensor(out=ot, in0=h1t, scalar=stat[:, 2:3], in1=gf_bc, op0=ALU.mult, op1=ALU.mult),
                      reads=[b_h1, b_stat, b_gf], writes=[b_ot])
                kb.dma("sp", lambda e, rows=rows: e.dma_start(out=out[rows, :], in_=ot), reads=[b_ot], writes=[b_out], accumulate=True)

            routing(0)
            for ch in range(cfg.main):
                kb.maybe_epoch()
                if ch + 1 < cfg.main:
                    kb.recording = True
                    routing(ch + 1)
                    kb.recording = False
                mix(ch)
                kb.flush()
        kb.barrier()

    if "out" in cfg.phases:
        phase_out()
    if "peer" in cfg.phases:
        phase_peer()

    kb.barrier()
    es_glob.close()
    return nc, kb


def make_consts():
    c = np.zeros((128, 4096), np.float64)
    i = np.arange(128)
    c[:, 0:128] = (i[None, :] >= i[:, None])
    c[:, 128:256] = (i[:, None] > i[None, :])
    c[:, 256:384] = 1.0
    gam = 1.0 - 2.0 ** (-5.0 - np.arange(8))
    lg = np.log(gam)
    c[:, 384:392] = np.exp(lg[None, :] * (i[:, None] + 1.0))
    c[:, 392:400] = np.exp(-lg[None, :] * (i[:, None] + 1.0)) / 16.0
    c[:, 400:408] = np.exp(lg[None, :] * (127.0 - i[:, None])) / 16.0
    c[:, 408:416] = np.exp(lg[None, :] * 128.0)
    return c.astype(np.float32)


def core_streams(x, meta_tokens, cfg):
    B, S, _ = x.shape
    maps = []
    inv = (10000.0 ** (-np.arange(128, dtype=np.float32) / np.float32(128))).astype(np.float32)
    for core in range(8):
        b, s = core // 2, core % 2
        full = np.zeros((LEAD + N_META + S, D), np.float32)
        full[LEAD:LEAD + N_META] = meta_tokens
        full[LEAD + N_META:] = x[b]
        valid = np.zeros((full.shape[0], 1), np.float32)
        valid[LEAD:] = 1.0
        pos = np.maximum(np.arange(full.shape[0]) - LEAD, 0).astype(np.float32)
        hin = np.zeros((cfg.nt, D), np.float32)
        msk = np.zeros((cfg.nt, 1), np.float32)
        p = np.zeros((cfg.nt,), np.float32)
        if s == 0:
            n = (cfg.main + 1) * 128
            r = (cfg.pre - 1) * 128
            hin[r:] = full[0:n]
            msk[r:] = valid[0:n]
            p[r:] = pos[0:n]
        else:
            hin[:] = full[0:cfg.nt]
            msk[:] = valid[0:cfg.nt]
            p[:] = pos[0:cfg.nt]
        ang = p[:, None] * inv[None, :]
        maps.append({"hin": hin, "rowmask": msk, "ropec": np.cos(ang).astype(np.float32),
                     "ropes": np.sin(ang).astype(np.float32)})
    return maps


_PROG = {}


def kernel(x, meta_tokens, norm_mix_g, w_in, conv_w, conv_b, dt_bias, a_log, d_skip, ssm_norm_g, w_ret_o,
           w_ssm_o, w_out, norm_ffn_g, peer_w_q, peer_sub_keys, peer_u, peer_v, norm_final_g):
    cfg = Cfg()
    x = np.asarray(x, np.float32)
    maps = core_streams(x, np.asarray(meta_tokens, np.float32), cfg)
    shared = {
        "cst_f": make_consts(),
        "norm_mix_g": np.asarray(norm_mix_g, np.float32).reshape(1, D),
        "w_in": np.asarray(w_in, np.float32).reshape(D, NPROJ),
        "conv_w": np.asarray(conv_w, np.float32).reshape(4, CONV_DIM),
        "conv_b": np.asarray(conv_b, np.float32).reshape(1, CONV_DIM),
        "dt_bias": np.asarray(dt_bias, np.float32).reshape(1, SH),
        "a_log": np.asarray(a_log, np.float32).reshape(1, SH),
        "d_skip": np.asarray(d_skip, np.float32).reshape(1, SH),
        "ssm_norm_g": np.asarray(ssm_norm_g, np.float32).reshape(1, SSM_INNER),
        "w_ret_o": np.asarray(w_ret_o, np.float32).reshape(RH * RDV, D),
        "w_ssm_o": np.asarray(w_ssm_o, np.float32).reshape(SSM_INNER, D),
        "w_out": np.asarray(w_out, np.float32).reshape(D, D),
        "norm_ffn_g": np.asarray(norm_ffn_g, np.float32).reshape(1, D),
        "peer_w_q": np.asarray(peer_w_q, np.float32).reshape(D, D),
        "peer_sub_keys": np.asarray(peer_sub_keys, np.float32).reshape(16 * 128, 128),
        "peer_u": np.asarray(peer_u, np.float32).reshape(NEXP, D),
        "peer_v": np.asarray(peer_v, np.float32).reshape(NEXP, D),
        "norm_final_g": np.asarray(norm_final_g, np.float32).reshape(1, D),
    }
    for m in maps:
        m.update(shared)
    nc, _ = build_program(cfg)
    res = run_bass_kernel_spmd(nc, maps, core_ids=list(range(8)))
    B, S, _ = x.shape
    outp = np.zeros((B, S, D), np.float32)
    half = cfg.main * 128
    for core in range(8):
        b, s = core // 2, core % 2
        outp[b, s * half:(s + 1) * half] = res.results[core]["out"]
    return outp
```

```python
import numpy as np
from contextlib import ExitStack
import concourse.bass as bass
import concourse.mybir as mybir
from concourse.bass_utils import run_bass_kernel_spmd

F32 = mybir.dt.float32
BF16 = mybir.dt.bfloat16
I32 = mybir.dt.int32
U32 = mybir.dt.uint32
AF = mybir.ActivationFunctionType
ALU = mybir.AluOpType
AX = mybir.AxisListType

D = 2048
N_META = 16
LEAD = 112
EPS = 1e-6
RH, RDK, RDV = 8, 256, 512
SSM_INNER, SH, SP_, SG, SN = 4096, 64, 64, 8, 128
CONV_DIM = 6144
NPROJ = 26688
C_Q, C_K, C_V, C_G, C_Z, C_XBC, C_DT, C_GR, C_GS = 0, 2048, 4096, 8192, 12288, 16384, 22528, 22592, 24640
PTM_W = 16384
PH, PNK, PTOPK, PHALF = 8, 128, 16, 128
NEXP = 16384
HALO = 8


class Buf:
    __slots__ = ("writers", "readers", "name")

    def __init__(self, name=""):
        self.writers = {}
        self.readers = {}
        self.name = name


class KB:
    NQ = 20

    def __init__(self, nc):
        self.nc = nc
        self.engs = {"pe": nc.tensor, "act": nc.scalar, "dve": nc.vector, "pool": nc.gpsimd, "sp": nc.sync}
        self.epoch = 0
        self.csem = {e: nc.alloc_semaphore("c_" + e) for e in ("pe", "act", "dve", "pool")}
        self.ccnt = {e: 0 for e in self.csem}
        self.dsem = {q: [nc.alloc_semaphore("d_%s%d" % (q, i)) for i in range(self.NQ)] for q in ("sp", "pool")}
        self.dcnt = {q: 0 for q in self.dsem}
        self.waited = {e: {} for e in self.engs}
        self.n_ins = 0
        self.limit = None
        self.n_ops = 0
        self.recording = False
        self.deferred_q = []

    def _wait(self, e, toks):
        w = self.waited[e]
        need = {}
        for key, (sem, val) in toks:
            if key[0] == "c":
                if key[2] < self.epoch:
                    continue
                if e == "pe" and key[1] == "pe":
                    continue
            if w.get(key, 0) >= val:
                continue
            if key not in need or need[key][1] < val:
                need[key] = (sem, val)
        for key, (sem, val) in need.items():
            self.engs[e].wait_ge(sem, val)
            w[key] = val
            self.n_ins += 1

    @staticmethod
    def _merge(dst, key, sv):
        if key not in dst or dst[key][1] < sv[1]:
            dst[key] = sv

    def _deps(self, reads, writes, accumulate=False):
        toks = []
        for b in reads:
            toks += list(b.writers.items())
        for b in writes:
            if not accumulate:
                toks += list(b.writers.items())
            toks += list(b.readers.items())
        return toks

    def _commit(self, key, sv, reads, writes, accumulate=False):
        for b in reads:
            self._merge(b.readers, key, sv)
        for b in writes:
            if accumulate:
                self._merge(b.writers, key, sv)
            else:
                b.writers = {key: sv}
                b.readers = {}

    def flush(self, n=None):
        q = self.deferred_q
        k = len(q) if n is None else min(n, len(q))
        rec, self.recording = self.recording, False
        for _ in range(k):
            kind, args = q.pop(0)
            (self.op if kind == "op" else self.dma)(*args)
        self.recording = rec

    def op(self, e, fn, reads=(), writes=()):
        if self.recording:
            self.deferred_q.append(("op", (e, fn, list(reads), list(writes))))
            return
        self.n_ops += 1
        if self.limit is not None and self.n_ops > self.limit:
            return
        self._wait(e, self._deps(reads, writes))
        ins = fn(self.engs[e])
        self.ccnt[e] += 1
        ins.then_inc(self.csem[e], 1)
        self.n_ins += 1
        self._commit(("c", e, self.epoch), (self.csem[e], self.ccnt[e]), reads, writes)

    def dma(self, q, fn, reads=(), writes=(), accumulate=False):
        if self.recording:
            self.deferred_q.append(("dma", (q, fn, list(reads), list(writes), accumulate)))
            return
        self.n_ops += 1
        if self.limit is not None and self.n_ops > self.limit:
            return
        i = self.dcnt[q]
        slot, gen = i % self.NQ, i // self.NQ
        key = ("d", q, slot)
        sem = self.dsem[q][slot]
        toks = self._deps(reads, writes, accumulate)
        if gen > 0:
            toks.append((key, (sem, 16 * gen)))
        self._wait(q, toks)
        ins = fn(self.engs[q])
        ins.then_inc(sem, 16)
        self.dcnt[q] += 1
        self.n_ins += 1
        self._commit(key, (sem, 16 * (gen + 1)), reads, writes, accumulate)

    def all_tokens(self):
        toks = [(("c", e, self.epoch), (self.csem[e], self.ccnt[e])) for e in self.csem if self.ccnt[e] > 0]
        for q in self.dsem:
            n = self.dcnt[q]
            for slot in range(min(n, self.NQ)):
                gens = (n - 1 - slot) // self.NQ + 1
                toks.append((("d", q, slot), (self.dsem[q][slot], 16 * gens)))
        return toks

    def barrier(self):
        toks = self.all_tokens()
        for e in self.engs:
            self._wait(e, toks)
        if self.limit is not None and self.n_ops > self.limit:
            return
        self.epoch += 1
        self.csem = {e: self.nc.alloc_semaphore("c_%s_%d" % (e, self.epoch)) for e in self.csem}
        self.ccnt = {e: 0 for e in self.csem}

    def maybe_epoch(self, thresh=30000):
        if max(self.ccnt.values()) > thresh:
            self.barrier()


def bc_mid(ap2d, n):
    p, f = ap2d.shape
    return ap2d.unsqueeze(1).to_broadcast([p, n, f])


def bc_last(ap2d, n):
    p, f = ap2d.shape
    return ap2d.unsqueeze(2).to_broadcast([p, f, n])


class Cfg:
    def __init__(self, pre=33, main=32, debug=False, phases=("proj", "ret", "ssd", "out", "peer"), tiny=()):
        self.tiny = set(tiny)
        self.pre = pre
        self.main = main
        self.debug = debug
        self.phases = phases
        self.nch = pre + main
        self.nt = self.nch * 128
        self.nt_main = main * 128


def build_program(cfg):
    nc = bass.Bass("TRN2", target_bir_lowering=False)
    kb = KB(nc)
    kb.limit = getattr(cfg, "limit", None)
    NT, NTM = cfg.nt, cfg.nt_main
    ROW0 = cfg.pre * 128
    dbg_kind = "ExternalOutput" if cfg.debug else "Internal"

    def ext_in(name, shape, dt=F32):
        if name in cfg.tiny:
            shape = [1, 8]
        return nc.dram_tensor(name, list(shape), dt, kind="ExternalInput").ap()

    hin = ext_in("hin", [NT, D])
    rowmask = ext_in("rowmask", [NT, 1])
    ropec = ext_in("ropec", [NT, 128])
    ropes = ext_in("ropes", [NT, 128])
    cst_f = ext_in("cst_f", [128, 4096])
    norm_mix_g = ext_in("norm_mix_g", [1, D])
    w_in = ext_in("w_in", [D, NPROJ])
    conv_w = ext_in("conv_w", [4, CONV_DIM])
    conv_b = ext_in("conv_b", [1, CONV_DIM])
    dt_bias = ext_in("dt_bias", [1, SH])
    a_log = ext_in("a_log", [1, SH])
    d_skip = ext_in("d_skip", [1, SH])
    ssm_norm_g = ext_in("ssm_norm_g", [1, SSM_INNER])
    w_ret_o = ext_in("w_ret_o", [RH * RDV, D])
    w_ssm_o = ext_in("w_ssm_o", [SSM_INNER, D])
    w_out = ext_in("w_out", [D, D])
    norm_ffn_g = ext_in("norm_ffn_g", [1, D])
    peer_w_q = ext_in("peer_w_q", [D, D])
    peer_sub_keys = ext_in("peer_sub_keys", [16 * 128, 128])
    peer_u = ext_in("peer_u", [NEXP, D])
    peer_v = ext_in("peer_v", [NEXP, D])
    norm_final_g = ext_in("norm_final_g", [1, D])
    out = nc.dram_tensor("out", [NTM, D], F32, kind="ExternalOutput").ap()

    PTMS = [nc.dram_tensor("PTM%d" % i, [NT, 4096], BF16, kind=dbg_kind).ap() for i in range(4)]

    class _PTM:
        def __getitem__(self, key):
            rows, cols = key
            i = cols.start // 4096
            assert (cols.stop - 1) // 4096 == i
            return PTMS[i][rows, cols.start - i * 4096:cols.stop - i * 4096]
    PTM = _PTM()
    XBCT = nc.dram_tensor("XBCT", [CONV_DIM, HALO + NT], BF16, kind=dbg_kind).ap()
    DTS = nc.dram_tensor("DTS", [NT, SH], F32, kind=dbg_kind).ap()
    GT = nc.dram_tensor("GT", [2 * D, NTM], BF16, kind=dbg_kind).ap()
    OGT = nc.dram_tensor("OGT", [RH * RDV, NTM], BF16, kind=dbg_kind).ap()
    YST = nc.dram_tensor("YST", [SSM_INNER, NTM], BF16, kind=dbg_kind).ap()
    H1 = nc.dram_tensor("H1", [NTM, D], F32, kind=dbg_kind).ap()
    XN2 = nc.dram_tensor("XN2", [NTM, D], BF16, kind=dbg_kind).ap()
    SC = nc.dram_tensor("SC", [NTM, 16 * 128], F32, kind=dbg_kind).ap()
    b_XN2, b_SC = Buf("XN2"), Buf("SC")
    NEX = 128 if "peer_u" in cfg.tiny else NEXP
    UVB = nc.dram_tensor("UVB", [NEX, 2 * D], BF16, kind="Internal").ap()
    b_UVB = Buf("UVB")
    WRB = nc.dram_tensor("WRB", [RH * RDV, D], BF16, kind="Internal").ap()
    WSB = nc.dram_tensor("WSB", [SSM_INNER, D], BF16, kind="Internal").ap()
    WOB = nc.dram_tensor("WOB", [D, D], BF16, kind="Internal").ap()
    WQB = nc.dram_tensor("WQB", [D, D], BF16, kind="Internal").ap()
    b_WB = Buf("WB")
    b_PTM, b_XBCT, b_DTS, b_GT, b_OGT, b_YST, b_H1, b_out = (Buf(n) for n in
                                                              ("PTM", "XBCT", "DTS", "GT", "OGT", "YST", "H1", "out"))

    es_glob = ExitStack()

    uniq = [0]

    def sb(es, name, shape, dt):
        uniq[0] += 1
        return es.enter_context(nc.sbuf_tensor("%s_%d" % (name, uniq[0]), list(shape), dt)).ap()

    def ps(es, name, shape, dt):
        uniq[0] += 1
        return es.enter_context(nc.psum_tensor("%s_%d" % (name, uniq[0]), list(shape), dt)).ap()

    ident_bf = sb(es_glob, "ident_bf", [128, 128], BF16)
    ident_f = sb(es_glob, "ident_f", [128, 128], F32)
    b_ident = Buf("ident")
    kb.op("pool", lambda e: e.memset(ident_f, 0.0), writes=[b_ident])
    kb.op("pool", lambda e: e.affine_select(out=ident_f, in_=ident_f, pattern=[[-1, 128]], compare_op=ALU.not_equal,
                                            fill=1.0, base=0, channel_multiplier=1), reads=[b_ident], writes=[b_ident])
    kb.op("dve", lambda e: e.tensor_copy(out=ident_bf, in_=ident_f), reads=[b_ident], writes=[b_ident])

    def phase_proj():
        with ExitStack() as es:
            TBMAX = 17 * 128
            g_bc = sb(es, "g_bc", [128, D], F32)
            b_g = Buf("g_bc")
            kb.dma("sp", lambda e: e.dma_start(out=g_bc, in_=norm_mix_g.partition_broadcast(128)), writes=[b_g])
            nT = sb(es, "nT", [128, 16, TBMAX], BF16)
            b_nT = Buf("nT")
            h_t = [sb(es, "h_t%d" % i, [128, D], F32) for i in range(2)]
            b_h = [Buf("h_t") for _ in range(2)]
            n_bf = [sb(es, "n_bf%d" % i, [128, D], BF16) for i in range(2)]
            b_n = [Buf("n_bf") for _ in range(2)]
            junk = sb(es, "junk", [128, D], BF16)
            b_junk = Buf("junk")
            stat = sb(es, "stat", [128, 4], F32)
            b_stat = Buf("stat")
            wst = [sb(es, "wst%d" % i, [128, 16, 512], F32) for i in range(2)]
            b_wst = [Buf("wst") for _ in range(2)]
            wbf = [sb(es, "wbf%d" % i, [128, 16, 512], BF16) for i in range(2)]
            b_wbf = [Buf("wbf") for _ in range(2)]
            ob = [sb(es, "ob%d" % i, [128, 512], BF16) for i in range(4)]
            b_ob = [Buf("ob") for _ in range(4)]
            obf = [sb(es, "obf%d" % i, [128, 64], F32) for i in range(2)]
            b_obf = [Buf("obf") for _ in range(2)]
            zt = sb(es, "zt", [128, HALO], BF16)
            b_zt = Buf("zt")
            ptr = ps(es, "ptr", [128, 16, 128], BF16)
            b_ptr = Buf("ptr")
            pmm = [ps(es, "pmm%d" % i, [128, 512], F32) for i in range(4)]
            b_pmm = [Buf("pmm") for _ in range(4)]

            kb.op("pool", lambda e: e.memset(zt, 0.0), writes=[b_zt])
            for r in range(CONV_DIM // 128):
                kb.dma("sp", lambda e, r=r: e.dma_start(out=XBCT[r * 128:(r + 1) * 128, 0:HALO], in_=zt),
                       reads=[b_zt], writes=[b_XBCT], accumulate=True)

            w_in_v = w_in.rearrange("(k p) n -> p k n", p=128)
            cnt = {"w": 0, "ob": 0, "pm": 0, "ev": 0, "obf": 0}

            def evac(dst, src, reads, writes):
                eng = "act" if cnt["ev"] % 2 == 0 else "dve"
                cnt["ev"] += 1
                if eng == "act":
                    kb.op("act", lambda e: e.activation(out=dst, in_=src, func=AF.Copy), reads=reads, writes=writes)
                else:
                    kb.op("dve", lambda e: e.tensor_copy(out=dst, in_=src), reads=reads, writes=writes)

            def do_block(ch0, nchk, colgroups):
                ntok = nchk * 128
                r0 = ch0 * 128
                for t in range(nchk):
                    i = t % 2
                    rows = slice(r0 + t * 128, r0 + (t + 1) * 128)
                    kb.dma("sp", lambda e, i=i, rows=rows: e.dma_start(out=h_t[i], in_=hin[rows, :]), writes=[b_h[i]])
                    kb.op("act", lambda e, i=i: e.activation(out=junk, in_=h_t[i], func=AF.Square, accum_out=stat[:, 0:1]),
                          reads=[b_h[i]], writes=[b_junk, b_stat])
                    kb.op("dve", lambda e: e.tensor_scalar(out=stat[:, 1:2], in0=stat[:, 0:1], scalar1=1.0 / D, scalar2=EPS,
                                                           op0=ALU.mult, op1=ALU.add), reads=[b_stat], writes=[b_stat])
                    kb.op("act", lambda e: e.activation(out=stat[:, 3:4], in_=stat[:, 1:2], func=AF.Sqrt),
                          reads=[b_stat], writes=[b_stat])
                    kb.op("dve", lambda e: e.reciprocal(out=stat[:, 2:3], in_=stat[:, 3:4]), reads=[b_stat], writes=[b_stat])
                    kb.op("dve", lambda e, i=i: e.scalar_tensor_tensor(out=n_bf[i], in0=h_t[i], scalar=stat[:, 2:3], in1=g_bc,
                                                                       op0=ALU.mult, op1=ALU.mult),
                          reads=[b_h[i], b_stat, b_g], writes=[b_n[i]])
                    for k in range(16):
                        kb.op("pe", lambda e, i=i, k=k: e.transpose(out=ptr[:, k, :], in_=n_bf[i][:, k * 128:(k + 1) * 128],
                                                                    identity=ident_bf),
                              reads=[b_n[i], b_ident], writes=[b_ptr])
                    evac(nT[:, :, t * 128:(t + 1) * 128], ptr, [b_ptr], [b_nT])
                for (kind, c0, ncol, dst_row0) in colgroups:
                    kb.maybe_epoch()
                    wi = cnt["w"] % 2
                    cnt["w"] += 1
                    kb.dma("sp", lambda e, wi=wi, c0=c0, ncol=ncol: e.dma_start(out=wst[wi][:, :, 0:ncol],
                                                                                 in_=w_in_v[:, :, c0:c0 + ncol]),
                           writes=[b_wst[wi]])
                    kb.op("pool", lambda e, wi=wi, ncol=ncol: e.tensor_copy(out=wbf[wi][:, :, 0:ncol], in_=wst[wi][:, :, 0:ncol]),
                          reads=[b_wst[wi]], writes=[b_wbf[wi]])
                    if kind == "tm":
                        for t in range(nchk):
                            pi = cnt["pm"] % 4
                            cnt["pm"] += 1
                            for k in range(16):
                                kb.op("pe", lambda e, pi=pi, k=k, t=t, wi=wi, ncol=ncol: e.matmul(
                                    pmm[pi][:, 0:ncol], lhsT=nT[:, k, t * 128:(t + 1) * 128], rhs=wbf[wi][:, k, 0:ncol],
                                    start=(k == 0), stop=(k == 15)), reads=[b_nT, b_wbf[wi]], writes=[b_pmm[pi]])
                            oi = cnt["ob"] % 4
                            cnt["ob"] += 1
                            evac(ob[oi][:, 0:ncol], pmm[pi][:, 0:ncol], [b_pmm[pi]], [b_ob[oi]])
                            rows = slice(r0 + t * 128, r0 + (t + 1) * 128)
                            kb.dma("sp", lambda e, oi=oi, rows=rows, c0=c0, ncol=ncol: e.dma_start(
                                out=PTM[rows, c0:c0 + ncol], in_=ob[oi][:, 0:ncol]),
                                reads=[b_ob[oi]], writes=[b_PTM], accumulate=True)
                    elif kind == "dt":
                        for t in range(nchk):
                            pi = cnt["pm"] % 4
                            cnt["pm"] += 1
                            for k in range(16):
                                kb.op("pe", lambda e, pi=pi, k=k, t=t, wi=wi: e.matmul(
                                    pmm[pi][:, 0:64], lhsT=nT[:, k, t * 128:(t + 1) * 128], rhs=wbf[wi][:, k, 0:64],
                                    start=(k == 0), stop=(k == 15)), reads=[b_nT, b_wbf[wi]], writes=[b_pmm[pi]])
                            oi = cnt["obf"] % 2
                            cnt["obf"] += 1
                            evac(obf[oi], pmm[pi][:, 0:64], [b_pmm[pi]], [b_obf[oi]])
                            rows = slice(r0 + t * 128, r0 + (t + 1) * 128)
                            kb.dma("sp", lambda e, oi=oi, rows=rows: e.dma_start(out=DTS[rows, :], in_=obf[oi]),
                                   reads=[b_obf[oi]], writes=[b_DTS], accumulate=True)
                    else:
                        for m in range(ncol // 128):
                            for tg in range(0, ntok, 512):
                                n = min(512, ntok - tg)
                                pi = cnt["pm"] % 4
                                cnt["pm"] += 1
                                for k in range(16):
                                    kb.op("pe", lambda e, pi=pi, k=k, m=m, tg=tg, n=n, wi=wi: e.matmul(
                                        pmm[pi][:, 0:n], lhsT=wbf[wi][:, k, m * 128:(m + 1) * 128], rhs=nT[:, k, tg:tg + n],
                                        start=(k == 0), stop=(k == 15)), reads=[b_nT, b_wbf[wi]], writes=[b_pmm[pi]])
                                oi = cnt["ob"] % 4
                                cnt["ob"] += 1
                                evac(ob[oi][:, 0:n], pmm[pi][:, 0:n], [b_pmm[pi]], [b_ob[oi]])
                                rr = dst_row0 + m * 128
                                if kind == "xbc":
                                    kb.dma("sp", lambda e, oi=oi, rr=rr, tg=tg, n=n: e.dma_start(
                                        out=XBCT[rr:rr + 128, HALO + r0 + tg:HALO + r0 + tg + n], in_=ob[oi][:, 0:n]),
                                        reads=[b_ob[oi]], writes=[b_XBCT], accumulate=True)
                                else:
                                    cc = r0 - ROW0 + tg
                                    kb.dma("sp", lambda e, oi=oi, rr=rr, cc=cc, n=n: e.dma_start(
                                        out=GT[rr:rr + 128, cc:cc + n], in_=ob[oi][:, 0:n]),
                                        reads=[b_ob[oi]], writes=[b_GT], accumulate=True)

            def groups(c_lo, c_hi, kind, dst_row0=0):
                return [(kind, c, min(512, c_hi - c), dst_row0 + (c - c_lo)) for c in range(c_lo, c_hi, 512)]

            pre_groups = (groups(C_K, C_G, "tm") + groups(C_XBC, C_DT, "xbc")
                          + [("dt", C_DT, 64, 0)])
            main_groups = (groups(0, PTM_W, "tm") + groups(C_XBC, C_DT, "xbc") + [("dt", C_DT, 64, 0)]
                           + groups(C_GR, NPROJ, "gt"))

            def blocks(c0, n):
                res, c = [], c0
                while n > 0:
                    m = min(n, 17 if n == 17 else 16)
                    res.append((c, m))
                    c += m
                    n -= m
                return res

            for (c, m) in blocks(0, cfg.pre):
                do_block(c, m, pre_groups)
            for (c, m) in blocks(cfg.pre, cfg.main):
                do_block(c, m, main_groups)
        kb.barrier()

    CO_UT, CO_SL, CO_ONE, CO_DQ, CO_DK, CO_DKZ, CO_G128 = 0, 128, 256, 384, 392, 400, 408

    def load_consts(es):
        cst = sb(es, "cst", [128, 512], F32)
        b_cst = Buf("cst")
        kb.dma("sp", lambda e: e.dma_start(out=cst, in_=cst_f[:, 0:512]), writes=[b_cst])
        return cst, b_cst

    def transpose_store(src_bf, b_src, nblk, ptr, b_ptr, oT, b_oT, dst_view, b_dst, col0):
        for r in range(0, nblk, 16):
            for k in range(16):
                kb.op("pe", lambda e, k=k, r=r: e.transpose(out=ptr[:, k, :], in_=src_bf[:, (r + k) * 128:(r + k + 1) * 128],
                                                            identity=ident_bf), reads=[b_src, b_ident], writes=[b_ptr])
            kb.op("act", lambda e, r=r: e.activation(out=oT[:, r:r + 16, :], in_=ptr, func=AF.Copy),
                  reads=[b_ptr], writes=[b_oT])
        kb.dma("sp", lambda e: e.dma_start(out=dst_view[:, :, col0:col0 + 128], in_=oT[:, 0:nblk, :]),
               reads=[b_oT], writes=[b_dst], accumulate=True)

    precast_state = {"done": False}

    def precast_units(es, ptile, b_ptile):
        gcol = sb(es, "gcol", [128, 32], F32); b_gcol = Buf()
        tmpg = sb(es, "tmpg", [32, 128], F32); b_tg = Buf()
        kb.dma("sp", lambda e: e.dma_start(out=tmpg, in_=ssm_norm_g.rearrange("o (kt p) -> (o kt) p", p=128)), writes=[b_tg])
        kb.op("pe", lambda e: e.transpose(out=ptile[:, 0:32], in_=tmpg, identity=ident_f[0:32, 0:32]),
              reads=[b_tg, b_ident], writes=[b_ptile])
        kb.op("dve", lambda e: e.tensor_copy(out=gcol, in_=ptile[:, 0:32]), reads=[b_ptile], writes=[b_gcol])
        stg = [sb(es, "stgw%d" % i, [128, D], F32) for i in range(3)]; b_stg = [Buf() for _ in range(3)]
        stb = [sb(es, "stbw%d" % i, [128, D], BF16) for i in range(3)]; b_stb = [Buf() for _ in range(3)]
        jobs = []
        for (src, dst, nblk, scale_g, b_dst) in ((w_ret_o, WRB, 32, False, b_WB), (w_ssm_o, WSB, 32, True, b_WB),
                                                 (w_out, WOB, 16, False, b_WB), (peer_w_q, WQB, 16, False, b_WB)):
            for r in range(nblk):
                jobs.append((src[r * 128:(r + 1) * 128, :], dst[r * 128:(r + 1) * 128, :], r if scale_g else None, b_dst))
        for r in range(NEX // 128):
            jobs.append((peer_u[r * 128:(r + 1) * 128, :], UVB[r * 128:(r + 1) * 128, 0:D], None, b_UVB))
            jobs.append((peer_v[r * 128:(r + 1) * 128, :], UVB[r * 128:(r + 1) * 128, D:2 * D], None, b_UVB))
        def ld(n):
            src_ap = jobs[n][0]
            i = n % 3
            kb.dma("sp", lambda e: e.dma_start(out=stg[i], in_=src_ap), writes=[b_stg[i]])

        def cast(n):
            i = n % 3
            gr = jobs[n][2]
            if gr is not None:
                kb.op("act", lambda e: e.activation(out=stb[i], in_=stg[i], func=AF.Copy, scale=gcol[:, gr:gr + 1]),
                      reads=[b_stg[i], b_gcol], writes=[b_stb[i]])
            else:
                kb.op("act", lambda e: e.activation(out=stb[i], in_=stg[i], func=AF.Copy), reads=[b_stg[i]], writes=[b_stb[i]])

        def st(n):
            i = n % 3
            dst_ap, b_dst = jobs[n][1], jobs[n][3]
            kb.dma("sp", lambda e: e.dma_start(out=dst_ap, in_=stb[i]), reads=[b_stb[i]], writes=[b_dst], accumulate=True)

        NJ = len(jobs)
        for n in range(NJ + 2):
            if n < NJ:
                ld(n)
            if 0 <= n - 1 < NJ:
                cast(n - 1)
            if 0 <= n - 2 < NJ:
                st(n - 2)
            yield n
        precast_state["done"] = True

    N_PRECAST = 96 + 2 * (NEX // 128) + 2

    def phase_ret():
        with ExitStack() as es:
            cst, b_cst = load_consts(es)
            UT = cst[:, CO_UT:CO_UT + 128]
            st_f = sb(es, "st_f", [128, RH, 2, 512], F32)
            st_b = sb(es, "st_b", [128, RH, 2, 512], BF16)
            b_stf = [Buf("stf") for _ in range(RH)]
            b_stb = [Buf("stb") for _ in range(RH)]
            for h in range(RH):
                kb.op("pool", lambda e, h=h: e.memset(st_f[:, h], 0.0), writes=[b_stf[h]])
                kb.op("pool", lambda e, h=h: e.memset(st_b[:, h], 0.0), writes=[b_stb[h]])
            k_in = sb(es, "k_in", [128, 2048], BF16); b_kin = Buf()
            q_in = sb(es, "q_in", [128, 2048], BF16); b_qin = Buf()
            v_in = sb(es, "v_in", [128, 4096], BF16); b_vin = Buf()
            g_in = sb(es, "g_in", [128, 4096], BF16); b_gin = Buf()
            cos_t = sb(es, "cos_t", [128, 128], F32); b_cos = Buf()
            sin_t = sb(es, "sin_t", [128, 128], F32); b_sin = Buf()
            tmp1 = sb(es, "tmp1", [128, 8, 128], F32); b_t1 = Buf()
            tmp2 = sb(es, "tmp2", [128, 8, 128], F32); b_t2 = Buf()
            kr = sb(es, "kr", [128, 8, 2, 128], F32); b_kr = Buf()
            kt_ = sb(es, "kt_", [128, 8, 256], BF16); b_kt = Buf()
            kz_ = sb(es, "kz_", [128, 8, 256], BF16); b_kz = Buf()
            qt_ = sb(es, "qt_", [128, 8, 256], BF16); b_qt = Buf()
            qT = sb(es, "qT", [128, 16, 128], BF16); b_qT = Buf()
            kT = sb(es, "kT", [128, 16, 128], BF16); b_kT = Buf()
            sTm = [sb(es, "sTm%d" % i, [128, 128], BF16) for i in range(2)]; b_sTm = [Buf(), Buf()]
            o_sb = sb(es, "o_sb", [128, RH, 512], F32); b_osb = [Buf() for _ in range(RH)]
            sg = sb(es, "sg", [128, RH, 512], BF16); b_sg = Buf()
            og = sb(es, "og", [128, RH * 512], BF16); b_og = Buf()
            oT = sb(es, "oT", [128, 32, 128], BF16); b_oT = Buf()
            junk = sb(es, "junkr", [128, 512], BF16); b_junk = Buf()
            ssq = sb(es, "ssq", [128, 32], F32); b_ssq = Buf()
            ptr = ps(es, "ptr_r", [128, 16, 128], BF16); b_ptr = Buf()
            ps_s = ps(es, "ps_s", [128, 512], F32); b_pss = Buf()
            ps_o = [ps(es, "ps_o%d" % i, [128, 512], F32) for i in range(2)]; b_pso = [Buf(), Buf()]
            ps_u = [ps(es, "ps_u%d" % i, [128, 512], F32) for i in range(2)]; b_psu = [Buf(), Buf()]
            OGT_v = OGT.rearrange("(kt p) n -> p kt n", p=128)
            pc_gen = precast_units(es, ps_s, b_pss)
            pc_per_chunk = -(-N_PRECAST // cfg.nch)

            def rope(src, b_src, dst_list):
                v4 = src.rearrange("p (h f two) -> p h f two", h=8, two=2)
                t1, t2 = v4[:, :, :, 0], v4[:, :, :, 1]
                cb_, sb_ = bc_mid(cos_t, 8), bc_mid(sin_t, 8)
                kb.op("dve", lambda e: e.tensor_tensor(out=tmp1, in0=t1, in1=cb_, op=ALU.mult), reads=[b_src, b_cos], writes=[b_t1])
                kb.op("dve", lambda e: e.tensor_tensor(out=tmp2, in0=t2, in1=sb_, op=ALU.mult), reads=[b_src, b_sin], writes=[b_t2])
                kb.op("dve", lambda e: e.tensor_tensor(out=kr[:, :, 0, :], in0=tmp1, in1=tmp2, op=ALU.subtract),
                      reads=[b_t1, b_t2], writes=[b_kr])
                kb.op("dve", lambda e: e.tensor_tensor(out=tmp1, in0=t1, in1=sb_, op=ALU.mult), reads=[b_src, b_sin], writes=[b_t1])
                kb.op("dve", lambda e: e.tensor_tensor(out=tmp2, in0=t2, in1=cb_, op=ALU.mult), reads=[b_src, b_cos], writes=[b_t2])
                kb.op("dve", lambda e: e.tensor_tensor(out=kr[:, :, 1, :], in0=tmp1, in1=tmp2, op=ALU.add),
                      reads=[b_t1, b_t2, b_kr], writes=[b_kr])
                krv = kr.rearrange("p h two f -> p h (two f)")
                for (dst, b_dst, co) in dst_list:
                    kb.op("dve", lambda e, dst=dst, co=co: e.tensor_tensor(out=dst, in0=krv, in1=bc_last(cst[:, co:co + 8], 256),
                                                                           op=ALU.mult), reads=[b_kr, b_cst], writes=[b_dst])

            for ch in range(cfg.nch):
                kb.maybe_epoch()
                for _ in range(pc_per_chunk):
                    next(pc_gen, None)
                main = ch >= cfg.pre
                r0 = ch * 128
                rows = slice(r0, r0 + 128)
                kb.dma("sp", lambda e, rows=rows: e.dma_start(out=k_in, in_=PTM[rows, C_K:C_K + 2048]), reads=[b_PTM], writes=[b_kin])
                kb.dma("sp", lambda e, rows=rows: e.dma_start(out=v_in, in_=PTM[rows, C_V:C_V + 4096]), reads=[b_PTM], writes=[b_vin])
                kb.dma("sp", lambda e, rows=rows: e.dma_start(out=cos_t, in_=ropec[rows, :]), writes=[b_cos])
                kb.dma("sp", lambda e, rows=rows: e.dma_start(out=sin_t, in_=ropes[rows, :]), writes=[b_sin])
                if main:
                    kb.dma("sp", lambda e, rows=rows: e.dma_start(out=q_in, in_=PTM[rows, C_Q:C_Q + 2048]), reads=[b_PTM], writes=[b_qin])
                    kb.dma("sp", lambda e, rows=rows: e.dma_start(out=g_in, in_=PTM[rows, C_G:C_G + 4096]), reads=[b_PTM], writes=[b_gin])
                    rope(k_in, b_kin, [(kt_, b_kt, CO_DK), (kz_, b_kz, CO_DKZ)])
                    rope(q_in, b_qin, [(qt_, b_qt, CO_DQ)])
                    kb.op("act", lambda e: e.activation(out=sg.rearrange("p h f -> p (h f)"), in_=g_in, func=AF.Silu),
                          reads=[b_gin], writes=[b_sg])
                    for (src, b_src, dstT, b_dstT) in ((qt_, b_qt, qT, b_qT), (kt_, b_kt, kT, b_kT)):
                        s2 = src.rearrange("p h f -> p (h f)")
                        for k in range(16):
                            kb.op("pe", lambda e, k=k, s2=s2: e.transpose(out=ptr[:, k, :], in_=s2[:, k * 128:(k + 1) * 128],
                                                                          identity=ident_bf), reads=[b_src, b_ident], writes=[b_ptr])
                        kb.op("act", lambda e, dstT=dstT: e.activation(out=dstT, in_=ptr, func=AF.Copy), reads=[b_ptr], writes=[b_dstT])
                else:
                    rope(k_in, b_kin, [(kz_, b_kz, CO_DKZ)])
                for h in range(RH):
                    vh = v_in[:, h * 512:(h + 1) * 512]
                    if main:
                        pi = h % 2
                        for c in range(2):
                            kb.op("pe", lambda e, h=h, c=c: e.matmul(ps_s[:, 0:128], lhsT=kT[:, 2 * h + c, :], rhs=qT[:, 2 * h + c, :],
                                                                     start=(c == 0), stop=(c == 1)), reads=[b_kT, b_qT], writes=[b_pss])
                        kb.op("dve", lambda e, pi=pi: e.tensor_tensor(out=sTm[pi], in0=ps_s[:, 0:128], in1=UT, op=ALU.mult),
                              reads=[b_pss, b_cst], writes=[b_sTm[pi]])
                        kb.op("pe", lambda e, pi=pi, vh=vh: e.matmul(ps_o[pi], lhsT=sTm[pi], rhs=vh, start=True, stop=False),
                              reads=[b_sTm[pi], b_vin], writes=[b_pso[pi]])
                        for c in range(2):
                            kb.op("pe", lambda e, pi=pi, h=h, c=c: e.matmul(ps_o[pi], lhsT=qT[:, 2 * h + c, :], rhs=st_b[:, h, c, :],
                                                                            start=False, stop=(c == 1)),
                                  reads=[b_qT, b_stb[h]], writes=[b_pso[pi]])
                        kb.op("dve", lambda e, pi=pi, h=h: e.tensor_copy(out=o_sb[:, h, :], in_=ps_o[pi]), reads=[b_pso[pi]], writes=[b_osb[h]])
                        kb.op("act", lambda e, h=h: e.activation(out=junk, in_=o_sb[:, h, :], func=AF.Square,
                                                                 accum_out=ssq[:, h:h + 1]), reads=[b_osb[h]], writes=[b_junk, b_ssq])
                    for c in range(2):
                        kb.op("pe", lambda e, h=h, c=c, vh=vh: e.matmul(ps_u[c], lhsT=kz_[:, h, c * 128:(c + 1) * 128], rhs=vh,
                                                                        start=True, stop=True), reads=[b_kz, b_vin], writes=[b_psu[c]])
                        kb.op("dve", lambda e, h=h, c=c: e.scalar_tensor_tensor(out=st_f[:, h, c, :], in0=st_f[:, h, c, :],
                                                                                 scalar=cst[:, CO_G128 + h:CO_G128 + h + 1],
                                                                                 in1=ps_u[c], op0=ALU.mult, op1=ALU.add),
                              reads=[b_psu[c], b_cst, b_stf[h]], writes=[b_stf[h]])
                    kb.op("act", lambda e, h=h: e.activation(out=st_b[:, h], in_=st_f[:, h], func=AF.Copy), reads=[b_stf[h]], writes=[b_stb[h]])
                if main:
                    kb.op("dve", lambda e: e.tensor_scalar(out=ssq[:, 8:16], in0=ssq[:, 0:8], scalar1=1.0 / RDV, scalar2=EPS,
                                                           op0=ALU.mult, op1=ALU.add), reads=[b_ssq], writes=[b_ssq])
                    kb.op("act", lambda e: e.activation(out=ssq[:, 16:24], in_=ssq[:, 8:16], func=AF.Sqrt), reads=[b_ssq], writes=[b_ssq])
                    kb.op("dve", lambda e: e.reciprocal(out=ssq[:, 24:32], in_=ssq[:, 16:24]), reads=[b_ssq], writes=[b_ssq])
                    kb.op("dve", lambda e: e.tensor_tensor(out=o_sb, in0=o_sb, in1=bc_last(ssq[:, 24:32], 512), op=ALU.mult),
                          reads=b_osb + [b_ssq], writes=b_osb)
                    kb.op("dve", lambda e: e.tensor_tensor(out=og.rearrange("p (h f) -> p h f", h=RH), in0=o_sb, in1=sg, op=ALU.mult),
                          reads=b_osb + [b_sg], writes=[b_og])
                    transpose_store(og, b_og, 32, ptr, b_ptr, oT, b_oT, OGT_v, b_OGT, r0 - ROW0)
            for _ in pc_gen:
                pass
        kb.barrier()

    if "proj" in cfg.phases:
        phase_proj()
    def phase_ssd():
        with ExitStack() as es:
            cst, b_cst = load_consts(es)
            UT = cst[:, CO_UT:CO_UT + 128]
            SLm = cst[:, CO_SL:CO_SL + 128]
            ONES = cst[:, CO_ONE:CO_ONE + 128]
            diag = sb(es, "diag", [128, 48, 4, 128], BF16); b_diag = Buf()
            cwT = sb(es, "cwT", [128, 48, 8], F32); b_cwT = Buf()
            cb_bc = sb(es, "cb_bc", [128, 5120], BF16); b_cb = Buf()
            par = sb(es, "par", [128, 4, 64], F32); b_par = Buf()
            sst_f = sb(es, "sst_f", [128, SH, SP_], F32)
            sst_b = sb(es, "sst_b", [128, SH * SP_], BF16)
            b_sf = [Buf() for _ in range(SG)]; b_sbb = [Buf() for _ in range(SG)]
            ptr = ps(es, "ptr_s", [128, 16, 128], BF16); b_ptr = Buf()
            pcA = ps(es, "pcA", [128, 512], F32); b_pcA = Buf()
            pcB = ps(es, "pcB", [128, 512], F32); b_pcB = Buf()
            psm = ps(es, "psm", [128, 512], F32); b_psm = Buf()
            pseg = ps(es, "pseg", [128, 1024], F32); b_pseg = Buf()
            pst = ps(es, "pst", [128, 512], F32); b_pst = Buf()
            YST_v = YST.rearrange("(kt p) n -> p kt n", p=128)
            XB_v = XBCT.rearrange("(t p) c -> p t c", p=128)

            with ExitStack() as es2:
                cw5 = sb(es2, "cw5", [8, CONV_DIM], F32); b_cw5 = Buf()
                cbs = sb(es2, "cbs", [128, 5120], F32); b_cbs = Buf()
                kb.dma("sp", lambda e: e.dma_start(out=cw5[0:4, :], in_=conv_w), writes=[b_cw5])
                kb.dma("sp", lambda e: e.dma_start(out=cw5[4:5, :], in_=conv_b), writes=[b_cw5], accumulate=True)
                kb.dma("sp", lambda e: e.dma_start(out=cbs, in_=conv_b[0:1, 0:5120].partition_broadcast(128)), writes=[b_cbs])
                kb.op("dve", lambda e: e.tensor_copy(out=cb_bc, in_=cbs), reads=[b_cbs], writes=[b_cb])
                for t in range(48):
                    kb.op("pe", lambda e, t=t: e.transpose(out=psm[:, t * 8:t * 8 + 5], in_=cw5[0:5, t * 128:(t + 1) * 128],
                                                           identity=ident_f[0:5, 0:5]), reads=[b_cw5, b_ident], writes=[b_psm])
                kb.op("dve", lambda e: e.tensor_copy(out=cwT[:, :, 0:5], in_=psm[:, 0:384].rearrange("p (t e) -> p t e", e=8)[:, :, 0:5]),
                      reads=[b_psm], writes=[b_cwT])
                for t in range(48):
                    for w in range(4):
                        kb.op("dve", lambda e, t=t, w=w: e.tensor_scalar(out=diag[:, t, w, :], in0=ident_f, scalar1=cwT[:, t, w:w + 1],
                                                                         scalar2=None, op0=ALU.mult),
                              reads=[b_ident, b_cwT], writes=[b_diag])
                kb.dma("sp", lambda e: e.dma_start(out=par[:, 0, :], in_=dt_bias.partition_broadcast(128)), writes=[b_par])
                kb.dma("sp", lambda e: e.dma_start(out=par[:, 3, :], in_=a_log.partition_broadcast(128)), writes=[b_par], accumulate=True)
                kb.dma("sp", lambda e: e.dma_start(out=par[:, 2, :], in_=d_skip.partition_broadcast(128)), writes=[b_par], accumulate=True)
                kb.op("act", lambda e: e.activation(out=par[:, 1, :], in_=par[:, 3, :], func=AF.Exp), reads=[b_par], writes=[b_par])
                kb.op("dve", lambda e: e.tensor_scalar(out=par[:, 1, :], in0=par[:, 1, :], scalar1=-1.0, scalar2=None, op0=ALU.mult),
                      reads=[b_par], writes=[b_par])
                for g in range(SG):
                    kb.op("pool", lambda e, g=g: e.memset(sst_f[:, g * 8:(g + 1) * 8, :], 0.0), writes=[b_sf[g]])
                    kb.op("pool", lambda e, g=g: e.memset(sst_b[:, g * 512:(g + 1) * 512], 0.0), writes=[b_sbb[g]])
                kb.barrier()
            xw = sb(es, "xw", [128, 48, 131], BF16); b_xw = Buf()
            z_in = sb(es, "z_in", [128, 4096], BF16); b_zin = Buf()
            xs = sb(es, "xs", [128, SH, SP_], F32); b_xs = [Buf() for _ in range(SG)]
            xdt = sb(es, "xdt", [128, SH, SP_], BF16); b_xdt = Buf()
            decx = sb(es, "decx", [128, SH * SP_], BF16); b_decx = [Buf() for _ in range(SG)]
            Bt = sb(es, "Bt", [128, 1024], BF16); b_Bt = Buf()
            BCT = sb(es, "BCT", [128, 16, 128], BF16); b_BCT = Buf()
            segL = [sb(es, "segL%d" % i, [128, 8, 128], F32) for i in range(2)]; b_segL = [Buf(), Buf()]
            Lg = [sb(es, "Lg%d" % i, [128, 8, 128], F32) for i in range(2)]; b_Lg = [Buf(), Buf()]
            MT = [sb(es, "MT%d" % i, [128, 8, 128], BF16) for i in range(2)]; b_MT = [Buf(), Buf()]
            cbm = sb(es, "cbm", [128, 128], F32); b_cbm = Buf()
            tmpc = [sb(es, "tmpc%d" % i, [128, 512], F32) for i in range(2)]; b_tmpc = [Buf(), Buf()]
            sz = sb(es, "sz", [128, 4096], BF16); b_sz = Buf()
            ys = sb(es, "ys", [128, 4096], BF16); b_ys = Buf()
            oT = sb(es, "oT_s", [128, 32, 128], BF16); b_oT = Buf()
            sm = sb(es, "sm", [128, 8, 64], F32); b_sm = Buf()
            sm2 = sb(es, "sm2", [128, 128], F32); b_sm2 = Buf()
            dtr = sb(es, "dtr", [128, 64], F32); b_dtr = Buf()
            msk = sb(es, "msk", [128, 1], F32); b_msk = Buf()
            junk = sb(es, "junks", [128, 512], BF16); b_junk = Buf()
            ssq = sb(es, "ssq2", [128, 32], F32); b_ssq = Buf()
            xs2 = xs.rearrange("p h f -> p (h f)")
            xdt2 = xdt.rearrange("p h f -> p (h f)")
            sst_f2 = sst_f.rearrange("p h f -> p (h f)")

            for ch in range(cfg.nch):
                kb.maybe_epoch()
                main = ch >= cfg.pre
                r0 = ch * 128
                rows = slice(r0, r0 + 128)
                c0 = HALO + r0 - 3
                T = 48 if main else 40
                kb.dma("sp", lambda e, T=T, c0=c0: e.dma_start(out=xw[:, 0:T, :], in_=XB_v[:, 0:T, c0:c0 + 131]),
                       reads=[b_XBCT], writes=[b_xw])
                kb.dma("sp", lambda e, rows=rows: e.dma_start(out=dtr, in_=DTS[rows, :]), reads=[b_DTS], writes=[b_dtr])
                kb.dma("sp", lambda e, rows=rows: e.dma_start(out=msk, in_=rowmask[rows, :]), writes=[b_msk])
                if main:
                    kb.dma("sp", lambda e, rows=rows: e.dma_start(out=z_in, in_=PTM[rows, C_Z:C_Z + 4096]), reads=[b_PTM], writes=[b_zin])
                S = lambda i: sm[:, i, :]
                kb.op("dve", lambda e: e.tensor_tensor(out=S(0), in0=dtr, in1=par[:, 0, :], op=ALU.add), reads=[b_dtr, b_par], writes=[b_sm])
                kb.op("act", lambda e: e.activation(out=S(1), in_=S(0), func=AF.Abs), reads=[b_sm], writes=[b_sm])
                kb.op("act", lambda e: e.activation(out=S(2), in_=S(1), func=AF.Exp, scale=-1.0), reads=[b_sm], writes=[b_sm])
                kb.op("act", lambda e: e.activation(out=S(3), in_=S(2), func=AF.Ln, bias=ONES[:, 0:1]), reads=[b_sm, b_cst], writes=[b_sm])
                kb.op("dve", lambda e: e.scalar_tensor_tensor(out=S(4), in0=S(0), scalar=0.0, in1=S(3), op0=ALU.max, op1=ALU.add),
                      reads=[b_sm], writes=[b_sm])
                kb.op("dve", lambda e: e.tensor_scalar(out=S(5), in0=S(4), scalar1=msk[:, 0:1], scalar2=None, op0=ALU.mult),
                      reads=[b_sm, b_msk], writes=[b_sm])
                kb.op("dve", lambda e: e.tensor_tensor(out=S(6), in0=S(4), in1=par[:, 1, :], op=ALU.mult), reads=[b_sm, b_par], writes=[b_sm])
                a_ap = S(6)
                for bnk in range(10):
                    pc, b_pc = (pcA, b_pcA) if bnk % 2 == 0 else (pcB, b_pcB)
                    for tt in range(4):
                        t = bnk * 4 + tt
                        for w in range(4):
                            kb.op("pe", lambda e, pc=pc, t=t, tt=tt, w=w: e.matmul(pc[:, tt * 128:(tt + 1) * 128], lhsT=xw[:, t, w:w + 128],
                                                                                   rhs=diag[:, t, w, :], start=(w == 0), stop=(w == 3)),
                                  reads=[b_xw, b_diag], writes=[b_pc])
                    ti = bnk % 2
                    kb.op("dve", lambda e, pc=pc, ti=ti, bnk=bnk: e.tensor_tensor(out=tmpc[ti], in0=pc, in1=cb_bc[:, bnk * 512:(bnk + 1) * 512],
                                                                                  op=ALU.add), reads=[b_pc, b_cb], writes=[b_tmpc[ti]])
                    if bnk < 8:
                        kb.op("act", lambda e, ti=ti, bnk=bnk: e.activation(out=xs2[:, bnk * 512:(bnk + 1) * 512], in_=tmpc[ti], func=AF.Silu),
                              reads=[b_tmpc[ti]], writes=[b_xs[bnk]])
                    else:
                        kb.op("act", lambda e, ti=ti, bnk=bnk: e.activation(out=Bt[:, (bnk - 8) * 512:(bnk - 7) * 512], in_=tmpc[ti], func=AF.Silu),
                              reads=[b_tmpc[ti]], writes=[b_Bt])
                for bnk in range(4 if main else 2):
                    pc, b_pc = (pcA, b_pcA) if bnk % 2 == 0 else (pcB, b_pcB)
                    for tt in range(4):
                        t = 32 + bnk * 4 + tt
                        for w in range(4):
                            kb.op("pe", lambda e, pc=pc, t=t, tt=tt, w=w: e.matmul(pc[:, tt * 128:(tt + 1) * 128], lhsT=diag[:, t, w, :],
                                                                                   rhs=xw[:, t, w:w + 128], start=(w == 0), stop=(w == 3)),
                                  reads=[b_xw, b_diag], writes=[b_pc])
                    for tt in range(4):
                        t = 32 + bnk * 4 + tt
                        kb.op("act", lambda e, pc=pc, t=t, tt=tt: e.activation(out=BCT[:, t - 32, :], in_=pc[:, tt * 128:(tt + 1) * 128],
                                                                               func=AF.Silu, bias=cwT[:, t, 4:5]),
                              reads=[b_pc, b_cwT], writes=[b_BCT])
                kb.op("dve", lambda e: e.tensor_tensor(out=xdt, in0=xs, in1=bc_last(S(5), 64), op=ALU.mult), reads=b_xs + [b_sm], writes=[b_xdt])
                if main:
                    kb.op("dve", lambda e: e.tensor_tensor(out=xs, in0=xs, in1=bc_last(par[:, 2, :], 64), op=ALU.mult),
                          reads=b_xs + [b_par], writes=b_xs)
                    kb.op("act", lambda e: e.activation(out=sz, in_=z_in, func=AF.Silu), reads=[b_zin], writes=[b_sz])
                kb.op("pe", lambda e: e.matmul(psm[:, 0:64], lhsT=UT, rhs=a_ap, start=True, stop=True), reads=[b_cst, b_sm], writes=[b_psm])
                kb.op("pe", lambda e: e.matmul(psm[:, 64:128], lhsT=ONES, rhs=a_ap, start=True, stop=True), reads=[b_cst, b_sm], writes=[b_psm])
                kb.op("act", lambda e: e.activation(out=sm2, in_=psm[:, 0:128], func=AF.Exp), reads=[b_psm], writes=[b_sm2])
                eacs, cdec = sm2[:, 0:64], sm2[:, 64:128]
                for g in range(SG):
                    i2 = g % 2
                    hs = slice(g * 8, (g + 1) * 8)
                    cs = slice(g * 512, (g + 1) * 512)
                    kb.op("pool", lambda e, i2=i2, hs=hs: e.tensor_tensor(out=segL[i2], in0=bc_mid(SLm, 8), in1=bc_last(a_ap[:, hs], 128),
                                                                          op=ALU.mult), reads=[b_cst, b_sm], writes=[b_segL[i2]])
                    for hh in range(8):
                        kb.op("pe", lambda e, i2=i2, hh=hh: e.matmul(pseg[:, hh * 128:(hh + 1) * 128], lhsT=segL[i2][:, hh, :], rhs=UT,
                                                                     start=True, stop=True), reads=[b_segL[i2], b_cst], writes=[b_pseg])
                    Lg2 = Lg[i2].rearrange("p h i -> p (h i)")
                    for hf in range(2):
                        kb.op("act", lambda e, Lg2=Lg2, hf=hf: e.activation(out=Lg2[:, hf * 512:(hf + 1) * 512],
                                                                            in_=pseg[:, hf * 512:(hf + 1) * 512], func=AF.Exp),
                              reads=[b_pseg], writes=[b_Lg[i2]])
                    dec_g = Lg[i2][:, :, 127]
                    kb.op("dve", lambda e, i2=i2, hs=hs, dec_g=dec_g: e.tensor_tensor(out=decx.rearrange("p (h f) -> p h f", f=64)[:, hs, :],
                                                                                      in0=xdt[:, hs, :],
                                                                                      in1=dec_g.unsqueeze(2).to_broadcast([128, 8, 64]),
                                                                                      op=ALU.mult),
                          reads=[b_xdt, b_Lg[i2]], writes=[b_decx[g]])
                    if main:
                        kb.op("pe", lambda e, g=g: e.matmul(psm[:, 128:256], lhsT=BCT[:, g, :], rhs=BCT[:, 8 + g, :], start=True, stop=True),
                              reads=[b_BCT], writes=[b_psm])
                        kb.op("dve", lambda e: e.tensor_tensor(out=cbm, in0=psm[:, 128:256], in1=UT, op=ALU.mult),
                              reads=[b_psm, b_cst], writes=[b_cbm])
                        kb.op("pool", lambda e, i2=i2: e.tensor_tensor(out=MT[i2], in0=Lg[i2], in1=bc_mid(cbm, 8), op=ALU.mult),
                              reads=[b_Lg[i2], b_cbm], writes=[b_MT[i2]])
                        for hh in range(8):
                            kb.op("pe", lambda e, i2=i2, hh=hh, g=g: e.matmul(pcA[:, hh * 64:(hh + 1) * 64], lhsT=MT[i2][:, hh, :],
                                                                              rhs=xdt[:, g * 8 + hh, :], start=True, stop=True),
                                  reads=[b_MT[i2], b_xdt], writes=[b_pcA])
                        kb.op("pe", lambda e, g=g, cs=cs: e.matmul(pcB, lhsT=BCT[:, 8 + g, :], rhs=sst_b[:, cs], start=True, stop=True),
                              reads=[b_BCT, b_sbb[g]], writes=[b_pcB])
                        kb.op("dve", lambda e, hs=hs: e.tensor_tensor(out=tmpc[0].rearrange("p (h f) -> p h f", f=64),
                                                                      in0=pcB.rearrange("p (h f) -> p h f", f=64),
                                                                      in1=bc_last(eacs[:, hs], 64), op=ALU.mult),
                              reads=[b_pcB, b_sm2], writes=[b_tmpc[0]])
                        kb.op("dve", lambda e: e.tensor_tensor(out=tmpc[1], in0=tmpc[0], in1=pcA, op=ALU.add),
                              reads=[b_tmpc[0], b_pcA], writes=[b_tmpc[1]])
                        kb.op("dve", lambda e, cs=cs: e.tensor_tensor(out=xs2[:, cs], in0=xs2[:, cs], in1=tmpc[1], op=ALU.add),
                              reads=[b_xs[g], b_tmpc[1]], writes=[b_xs[g]])
                    kb.op("pe", lambda e, g=g, cs=cs: e.matmul(pst, lhsT=Bt[:, g * 128:(g + 1) * 128], rhs=decx[:, cs], start=True, stop=True),
                          reads=[b_Bt, b_decx[g]], writes=[b_pst])
                    kb.op("dve", lambda e, hs=hs: e.tensor_tensor(out=sst_f[:, hs, :], in0=sst_f[:, hs, :], in1=bc_last(cdec[:, hs], 64),
                                                                  op=ALU.mult), reads=[b_sf[g], b_sm2], writes=[b_sf[g]])
                    kb.op("dve", lambda e, cs=cs: e.tensor_tensor(out=sst_f2[:, cs], in0=sst_f2[:, cs], in1=pst, op=ALU.add),
                          reads=[b_sf[g], b_pst], writes=[b_sf[g]])
                    kb.op("act", lambda e, cs=cs: e.activation(out=sst_b[:, cs], in_=sst_f2[:, cs], func=AF.Copy),
                          reads=[b_sf[g]], writes=[b_sbb[g]])
                if main:
                    kb.op("dve", lambda e: e.tensor_tensor(out=xs2, in0=xs2, in1=sz, op=ALU.mult), reads=b_xs + [b_sz], writes=b_xs)
                    for g in range(SG):
                        kb.op("act", lambda e, g=g: e.activation(out=junk, in_=xs2[:, g * 512:(g + 1) * 512], func=AF.Square,
                                                                 accum_out=ssq[:, g:g + 1]), reads=[b_xs[g]], writes=[b_junk, b_ssq])
                    kb.op("dve", lambda e: e.tensor_scalar(out=ssq[:, 8:16], in0=ssq[:, 0:8], scalar1=1.0 / 512, scalar2=EPS,
                                                           op0=ALU.mult, op1=ALU.add), reads=[b_ssq], writes=[b_ssq])
                    kb.op("act", lambda e: e.activation(out=ssq[:, 16:24], in_=ssq[:, 8:16], func=AF.Sqrt), reads=[b_ssq], writes=[b_ssq])
                    kb.op("dve", lambda e: e.reciprocal(out=ssq[:, 24:32], in_=ssq[:, 16:24]), reads=[b_ssq], writes=[b_ssq])
                    kb.op("dve", lambda e: e.tensor_tensor(out=ys.rearrange("p (g f) -> p g f", f=512),
                                                           in0=xs2.rearrange("p (g f) -> p g f", f=512),
                                                           in1=bc_last(ssq[:, 24:32], 512), op=ALU.mult),
                          reads=b_xs + [b_ssq], writes=[b_ys])
                    transpose_store(ys, b_ys, 32, ptr, b_ptr, oT, b_oT, YST_v, b_YST, r0 - ROW0)
        kb.barrier()

    if "ret" in cfg.phases:
        phase_ret()
    def phase_out():
        with ExitStack() as es:
            TBC = 3
            TBM = TBC * 128
            g_bc = sb(es, "gf_bc", [128, D], F32); b_g = Buf()
            kb.dma("sp", lambda e: e.dma_start(out=g_bc, in_=norm_ffn_g.partition_broadcast(128)), writes=[b_g])
            skT = sb(es, "skT", [128, 16, 128], F32); b_skT = Buf()
            ptr = ps(es, "ptr_o", [128, 16, 128], BF16); b_ptr = Buf()
            pA = ps(es, "pA", [128, 512], F32); b_pA = Buf()
            pB = ps(es, "pB", [128, 512], F32); b_pB = Buf()
            pC = [ps(es, "pC%d" % i, [128, 512], F32) for i in range(2)]; b_pC = [Buf(), Buf()]
            pD = ps(es, "pD", [128, 512], F32); b_pD = Buf()

            with ExitStack() as es2:
                skr = sb(es2, "skr", [128, 16, 128], F32); b_skr = Buf()
                kb.dma("sp", lambda e: e.dma_start(out=skr, in_=peer_sub_keys.rearrange("(j k) d -> k j d", k=128)), writes=[b_skr])
                for j in range(16):
                    kb.op("pe", lambda e, j=j: e.transpose(out=pC[j % 2][:, 0:128], in_=skr[:, j, :], identity=ident_f),
                          reads=[b_skr, b_ident], writes=[b_pC[j % 2]])
                    kb.op("dve", lambda e, j=j: e.tensor_copy(out=skT[:, j, :], in_=pC[j % 2][:, 0:128]), reads=[b_pC[j % 2]], writes=[b_skT])
                if not precast_state["done"]:
                    for _ in precast_units(es2, pA, b_pA):
                        pass
                kb.barrier()

            ogT = sb(es, "ogT", [128, 32, TBM], BF16); b_ogT = Buf()
            ysT = sb(es, "ysT", [128, 32, TBM], BF16); b_ysT = Buf()
            mT = sb(es, "mT", [128, 16, TBM], BF16); b_mT = Buf()
            xT = sb(es, "xT", [128, 16, TBM], BF16); b_xT = Buf()
            hb = sb(es, "hb", [128, TBC, D], F32); b_hb = [Buf() for _ in range(TBC)]
            NWB = 4
            wbf = [sb(es, "wbf_o%d" % i, [128, 4096], BF16) for i in range(NWB)]; b_wbf = [Buf() for _ in range(NWB)]
            grt = [sb(es, "grt%d" % i, [128, 2, TBM], BF16) for i in range(2)]; b_grt = [Buf(), Buf()]
            sgt = [sb(es, "sgt%d" % i, [128, 2, TBM], F32) for i in range(2)]; b_sgt = [Buf(), Buf()]
            t1 = sb(es, "t1o", [128, TBM], F32); b_t1 = Buf()
            t2 = sb(es, "t2o", [128, TBM], F32); b_t2 = Buf()
            n_bf = sb(es, "n_bf_o", [128, D], BF16); b_n = Buf()
            junk = sb(es, "junk_o", [128, D], BF16); b_junk = Buf()
            stat = sb(es, "stat_o", [128, 4], F32); b_stat = Buf()
            qpT = sb(es, "qpT", [128, TBM], F32); b_qpT = Buf()
            sct = sb(es, "sct", [128, TBC, 128], F32); b_sct = Buf()
            OGT_v = OGT.rearrange("(kt p) n -> p kt n", p=128)
            YST_v = YST.rearrange("(kt p) n -> p kt n", p=128)
            wr_v = WRB.rearrange("(kt p) c -> p kt c", p=128)
            ws_v = WSB.rearrange("(kt p) c -> p kt c", p=128)
            wo_v = WOB.rearrange("(kt p) c -> p kt c", p=128)
            wq_v = WQB.rearrange("(kt p) c -> p kt c", p=128)
            SC_v = SC.rearrange("(t p) c -> p t c", p=128)
            cnt = {"w": 0, "g": 0, "pc": 0}

            def load_w(view, kts, c0, ncol, scale_g=False):
                wi = cnt["w"] % NWB
                cnt["w"] += 1
                dsb = wbf[wi][:, 0:kts * ncol].rearrange("p (k c) -> p k c", c=ncol)
                kb.dma("sp", lambda e: e.dma_start(out=dsb, in_=view[:, :, c0:c0 + ncol]), reads=[b_WB], writes=[b_wbf[wi]])
                return dsb, b_wbf[wi]

            ch = 0
            while ch < cfg.main:
                nsub = min(TBC, cfg.main - ch)
                ntok = nsub * 128
                c0 = ch * 128
                kb.dma("sp", lambda e, c0=c0, ntok=ntok: e.dma_start(out=ogT[:, :, 0:ntok], in_=OGT_v[:, :, c0:c0 + ntok]),
                       reads=[b_OGT], writes=[b_ogT])
                kb.dma("sp", lambda e, c0=c0, ntok=ntok: e.dma_start(out=ysT[:, :, 0:ntok], in_=YST_v[:, :, c0:c0 + ntok]),
                       reads=[b_YST], writes=[b_ysT])
                for t in range(nsub):
                    rows = slice(ROW0 + c0 + t * 128, ROW0 + c0 + (t + 1) * 128)
                    kb.dma("sp", lambda e, t=t, rows=rows: e.dma_start(out=hb[:, t, :], in_=hin[rows, :]), writes=[b_hb[t]])
                for m in range(16):
                    kb.maybe_epoch()
                    wr, b_wr = load_w(wr_v, 32, m * 128, 128)
                    for kt in range(32):
                        kb.op("pe", lambda e, kt=kt, wr=wr, ntok=ntok: e.matmul(pA[:, 0:ntok], lhsT=wr[:, kt, :], rhs=ogT[:, kt, 0:ntok],
                                                                                start=(kt == 0), stop=(kt == 31)),
                              reads=[b_wr, b_ogT], writes=[b_pA])
                    ws, b_ws = load_w(ws_v, 32, m * 128, 128, scale_g=True)
                    for kt in range(32):
                        kb.op("pe", lambda e, kt=kt, ws=ws, ntok=ntok: e.matmul(pB[:, 0:ntok], lhsT=ws[:, kt, :], rhs=ysT[:, kt, 0:ntok],
                                                                                start=(kt == 0), stop=(kt == 31)),
                              reads=[b_ws, b_ysT], writes=[b_pB])
                    gi = cnt["g"] % 2
                    cnt["g"] += 1
                    kb.dma("sp", lambda e, gi=gi, m=m, c0=c0, ntok=ntok: e.dma_start(out=grt[gi][:, 0, 0:ntok],
                                                                                     in_=GT[m * 128:(m + 1) * 128, c0:c0 + ntok]),
                           reads=[b_GT], writes=[b_grt[gi]])
                    kb.dma("sp", lambda e, gi=gi, m=m, c0=c0, ntok=ntok: e.dma_start(out=grt[gi][:, 1, 0:ntok],
                                                                                     in_=GT[D + m * 128:D + (m + 1) * 128, c0:c0 + ntok]),
                           reads=[b_GT], writes=[b_grt[gi]], accumulate=True)
                    kb.op("act", lambda e, gi=gi, ntok=ntok: e.activation(out=sgt[gi][:, :, 0:ntok], in_=grt[gi][:, :, 0:ntok], func=AF.Sigmoid),
                          reads=[b_grt[gi]], writes=[b_sgt[gi]])
                    kb.op("dve", lambda e, gi=gi, ntok=ntok: e.tensor_tensor(out=t1[:, 0:ntok], in0=pA[:, 0:ntok], in1=sgt[gi][:, 0, 0:ntok],
                                                                             op=ALU.mult), reads=[b_pA, b_sgt[gi]], writes=[b_t1])
                    kb.op("dve", lambda e, gi=gi, ntok=ntok: e.tensor_tensor(out=t2[:, 0:ntok], in0=pB[:, 0:ntok], in1=sgt[gi][:, 1, 0:ntok],
                                                                             op=ALU.mult), reads=[b_pB, b_sgt[gi]], writes=[b_t2])
                    kb.op("dve", lambda e, m=m, ntok=ntok: e.tensor_tensor(out=mT[:, m, 0:ntok], in0=t1[:, 0:ntok], in1=t2[:, 0:ntok],
                                                                           op=ALU.add), reads=[b_t1, b_t2], writes=[b_mT])
                for nb in range(8):
                    wo, b_wo = load_w(wo_v, 16, nb * 256, 256)
                    for t in range(nsub):
                        pi = cnt["pc"] % 2
                        cnt["pc"] += 1
                        for kt in range(16):
                            kb.op("pe", lambda e, pi=pi, kt=kt, t=t, wo=wo: e.matmul(pC[pi][:, 0:256], lhsT=mT[:, kt, t * 128:(t + 1) * 128],
                                                                                     rhs=wo[:, kt, :], start=(kt == 0), stop=(kt == 15)),
                                  reads=[b_mT, b_wo], writes=[b_pC[pi]])
                        kb.op("dve", lambda e, pi=pi, t=t, nb=nb: e.tensor_tensor(out=hb[:, t, nb * 256:(nb + 1) * 256],
                                                                                  in0=hb[:, t, nb * 256:(nb + 1) * 256], in1=pC[pi][:, 0:256],
                                                                                  op=ALU.add), reads=[b_hb[t], b_pC[pi]], writes=[b_hb[t]])
                for t in range(nsub):
                    rows = slice(c0 + t * 128, c0 + (t + 1) * 128)
                    kb.dma("sp", lambda e, t=t, rows=rows: e.dma_start(out=H1[rows, :], in_=hb[:, t, :]), reads=[b_hb[t]], writes=[b_H1],
                           accumulate=True)
                    kb.op("act", lambda e, t=t: e.activation(out=junk, in_=hb[:, t, :], func=AF.Square, accum_out=stat[:, 0:1]),
                          reads=[b_hb[t]], writes=[b_junk, b_stat])
                    kb.op("dve", lambda e: e.tensor_scalar(out=stat[:, 1:2], in0=stat[:, 0:1], scalar1=1.0 / D, scalar2=EPS,
                                                           op0=ALU.mult, op1=ALU.add), reads=[b_stat], writes=[b_stat])
                    kb.op("act", lambda e: e.activation(out=stat[:, 3:4], in_=stat[:, 1:2], func=AF.Sqrt), reads=[b_stat], writes=[b_stat])
                    kb.op("dve", lambda e: e.reciprocal(out=stat[:, 2:3], in_=stat[:, 3:4]), reads=[b_stat], writes=[b_stat])
                    kb.op("dve", lambda e, t=t: e.scalar_tensor_tensor(out=n_bf, in0=hb[:, t, :], scalar=stat[:, 2:3], in1=g_bc,
                                                                       op0=ALU.mult, op1=ALU.mult), reads=[b_hb[t], b_stat, b_g], writes=[b_n])
                    kb.dma("sp", lambda e, rows=rows: e.dma_start(out=XN2[rows, :], in_=n_bf), reads=[b_n], writes=[b_XN2], accumulate=True)
                    for k in range(16):
                        kb.op("pe", lambda e, k=k: e.transpose(out=ptr[:, k, :], in_=n_bf[:, k * 128:(k + 1) * 128], identity=ident_bf),
                              reads=[b_n, b_ident], writes=[b_ptr])
                    kb.op("act", lambda e, t=t: e.activation(out=xT[:, :, t * 128:(t + 1) * 128], in_=ptr, func=AF.Copy),
                          reads=[b_ptr], writes=[b_xT])
                for j in range(16):
                    wq, b_wq = load_w(wq_v, 16, j * 128, 128)
                    pi = cnt["pc"] % 2
                    cnt["pc"] += 1
                    for kt in range(16):
                        kb.op("pe", lambda e, pi=pi, kt=kt, wq=wq, ntok=ntok: e.matmul(pC[pi][:, 0:ntok], lhsT=wq[:, kt, :], rhs=xT[:, kt, 0:ntok],
                                                                                       start=(kt == 0), stop=(kt == 15)),
                              reads=[b_wq, b_xT], writes=[b_pC[pi]])
                    kb.op("act", lambda e, pi=pi, ntok=ntok: e.activation(out=qpT[:, 0:ntok], in_=pC[pi][:, 0:ntok], func=AF.Copy),
                          reads=[b_pC[pi]], writes=[b_qpT])
                    for t in range(nsub):
                        kb.op("pe", lambda e, t=t, j=j: e.matmul(pD[:, t * 128:(t + 1) * 128], lhsT=qpT[:, t * 128:(t + 1) * 128], rhs=skT[:, j, :],
                                                                 start=True, stop=True), reads=[b_qpT, b_skT], writes=[b_pD])
                    kb.op("dve", lambda e, nsub=nsub, ntok=ntok: e.tensor_copy(out=sct[:, 0:nsub, :],
                                                                               in_=pD[:, 0:ntok].rearrange("p (t k) -> p t k", k=128)),
                          reads=[b_pD], writes=[b_sct])
                    kb.dma("sp", lambda e, j=j, ch=ch, nsub=nsub: e.dma_start(out=SC_v[:, ch:ch + nsub, j * 128:(j + 1) * 128], in_=sct[:, 0:nsub, :]),
                           reads=[b_sct], writes=[b_SC], accumulate=True)
                ch += nsub
        kb.barrier()

    if "ssd" in cfg.phases:
        phase_ssd()
    def phase_peer():
        with ExitStack() as es:
            NEG = -1.0e30
            NB = 6
            gf_bc = sb(es, "gfin_bc", [128, D], F32); b_gf = Buf()
            kb.dma("sp", lambda e: e.dma_start(out=gf_bc, in_=norm_final_g.partition_broadcast(128)), writes=[b_gf])
            iota = sb(es, "iota", [128, 256], F32); b_iota = Buf()
            iota_i = sb(es, "iota_i", [128, 256], I32)
            kb.op("pool", lambda e: e.iota(iota_i, pattern=[[1, 256]], base=0, channel_multiplier=0), writes=[b_iota])
            kb.op("dve", lambda e: e.tensor_copy(out=iota, in_=iota_i), reads=[b_iota], writes=[b_iota])
            if not precast_state["done"]:
                with ExitStack() as es2:
                    ptmp = ps(es2, "ptmp", [128, 512], F32); b_ptmp = Buf()
                    for _ in precast_units(es2, ptmp, b_ptmp):
                        pass
                    kb.barrier()
            sc = sb(es, "sc", [128, 16, 128], F32); b_sc = Buf()
            work = sb(es, "work", [128, 256], F32); b_work = Buf()
            v16 = sb(es, "v16", [128, 16, 16], F32); b_v16 = Buf()
            i16 = sb(es, "i16", [128, 16, 16], U32); b_i16 = Buf()
            i16f = sb(es, "i16f", [128, 16, 16], F32); b_i16f = Buf()
            i16s = sb(es, "i16s", [128, 16, 16], F32); b_i16s = Buf()
            cand = sb(es, "cand", [128, 8, 256], F32); b_cand = Buf()
            cid = sb(es, "cid", [128, 8, 256], F32); b_cid = Buf()
            top = sb(es, "top", [128, 8, 16], F32); b_top = Buf()
            pos = sb(es, "pos", [128, 8, 16], U32); b_pos = Buf()
            posf = sb(es, "posf", [128, 8, 16], F32); b_posf = Buf()
            eq = sb(es, "eq", [128, 16, 256], F32); b_eq = Buf()
            eidf = sb(es, "eidf", [128, 128], F32); b_eidf = Buf()
            eid2 = [sb(es, "eid%d" % i, [128, 128], I32) for i in range(2)]; b_eid2 = [Buf(), Buf()]
            gsm2 = [sb(es, "gsm%d" % i, [128, 4, 128], F32) for i in range(2)]; b_gsm2 = [Buf(), Buf()]
            gz = sb(es, "gz", [128, 16], F32); b_gz = Buf()
            act_t = sb(es, "act_t", [128, 128], F32); b_act = Buf()
            wgt = sb(es, "wgt", [128, 128], F32); b_wgt = Buf()
            GS, NGB = 4, 3
            dgs = [sb(es, "dgs%d" % i, [128, GS, 128], BF16) for i in range(NGB)]; b_dgs = [Buf() for _ in range(NGB)]
            uv = [sb(es, "uv%d" % i, [128, GS, 2 * D], BF16) for i in range(NGB)]
            b_uv = [[Buf() for _ in range(GS)] for _ in range(NGB)]
            b_actg = [Buf() for _ in range(128 // GS)]
            xn2_ = [sb(es, "xn%d" % i, [128, D], BF16) for i in range(2)]; b_xn2 = [Buf(), Buf()]
            h1t2 = [sb(es, "h1t%d" % i, [128, D], F32) for i in range(2)]; b_h12 = [Buf(), Buf()]
            junk = sb(es, "junk_p", [128, D], BF16); b_junk = Buf()
            prod = [sb(es, "prod%d" % i, [128, D], BF16) for i in range(2)]; b_prod = [Buf(), Buf()]
            stat = sb(es, "stat_p", [128, 4], F32); b_stat = Buf()
            ot = sb(es, "ot", [128, D], F32); b_ot = Buf()
            pacc = [ps(es, "pacc%d" % i, [128, 512], F32) for i in range(4)]; b_pacc = [Buf() for _ in range(4)]
            cnt = {"u": 0, "v": 0}

            def routing(ch):
                rows = slice(ch * 128, (ch + 1) * 128)
                par = ch % 2
                eid, b_eid, gsm, b_gsm = eid2[par], b_eid2[par], gsm2[par], b_gsm2[par]
                xn, b_xn, h1t, b_h1 = xn2_[par], b_xn2[par], h1t2[par], b_h12[par]
                kb.dma("sp", lambda e, rows=rows: e.dma_start(out=sc.rearrange("p j k -> p (j k)"), in_=SC[rows, :]), reads=[b_SC], writes=[b_sc])
                kb.dma("sp", lambda e, rows=rows: e.dma_start(out=xn, in_=XN2[rows, :]), reads=[b_XN2], writes=[b_xn])
                kb.dma("sp", lambda e, rows=rows: e.dma_start(out=h1t, in_=H1[rows, :]), reads=[b_H1], writes=[b_h1])
                for j in range(16):
                    kb.op("dve", lambda e, j=j: e.max(out=v16[:, j, 0:8], in_=sc[:, j, :]), reads=[b_sc], writes=[b_v16])
                    kb.op("dve", lambda e, j=j: e.match_replace(out=work[:, 0:128], in_to_replace=v16[:, j, 0:8], in_values=sc[:, j, :],
                                                                imm_value=NEG), reads=[b_sc, b_v16], writes=[b_work])
                    kb.op("dve", lambda e, j=j: e.max(out=v16[:, j, 8:16], in_=work[:, 0:128]), reads=[b_work, b_v16], writes=[b_v16])
                    kb.op("dve", lambda e, j=j: e.max_index(out=i16[:, j, 0:8], in_max=v16[:, j, 0:8], in_values=sc[:, j, :]),
                          reads=[b_sc, b_v16], writes=[b_i16])
                    kb.op("dve", lambda e, j=j: e.max_index(out=i16[:, j, 8:16], in_max=v16[:, j, 8:16], in_values=sc[:, j, :]),
                          reads=[b_sc, b_v16, b_i16], writes=[b_i16])
                kb.op("dve", lambda e: e.tensor_copy(out=i16f, in_=i16), reads=[b_i16], writes=[b_i16f])
                v4 = v16.rearrange("p (h c) k -> p h c k", c=2)
                f4 = i16f.rearrange("p (h c) k -> p h c k", c=2)
                s4 = i16s.rearrange("p (h c) k -> p h c k", c=2)
                c4 = cand.rearrange("p h (a b) -> p h a b", b=16)
                d4 = cid.rearrange("p h (a b) -> p h a b", b=16)
                kb.op("dve", lambda e: e.tensor_tensor(out=c4, in0=v4[:, :, 0, :].unsqueeze(3).to_broadcast([128, 8, 16, 16]),
                                                       in1=v4[:, :, 1, :].unsqueeze(2).to_broadcast([128, 8, 16, 16]), op=ALU.add),
                      reads=[b_v16], writes=[b_cand])
                kb.op("dve", lambda e: e.tensor_scalar(out=i16s, in0=i16f, scalar1=float(PNK), scalar2=None, op0=ALU.mult),
                      reads=[b_i16f], writes=[b_i16s])
                kb.op("dve", lambda e: e.tensor_tensor(out=d4, in0=s4[:, :, 0, :].unsqueeze(3).to_broadcast([128, 8, 16, 16]),
                                                       in1=f4[:, :, 1, :].unsqueeze(2).to_broadcast([128, 8, 16, 16]), op=ALU.add),
                      reads=[b_i16f, b_i16s], writes=[b_cid])
                for h in range(PH):
                    kb.op("dve", lambda e, h=h: e.max(out=top[:, h, 0:8], in_=cand[:, h, :]), reads=[b_cand], writes=[b_top])
                    kb.op("dve", lambda e, h=h: e.match_replace(out=work, in_to_replace=top[:, h, 0:8], in_values=cand[:, h, :],
                                                                imm_value=NEG), reads=[b_cand, b_top], writes=[b_work])
                    kb.op("dve", lambda e, h=h: e.max(out=top[:, h, 8:16], in_=work), reads=[b_work, b_top], writes=[b_top])
                    kb.op("dve", lambda e, h=h: e.max_index(out=pos[:, h, 0:8], in_max=top[:, h, 0:8], in_values=cand[:, h, :]),
                          reads=[b_cand, b_top], writes=[b_pos])
                    kb.op("dve", lambda e, h=h: e.max_index(out=pos[:, h, 8:16], in_max=top[:, h, 8:16], in_values=cand[:, h, :]),
                          reads=[b_cand, b_top, b_pos], writes=[b_pos])
                kb.op("dve", lambda e: e.tensor_copy(out=posf, in_=pos), reads=[b_pos], writes=[b_posf])
                for h in range(PH):
                    kb.op("dve", lambda e, h=h: e.tensor_tensor(out=eq, in0=iota.unsqueeze(1).to_broadcast([128, 16, 256]),
                                                                in1=posf[:, h, :].unsqueeze(2).to_broadcast([128, 16, 256]),
                                                                op=ALU.is_equal), reads=[b_iota, b_posf], writes=[b_eq])
                    kb.op("dve", lambda e, h=h: e.tensor_tensor(out=eq, in0=eq, in1=cid[:, h, :].unsqueeze(1).to_broadcast([128, 16, 256]),
                                                                op=ALU.mult), reads=[b_eq, b_cid], writes=[b_eq])
                    kb.op("dve", lambda e, h=h: e.tensor_reduce(out=eidf[:, h * 16:(h + 1) * 16], in_=eq, axis=AX.X, op=ALU.add),
                          reads=[b_eq], writes=[b_eidf])
                kb.op("dve", lambda e: e.tensor_copy(out=eid, in_=eidf), reads=[b_eidf], writes=[b_eid])
                G = lambda i: gsm[:, i, :]
                t3 = top
                kb.op("dve", lambda e: e.tensor_tensor(out=G(0).rearrange("p (h k) -> p h k", k=16), in0=t3,
                                                       in1=t3[:, :, 0:1].to_broadcast([128, 8, 16]), op=ALU.subtract),
                      reads=[b_top], writes=[b_gsm])
                kb.op("act", lambda e: e.activation(out=G(1), in_=G(0), func=AF.Exp), reads=[b_gsm], writes=[b_gsm])
                kb.op("dve", lambda e: e.tensor_reduce(out=gz[:, 0:8], in_=G(1).rearrange("p (h k) -> p h k", k=16), axis=AX.X, op=ALU.add),
                      reads=[b_gsm], writes=[b_gz])
                kb.op("dve", lambda e: e.reciprocal(out=gz[:, 8:16], in_=gz[:, 0:8]), reads=[b_gz], writes=[b_gz])
                kb.op("dve", lambda e: e.tensor_tensor(out=G(2).rearrange("p (h k) -> p h k", k=16),
                                                       in0=G(1).rearrange("p (h k) -> p h k", k=16),
                                                       in1=bc_last(gz[:, 8:16], 16), op=ALU.mult), reads=[b_gsm, b_gz], writes=[b_gsm])

            def mix(ch):
                rows = slice(ch * 128, (ch + 1) * 128)
                par = ch % 2
                eid, b_eid, gsm, b_gsm = eid2[par], b_eid2[par], gsm2[par], b_gsm2[par]
                xn, b_xn, h1t, b_h1 = xn2_[par], b_xn2[par], h1t2[par], b_h12[par]
                NG = 128 // GS

                def finish_group(gq):
                    bi = gq % NGB
                    s0 = gq * GS
                    kb.op("act", lambda e: e.activation(out=gsm[:, 3, s0:s0 + GS], in_=act_t[:, s0:s0 + GS], func=AF.Gelu),
                          reads=[b_actg[gq]], writes=[b_actg[gq]])
                    kb.op("dve", lambda e: e.tensor_tensor(out=wgt[:, s0:s0 + GS], in0=gsm[:, 3, s0:s0 + GS], in1=gsm[:, 2, s0:s0 + GS],
                                                           op=ALU.mult), reads=[b_actg[gq], b_gsm], writes=[b_actg[gq]])
                    kb.op("dve", lambda e: e.tensor_tensor(out=dgs[bi], in0=ident_f.unsqueeze(1).to_broadcast([128, GS, 128]),
                                                           in1=bc_last(wgt[:, s0:s0 + GS], 128), op=ALU.mult),
                          reads=[b_ident, b_actg[gq]], writes=[b_dgs[bi]])
                    for i in range(GS):
                        s_ = s0 + i
                        for nb in range(4):
                            kb.op("pe", lambda e, i=i, s_=s_, nb=nb: e.matmul(pacc[nb], lhsT=dgs[bi][:, i, :],
                                                                              rhs=uv[bi][:, i, D + nb * 512:D + (nb + 1) * 512],
                                                                              start=(s_ == 0), stop=(s_ == 127)),
                                  reads=[b_dgs[bi], b_uv[bi][i]], writes=[b_pacc[nb]])

                for gq in range(NG):
                    bi = gq % NGB
                    s0 = gq * GS
                    for i in range(GS):
                        s_ = s0 + i
                        kb.dma("pool", lambda e, bi=bi, i=i, s_=s_: e.indirect_dma_start(
                            out=uv[bi][:, i, :], out_offset=None, in_=UVB,
                            in_offset=bass.IndirectOffsetOnAxis(ap=eid[:, s_:s_ + 1], axis=0)),
                            reads=[b_eid, b_UVB], writes=[b_uv[bi][i]])
                    for i in range(GS):
                        s_ = s0 + i
                        pj = s_ % 2
                        kb.op("dve", lambda e, bi=bi, i=i, pj=pj: e.tensor_tensor(out=prod[pj], in0=uv[bi][:, i, 0:D], in1=xn, op=ALU.mult),
                              reads=[b_uv[bi][i], b_xn], writes=[b_prod[pj]])
                        kb.op("act", lambda e, pj=pj, s_=s_: e.activation(out=junk, in_=prod[pj], func=AF.Copy, accum_out=act_t[:, s_:s_ + 1]),
                              reads=[b_prod[pj]], writes=[b_junk, b_actg[gq]])
                    if gq >= 1:
                        finish_group(gq - 1)
                    kb.flush(6)
                finish_group(NG - 1)
                for nb in range(4):
                    kb.op("dve", lambda e, nb=nb: e.tensor_tensor(out=h1t[:, nb * 512:(nb + 1) * 512], in0=h1t[:, nb * 512:(nb + 1) * 512],
                                                                  in1=pacc[nb], op=ALU.add), reads=[b_h1, b_pacc[nb]], writes=[b_h1])
                kb.op("act", lambda e: e.activation(out=junk, in_=h1t, func=AF.Square, accum_out=stat[:, 0:1]),
                      reads=[b_h1], writes=[b_junk, b_stat])
                kb.op("dve", lambda e: e.tensor_scalar(out=stat[:, 1:2], in0=stat[:, 0:1], scalar1=1.0 / D, scalar2=EPS,
                                                       op0=ALU.mult, op1=ALU.add), reads=[b_stat], writes=[b_stat])
                kb.op("act", lambda e: e.activation(out=stat[:, 3:4], in_=stat[:, 1:2], func=AF.Sqrt), reads=[b_stat], writes=[b_stat])
                kb.op("dve", lambda e: e.reciprocal(out=stat[:, 2:3], in_=stat[:, 3:4]), reads=[b_stat], writes=[b_stat])
                kb.op("dve", lambda e: e.scalar_tensor_tensor(out=ot, in0=h1t, scalar=stat[:, 2:3], in1=gf_bc, op0=ALU.mult, op1=ALU.mult),
                      reads=[b_h1, b_stat, b_gf], writes=[b_ot])
                kb.dma("sp", lambda e, rows=rows: e.dma_start(out=out[rows, :], in_=ot), reads=[b_ot], writes=[b_out], accumulate=True)

            routing(0)
            for ch in range(cfg.main):
                kb.maybe_epoch()
                if ch + 1 < cfg.main:
                    kb.recording = True
                    routing(ch + 1)
                    kb.recording = False
                mix(ch)
                kb.flush()
        kb.barrier()

    if "out" in cfg.phases:
        phase_out()
    if "peer" in cfg.phases:
        phase_peer()

    kb.barrier()
    es_glob.close()
    return nc, kb


def make_consts():
    c = np.zeros((128, 4096), np.float64)
    i = np.arange(128)
    c[:, 0:128] = (i[None, :] >= i[:, None])
    c[:, 128:256] = (i[:, None] > i[None, :])
    c[:, 256:384] = 1.0
    gam = 1.0 - 2.0 ** (-5.0 - np.arange(8))
    lg = np.log(gam)
    c[:, 384:392] = np.exp(lg[None, :] * (i[:, None] + 1.0))
    c[:, 392:400] = np.exp(-lg[None, :] * (i[:, None] + 1.0)) / 16.0
    c[:, 400:408] = np.exp(lg[None, :] * (127.0 - i[:, None])) / 16.0
    c[:, 408:416] = np.exp(lg[None, :] * 128.0)
    return c.astype(np.float32)


def core_streams(x, meta_tokens, cfg):
    B, S, _ = x.shape
    maps = []
    inv = (10000.0 ** (-np.arange(128, dtype=np.float32) / np.float32(128))).astype(np.float32)
    for core in range(8):
        b, s = core // 2, core % 2
        full = np.zeros((LEAD + N_META + S, D), np.float32)
        full[LEAD:LEAD + N_META] = meta_tokens
        full[LEAD + N_META:] = x[b]
        valid = np.zeros((full.shape[0], 1), np.float32)
        valid[LEAD:] = 1.0
        pos = np.maximum(np.arange(full.shape[0]) - LEAD, 0).astype(np.float32)
        hin = np.zeros((cfg.nt, D), np.float32)
        msk = np.zeros((cfg.nt, 1), np.float32)
        p = np.zeros((cfg.nt,), np.float32)
        if s == 0:
            n = (cfg.main + 1) * 128
            r = (cfg.pre - 1) * 128
            hin[r:] = full[0:n]
            msk[r:] = valid[0:n]
            p[r:] = pos[0:n]
        else:
            hin[:] = full[0:cfg.nt]
            msk[:] = valid[0:cfg.nt]
            p[:] = pos[0:cfg.nt]
        ang = p[:, None] * inv[None, :]
        maps.append({"hin": hin, "rowmask": msk, "ropec": np.cos(ang).astype(np.float32),
                     "ropes": np.sin(ang).astype(np.float32)})
    return maps


_PROG = {}


def kernel(x, meta_tokens, norm_mix_g, w_in, conv_w, conv_b, dt_bias, a_log, d_skip, ssm_norm_g, w_ret_o,
           w_ssm_o, w_out, norm_ffn_g, peer_w_q, peer_sub_keys, peer_u, peer_v, norm_final_g):
    cfg = Cfg()
    x = np.asarray(x, np.float32)
    maps = core_streams(x, np.asarray(meta_tokens, np.float32), cfg)
    shared = {
        "cst_f": make_consts(),
        "norm_mix_g": np.asarray(norm_mix_g, np.float32).reshape(1, D),
        "w_in": np.asarray(w_in, np.float32).reshape(D, NPROJ),
        "conv_w": np.asarray(conv_w, np.float32).reshape(4, CONV_DIM),
        "conv_b": np.asarray(conv_b, np.float32).reshape(1, CONV_DIM),
        "dt_bias": np.asarray(dt_bias, np.float32).reshape(1, SH),
        "a_log": np.asarray(a_log, np.float32).reshape(1, SH),
        "d_skip": np.asarray(d_skip, np.float32).reshape(1, SH),
        "ssm_norm_g": np.asarray(ssm_norm_g, np.float32).reshape(1, SSM_INNER),
        "w_ret_o": np.asarray(w_ret_o, np.float32).reshape(RH * RDV, D),
        "w_ssm_o": np.asarray(w_ssm_o, np.float32).reshape(SSM_INNER, D),
        "w_out": np.asarray(w_out, np.float32).reshape(D, D),
        "norm_ffn_g": np.asarray(norm_ffn_g, np.float32).reshape(1, D),
        "peer_w_q": np.asarray(peer_w_q, np.float32).reshape(D, D),
        "peer_sub_keys": np.asarray(peer_sub_keys, np.float32).reshape(16 * 128, 128),
        "peer_u": np.asarray(peer_u, np.float32).reshape(NEXP, D),
        "peer_v": np.asarray(peer_v, np.float32).reshape(NEXP, D),
        "norm_final_g": np.asarray(norm_final_g, np.float32).reshape(1, D),
    }
    for m in maps:
        m.update(shared)
    nc, _ = build_program(cfg)
    res = run_bass_kernel_spmd(nc, maps, core_ids=list(range(8)))
    B, S, _ = x.shape
    outp = np.zeros((B, S, D), np.float32)
    half = cfg.main * 128
    for core in range(8):
        b, s = core // 2, core % 2
        outp[b, s * half:(s + 1) * half] = res.results[core]["out"]
    return outp
```

```python
import numpy as np
from contextlib import ExitStack
import concourse.bass as bass
import concourse.mybir as mybir
from concourse.bass_utils import run_bass_kernel_spmd

F32 = mybir.dt.float32
BF16 = mybir.dt.bfloat16
I32 = mybir.dt.int32
U32 = mybir.dt.uint32
AF = mybir.ActivationFunctionType
ALU = mybir.AluOpType
AX = mybir.AxisListType

D = 2048
N_META = 16
LEAD = 112
EPS = 1e-6
RH, RDK, RDV = 8, 256, 512
SSM_INNER, SH, SP_, SG, SN = 4096, 64, 64, 8, 128
CONV_DIM = 6144
NPROJ = 26688
C_Q, C_K, C_V, C_G, C_Z, C_XBC, C_DT, C_GR, C_GS = 0, 2048, 4096, 8192, 12288, 16384, 22528, 22592, 24640
PTM_W = 16384
PH, PNK, PTOPK, PHALF = 8, 128, 16, 128
NEXP = 16384
HALO = 8


class Buf:
    __slots__ = ("writers", "readers", "name")

    def __init__(self, name=""):
        self.writers = {}
        self.readers = {}
        self.name = name


class KB:
    NQ = 20

    def __init__(self, nc):
        self.nc = nc
        self.engs = {"pe": nc.tensor, "act": nc.scalar, "dve": nc.vector, "pool": nc.gpsimd, "sp": nc.sync}
        self.epoch = 0
        self.csem = {e: nc.alloc_semaphore("c_" + e) for e in ("pe", "act", "dve", "pool")}
        self.ccnt = {e: 0 for e in self.csem}
        self.dsem = {q: [nc.alloc_semaphore("d_%s%d" % (q, i)) for i in range(self.NQ)] for q in ("sp", "pool")}
        self.dcnt = {q: 0 for q in self.dsem}
        self.waited = {e: {} for e in self.engs}
        self.n_ins = 0
        self.limit = None
        self.n_ops = 0
        self.recording = False
        self.deferred_q = []

    def _wait(self, e, toks):
        w = self.waited[e]
        need = {}
        for key, (sem, val) in toks:
            if key[0] == "c":
                if key[2] < self.epoch:
                    continue
                if e == "pe" and key[1] == "pe":
                    continue
            if w.get(key, 0) >= val:
                continue
            if key not in need or need[key][1] < val:
                need[key] = (sem, val)
        for key, (sem, val) in need.items():
            self.engs[e].wait_ge(sem, val)
            w[key] = val
            self.n_ins += 1

    @staticmethod
    def _merge(dst, key, sv):
        if key not in dst or dst[key][1] < sv[1]:
            dst[key] = sv

    def _deps(self, reads, writes, accumulate=False):
        toks = []
        for b in reads:
            toks += list(b.writers.items())
        for b in writes:
            if not accumulate:
                toks += list(b.writers.items())
            toks += list(b.readers.items())
        return toks

    def _commit(self, key, sv, reads, writes, accumulate=False):
        for b in reads:
            self._merge(b.readers, key, sv)
        for b in writes:
            if accumulate:
                self._merge(b.writers, key, sv)
            else:
                b.writers = {key: sv}
                b.readers = {}

    def flush(self, n=None):
        q = self.deferred_q
        k = len(q) if n is None else min(n, len(q))
        rec, self.recording = self.recording, False
        for _ in range(k):
            kind, args = q.pop(0)
            (self.op if kind == "op" else self.dma)(*args)
        self.recording = rec

    def op(self, e, fn, reads=(), writes=()):
        if self.recording:
            self.deferred_q.append(("op", (e, fn, list(reads), list(writes))))
            return
        self.n_ops += 1
        if self.limit is not None and self.n_ops > self.limit:
            return
        self._wait(e, self._deps(reads, writes))
        ins = fn(self.engs[e])
        self.ccnt[e] += 1
        ins.then_inc(self.csem[e], 1)
        self.n_ins += 1
        self._commit(("c", e, self.epoch), (self.csem[e], self.ccnt[e]), reads, writes)

    def dma(self, q, fn, reads=(), writes=(), accumulate=False):
        if self.recording:
            self.deferred_q.append(("dma", (q, fn, list(reads), list(writes), accumulate)))
            return
        self.n_ops += 1
        if self.limit is not None and self.n_ops > self.limit:
            return
        i = self.dcnt[q]
        slot, gen = i % self.NQ, i // self.NQ
        key = ("d", q, slot)
        sem = self.dsem[q][slot]
        toks = self._deps(reads, writes, accumulate)
        if gen > 0:
            toks.append((key, (sem, 16 * gen)))
        self._wait(q, toks)
        ins = fn(self.engs[q])
        ins.then_inc(sem, 16)
        self.dcnt[q] += 1
        self.n_ins += 1
        self._commit(key, (sem, 16 * (gen + 1)), reads, writes, accumulate)

    def all_tokens(self):
        toks = [(("c", e, self.epoch), (self.csem[e], self.ccnt[e])) for e in self.csem if self.ccnt[e] > 0]
        for q in self.dsem:
            n = self.dcnt[q]
            for slot in range(min(n, self.NQ)):
                gens = (n - 1 - slot) // self.NQ + 1
                toks.append((("d", q, slot), (self.dsem[q][slot], 16 * gens)))
        return toks

    def barrier(self):
        toks = self.all_tokens()
        for e in self.engs:
            self._wait(e, toks)
        if self.limit is not None and self.n_ops > self.limit:
            return
        self.epoch += 1
        self.csem = {e: self.nc.alloc_semaphore("c_%s_%d" % (e, self.epoch)) for e in self.csem}
        self.ccnt = {e: 0 for e in self.csem}

    def maybe_epoch(self, thresh=30000):
        if max(self.ccnt.values()) > thresh:
            self.barrier()


def bc_mid(ap2d, n):
    p, f = ap2d.shape
    return ap2d.unsqueeze(1).to_broadcast([p, n, f])


def bc_last(ap2d, n):
    p, f = ap2d.shape
    return ap2d.unsqueeze(2).to_broadcast([p, f, n])


class Cfg:
    def __init__(self, pre=33, main=32, debug=False, phases=("proj", "ret", "ssd", "out", "peer"), tiny=()):
        self.tiny = set(tiny)
        self.pre = pre
        self.main = main
        self.debug = debug
        self.phases = phases
        self.nch = pre + main
        self.nt = self.nch * 128
        self.nt_main = main * 128


def build_program(cfg):
    nc = bass.Bass("TRN2", target_bir_lowering=False)
    kb = KB(nc)
    kb.limit = getattr(cfg, "limit", None)
    NT, NTM = cfg.nt, cfg.nt_main
    ROW0 = cfg.pre * 128
    dbg_kind = "ExternalOutput" if cfg.debug else "Internal"

    def ext_in(name, shape, dt=F32):
        if name in cfg.tiny:
            shape = [1, 8]
        return nc.dram_tensor(name, list(shape), dt, kind="ExternalInput").ap()

    hin = ext_in("hin", [NT, D])
    rowmask = ext_in("rowmask", [NT, 1])
    ropec = ext_in("ropec", [NT, 128])
    ropes = ext_in("ropes", [NT, 128])
    cst_f = ext_in("cst_f", [128, 4096])
    norm_mix_g = ext_in("norm_mix_g", [1, D])
    w_in = ext_in("w_in", [D, NPROJ])
    conv_w = ext_in("conv_w", [4, CONV_DIM])
    conv_b = ext_in("conv_b", [1, CONV_DIM])
    dt_bias = ext_in("dt_bias", [1, SH])
    a_log = ext_in("a_log", [1, SH])
    d_skip = ext_in("d_skip", [1, SH])
    ssm_norm_g = ext_in("ssm_norm_g", [1, SSM_INNER])
    w_ret_o = ext_in("w_ret_o", [RH * RDV, D])
    w_ssm_o = ext_in("w_ssm_o", [SSM_INNER, D])
    w_out = ext_in("w_out", [D, D])
    norm_ffn_g = ext_in("norm_ffn_g", [1, D])
    peer_w_q = ext_in("peer_w_q", [D, D])
    peer_sub_keys = ext_in("peer_sub_keys", [16 * 128, 128])
    peer_u = ext_in("peer_u", [NEXP, D])
    peer_v = ext_in("peer_v", [NEXP, D])
    norm_final_g = ext_in("norm_final_g", [1, D])
    out = nc.dram_tensor("out", [NTM, D], F32, kind="ExternalOutput").ap()

    PTMS = [nc.dram_tensor("PTM%d" % i, [NT, 4096], BF16, kind=dbg_kind).ap() for i in range(4)]

    class _PTM:
        def __getitem__(self, key):
            rows, cols = key
            i = cols.start // 4096
            assert (cols.stop - 1) // 4096 == i
            return PTMS[i][rows, cols.start - i * 4096:cols.stop - i * 4096]
    PTM = _PTM()
    XBCT = nc.dram_tensor("XBCT", [CONV_DIM, HALO + NT], BF16, kind=dbg_kind).ap()
    DTS = nc.dram_tensor("DTS", [NT, SH], F32, kind=dbg_kind).ap()
    GT = nc.dram_tensor("GT", [2 * D, NTM], BF16, kind=dbg_kind).ap()
    OGT = nc.dram_tensor("OGT", [RH * RDV, NTM], BF16, kind=dbg_kind).ap()
    YST = nc.dram_tensor("YST", [SSM_INNER, NTM], BF16, kind=dbg_kind).ap()
    H1 = nc.dram_tensor("H1", [NTM, D], F32, kind=dbg_kind).ap()
    XN2 = nc.dram_tensor("XN2", [NTM, D], BF16, kind=dbg_kind).ap()
    SC = nc.dram_tensor("SC", [NTM, 16 * 128], F32, kind=dbg_kind).ap()
    b_XN2, b_SC = Buf("XN2"), Buf("SC")
    NEX = 128 if "peer_u" in cfg.tiny else NEXP
    UVB = nc.dram_tensor("UVB", [NEX, 2 * D], BF16, kind="Internal").ap()
    b_UVB = Buf("UVB")
    WRB = nc.dram_tensor("WRB", [RH * RDV, D], BF16, kind="Internal").ap()
    WSB = nc.dram_tensor("WSB", [SSM_INNER, D], BF16, kind="Internal").ap()
    WOB = nc.dram_tensor("WOB", [D, D], BF16, kind="Internal").ap()
    WQB = nc.dram_tensor("WQB", [D, D], BF16, kind="Internal").ap()
    b_WB = Buf("WB")
    b_PTM, b_XBCT, b_DTS, b_GT, b_OGT, b_YST, b_H1, b_out = (Buf(n) for n in
                                                              ("PTM", "XBCT", "DTS", "GT", "OGT", "YST", "H1", "out"))

    es_glob = ExitStack()

    uniq = [0]

    def sb(es, name, shape, dt):
        uniq[0] += 1
        return es.enter_context(nc.sbuf_tensor("%s_%d" % (name, uniq[0]), list(shape), dt)).ap()

    def ps(es, name, shape, dt):
        uniq[0] += 1
        return es.enter_context(nc.psum_tensor("%s_%d" % (name, uniq[0]), list(shape), dt)).ap()

    ident_bf = sb(es_glob, "ident_bf", [128, 128], BF16)
    ident_f = sb(es_glob, "ident_f", [128, 128], F32)
    b_ident = Buf("ident")
    kb.op("pool", lambda e: e.memset(ident_f, 0.0), writes=[b_ident])
    kb.op("pool", lambda e: e.affine_select(out=ident_f, in_=ident_f, pattern=[[-1, 128]], compare_op=ALU.not_equal,
                                            fill=1.0, base=0, channel_multiplier=1), reads=[b_ident], writes=[b_ident])
    kb.op("dve", lambda e: e.tensor_copy(out=ident_bf, in_=ident_f), reads=[b_ident], writes=[b_ident])

    def phase_proj():
        with ExitStack() as es:
            TBMAX = 17 * 128
            g_bc = sb(es, "g_bc", [128, D], F32)
            b_g = Buf("g_bc")
            kb.dma("sp", lambda e: e.dma_start(out=g_bc, in_=norm_mix_g.partition_broadcast(128)), writes=[b_g])
            nT = sb(es, "nT", [128, 16, TBMAX], BF16)
            b_nT = Buf("nT")
            h_t = [sb(es, "h_t%d" % i, [128, D], F32) for i in range(2)]
            b_h = [Buf("h_t") for _ in range(2)]
            n_bf = [sb(es, "n_bf%d" % i, [128, D], BF16) for i in range(2)]
            b_n = [Buf("n_bf") for _ in range(2)]
            junk = sb(es, "junk", [128, D], BF16)
            b_junk = Buf("junk")
            stat = sb(es, "stat", [128, 4], F32)
            b_stat = Buf("stat")
            wst = [sb(es, "wst%d" % i, [128, 16, 512], F32) for i in range(2)]
            b_wst = [Buf("wst") for _ in range(2)]
            wbf = [sb(es, "wbf%d" % i, [128, 16, 512], BF16) for i in range(2)]
            b_wbf = [Buf("wbf") for _ in range(2)]
            ob = [sb(es, "ob%d" % i, [128, 512], BF16) for i in range(4)]
            b_ob = [Buf("ob") for _ in range(4)]
            obf = [sb(es, "obf%d" % i, [128, 64], F32) for i in range(2)]
            b_obf = [Buf("obf") for _ in range(2)]
            zt = sb(es, "zt", [128, HALO], BF16)
            b_zt = Buf("zt")
            ptr = ps(es, "ptr", [128, 16, 128], BF16)
            b_ptr = Buf("ptr")
            pmm = [ps(es, "pmm%d" % i, [128, 512], F32) for i in range(4)]
            b_pmm = [Buf("pmm") for _ in range(4)]

            kb.op("pool", lambda e: e.memset(zt, 0.0), writes=[b_zt])
            for r in range(CONV_DIM // 128):
                kb.dma("sp", lambda e, r=r: e.dma_start(out=XBCT[r * 128:(r + 1) * 128, 0:HALO], in_=zt),
                       reads=[b_zt], writes=[b_XBCT], accumulate=True)

            w_in_v = w_in.rearrange("(k p) n -> p k n", p=128)
            cnt = {"w": 0, "ob": 0, "pm": 0, "ev": 0, "obf": 0}

            def evac(dst, src, reads, writes):
                eng = "act" if cnt["ev"] % 2 == 0 else "dve"
                cnt["ev"] += 1
                if eng == "act":
                    kb.op("act", lambda e: e.activation(out=dst, in_=src, func=AF.Copy), reads=reads, writes=writes)
                else:
                    kb.op("dve", lambda e: e.tensor_copy(out=dst, in_=src), reads=reads, writes=writes)

            def do_block(ch0, nchk, colgroups):
                ntok = nchk * 128
                r0 = ch0 * 128
                for t in range(nchk):
                    i = t % 2
                    rows = slice(r0 + t * 128, r0 + (t + 1) * 128)
                    kb.dma("sp", lambda e, i=i, rows=rows: e.dma_start(out=h_t[i], in_=hin[rows, :]), writes=[b_h[i]])
                    kb.op("act", lambda e, i=i: e.activation(out=junk, in_=h_t[i], func=AF.Square, accum_out=stat[:, 0:1]),
                          reads=[b_h[i]], writes=[b_junk, b_stat])
                    kb.op("dve", lambda e: e.tensor_scalar(out=stat[:, 1:2], in0=stat[:, 0:1], scalar1=1.0 / D, scalar2=EPS,
                                                           op0=ALU.mult, op1=ALU.add), reads=[b_stat], writes=[b_stat])
                    kb.op("act", lambda e: e.activation(out=stat[:, 3:4], in_=stat[:, 1:2], func=AF.Sqrt),
                          reads=[b_stat], writes=[b_stat])
                    kb.op("dve", lambda e: e.reciprocal(out=stat[:, 2:3], in_=stat[:, 3:4]), reads=[b_stat], writes=[b_stat])
                    kb.op("dve", lambda e, i=i: e.scalar_tensor_tensor(out=n_bf[i], in0=h_t[i], scalar=stat[:, 2:3], in1=g_bc,
                                                                       op0=ALU.mult, op1=ALU.mult),
                          reads=[b_h[i], b_stat, b_g], writes=[b_n[i]])
                    for k in range(16):
                        kb.op("pe", lambda e, i=i, k=k: e.transpose(out=ptr[:, k, :], in_=n_bf[i][:, k * 128:(k + 1) * 128],
                                                                    identity=ident_bf),
                              reads=[b_n[i], b_ident], writes=[b_ptr])
                    evac(nT[:, :, t * 128:(t + 1) * 128], ptr, [b_ptr], [b_nT])
                for (kind, c0, ncol, dst_row0) in colgroups:
                    kb.maybe_epoch()
                    wi = cnt["w"] % 2
                    cnt["w"] += 1
                    kb.dma("sp", lambda e, wi=wi, c0=c0, ncol=ncol: e.dma_start(out=wst[wi][:, :, 0:ncol],
                                                                                 in_=w_in_v[:, :, c0:c0 + ncol]),
                           writes=[b_wst[wi]])
                    kb.op("pool", lambda e, wi=wi, ncol=ncol: e.tensor_copy(out=wbf[wi][:, :, 0:ncol], in_=wst[wi][:, :, 0:ncol]),
                          reads=[b_wst[wi]], writes=[b_wbf[wi]])
                    if kind == "tm":
                        for t in range(nchk):
                            pi = cnt["pm"] % 4
                            cnt["pm"] += 1
                            for k in range(16):
                                kb.op("pe", lambda e, pi=pi, k=k, t=t, wi=wi, ncol=ncol: e.matmul(
                                    pmm[pi][:, 0:ncol], lhsT=nT[:, k, t * 128:(t + 1) * 128], rhs=wbf[wi][:, k, 0:ncol],
                                    start=(k == 0), stop=(k == 15)), reads=[b_nT, b_wbf[wi]], writes=[b_pmm[pi]])
                            oi = cnt["ob"] % 4
                            cnt["ob"] += 1
                            evac(ob[oi][:, 0:ncol], pmm[pi][:, 0:ncol], [b_pmm[pi]], [b_ob[oi]])
                            rows = slice(r0 + t * 128, r0 + (t + 1) * 128)
                            kb.dma("sp", lambda e, oi=oi, rows=rows, c0=c0, ncol=ncol: e.dma_start(
                                out=PTM[rows, c0:c0 + ncol], in_=ob[oi][:, 0:ncol]),
                                reads=[b_ob[oi]], writes=[b_PTM], accumulate=True)
                    elif kind == "dt":
                        for t in range(nchk):
                            pi = cnt["pm"] % 4
                            cnt["pm"] += 1
                            for k in range(16):
                                kb.op("pe", lambda e, pi=pi, k=k, t=t, wi=wi: e.matmul(
                                    pmm[pi][:, 0:64], lhsT=nT[:, k, t * 128:(t + 1) * 128], rhs=wbf[wi][:, k, 0:64],
                                    start=(k == 0), stop=(k == 15)), reads=[b_nT, b_wbf[wi]], writes=[b_pmm[pi]])
                            oi = cnt["obf"] % 2
                            cnt["obf"] += 1
                            evac(obf[oi], pmm[pi][:, 0:64], [b_pmm[pi]], [b_obf[oi]])
                            rows = slice(r0 + t * 128, r0 + (t + 1) * 128)
                            kb.dma("sp", lambda e, oi=oi, rows=rows: e.dma_start(out=DTS[rows, :], in_=obf[oi]),
                                   reads=[b_obf[oi]], writes=[b_DTS], accumulate=True)
                    else:
                        for m in range(ncol // 128):
                            for tg in range(0, ntok, 512):
                                n = min(512, ntok - tg)
                                pi = cnt["pm"] % 4
                                cnt["pm"] += 1
                                for k in range(16):
                                    kb.op("pe", lambda e, pi=pi, k=k, m=m, tg=tg, n=n, wi=wi: e.matmul(
                                        pmm[pi][:, 0:n], lhsT=wbf[wi][:, k, m * 128:(m + 1) * 128], rhs=nT[:, k, tg:tg + n],
                                        start=(k == 0), stop=(k == 15)), reads=[b_nT, b_wbf[wi]], writes=[b_pmm[pi]])
                                oi = cnt["ob"] % 4
                                cnt["ob"] += 1
                                evac(ob[oi][:, 0:n], pmm[pi][:, 0:n], [b_pmm[pi]], [b_ob[oi]])
                                rr = dst_row0 + m * 128
                                if kind == "xbc":
                                    kb.dma("sp", lambda e, oi=oi, rr=rr, tg=tg, n=n: e.dma_start(
                                        out=XBCT[rr:rr + 128, HALO + r0 + tg:HALO + r0 + tg + n], in_=ob[oi][:, 0:n]),
                                        reads=[b_ob[oi]], writes=[b_XBCT], accumulate=True)
                                else:
                                    cc = r0 - ROW0 + tg
                                    kb.dma("sp", lambda e, oi=oi, rr=rr, cc=cc, n=n: e.dma_start(
                                        out=GT[rr:rr + 128, cc:cc + n], in_=ob[oi][:, 0:n]),
                                        reads=[b_ob[oi]], writes=[b_GT], accumulate=True)

            def groups(c_lo, c_hi, kind, dst_row0=0):
                return [(kind, c, min(512, c_hi - c), dst_row0 + (c - c_lo)) for c in range(c_lo, c_hi, 512)]

            pre_groups = (groups(C_K, C_G, "tm") + groups(C_XBC, C_DT, "xbc")
                          + [("dt", C_DT, 64, 0)])
            main_groups = (groups(0, PTM_W, "tm") + groups(C_XBC, C_DT, "xbc") + [("dt", C_DT, 64, 0)]
                           + groups(C_GR, NPROJ, "gt"))

            def blocks(c0, n):
                res, c = [], c0
                while n > 0:
                    m = min(n, 17 if n == 17 else 16)
                    res.append((c, m))
                    c += m
                    n -= m
                return res

            for (c, m) in blocks(0, cfg.pre):
                do_block(c, m, pre_groups)
            for (c, m) in blocks(cfg.pre, cfg.main):
                do_block(c, m, main_groups)
        kb.barrier()

    CO_UT, CO_SL, CO_ONE, CO_DQ, CO_DK, CO_DKZ, CO_G128 = 0, 128, 256, 384, 392, 400, 408

    def load_consts(es):
        cst = sb(es, "cst", [128, 512], F32)
        b_cst = Buf("cst")
        kb.dma("sp", lambda e: e.dma_start(out=cst, in_=cst_f[:, 0:512]), writes=[b_cst])
        return cst, b_cst

    def transpose_store(src_bf, b_src, nblk, ptr, b_ptr, oT, b_oT, dst_view, b_dst, col0):
        for r in range(0, nblk, 16):
            for k in range(16):
                kb.op("pe", lambda e, k=k, r=r: e.transpose(out=ptr[:, k, :], in_=src_bf[:, (r + k) * 128:(r + k + 1) * 128],
                                                            identity=ident_bf), reads=[b_src, b_ident], writes=[b_ptr])
            kb.op("act", lambda e, r=r: e.activation(out=oT[:, r:r + 16, :], in_=ptr, func=AF.Copy),
                  reads=[b_ptr], writes=[b_oT])
        kb.dma("sp", lambda e: e.dma_start(out=dst_view[:, :, col0:col0 + 128], in_=oT[:, 0:nblk, :]),
               reads=[b_oT], writes=[b_dst], accumulate=True)

    precast_state = {"done": False}

    def precast_units(es, ptile, b_ptile):
        gcol = sb(es, "gcol", [128, 32], F32); b_gcol = Buf()
        tmpg = sb(es, "tmpg", [32, 128], F32); b_tg = Buf()
        kb.dma("sp", lambda e: e.dma_start(out=tmpg, in_=ssm_norm_g.rearrange("o (kt p) -> (o kt) p", p=128)), writes=[b_tg])
        kb.op("pe", lambda e: e.transpose(out=ptile[:, 0:32], in_=tmpg, identity=ident_f[0:32, 0:32]),
              reads=[b_tg, b_ident], writes=[b_ptile])
        kb.op("dve", lambda e: e.tensor_copy(out=gcol, in_=ptile[:, 0:32]), reads=[b_ptile], writes=[b_gcol])
        stg = [sb(es, "stgw%d" % i, [128, D], F32) for i in range(3)]; b_stg = [Buf() for _ in range(3)]
        stb = [sb(es, "stbw%d" % i, [128, D], BF16) for i in range(3)]; b_stb = [Buf() for _ in range(3)]
        jobs = []
        for (src, dst, nblk, scale_g, b_dst) in ((w_ret_o, WRB, 32, False, b_WB), (w_ssm_o, WSB, 32, True, b_WB),
                                                 (w_out, WOB, 16, False, b_WB), (peer_w_q, WQB, 16, False, b_WB)):
            for r in range(nblk):
                jobs.append((src[r * 128:(r + 1) * 128, :], dst[r * 128:(r + 1) * 128, :], r if scale_g else None, b_dst))
        for r in range(NEX // 128):
            jobs.append((peer_u[r * 128:(r + 1) * 128, :], UVB[r * 128:(r + 1) * 128, 0:D], None, b_UVB))
            jobs.append((peer_v[r * 128:(r + 1) * 128, :], UVB[r * 128:(r + 1) * 128, D:2 * D], None, b_UVB))
        def ld(n):
            src_ap = jobs[n][0]
            i = n % 3
            kb.dma("sp", lambda e: e.dma_start(out=stg[i], in_=src_ap), writes=[b_stg[i]])

        def cast(n):
            i = n % 3
            gr = jobs[n][2]
            if gr is not None:
                kb.op("act", lambda e: e.activation(out=stb[i], in_=stg[i], func=AF.Copy, scale=gcol[:, gr:gr + 1]),
                      reads=[b_stg[i], b_gcol], writes=[b_stb[i]])
            else:
                kb.op("act", lambda e: e.activation(out=stb[i], in_=stg[i], func=AF.Copy), reads=[b_stg[i]], writes=[b_stb[i]])

        def st(n):
            i = n % 3
            dst_ap, b_dst = jobs[n][1], jobs[n][3]
            kb.dma("sp", lambda e: e.dma_start(out=dst_ap, in_=stb[i]), reads=[b_stb[i]], writes=[b_dst], accumulate=True)

        NJ = len(jobs)
        for n in range(NJ + 2):
            if n < NJ:
                ld(n)
            if 0 <= n - 1 < NJ:
                cast(n - 1)
            if 0 <= n - 2 < NJ:
                st(n - 2)
            yield n
        precast_state["done"] = True

    N_PRECAST = 96 + 2 * (NEX // 128) + 2

    def phase_ret():
        with ExitStack() as es:
            cst, b_cst = load_consts(es)
            UT = cst[:, CO_UT:CO_UT + 128]
            st_f = sb(es, "st_f", [128, RH, 2, 512], F32)
            st_b = sb(es, "st_b", [128, RH, 2, 512], BF16)
            b_stf = [Buf("stf") for _ in range(RH)]
            b_stb = [Buf("stb") for _ in range(RH)]
            for h in range(RH):
                kb.op("pool", lambda e, h=h: e.memset(st_f[:, h], 0.0), writes=[b_stf[h]])
                kb.op("pool", lambda e, h=h: e.memset(st_b[:, h], 0.0), writes=[b_stb[h]])
            k_in = sb(es, "k_in", [128, 2048], BF16); b_kin = Buf()
            q_in = sb(es, "q_in", [128, 2048], BF16); b_qin = Buf()
            v_in = sb(es, "v_in", [128, 4096], BF16); b_vin = Buf()
            g_in = sb(es, "g_in", [128, 4096], BF16); b_gin = Buf()
            cos_t = sb(es, "cos_t", [128, 128], F32); b_cos = Buf()
            sin_t = sb(es, "sin_t", [128, 128], F32); b_sin = Buf()
            tmp1 = sb(es, "tmp1", [128, 8, 128], F32); b_t1 = Buf()
            tmp2 = sb(es, "tmp2", [128, 8, 128], F32); b_t2 = Buf()
            kr = sb(es, "kr", [128, 8, 2, 128], F32); b_kr = Buf()
            kt_ = sb(es, "kt_", [128, 8, 256], BF16); b_kt = Buf()
            kz_ = sb(es, "kz_", [128, 8, 256], BF16); b_kz = Buf()
            qt_ = sb(es, "qt_", [128, 8, 256], BF16); b_qt = Buf()
            qT = sb(es, "qT", [128, 16, 128], BF16); b_qT = Buf()
            kT = sb(es, "kT", [128, 16, 128], BF16); b_kT = Buf()
            sTm = [sb(es, "sTm%d" % i, [128, 128], BF16) for i in range(2)]; b_sTm = [Buf(), Buf()]
            o_sb = sb(es, "o_sb", [128, RH, 512], F32); b_osb = [Buf() for _ in range(RH)]
            sg = sb(es, "sg", [128, RH, 512], BF16); b_sg = Buf()
            og = sb(es, "og", [128, RH * 512], BF16); b_og = Buf()
            oT = sb(es, "oT", [128, 32, 128], BF16); b_oT = Buf()
            junk = sb(es, "junkr", [128, 512], BF16); b_junk = Buf()
            ssq = sb(es, "ssq", [128, 32], F32); b_ssq = Buf()
            ptr = ps(es, "ptr_r", [128, 16, 128], BF16); b_ptr = Buf()
            ps_s = ps(es, "ps_s", [128, 512], F32); b_pss = Buf()
            ps_o = [ps(es, "ps_o%d" % i, [128, 512], F32) for i in range(2)]; b_pso = [Buf(), Buf()]
            ps_u = [ps(es, "ps_u%d" % i, [128, 512], F32) for i in range(2)]; b_psu = [Buf(), Buf()]
            OGT_v = OGT.rearrange("(kt p) n -> p kt n", p=128)
            pc_gen = precast_units(es, ps_s, b_pss)
            pc_per_chunk = -(-N_PRECAST // cfg.nch)

            def rope(src, b_src, dst_list):
                v4 = src.rearrange("p (h f two) -> p h f two", h=8, two=2)
                t1, t2 = v4[:, :, :, 0], v4[:, :, :, 1]
                cb_, sb_ = bc_mid(cos_t, 8), bc_mid(sin_t, 8)
                kb.op("dve", lambda e: e.tensor_tensor(out=tmp1, in0=t1, in1=cb_, op=ALU.mult), reads=[b_src, b_cos], writes=[b_t1])
                kb.op("dve", lambda e: e.tensor_tensor(out=tmp2, in0=t2, in1=sb_, op=ALU.mult), reads=[b_src, b_sin], writes=[b_t2])
                kb.op("dve", lambda e: e.tensor_tensor(out=kr[:, :, 0, :], in0=tmp1, in1=tmp2, op=ALU.subtract),
                      reads=[b_t1, b_t2], writes=[b_kr])
                kb.op("dve", lambda e: e.tensor_tensor(out=tmp1, in0=t1, in1=sb_, op=ALU.mult), reads=[b_src, b_sin], writes=[b_t1])
                kb.op("dve", lambda e: e.tensor_tensor(out=tmp2, in0=t2, in1=cb_, op=ALU.mult), reads=[b_src, b_cos], writes=[b_t2])
                kb.op("dve", lambda e: e.tensor_tensor(out=kr[:, :, 1, :], in0=tmp1, in1=tmp2, op=ALU.add),
                      reads=[b_t1, b_t2, b_kr], writes=[b_kr])
                krv = kr.rearrange("p h two f -> p h (two f)")
                for (dst, b_dst, co) in dst_list:
                    kb.op("dve", lambda e, dst=dst, co=co: e.tensor_tensor(out=dst, in0=krv, in1=bc_last(cst[:, co:co + 8], 256),
                                                                           op=ALU.mult), reads=[b_kr, b_cst], writes=[b_dst])

            for ch in range(cfg.nch):
                kb.maybe_epoch()
                pc_left = pc_per_chunk
                main = ch >= cfg.pre
                r0 = ch * 128
                rows = slice(r0, r0 + 128)
                kb.dma("sp", lambda e, rows=rows: e.dma_start(out=k_in, in_=PTM[rows, C_K:C_K + 2048]), reads=[b_PTM], writes=[b_kin])
                kb.dma("sp", lambda e, rows=rows: e.dma_start(out=v_in, in_=PTM[rows, C_V:C_V + 4096]), reads=[b_PTM], writes=[b_vin])
                kb.dma("sp", lambda e, rows=rows: e.dma_start(out=cos_t, in_=ropec[rows, :]), writes=[b_cos])
                kb.dma("sp", lambda e, rows=rows: e.dma_start(out=sin_t, in_=ropes[rows, :]), writes=[b_sin])
                if main:
                    kb.dma("sp", lambda e, rows=rows: e.dma_start(out=q_in, in_=PTM[rows, C_Q:C_Q + 2048]), reads=[b_PTM], writes=[b_qin])
                    kb.dma("sp", lambda e, rows=rows: e.dma_start(out=g_in, in_=PTM[rows, C_G:C_G + 4096]), reads=[b_PTM], writes=[b_gin])
                    rope(k_in, b_kin, [(kt_, b_kt, CO_DK), (kz_, b_kz, CO_DKZ)])
                    rope(q_in, b_qin, [(qt_, b_qt, CO_DQ)])
                    kb.op("act", lambda e: e.activation(out=sg.rearrange("p h f -> p (h f)"), in_=g_in, func=AF.Silu),
                          reads=[b_gin], writes=[b_sg])
                    for (src, b_src, dstT, b_dstT) in ((qt_, b_qt, qT, b_qT), (kt_, b_kt, kT, b_kT)):
                        s2 = src.rearrange("p h f -> p (h f)")
                        for k in range(16):
                            kb.op("pe", lambda e, k=k, s2=s2: e.transpose(out=ptr[:, k, :], in_=s2[:, k * 128:(k + 1) * 128],
                                                                          identity=ident_bf), reads=[b_src, b_ident], writes=[b_ptr])
                        kb.op("act", lambda e, dstT=dstT: e.activation(out=dstT, in_=ptr, func=AF.Copy), reads=[b_ptr], writes=[b_dstT])
                else:
                    rope(k_in, b_kin, [(kz_, b_kz, CO_DKZ)])
                for h in range(RH):
                    for _ in range(-(-pc_left // (RH - h))):
                        next(pc_gen, None)
                        pc_left -= 1
                    vh = v_in[:, h * 512:(h + 1) * 512]
                    if main:
                        pi = h % 2
                        for c in range(2):
                            kb.op("pe", lambda e, h=h, c=c: e.matmul(ps_s[:, 0:128], lhsT=kT[:, 2 * h + c, :], rhs=qT[:, 2 * h + c, :],
                                                                     start=(c == 0), stop=(c == 1)), reads=[b_kT, b_qT], writes=[b_pss])
                        kb.op("dve", lambda e, pi=pi: e.tensor_tensor(out=sTm[pi], in0=ps_s[:, 0:128], in1=UT, op=ALU.mult),
                              reads=[b_pss, b_cst], writes=[b_sTm[pi]])
                        kb.op("pe", lambda e, pi=pi, vh=vh: e.matmul(ps_o[pi], lhsT=sTm[pi], rhs=vh, start=True, stop=False),
                              reads=[b_sTm[pi], b_vin], writes=[b_pso[pi]])
                        for c in range(2):
                            kb.op("pe", lambda e, pi=pi, h=h, c=c: e.matmul(ps_o[pi], lhsT=qT[:, 2 * h + c, :], rhs=st_b[:, h, c, :],
                                                                            start=False, stop=(c == 1)),
                                  reads=[b_qT, b_stb[h]], writes=[b_pso[pi]])
                        kb.op("dve", lambda e, pi=pi, h=h: e.tensor_copy(out=o_sb[:, h, :], in_=ps_o[pi]), reads=[b_pso[pi]], writes=[b_osb[h]])
                        kb.op("act", lambda e, h=h: e.activation(out=junk, in_=o_sb[:, h, :], func=AF.Square,
                                                                 accum_out=ssq[:, h:h + 1]), reads=[b_osb[h]], writes=[b_junk, b_ssq])
                    for c in range(2):
                        kb.op("pe", lambda e, h=h, c=c, vh=vh: e.matmul(ps_u[c], lhsT=kz_[:, h, c * 128:(c + 1) * 128], rhs=vh,
                                                                        start=True, stop=True), reads=[b_kz, b_vin], writes=[b_psu[c]])
                        kb.op("dve", lambda e, h=h, c=c: e.scalar_tensor_tensor(out=st_f[:, h, c, :], in0=st_f[:, h, c, :],
                                                                                 scalar=cst[:, CO_G128 + h:CO_G128 + h + 1],
                                                                                 in1=ps_u[c], op0=ALU.mult, op1=ALU.add),
                              reads=[b_psu[c], b_cst, b_stf[h]], writes=[b_stf[h]])
                    kb.op("act", lambda e, h=h: e.activation(out=st_b[:, h], in_=st_f[:, h], func=AF.Copy), reads=[b_stf[h]], writes=[b_stb[h]])
                if main:
                    kb.op("dve", lambda e: e.tensor_scalar(out=ssq[:, 8:16], in0=ssq[:, 0:8], scalar1=1.0 / RDV, scalar2=EPS,
                                                           op0=ALU.mult, op1=ALU.add), reads=[b_ssq], writes=[b_ssq])
                    kb.op("act", lambda e: e.activation(out=ssq[:, 16:24], in_=ssq[:, 8:16], func=AF.Sqrt), reads=[b_ssq], writes=[b_ssq])
                    kb.op("dve", lambda e: e.reciprocal(out=ssq[:, 24:32], in_=ssq[:, 16:24]), reads=[b_ssq], writes=[b_ssq])
                    kb.op("dve", lambda e: e.tensor_tensor(out=o_sb, in0=o_sb, in1=bc_last(ssq[:, 24:32], 512), op=ALU.mult),
                          reads=b_osb + [b_ssq], writes=b_osb)
                    kb.op("dve", lambda e: e.tensor_tensor(out=og.rearrange("p (h f) -> p h f", h=RH), in0=o_sb, in1=sg, op=ALU.mult),
                          reads=b_osb + [b_sg], writes=[b_og])
                    transpose_store(og, b_og, 32, ptr, b_ptr, oT, b_oT, OGT_v, b_OGT, r0 - ROW0)
            for _ in pc_gen:
                pass
        kb.barrier()

    if "proj" in cfg.phases:
        phase_proj()
    def phase_ssd():
        with ExitStack() as es:
            cst, b_cst = load_consts(es)
            UT = cst[:, CO_UT:CO_UT + 128]
            SLm = cst[:, CO_SL:CO_SL + 128]
            ONES = cst[:, CO_ONE:CO_ONE + 128]
            diag = sb(es, "diag", [128, 48, 4, 128], BF16); b_diag = Buf()
            cwT = sb(es, "cwT", [128, 48, 8], F32); b_cwT = Buf()
            cb_bc = sb(es, "cb_bc", [128, 5120], BF16); b_cb = Buf()
            par = sb(es, "par", [128, 4, 64], F32); b_par = Buf()
            sst_f = sb(es, "sst_f", [128, SH, SP_], F32)
            sst_b = sb(es, "sst_b", [128, SH * SP_], BF16)
            b_sf = [Buf() for _ in range(SG)]; b_sbb = [Buf() for _ in range(SG)]
            ptr = ps(es, "ptr_s", [128, 16, 128], BF16); b_ptr = Buf()
            pcA = ps(es, "pcA", [128, 512], F32); b_pcA = Buf()
            pcB = ps(es, "pcB", [128, 512], F32); b_pcB = Buf()
            psm = ps(es, "psm", [128, 512], F32); b_psm = Buf()
            pseg = ps(es, "pseg", [128, 1024], F32); b_pseg = Buf()
            pst = ps(es, "pst", [128, 512], F32); b_pst = Buf()
            YST_v = YST.rearrange("(kt p) n -> p kt n", p=128)
            XB_v = XBCT.rearrange("(t p) c -> p t c", p=128)

            with ExitStack() as es2:
                cw5 = sb(es2, "cw5", [8, CONV_DIM], F32); b_cw5 = Buf()
                cbs = sb(es2, "cbs", [128, 5120], F32); b_cbs = Buf()
                kb.dma("sp", lambda e: e.dma_start(out=cw5[0:4, :], in_=conv_w), writes=[b_cw5])
                kb.dma("sp", lambda e: e.dma_start(out=cw5[4:5, :], in_=conv_b), writes=[b_cw5], accumulate=True)
                kb.dma("sp", lambda e: e.dma_start(out=cbs, in_=conv_b[0:1, 0:5120].partition_broadcast(128)), writes=[b_cbs])
                kb.op("dve", lambda e: e.tensor_copy(out=cb_bc, in_=cbs), reads=[b_cbs], writes=[b_cb])
                for t in range(48):
                    kb.op("pe", lambda e, t=t: e.transpose(out=psm[:, t * 8:t * 8 + 5], in_=cw5[0:5, t * 128:(t + 1) * 128],
                                                           identity=ident_f[0:5, 0:5]), reads=[b_cw5, b_ident], writes=[b_psm])
                kb.op("dve", lambda e: e.tensor_copy(out=cwT[:, :, 0:5], in_=psm[:, 0:384].rearrange("p (t e) -> p t e", e=8)[:, :, 0:5]),
                      reads=[b_psm], writes=[b_cwT])
                for t in range(48):
                    for w in range(4):
                        kb.op("dve", lambda e, t=t, w=w: e.tensor_scalar(out=diag[:, t, w, :], in0=ident_f, scalar1=cwT[:, t, w:w + 1],
                                                                         scalar2=None, op0=ALU.mult),
                              reads=[b_ident, b_cwT], writes=[b_diag])
                kb.dma("sp", lambda e: e.dma_start(out=par[:, 0, :], in_=dt_bias.partition_broadcast(128)), writes=[b_par])
                kb.dma("sp", lambda e: e.dma_start(out=par[:, 3, :], in_=a_log.partition_broadcast(128)), writes=[b_par], accumulate=True)
                kb.dma("sp", lambda e: e.dma_start(out=par[:, 2, :], in_=d_skip.partition_broadcast(128)), writes=[b_par], accumulate=True)
                kb.op("act", lambda e: e.activation(out=par[:, 1, :], in_=par[:, 3, :], func=AF.Exp), reads=[b_par], writes=[b_par])
                kb.op("dve", lambda e: e.tensor_scalar(out=par[:, 1, :], in0=par[:, 1, :], scalar1=-1.0, scalar2=None, op0=ALU.mult),
                      reads=[b_par], writes=[b_par])
                for g in range(SG):
                    kb.op("pool", lambda e, g=g: e.memset(sst_f[:, g * 8:(g + 1) * 8, :], 0.0), writes=[b_sf[g]])
                    kb.op("pool", lambda e, g=g: e.memset(sst_b[:, g * 512:(g + 1) * 512], 0.0), writes=[b_sbb[g]])
                kb.barrier()
            xw = sb(es, "xw", [128, 48, 131], BF16); b_xw = Buf()
            z_in = sb(es, "z_in", [128, 4096], BF16); b_zin = Buf()
            xs = sb(es, "xs", [128, SH, SP_], F32); b_xs = [Buf() for _ in range(SG)]
            xdt = sb(es, "xdt", [128, SH, SP_], BF16); b_xdt = Buf()
            decx = sb(es, "decx", [128, SH * SP_], BF16); b_decx = [Buf() for _ in range(SG)]
            Bt = sb(es, "Bt", [128, 1024], BF16); b_Bt = Buf()
            BCT = sb(es, "BCT", [128, 16, 128], BF16); b_BCT = Buf()
            segL = [sb(es, "segL%d" % i, [128, 8, 128], F32) for i in range(2)]; b_segL = [Buf(), Buf()]
            Lg = [sb(es, "Lg%d" % i, [128, 8, 128], F32) for i in range(2)]; b_Lg = [Buf(), Buf()]
            MT = [sb(es, "MT%d" % i, [128, 8, 128], BF16) for i in range(2)]; b_MT = [Buf(), Buf()]
            cbm = sb(es, "cbm", [128, 128], F32); b_cbm = Buf()
            tmpc = [sb(es, "tmpc%d" % i, [128, 512], F32) for i in range(2)]; b_tmpc = [Buf(), Buf()]
            sz = sb(es, "sz", [128, 4096], BF16); b_sz = Buf()
            ys = sb(es, "ys", [128, 4096], BF16); b_ys = Buf()
            oT = sb(es, "oT_s", [128, 32, 128], BF16); b_oT = Buf()
            sm = sb(es, "sm", [128, 8, 64], F32); b_sm = Buf()
            sm2 = sb(es, "sm2", [128, 128], F32); b_sm2 = Buf()
            dtr = sb(es, "dtr", [128, 64], F32); b_dtr = Buf()
            msk = sb(es, "msk", [128, 1], F32); b_msk = Buf()
            junk = sb(es, "junks", [128, 512], BF16); b_junk = Buf()
            ssq = sb(es, "ssq2", [128, 32], F32); b_ssq = Buf()
            xs2 = xs.rearrange("p h f -> p (h f)")
            xdt2 = xdt.rearrange("p h f -> p (h f)")
            sst_f2 = sst_f.rearrange("p h f -> p (h f)")

            for ch in range(cfg.nch):
                kb.maybe_epoch()
                main = ch >= cfg.pre
                r0 = ch * 128
                rows = slice(r0, r0 + 128)
                c0 = HALO + r0 - 3
                T = 48 if main else 40
                kb.dma("sp", lambda e, T=T, c0=c0: e.dma_start(out=xw[:, 0:T, :], in_=XB_v[:, 0:T, c0:c0 + 131]),
                       reads=[b_XBCT], writes=[b_xw])
                kb.dma("sp", lambda e, rows=rows: e.dma_start(out=dtr, in_=DTS[rows, :]), reads=[b_DTS], writes=[b_dtr])
                kb.dma("sp", lambda e, rows=rows: e.dma_start(out=msk, in_=rowmask[rows, :]), writes=[b_msk])
                if main:
                    kb.dma("sp", lambda e, rows=rows: e.dma_start(out=z_in, in_=PTM[rows, C_Z:C_Z + 4096]), reads=[b_PTM], writes=[b_zin])
                S = lambda i: sm[:, i, :]
                kb.op("dve", lambda e: e.tensor_tensor(out=S(0), in0=dtr, in1=par[:, 0, :], op=ALU.add), reads=[b_dtr, b_par], writes=[b_sm])
                kb.op("act", lambda e: e.activation(out=S(1), in_=S(0), func=AF.Abs), reads=[b_sm], writes=[b_sm])
                kb.op("act", lambda e: e.activation(out=S(2), in_=S(1), func=AF.Exp, scale=-1.0), reads=[b_sm], writes=[b_sm])
                kb.op("act", lambda e: e.activation(out=S(3), in_=S(2), func=AF.Ln, bias=ONES[:, 0:1]), reads=[b_sm, b_cst], writes=[b_sm])
                kb.op("dve", lambda e: e.scalar_tensor_tensor(out=S(4), in0=S(0), scalar=0.0, in1=S(3), op0=ALU.max, op1=ALU.add),
                      reads=[b_sm], writes=[b_sm])
                kb.op("dve", lambda e: e.tensor_scalar(out=S(5), in0=S(4), scalar1=msk[:, 0:1], scalar2=None, op0=ALU.mult),
                      reads=[b_sm, b_msk], writes=[b_sm])
                kb.op("dve", lambda e: e.tensor_tensor(out=S(6), in0=S(4), in1=par[:, 1, :], op=ALU.mult), reads=[b_sm, b_par], writes=[b_sm])
                a_ap = S(6)
                for bnk in range(10):
                    pc, b_pc = (pcA, b_pcA) if bnk % 2 == 0 else (pcB, b_pcB)
                    for tt in range(4):
                        t = bnk * 4 + tt
                        for w in range(4):
                            kb.op("pe", lambda e, pc=pc, t=t, tt=tt, w=w: e.matmul(pc[:, tt * 128:(tt + 1) * 128], lhsT=xw[:, t, w:w + 128],
                                                                                   rhs=diag[:, t, w, :], start=(w == 0), stop=(w == 3)),
                                  reads=[b_xw, b_diag], writes=[b_pc])
                    ti = bnk % 2
                    kb.op("dve", lambda e, pc=pc, ti=ti, bnk=bnk: e.tensor_tensor(out=tmpc[ti], in0=pc, in1=cb_bc[:, bnk * 512:(bnk + 1) * 512],
                                                                                  op=ALU.add), reads=[b_pc, b_cb], writes=[b_tmpc[ti]])
                    if bnk < 8:
                        kb.op("act", lambda e, ti=ti, bnk=bnk: e.activation(out=xs2[:, bnk * 512:(bnk + 1) * 512], in_=tmpc[ti], func=AF.Silu),
                              reads=[b_tmpc[ti]], writes=[b_xs[bnk]])
                    else:
                        kb.op("act", lambda e, ti=ti, bnk=bnk: e.activation(out=Bt[:, (bnk - 8) * 512:(bnk - 7) * 512], in_=tmpc[ti], func=AF.Silu),
                              reads=[b_tmpc[ti]], writes=[b_Bt])
                for bnk in range(4 if main else 2):
                    pc, b_pc = (pcA, b_pcA) if bnk % 2 == 0 else (pcB, b_pcB)
                    for tt in range(4):
                        t = 32 + bnk * 4 + tt
                        for w in range(4):
                            kb.op("pe", lambda e, pc=pc, t=t, tt=tt, w=w: e.matmul(pc[:, tt * 128:(tt + 1) * 128], lhsT=diag[:, t, w, :],
                                                                                   rhs=xw[:, t, w:w + 128], start=(w == 0), stop=(w == 3)),
                                  reads=[b_xw, b_diag], writes=[b_pc])
                    for tt in range(4):
                        t = 32 + bnk * 4 + tt
                        kb.op("act", lambda e, pc=pc, t=t, tt=tt: e.activation(out=BCT[:, t - 32, :], in_=pc[:, tt * 128:(tt + 1) * 128],
                                                                               func=AF.Silu, bias=cwT[:, t, 4:5]),
                              reads=[b_pc, b_cwT], writes=[b_BCT])
                kb.op("dve", lambda e: e.tensor_tensor(out=xdt, in0=xs, in1=bc_last(S(5), 64), op=ALU.mult), reads=b_xs + [b_sm], writes=[b_xdt])
                if main:
                    kb.op("dve", lambda e: e.tensor_tensor(out=xs, in0=xs, in1=bc_last(par[:, 2, :], 64), op=ALU.mult),
                          reads=b_xs + [b_par], writes=b_xs)
                    kb.op("act", lambda e: e.activation(out=sz, in_=z_in, func=AF.Silu), reads=[b_zin], writes=[b_sz])
                kb.op("pe", lambda e: e.matmul(psm[:, 0:64], lhsT=UT, rhs=a_ap, start=True, stop=True), reads=[b_cst, b_sm], writes=[b_psm])
                kb.op("pe", lambda e: e.matmul(psm[:, 64:128], lhsT=ONES, rhs=a_ap, start=True, stop=True), reads=[b_cst, b_sm], writes=[b_psm])
                kb.op("act", lambda e: e.activation(out=sm2, in_=psm[:, 0:128], func=AF.Exp), reads=[b_psm], writes=[b_sm2])
                eacs, cdec = sm2[:, 0:64], sm2[:, 64:128]
                for g in range(SG):
                    i2 = g % 2
                    hs = slice(g * 8, (g + 1) * 8)
                    cs = slice(g * 512, (g + 1) * 512)
                    kb.op("pool", lambda e, i2=i2, hs=hs: e.tensor_tensor(out=segL[i2], in0=bc_mid(SLm, 8), in1=bc_last(a_ap[:, hs], 128),
                                                                          op=ALU.mult), reads=[b_cst, b_sm], writes=[b_segL[i2]])
                    for hh in range(8):
                        kb.op("pe", lambda e, i2=i2, hh=hh: e.matmul(pseg[:, hh * 128:(hh + 1) * 128], lhsT=segL[i2][:, hh, :], rhs=UT,
                                                                     start=True, stop=True), reads=[b_segL[i2], b_cst], writes=[b_pseg])
                    Lg2 = Lg[i2].rearrange("p h i -> p (h i)")
                    for hf in range(2):
                        kb.op("act", lambda e, Lg2=Lg2, hf=hf: e.activation(out=Lg2[:, hf * 512:(hf + 1) * 512],
                                                                            in_=pseg[:, hf * 512:(hf + 1) * 512], func=AF.Exp),
                              reads=[b_pseg], writes=[b_Lg[i2]])
                    dec_g = Lg[i2][:, :, 127]
                    kb.op("dve", lambda e, i2=i2, hs=hs, dec_g=dec_g: e.tensor_tensor(out=decx.rearrange("p (h f) -> p h f", f=64)[:, hs, :],
                                                                                      in0=xdt[:, hs, :],
                                                                                      in1=dec_g.unsqueeze(2).to_broadcast([128, 8, 64]),
                                                                                      op=ALU.mult),
                          reads=[b_xdt, b_Lg[i2]], writes=[b_decx[g]])
                    if main:
                        kb.op("pe", lambda e, g=g: e.matmul(psm[:, 128:256], lhsT=BCT[:, g, :], rhs=BCT[:, 8 + g, :], start=True, stop=True),
                              reads=[b_BCT], writes=[b_psm])
                        kb.op("dve", lambda e: e.tensor_tensor(out=cbm, in0=psm[:, 128:256], in1=UT, op=ALU.mult),
                              reads=[b_psm, b_cst], writes=[b_cbm])
                        kb.op("pool", lambda e, i2=i2: e.tensor_tensor(out=MT[i2], in0=Lg[i2], in1=bc_mid(cbm, 8), op=ALU.mult),
                              reads=[b_Lg[i2], b_cbm], writes=[b_MT[i2]])
                        for hh in range(8):
                            kb.op("pe", lambda e, i2=i2, hh=hh, g=g: e.matmul(pcA[:, hh * 64:(hh + 1) * 64], lhsT=MT[i2][:, hh, :],
                                                                              rhs=xdt[:, g * 8 + hh, :], start=True, stop=True),
                                  reads=[b_MT[i2], b_xdt], writes=[b_pcA])
                        kb.op("pe", lambda e, g=g, cs=cs: e.matmul(pcB, lhsT=BCT[:, 8 + g, :], rhs=sst_b[:, cs], start=True, stop=True),
                              reads=[b_BCT, b_sbb[g]], writes=[b_pcB])
                        kb.op("dve", lambda e, hs=hs: e.tensor_tensor(out=tmpc[0].rearrange("p (h f) -> p h f", f=64),
                                                                      in0=pcB.rearrange("p (h f) -> p h f", f=64),
                                                                      in1=bc_last(eacs[:, hs], 64), op=ALU.mult),
                              reads=[b_pcB, b_sm2], writes=[b_tmpc[0]])
                        kb.op("dve", lambda e: e.tensor_tensor(out=tmpc[1], in0=tmpc[0], in1=pcA, op=ALU.add),
                              reads=[b_tmpc[0], b_pcA], writes=[b_tmpc[1]])
                        kb.op("dve", lambda e, cs=cs: e.tensor_tensor(out=xs2[:, cs], in0=xs2[:, cs], in1=tmpc[1], op=ALU.add),
                              reads=[b_xs[g], b_tmpc[1]], writes=[b_xs[g]])
                    kb.op("pe", lambda e, g=g, cs=cs: e.matmul(pst, lhsT=Bt[:, g * 128:(g + 1) * 128], rhs=decx[:, cs], start=True, stop=True),
                          reads=[b_Bt, b_decx[g]], writes=[b_pst])
                    kb.op("dve", lambda e, hs=hs: e.tensor_tensor(out=sst_f[:, hs, :], in0=sst_f[:, hs, :], in1=bc_last(cdec[:, hs], 64),
                                                                  op=ALU.mult), reads=[b_sf[g], b_sm2], writes=[b_sf[g]])
                    kb.op("dve", lambda e, cs=cs: e.tensor_tensor(out=sst_f2[:, cs], in0=sst_f2[:, cs], in1=pst, op=ALU.add),
                          reads=[b_sf[g], b_pst], writes=[b_sf[g]])
                    kb.op("act", lambda e, cs=cs: e.activation(out=sst_b[:, cs], in_=sst_f2[:, cs], func=AF.Copy),
                          reads=[b_sf[g]], writes=[b_sbb[g]])
                if main:
                    kb.op("dve", lambda e: e.tensor_tensor(out=xs2, in0=xs2, in1=sz, op=ALU.mult), reads=b_xs + [b_sz], writes=b_xs)
                    for g in range(SG):
                        kb.op("act", lambda e, g=g: e.activation(out=junk, in_=xs2[:, g * 512:(g + 1) * 512], func=AF.Square,
                                                                 accum_out=ssq[:, g:g + 1]), reads=[b_xs[g]], writes=[b_junk, b_ssq])
                    kb.op("dve", lambda e: e.tensor_scalar(out=ssq[:, 8:16], in0=ssq[:, 0:8], scalar1=1.0 / 512, scalar2=EPS,
                                                           op0=ALU.mult, op1=ALU.add), reads=[b_ssq], writes=[b_ssq])
                    kb.op("act", lambda e: e.activation(out=ssq[:, 16:24], in_=ssq[:, 8:16], func=AF.Sqrt), reads=[b_ssq], writes=[b_ssq])
                    kb.op("dve", lambda e: e.reciprocal(out=ssq[:, 24:32], in_=ssq[:, 16:24]), reads=[b_ssq], writes=[b_ssq])
                    kb.op("dve", lambda e: e.tensor_tensor(out=ys.rearrange("p (g f) -> p g f", f=512),
                                                           in0=xs2.rearrange("p (g f) -> p g f", f=512),
                                                           in1=bc_last(ssq[:, 24:32], 512), op=ALU.mult),
                          reads=b_xs + [b_ssq], writes=[b_ys])
                    transpose_store(ys, b_ys, 32, ptr, b_ptr, oT, b_oT, YST_v, b_YST, r0 - ROW0)
        kb.barrier()

    if "ret" in cfg.phases:
        phase_ret()
    def phase_out():
        with ExitStack() as es:
            TBC = 3
            TBM = TBC * 128
            g_bc = sb(es, "gf_bc", [128, D], F32); b_g = Buf()
            kb.dma("sp", lambda e: e.dma_start(out=g_bc, in_=norm_ffn_g.partition_broadcast(128)), writes=[b_g])
            skT = sb(es, "skT", [128, 16, 128], F32); b_skT = Buf()
            ptr = ps(es, "ptr_o", [128, 16, 128], BF16); b_ptr = Buf()
            pA = ps(es, "pA", [128, 512], F32); b_pA = Buf()
            pB = ps(es, "pB", [128, 512], F32); b_pB = Buf()
            pC = [ps(es, "pC%d" % i, [128, 512], F32) for i in range(2)]; b_pC = [Buf(), Buf()]
            pD = ps(es, "pD", [128, 512], F32); b_pD = Buf()

            with ExitStack() as es2:
                skr = sb(es2, "skr", [128, 16, 128], F32); b_skr = Buf()
                kb.dma("sp", lambda e: e.dma_start(out=skr, in_=peer_sub_keys.rearrange("(j k) d -> k j d", k=128)), writes=[b_skr])
                for j in range(16):
                    kb.op("pe", lambda e, j=j: e.transpose(out=pC[j % 2][:, 0:128], in_=skr[:, j, :], identity=ident_f),
                          reads=[b_skr, b_ident], writes=[b_pC[j % 2]])
                    kb.op("dve", lambda e, j=j: e.tensor_copy(out=skT[:, j, :], in_=pC[j % 2][:, 0:128]), reads=[b_pC[j % 2]], writes=[b_skT])
                if not precast_state["done"]:
                    for _ in precast_units(es2, pA, b_pA):
                        pass
                kb.barrier()

            ogT = sb(es, "ogT", [128, 32, TBM], BF16); b_ogT = Buf()
            ysT = sb(es, "ysT", [128, 32, TBM], BF16); b_ysT = Buf()
            mT = sb(es, "mT", [128, 16, TBM], BF16); b_mT = Buf()
            xT = sb(es, "xT", [128, 16, TBM], BF16); b_xT = Buf()
            hb = sb(es, "hb", [128, TBC, D], F32); b_hb = [Buf() for _ in range(TBC)]
            NWB = 4
            wbf = [sb(es, "wbf_o%d" % i, [128, 4096], BF16) for i in range(NWB)]; b_wbf = [Buf() for _ in range(NWB)]
            grt = [sb(es, "grt%d" % i, [128, 2, TBM], BF16) for i in range(2)]; b_grt = [Buf(), Buf()]
            sgt = [sb(es, "sgt%d" % i, [128, 2, TBM], F32) for i in range(2)]; b_sgt = [Buf(), Buf()]
            t1 = sb(es, "t1o", [128, TBM], F32); b_t1 = Buf()
            t2 = sb(es, "t2o", [128, TBM], F32); b_t2 = Buf()
            n_bf = sb(es, "n_bf_o", [128, D], BF16); b_n = Buf()
            junk = sb(es, "junk_o", [128, D], BF16); b_junk = Buf()
            stat = sb(es, "stat_o", [128, 4], F32); b_stat = Buf()
            qpT = sb(es, "qpT", [128, TBM], F32); b_qpT = Buf()
            sct = sb(es, "sct", [128, TBC, 128], F32); b_sct = Buf()
            OGT_v = OGT.rearrange("(kt p) n -> p kt n", p=128)
            YST_v = YST.rearrange("(kt p) n -> p kt n", p=128)
            wr_v = WRB.rearrange("(kt p) c -> p kt c", p=128)
            ws_v = WSB.rearrange("(kt p) c -> p kt c", p=128)
            wo_v = WOB.rearrange("(kt p) c -> p kt c", p=128)
            wq_v = WQB.rearrange("(kt p) c -> p kt c", p=128)
            SC_v = SC.rearrange("(t p) c -> p t c", p=128)
            cnt = {"w": 0, "g": 0, "pc": 0}

            def load_w(view, kts, c0, ncol, scale_g=False):
                wi = cnt["w"] % NWB
                cnt["w"] += 1
                dsb = wbf[wi][:, 0:kts * ncol].rearrange("p (k c) -> p k c", c=ncol)
                kb.dma("sp", lambda e: e.dma_start(out=dsb, in_=view[:, :, c0:c0 + ncol]), reads=[b_WB], writes=[b_wbf[wi]])
                return dsb, b_wbf[wi]

            ch = 0
            while ch < cfg.main:
                nsub = min(TBC, cfg.main - ch)
                ntok = nsub * 128
                c0 = ch * 128
                kb.dma("sp", lambda e, c0=c0, ntok=ntok: e.dma_start(out=ogT[:, :, 0:ntok], in_=OGT_v[:, :, c0:c0 + ntok]),
                       reads=[b_OGT], writes=[b_ogT])
                kb.dma("sp", lambda e, c0=c0, ntok=ntok: e.dma_start(out=ysT[:, :, 0:ntok], in_=YST_v[:, :, c0:c0 + ntok]),
                       reads=[b_YST], writes=[b_ysT])
                for t in range(nsub):
                    rows = slice(ROW0 + c0 + t * 128, ROW0 + c0 + (t + 1) * 128)
                    kb.dma("sp", lambda e, t=t, rows=rows: e.dma_start(out=hb[:, t, :], in_=hin[rows, :]), writes=[b_hb[t]])
                for m in range(16):
                    kb.maybe_epoch()
                    wr, b_wr = load_w(wr_v, 32, m * 128, 128)
                    for kt in range(32):
                        kb.op("pe", lambda e, kt=kt, wr=wr, ntok=ntok: e.matmul(pA[:, 0:ntok], lhsT=wr[:, kt, :], rhs=ogT[:, kt, 0:ntok],
                                                                                start=(kt == 0), stop=(kt == 31)),
                              reads=[b_wr, b_ogT], writes=[b_pA])
                    ws, b_ws = load_w(ws_v, 32, m * 128, 128, scale_g=True)
                    for kt in range(32):
                        kb.op("pe", lambda e, kt=kt, ws=ws, ntok=ntok: e.matmul(pB[:, 0:ntok], lhsT=ws[:, kt, :], rhs=ysT[:, kt, 0:ntok],
                                                                                start=(kt == 0), stop=(kt == 31)),
                              reads=[b_ws, b_ysT], writes=[b_pB])
                    gi = cnt["g"] % 2
                    cnt["g"] += 1
                    kb.dma("sp", lambda e, gi=gi, m=m, c0=c0, ntok=ntok: e.dma_start(out=grt[gi][:, 0, 0:ntok],
                                                                                     in_=GT[m * 128:(m + 1) * 128, c0:c0 + ntok]),
                           reads=[b_GT], writes=[b_grt[gi]])
                    kb.dma("sp", lambda e, gi=gi, m=m, c0=c0, ntok=ntok: e.dma_start(out=grt[gi][:, 1, 0:ntok],
                                                                                     in_=GT[D + m * 128:D + (m + 1) * 128, c0:c0 + ntok]),
                           reads=[b_GT], writes=[b_grt[gi]], accumulate=True)
                    kb.op("act", lambda e, gi=gi, ntok=ntok: e.activation(out=sgt[gi][:, :, 0:ntok], in_=grt[gi][:, :, 0:ntok], func=AF.Sigmoid),
                          reads=[b_grt[gi]], writes=[b_sgt[gi]])
                    kb.op("dve", lambda e, gi=gi, ntok=ntok: e.tensor_tensor(out=t1[:, 0:ntok], in0=pA[:, 0:ntok], in1=sgt[gi][:, 0, 0:ntok],
                                                                             op=ALU.mult), reads=[b_pA, b_sgt[gi]], writes=[b_t1])
                    kb.op("dve", lambda e, gi=gi, ntok=ntok: e.tensor_tensor(out=t2[:, 0:ntok], in0=pB[:, 0:ntok], in1=sgt[gi][:, 1, 0:ntok],
                                                                             op=ALU.mult), reads=[b_pB, b_sgt[gi]], writes=[b_t2])
                    kb.op("dve", lambda e, m=m, ntok=ntok: e.tensor_tensor(out=mT[:, m, 0:ntok], in0=t1[:, 0:ntok], in1=t2[:, 0:ntok],
                                                                           op=ALU.add), reads=[b_t1, b_t2], writes=[b_mT])
                for nb in range(8):
                    wo, b_wo = load_w(wo_v, 16, nb * 256, 256)
                    for t in range(nsub):
                        pi = cnt["pc"] % 2
                        cnt["pc"] += 1
                        for kt in range(16):
                            kb.op("pe", lambda e, pi=pi, kt=kt, t=t, wo=wo: e.matmul(pC[pi][:, 0:256], lhsT=mT[:, kt, t * 128:(t + 1) * 128],
                                                                                     rhs=wo[:, kt, :], start=(kt == 0), stop=(kt == 15)),
                                  reads=[b_mT, b_wo], writes=[b_pC[pi]])
                        kb.op("dve", lambda e, pi=pi, t=t, nb=nb: e.tensor_tensor(out=hb[:, t, nb * 256:(nb + 1) * 256],
                                                                                  in0=hb[:, t, nb * 256:(nb + 1) * 256], in1=pC[pi][:, 0:256],
                                                                                  op=ALU.add), reads=[b_hb[t], b_pC[pi]], writes=[b_hb[t]])
                for t in range(nsub):
                    rows = slice(c0 + t * 128, c0 + (t + 1) * 128)
                    kb.dma("sp", lambda e, t=t, rows=rows: e.dma_start(out=H1[rows, :], in_=hb[:, t, :]), reads=[b_hb[t]], writes=[b_H1],
                           accumulate=True)
                    kb.op("act", lambda e, t=t: e.activation(out=junk, in_=hb[:, t, :], func=AF.Square, accum_out=stat[:, 0:1]),
                          reads=[b_hb[t]], writes=[b_junk, b_stat])
                    kb.op("dve", lambda e: e.tensor_scalar(out=stat[:, 1:2], in0=stat[:, 0:1], scalar1=1.0 / D, scalar2=EPS,
                                                           op0=ALU.mult, op1=ALU.add), reads=[b_stat], writes=[b_stat])
                    kb.op("act", lambda e: e.activation(out=stat[:, 3:4], in_=stat[:, 1:2], func=AF.Sqrt), reads=[b_stat], writes=[b_stat])
                    kb.op("dve", lambda e: e.reciprocal(out=stat[:, 2:3], in_=stat[:, 3:4]), reads=[b_stat], writes=[b_stat])
                    kb.op("dve", lambda e, t=t: e.scalar_tensor_tensor(out=n_bf, in0=hb[:, t, :], scalar=stat[:, 2:3], in1=g_bc,
                                                                       op0=ALU.mult, op1=ALU.mult), reads=[b_hb[t], b_stat, b_g], writes=[b_n])
                    kb.dma("sp", lambda e, rows=rows: e.dma_start(out=XN2[rows, :], in_=n_bf), reads=[b_n], writes=[b_XN2], accumulate=True)
                    for k in range(16):
                        kb.op("pe", lambda e, k=k: e.transpose(out=ptr[:, k, :], in_=n_bf[:, k * 128:(k + 1) * 128], identity=ident_bf),
                              reads=[b_n, b_ident], writes=[b_ptr])
                    kb.op("act", lambda e, t=t: e.activation(out=xT[:, :, t * 128:(t + 1) * 128], in_=ptr, func=AF.Copy),
                          reads=[b_ptr], writes=[b_xT])
                for j in range(16):
                    wq, b_wq = load_w(wq_v, 16, j * 128, 128)
                    pi = cnt["pc"] % 2
                    cnt["pc"] += 1
                    for kt in range(16):
                        kb.op("pe", lambda e, pi=pi, kt=kt, wq=wq, ntok=ntok: e.matmul(pC[pi][:, 0:ntok], lhsT=wq[:, kt, :], rhs=xT[:, kt, 0:ntok],
                                                                                       start=(kt == 0), stop=(kt == 15)),
                              reads=[b_wq, b_xT], writes=[b_pC[pi]])
                    kb.op("act", lambda e, pi=pi, ntok=ntok: e.activation(out=qpT[:, 0:ntok], in_=pC[pi][:, 0:ntok], func=AF.Copy),
                          reads=[b_pC[pi]], writes=[b_qpT])
                    for t in range(nsub):
                        kb.op("pe", lambda e, t=t, j=j: e.matmul(pD[:, t * 128:(t + 1) * 128], lhsT=qpT[:, t * 128:(t + 1) * 128], rhs=skT[:, j, :],
                                                                 start=True, stop=True), reads=[b_qpT, b_skT], writes=[b_pD])
                    kb.op("dve", lambda e, nsub=nsub, ntok=ntok: e.tensor_copy(out=sct[:, 0:nsub, :],
                                                                               in_=pD[:, 0:ntok].rearrange("p (t k) -> p t k", k=128)),
                          reads=[b_pD], writes=[b_sct])
                    kb.dma("sp", lambda e, j=j, ch=ch, nsub=nsub: e.dma_start(out=SC_v[:, ch:ch + nsub, j * 128:(j + 1) * 128], in_=sct[:, 0:nsub, :]),
                           reads=[b_sct], writes=[b_SC], accumulate=True)
                ch += nsub
        kb.barrier()

    if "ssd" in cfg.phases:
        phase_ssd()
    def phase_peer():
        with ExitStack() as es:
            NEG = -1.0e30
            NB = 6
            gf_bc = sb(es, "gfin_bc", [128, D], F32); b_gf = Buf()
            kb.dma("sp", lambda e: e.dma_start(out=gf_bc, in_=norm_final_g.partition_broadcast(128)), writes=[b_gf])
            iota = sb(es, "iota", [128, 256], F32); b_iota = Buf()
            iota_i = sb(es, "iota_i", [128, 256], I32)
            kb.op("pool", lambda e: e.iota(iota_i, pattern=[[1, 256]], base=0, channel_multiplier=0), writes=[b_iota])
            kb.op("dve", lambda e: e.tensor_copy(out=iota, in_=iota_i), reads=[b_iota], writes=[b_iota])
            if not precast_state["done"]:
                with ExitStack() as es2:
                    ptmp = ps(es2, "ptmp", [128, 512], F32); b_ptmp = Buf()
                    for _ in precast_units(es2, ptmp, b_ptmp):
                        pass
                    kb.barrier()
            sc = sb(es, "sc", [128, 16, 128], F32); b_sc = Buf()
            work = sb(es, "work", [128, 256], F32); b_work = Buf()
            v16 = sb(es, "v16", [128, 16, 16], F32); b_v16 = Buf()
            i16 = sb(es, "i16", [128, 16, 16], U32); b_i16 = Buf()
            i16f = sb(es, "i16f", [128, 16, 16], F32); b_i16f = Buf()
            i16s = sb(es, "i16s", [128, 16, 16], F32); b_i16s = Buf()
            cand = sb(es, "cand", [128, 8, 256], F32); b_cand = Buf()
            cid = sb(es, "cid", [128, 8, 256], F32); b_cid = Buf()
            top = sb(es, "top", [128, 8, 16], F32); b_top = Buf()
            pos = sb(es, "pos", [128, 8, 16], U32); b_pos = Buf()
            posf = sb(es, "posf", [128, 8, 16], F32); b_posf = Buf()
            eq = sb(es, "eq", [128, 16, 256], F32); b_eq = Buf()
            eidf = sb(es, "eidf", [128, 128], F32); b_eidf = Buf()
            eid2 = [sb(es, "eid%d" % i, [128, 128], I32) for i in range(2)]; b_eid2 = [Buf(), Buf()]
            gsm2 = [sb(es, "gsm%d" % i, [128, 4, 128], F32) for i in range(2)]; b_gsm2 = [Buf(), Buf()]
            gz = sb(es, "gz", [128, 16], F32); b_gz = Buf()
            act_t = sb(es, "act_t", [128, 128], F32); b_act = Buf()
            wgt = sb(es, "wgt", [128, 128], F32); b_wgt = Buf()
            GS, NGB = 4, 3
            dgs = [sb(es, "dgs%d" % i, [128, GS, 128], BF16) for i in range(NGB)]; b_dgs = [Buf() for _ in range(NGB)]
            uv = [sb(es, "uv%d" % i, [128, GS, 2 * D], BF16) for i in range(NGB)]
            b_uv = [[Buf() for _ in range(GS)] for _ in range(NGB)]
            b_actg = [Buf() for _ in range(128 // GS)]
            xn2_ = [sb(es, "xn%d" % i, [128, D], BF16) for i in range(2)]; b_xn2 = [Buf(), Buf()]
            h1t2 = [sb(es, "h1t%d" % i, [128, D], F32) for i in range(2)]; b_h12 = [Buf(), Buf()]
            junk = sb(es, "junk_p", [128, D], BF16); b_junk = Buf()
            prod = [sb(es, "prod%d" % i, [128, D], BF16) for i in range(2)]; b_prod = [Buf(), Buf()]
            stat = sb(es, "stat_p", [128, 4], F32); b_stat = Buf()
            ot = sb(es, "ot", [128, D], F32); b_ot = Buf()
            pacc = [ps(es, "pacc%d" % i, [128, 512], F32) for i in range(4)]; b_pacc = [Buf() for _ in range(4)]
            cnt = {"u": 0, "v": 0}

            def routing(ch):
                rows = slice(ch * 128, (ch + 1) * 128)
                par = ch % 2
                eid, b_eid, gsm, b_gsm = eid2[par], b_eid2[par], gsm2[par], b_gsm2[par]
                xn, b_xn, h1t, b_h1 = xn2_[par], b_xn2[par], h1t2[par], b_h12[par]
                kb.dma("sp", lambda e, rows=rows: e.dma_start(out=sc.rearrange("p j k -> p (j k)"), in_=SC[rows, :]), reads=[b_SC], writes=[b_sc])
                kb.dma("sp", lambda e, rows=rows: e.dma_start(out=xn, in_=XN2[rows, :]), reads=[b_XN2], writes=[b_xn])
                kb.dma("sp", lambda e, rows=rows: e.dma_start(out=h1t, in_=H1[rows, :]), reads=[b_H1], writes=[b_h1])
                for j in range(16):
                    kb.op("dve", lambda e, j=j: e.max(out=v16[:, j, 0:8], in_=sc[:, j, :]), reads=[b_sc], writes=[b_v16])
                    kb.op("dve", lambda e, j=j: e.match_replace(out=work[:, 0:128], in_to_replace=v16[:, j, 0:8], in_values=sc[:, j, :],
                                                                imm_value=NEG), reads=[b_sc, b_v16], writes=[b_work])
                    kb.op("dve", lambda e, j=j: e.max(out=v16[:, j, 8:16], in_=work[:, 0:128]), reads=[b_work, b_v16], writes=[b_v16])
                    kb.op("dve", lambda e, j=j: e.max_index(out=i16[:, j, 0:8], in_max=v16[:, j, 0:8], in_values=sc[:, j, :]),
                          reads=[b_sc, b_v16], writes=[b_i16])
                    kb.op("dve", lambda e, j=j: e.max_index(out=i16[:, j, 8:16], in_max=v16[:, j, 8:16], in_values=sc[:, j, :]),
                          reads=[b_sc, b_v16, b_i16], writes=[b_i16])
                kb.op("dve", lambda e: e.tensor_copy(out=i16f, in_=i16), reads=[b_i16], writes=[b_i16f])
                v4 = v16.rearrange("p (h c) k -> p h c k", c=2)
                f4 = i16f.rearrange("p (h c) k -> p h c k", c=2)
                s4 = i16s.rearrange("p (h c) k -> p h c k", c=2)
                c4 = cand.rearrange("p h (a b) -> p h a b", b=16)
                d4 = cid.rearrange("p h (a b) -> p h a b", b=16)
                kb.op("dve", lambda e: e.tensor_tensor(out=c4, in0=v4[:, :, 0, :].unsqueeze(3).to_broadcast([128, 8, 16, 16]),
                                                       in1=v4[:, :, 1, :].unsqueeze(2).to_broadcast([128, 8, 16, 16]), op=ALU.add),
                      reads=[b_v16], writes=[b_cand])
                kb.op("dve", lambda e: e.tensor_scalar(out=i16s, in0=i16f, scalar1=float(PNK), scalar2=None, op0=ALU.mult),
                      reads=[b_i16f], writes=[b_i16s])
                kb.op("dve", lambda e: e.tensor_tensor(out=d4, in0=s4[:, :, 0, :].unsqueeze(3).to_broadcast([128, 8, 16, 16]),
                                                       in1=f4[:, :, 1, :].unsqueeze(2).to_broadcast([128, 8, 16, 16]), op=ALU.add),
                      reads=[b_i16f, b_i16s], writes=[b_cid])
                for h in range(PH):
                    kb.op("dve", lambda e, h=h: e.max(out=top[:, h, 0:8], in_=cand[:, h, :]), reads=[b_cand], writes=[b_top])
                    kb.op("dve", lambda e, h=h: e.match_replace(out=work, in_to_replace=top[:, h, 0:8], in_values=cand[:, h, :],
                                                                imm_value=NEG), reads=[b_cand, b_top], writes=[b_work])
                    kb.op("dve", lambda e, h=h: e.max(out=top[:, h, 8:16], in_=work), reads=[b_work, b_top], writes=[b_top])
                    kb.op("dve", lambda e, h=h: e.max_index(out=pos[:, h, 0:8], in_max=top[:, h, 0:8], in_values=cand[:, h, :]),
                          reads=[b_cand, b_top], writes=[b_pos])
                    kb.op("dve", lambda e, h=h: e.max_index(out=pos[:, h, 8:16], in_max=top[:, h, 8:16], in_values=cand[:, h, :]),
                          reads=[b_cand, b_top, b_pos], writes=[b_pos])
                kb.op("dve", lambda e: e.tensor_copy(out=posf, in_=pos), reads=[b_pos], writes=[b_posf])
                for h in range(PH):
                    kb.op("dve", lambda e, h=h: e.tensor_tensor(out=eq, in0=iota.unsqueeze(1).to_broadcast([128, 16, 256]),
                                                                in1=posf[:, h, :].unsqueeze(2).to_broadcast([128, 16, 256]),
                                                                op=ALU.is_equal), reads=[b_iota, b_posf], writes=[b_eq])
                    kb.op("dve", lambda e, h=h: e.tensor_tensor(out=eq, in0=eq, in1=cid[:, h, :].unsqueeze(1).to_broadcast([128, 16, 256]),
                                                                op=ALU.mult), reads=[b_eq, b_cid], writes=[b_eq])
                    kb.op("dve", lambda e, h=h: e.tensor_reduce(out=eidf[:, h * 16:(h + 1) * 16], in_=eq, axis=AX.X, op=ALU.add),
                          reads=[b_eq], writes=[b_eidf])
                kb.op("dve", lambda e: e.tensor_copy(out=eid, in_=eidf), reads=[b_eidf], writes=[b_eid])
                G = lambda i: gsm[:, i, :]
                t3 = top
                kb.op("dve", lambda e: e.tensor_tensor(out=G(0).rearrange("p (h k) -> p h k", k=16), in0=t3,
                                                       in1=t3[:, :, 0:1].to_broadcast([128, 8, 16]), op=ALU.subtract),
                      reads=[b_top], writes=[b_gsm])
                kb.op("act", lambda e: e.activation(out=G(1), in_=G(0), func=AF.Exp), reads=[b_gsm], writes=[b_gsm])
                kb.op("dve", lambda e: e.tensor_reduce(out=gz[:, 0:8], in_=G(1).rearrange("p (h k) -> p h k", k=16), axis=AX.X, op=ALU.add),
                      reads=[b_gsm], writes=[b_gz])
                kb.op("dve", lambda e: e.reciprocal(out=gz[:, 8:16], in_=gz[:, 0:8]), reads=[b_gz], writes=[b_gz])
                kb.op("dve", lambda e: e.tensor_tensor(out=G(2).rearrange("p (h k) -> p h k", k=16),
                                                       in0=G(1).rearrange("p (h k) -> p h k", k=16),
                                                       in1=bc_last(gz[:, 8:16], 16), op=ALU.mult), reads=[b_gsm, b_gz], writes=[b_gsm])

            def mix(ch):
                rows = slice(ch * 128, (ch + 1) * 128)
                par = ch % 2
                eid, b_eid, gsm, b_gsm = eid2[par], b_eid2[par], gsm2[par], b_gsm2[par]
                xn, b_xn, h1t, b_h1 = xn2_[par], b_xn2[par], h1t2[par], b_h12[par]
                NG = 128 // GS

                def finish_group(gq):
                    bi = gq % NGB
                    s0 = gq * GS
                    kb.op("act", lambda e: e.activation(out=gsm[:, 3, s0:s0 + GS], in_=act_t[:, s0:s0 + GS], func=AF.Gelu),
                          reads=[b_actg[gq]], writes=[b_actg[gq]])
                    kb.op("dve", lambda e: e.tensor_tensor(out=wgt[:, s0:s0 + GS], in0=gsm[:, 3, s0:s0 + GS], in1=gsm[:, 2, s0:s0 + GS],
                                                           op=ALU.mult), reads=[b_actg[gq], b_gsm], writes=[b_actg[gq]])
                    kb.op("dve", lambda e: e.tensor_tensor(out=dgs[bi], in0=ident_f.unsqueeze(1).to_broadcast([128, GS, 128]),
                                                           in1=bc_last(wgt[:, s0:s0 + GS], 128), op=ALU.mult),
                          reads=[b_ident, b_actg[gq]], writes=[b_dgs[bi]])
                    for i in range(GS):
                        s_ = s0 + i
                        for nb in range(4):
                            kb.op("pe", lambda e, i=i, s_=s_, nb=nb: e.matmul(pacc[nb], lhsT=dgs[bi][:, i, :],
                                                                              rhs=uv[bi][:, i, D + nb * 512:D + (nb + 1) * 512],
                                                                              start=(s_ == 0), stop=(s_ == 127)),
                                  reads=[b_dgs[bi], b_uv[bi][i]], writes=[b_pacc[nb]])

                for gq in range(NG):
                    bi = gq % NGB
                    s0 = gq * GS
                    for i in range(GS):
                        s_ = s0 + i
                        kb.dma("pool", lambda e, bi=bi, i=i, s_=s_: e.indirect_dma_start(
                            out=uv[bi][:, i, :], out_offset=None, in_=UVB,
                            in_offset=bass.IndirectOffsetOnAxis(ap=eid[:, s_:s_ + 1], axis=0)),
                            reads=[b_eid, b_UVB], writes=[b_uv[bi][i]])
                    for i in range(GS):
                        s_ = s0 + i
                        pj = s_ % 2
                        kb.op("dve", lambda e, bi=bi, i=i, pj=pj: e.tensor_tensor(out=prod[pj], in0=uv[bi][:, i, 0:D], in1=xn, op=ALU.mult),
                              reads=[b_uv[bi][i], b_xn], writes=[b_prod[pj]])
                        kb.op("act", lambda e, pj=pj, s_=s_: e.activation(out=junk, in_=prod[pj], func=AF.Copy, accum_out=act_t[:, s_:s_ + 1]),
                              reads=[b_prod[pj]], writes=[b_junk, b_actg[gq]])
                    if gq >= 1:
                        finish_group(gq - 1)
                    kb.flush(6)
                finish_group(NG - 1)
                for nb in range(4):
                    kb.op("dve", lambda e, nb=nb: e.tensor_tensor(out=h1t[:, nb * 512:(nb + 1) * 512], in0=h1t[:, nb * 512:(nb + 1) * 512],
                                                                  in1=pacc[nb], op=ALU.add), reads=[b_h1, b_pacc[nb]], writes=[b_h1])
                kb.op("act", lambda e: e.activation(out=junk, in_=h1t, func=AF.Square, accum_out=stat[:, 0:1]),
                      reads=[b_h1], writes=[b_junk, b_stat])
                kb.op("dve", lambda e: e.tensor_scalar(out=stat[:, 1:2], in0=stat[:, 0:1], scalar1=1.0 / D, scalar2=EPS,
                                                       op0=ALU.mult, op1=ALU.add), reads=[b_stat], writes=[b_stat])
                kb.op("act", lambda e: e.activation(out=stat[:, 3:4], in_=stat[:, 1:2], func=AF.Sqrt), reads=[b_stat], writes=[b_stat])
                kb.op("dve", lambda e: e.reciprocal(out=stat[:, 2:3], in_=stat[:, 3:4]), reads=[b_stat], writes=[b_stat])
                kb.op("dve", lambda e: e.scalar_tensor_tensor(out=ot, in0=h1t, scalar=stat[:, 2:3], in1=gf_bc, op0=ALU.mult, op1=ALU.mult),
                      reads=[b_h1, b_stat, b_gf], writes=[b_ot])
                kb.dma("sp", lambda e, rows=rows: e.dma_start(out=out[rows, :], in_=ot), reads=[b_ot], writes=[b_out], accumulate=True)

            routing(0)
            for ch in range(cfg.main):
                kb.maybe_epoch()
                if ch + 1 < cfg.main:
                    kb.recording = True
                    routing(ch + 1)
                    kb.recording = False
                mix(ch)
                kb.flush()
        kb.barrier()

    if "out" in cfg.phases:
        phase_out()
    if "peer" in cfg.phases:
        phase_peer()

    kb.barrier()
    es_glob.close()
    return nc, kb


def make_consts():
    c = np.zeros((128, 4096), np.float64)
    i = np.arange(128)
    c[:, 0:128] = (i[None, :] >= i[:, None])
    c[:, 128:256] = (i[:, None] > i[None, :])
    c[:, 256:384] = 1.0
    gam = 1.0 - 2.0 ** (-5.0 - np.arange(8))
    lg = np.log(gam)
    c[:, 384:392] = np.exp(lg[None, :] * (i[:, None] + 1.0))
    c[:, 392:400] = np.exp(-lg[None, :] * (i[:, None] + 1.0)) / 16.0
    c[:, 400:408] = np.exp(lg[None, :] * (127.0 - i[:, None])) / 16.0
    c[:, 408:416] = np.exp(lg[None, :] * 128.0)
    return c.astype(np.float32)


def core_streams(x, meta_tokens, cfg):
    B, S, _ = x.shape
    maps = []
    inv = (10000.0 ** (-np.arange(128, dtype=np.float32) / np.float32(128))).astype(np.float32)
    for core in range(8):
        b, s = core // 2, core % 2
        full = np.zeros((LEAD + N_META + S, D), np.float32)
        full[LEAD:LEAD + N_META] = meta_tokens
        full[LEAD + N_META:] = x[b]
        valid = np.zeros((full.shape[0], 1), np.float32)
        valid[LEAD:] = 1.0
        pos = np.maximum(np.arange(full.shape[0]) - LEAD, 0).astype(np.float32)
        hin = np.zeros((cfg.nt, D), np.float32)
        msk = np.zeros((cfg.nt, 1), np.float32)
        p = np.zeros((cfg.nt,), np.float32)
        if s == 0:
            n = (cfg.main + 1) * 128
            r = (cfg.pre - 1) * 128
            hin[r:] = full[0:n]
            msk[r:] = valid[0:n]
            p[r:] = pos[0:n]
        else:
            hin[:] = full[0:cfg.nt]
            msk[:] = valid[0:cfg.nt]
            p[:] = pos[0:cfg.nt]
        ang = p[:, None] * inv[None, :]
        maps.append({"hin": hin, "rowmask": msk, "ropec": np.cos(ang).astype(np.float32),
                     "ropes": np.sin(ang).astype(np.float32)})
    return maps


_PROG = {}


def kernel(x, meta_tokens, norm_mix_g, w_in, conv_w, conv_b, dt_bias, a_log, d_skip, ssm_norm_g, w_ret_o,
           w_ssm_o, w_out, norm_ffn_g, peer_w_q, peer_sub_keys, peer_u, peer_v, norm_final_g):
    cfg = Cfg()
    x = np.asarray(x, np.float32)
    maps = core_streams(x, np.asarray(meta_tokens, np.float32), cfg)
    shared = {
        "cst_f": make_consts(),
        "norm_mix_g": np.asarray(norm_mix_g, np.float32).reshape(1, D),
        "w_in": np.asarray(w_in, np.float32).reshape(D, NPROJ),
        "conv_w": np.asarray(conv_w, np.float32).reshape(4, CONV_DIM),
        "conv_b": np.asarray(conv_b, np.float32).reshape(1, CONV_DIM),
        "dt_bias": np.asarray(dt_bias, np.float32).reshape(1, SH),
        "a_log": np.asarray(a_log, np.float32).reshape(1, SH),
        "d_skip": np.asarray(d_skip, np.float32).reshape(1, SH),
        "ssm_norm_g": np.asarray(ssm_norm_g, np.float32).reshape(1, SSM_INNER),
        "w_ret_o": np.asarray(w_ret_o, np.float32).reshape(RH * RDV, D),
        "w_ssm_o": np.asarray(w_ssm_o, np.float32).reshape(SSM_INNER, D),
        "w_out": np.asarray(w_out, np.float32).reshape(D, D),
        "norm_ffn_g": np.asarray(norm_ffn_g, np.float32).reshape(1, D),
        "peer_w_q": np.asarray(peer_w_q, np.float32).reshape(D, D),
        "peer_sub_keys": np.asarray(peer_sub_keys, np.float32).reshape(16 * 128, 128),
        "peer_u": np.asarray(peer_u, np.float32).reshape(NEXP, D),
        "peer_v": np.asarray(peer_v, np.float32).reshape(NEXP, D),
        "norm_final_g": np.asarray(norm_final_g, np.float32).reshape(1, D),
    }
    for m in maps:
        m.update(shared)
    nc, _ = build_program(cfg)
    res = run_bass_kernel_spmd(nc, maps, core_ids=list(range(8)))
    B, S, _ = x.shape
    outp = np.zeros((B, S, D), np.float32)
    half = cfg.main * 128
    for core in range(8):
        b, s = core // 2, core % 2
        outp[b, s * half:(s + 1) * half] = res.results[core]["out"]
    return outp
```
